# Optimizing a Trainium2 kernel written in Bass

```python
import math
import jax, jax.numpy as jnp
from jax import lax
import numpy as np

D_MODEL = 1024
BATCH = 8
SEQ = 4096
DEPTH = 4

N_EVEN = (DEPTH + 1) // 2
N_ODD = DEPTH // 2
W_A = D_MODEL // 2
CONV_A = 3
W_B = D_MODEL // 2
SG_HEADS = 8
SG_HEAD_DIM = W_B // SG_HEADS
CHUNK = 128
EVEN_IN = 3 * W_A + 2 * W_B
EVEN_OUT = W_A + W_B
W_C = D_MODEL // 2
CONV_C = 31
MLA_HEADS = 8
QK_NOPE = 64
QK_ROPE = 32
V_DIM = 64
Q_RANK = 256
KV_RANK = 128
ROPE_THETA = 10000.0
Q_BLOCK = 128
ODD_IN = 2 * W_C + Q_RANK + KV_RANK + QK_ROPE
ODD_OUT = W_C + MLA_HEADS * V_DIM
N_EXPERTS = 32
TOP_K = 4
D_EXPERT = D_MODEL
SWIGLU_LIMIT = 7.0
SWIGLU_ALPHA = 1.702
EXPERT_BLOCK = 128
DEEPNORM_ALPHA = (2.0 * DEPTH) ** 0.25
DEEPNORM_BETA = (8.0 * DEPTH) ** -0.25
LN_EPS = 1e-5
RMS_EPS = 1e-6

kernel_name = 'hybrid_shortconv_sgmlp_conformer_mla_moe_trunk'


def layer_norm(x, g, b):
    xf = x.astype(jnp.float32)
    mu = jnp.mean(xf, axis=-1, keepdims=True)
    var = jnp.mean(jnp.square(xf - mu), axis=-1, keepdims=True)
    y = (xf - mu) * lax.rsqrt(var + LN_EPS)
    return (y * g.astype(jnp.float32) + b.astype(jnp.float32)).astype(x.dtype)


def rms_norm(x, g):
    xf = x.astype(jnp.float32)
    y = xf * lax.rsqrt(jnp.mean(jnp.square(xf), axis=-1, keepdims=True) + RMS_EPS)
    return (y * g.astype(jnp.float32)).astype(x.dtype)


def causal_dwconv(x, w):
    k = w.shape[0]
    return lax.conv_general_dilated(
        x, w[:, None, :].astype(x.dtype), window_strides=(1,), padding=[(k - 1, 0)],
        dimension_numbers=('NWC', 'WIO', 'NWC'), feature_group_count=x.shape[-1])


def rope_tables(positions):
    inv_freq = ROPE_THETA ** (-jnp.arange(0, QK_ROPE, 2, dtype=jnp.float32) / QK_ROPE)
    ang = positions.astype(jnp.float32)[..., None] * inv_freq
    return jnp.cos(ang), jnp.sin(ang)


def apply_rope(x, cos, sin):
    half = x.shape[-1] // 2
    x1 = x[..., :half].astype(jnp.float32)
    x2 = x[..., half:].astype(jnp.float32)
    return jnp.concatenate([x1 * cos - x2 * sin, x2 * cos + x1 * sin], axis=-1).astype(x.dtype)


def causal_block_attention(q, k, v):
    bsz, seq, heads, dqk = q.shape
    n_blk = seq // Q_BLOCK
    scale = 1.0 / math.sqrt(dqk)
    q_blocks = jnp.moveaxis(q.reshape(bsz, n_blk, Q_BLOCK, heads, dqk), 1, 0)
    k_pos = jnp.arange(seq)

    def attend(args):
        qb, i = args
        s = jnp.einsum('bqhd,bkhd->bhqk', qb, k, preferred_element_type=jnp.float32) * scale
        q_pos = i * Q_BLOCK + jnp.arange(Q_BLOCK)
        s = jnp.where(k_pos[None, :] <= q_pos[:, None], s, -1e30)
        p = jax.nn.softmax(s, axis=-1).astype(v.dtype)
        return jnp.einsum('bhqk,bkhd->bqhd', p, v)

    out = lax.map(attend, (q_blocks, jnp.arange(n_blk)))
    return jnp.moveaxis(out, 0, 1).reshape(bsz, seq, heads, v.shape[-1])


def even_mixer(u, w_in, conv_w, sg_w, sg_b, vn_g, vn_b, w_out):
    bsz, seq, _ = u.shape
    proj = u @ w_in
    b_gate, c_gate, xa, zu, zv = jnp.split(proj, [W_A, 2 * W_A, 3 * W_A, 3 * W_A + W_B], axis=-1)
    y_a = b_gate * causal_dwconv(c_gate * xa, conv_w)
    zu = jax.nn.gelu(zu, approximate=False)
    zv = layer_norm(jax.nn.gelu(zv, approximate=False), vn_g, vn_b)
    zv = zv.reshape(bsz, seq // CHUNK, CHUNK, SG_HEADS, SG_HEAD_DIM)
    causal = jnp.tril(jnp.ones((CHUNK, CHUNK), dtype=sg_w.dtype))
    mixed = jnp.einsum('hij,bnjhd->bnihd', sg_w * causal, zv) + jnp.swapaxes(sg_b, 0, 1)[:, :, None]
    y_b = zu * mixed.reshape(bsz, seq, W_B)
    return jnp.concatenate([y_a, y_b], axis=-1) @ w_out


def odd_mixer(u, cos, sin, w_in, dw_w, dw_b, cn_g, cn_b, qn_g, w_uq, kvn_g, w_ukv, w_out):
    bsz, seq, _ = u.shape
    proj = u @ w_in
    ga, gb, cq, ckv, k_rope = jnp.split(
        proj, [W_C, 2 * W_C, 2 * W_C + Q_RANK, 2 * W_C + Q_RANK + KV_RANK], axis=-1)
    h = causal_dwconv(ga * jax.nn.sigmoid(gb), dw_w) + dw_b
    y_c = jax.nn.silu(layer_norm(h, cn_g, cn_b))
    q = (rms_norm(cq, qn_g) @ w_uq).reshape(bsz, seq, MLA_HEADS, QK_NOPE + QK_ROPE)
    kv = (rms_norm(ckv, kvn_g) @ w_ukv).reshape(bsz, seq, MLA_HEADS, QK_NOPE + V_DIM)
    q = jnp.concatenate(
        [q[..., :QK_NOPE], apply_rope(q[..., QK_NOPE:], cos[:, :, None], sin[:, :, None])], axis=-1)
    k_rope = apply_rope(k_rope, cos, sin)[:, :, None, :]
    k = jnp.concatenate(
        [kv[..., :QK_NOPE], jnp.broadcast_to(k_rope, (bsz, seq, MLA_HEADS, QK_ROPE))], axis=-1)
    v = kv[..., QK_NOPE:]
    y_d = causal_block_attention(q, k, v).reshape(bsz, seq, MLA_HEADS * V_DIM)
    return jnp.concatenate([y_c, y_d], axis=-1) @ w_out


def clamped_swiglu(xb, w_gu, b_gu, w_dn, b_dn):
    gu = xb @ w_gu + b_gu
    gate, up = gu[..., :D_EXPERT], gu[..., D_EXPERT:]
    gate = jnp.minimum(gate, SWIGLU_LIMIT)
    up = jnp.clip(up, -SWIGLU_LIMIT, SWIGLU_LIMIT)
    glu = gate * jax.nn.sigmoid(SWIGLU_ALPHA * gate)
    return ((up + 1.0) * glu) @ w_dn + b_dn


def moe_ffn(h, router_w, router_b, w_gu, b_gu, w_dn, b_dn):
    bsz, seq, d = h.shape
    n_tok = bsz * seq
    n_assign = n_tok * TOP_K
    xt = h.reshape(n_tok, d)
    logits = (xt @ router_w + router_b).astype(jnp.float32)
    top_val, top_exp = lax.top_k(logits, TOP_K)
    gates = jax.nn.softmax(top_val, axis=-1)
    flat_exp = top_exp.reshape(-1)
    flat_tok = jnp.arange(n_assign, dtype=jnp.int32) // TOP_K
    flat_gate = gates.reshape(-1)
    order = jnp.argsort(flat_exp)
    sorted_exp = flat_exp[order]
    counts = jnp.bincount(flat_exp, length=N_EXPERTS)
    padded = (counts + EXPERT_BLOCK - 1) // EXPERT_BLOCK * EXPERT_BLOCK
    start = jnp.cumsum(counts) - counts
    pad_end = jnp.cumsum(padded)
    pad_start = pad_end - padded
    dest = pad_start[sorted_exp] + jnp.arange(n_assign, dtype=jnp.int32) - start[sorted_exp]
    n_slots = n_assign + N_EXPERTS * EXPERT_BLOCK
    n_blocks = n_slots // EXPERT_BLOCK
    slot_tok = jnp.full((n_slots,), n_tok, jnp.int32).at[dest].set(flat_tok[order])
    slot_gate = jnp.zeros((n_slots,), jnp.float32).at[dest].set(flat_gate[order])
    block_exp = jnp.minimum(
        jnp.searchsorted(pad_end, jnp.arange(n_blocks, dtype=jnp.int32) * EXPERT_BLOCK, side='right'),
        N_EXPERTS - 1)
    x_pad = jnp.concatenate([xt, jnp.zeros((1, d), xt.dtype)], axis=0)
    xs = x_pad[slot_tok].reshape(n_blocks, EXPERT_BLOCK, d)

    def run_block(args):
        xb, e = args
        return clamped_swiglu(xb, w_gu[e], b_gu[e], w_dn[e], b_dn[e])

    ys = lax.map(run_block, (xs, block_exp)).reshape(n_slots, d)
    out = jnp.zeros((n_tok + 1, d), ys.dtype).at[slot_tok].add(ys * slot_gate[:, None].astype(ys.dtype))
    return out[:n_tok].reshape(bsz, seq, d)


def setup_inputs(seed: int = 0) -> dict:
    key = jax.random.key(seed)
    ks = iter(jax.random.split(key, 48))

    def nrm(shape, scale):
        return jax.random.normal(next(ks), shape, jnp.float32) * scale

    def gain(shape):
        return 1.0 + nrm(shape, 0.02)

    d = D_MODEL
    x = nrm((BATCH, SEQ, d), 1.0)
    c = nrm((BATCH, d), 1.0)
    offsets = jax.random.randint(next(ks), (BATCH, 1), 0, 1024, dtype=jnp.int32)
    positions = (offsets + jnp.arange(SEQ, dtype=jnp.int32)[None, :]).astype(jnp.int32)
    return {
        'x': x,
        'c': c,
        'positions': positions,
        'ada_w': nrm((DEPTH, d, 6 * d), 0.5 * d ** -0.5),
        'ada_b': nrm((DEPTH, 6 * d), 0.02),
        'ln_mix_g': gain((DEPTH, d)),
        'ln_mix_b': nrm((DEPTH, d), 0.02),
        'ln_ffn_g': gain((DEPTH, d)),
        'ln_ffn_b': nrm((DEPTH, d), 0.02),
        'ev_w_in': nrm((N_EVEN, d, EVEN_IN), d ** -0.5),
        'ev_conv_w': nrm((N_EVEN, CONV_A, W_A), CONV_A ** -0.5),
        'ev_sg_w': nrm((N_EVEN, SG_HEADS, CHUNK, CHUNK), CHUNK ** -0.5),
        'ev_sg_b': 1.0 + nrm((N_EVEN, SG_HEADS, CHUNK), 0.1),
        'ev_vn_g': gain((N_EVEN, W_B)),
        'ev_vn_b': nrm((N_EVEN, W_B), 0.02),
        'ev_w_out': nrm((N_EVEN, EVEN_OUT, d), EVEN_OUT ** -0.5 * DEEPNORM_BETA),
        'od_w_in': nrm((N_ODD, d, ODD_IN), d ** -0.5),
        'od_dw_w': nrm((N_ODD, CONV_C, W_C), CONV_C ** -0.5),
        'od_dw_b': nrm((N_ODD, W_C), 0.02),
        'od_cn_g': gain((N_ODD, W_C)),
        'od_cn_b': nrm((N_ODD, W_C), 0.02),
        'od_qn_g': gain((N_ODD, Q_RANK)),
        'od_w_uq': nrm((N_ODD, Q_RANK, MLA_HEADS * (QK_NOPE + QK_ROPE)), Q_RANK ** -0.5),
        'od_kvn_g': gain((N_ODD, KV_RANK)),
        'od_w_ukv': nrm((N_ODD, KV_RANK, MLA_HEADS * (QK_NOPE + V_DIM)), KV_RANK ** -0.5),
        'od_w_out': nrm((N_ODD, ODD_OUT, d), ODD_OUT ** -0.5 * DEEPNORM_BETA),
        'moe_router_w': nrm((DEPTH, d, N_EXPERTS), d ** -0.5),
        'moe_router_b': nrm((DEPTH, N_EXPERTS), 0.01),
        'moe_w_gu': nrm((DEPTH, N_EXPERTS, d, 2 * D_EXPERT), d ** -0.5),
        'moe_b_gu': nrm((DEPTH, N_EXPERTS, 2 * D_EXPERT), 0.02),
        'moe_w_dn': nrm((DEPTH, N_EXPERTS, D_EXPERT, d), D_EXPERT ** -0.5 * DEEPNORM_BETA),
        'moe_b_dn': nrm((DEPTH, N_EXPERTS, d), 0.02),
    }


def reference(x, c, positions, ada_w, ada_b, ln_mix_g, ln_mix_b, ln_ffn_g, ln_ffn_b,
              ev_w_in, ev_conv_w, ev_sg_w, ev_sg_b, ev_vn_g, ev_vn_b, ev_w_out,
              od_w_in, od_dw_w, od_dw_b, od_cn_g, od_cn_b, od_qn_g, od_w_uq, od_kvn_g, od_w_ukv, od_w_out,
              moe_router_w, moe_router_b, moe_w_gu, moe_b_gu, moe_w_dn, moe_b_dn):
    cos, sin = rope_tables(positions)
    cond = jax.nn.silu(c)
    for layer in range(DEPTH):
        mod = (cond @ ada_w[layer] + ada_b[layer])[:, None, :]
        shift_m, scale_m, gate_m, shift_f, scale_f, gate_f = jnp.split(mod, 6, axis=-1)
        u = x * (1.0 + scale_m) + shift_m
        i = layer // 2
        if layer % 2 == 0:
            y = even_mixer(u, ev_w_in[i], ev_conv_w[i], ev_sg_w[i], ev_sg_b[i],
                           ev_vn_g[i], ev_vn_b[i], ev_w_out[i])
        else:
            y = odd_mixer(u, cos, sin, od_w_in[i], od_dw_w[i], od_dw_b[i], od_cn_g[i], od_cn_b[i],
                          od_qn_g[i], od_w_uq[i], od_kvn_g[i], od_w_ukv[i], od_w_out[i])
        x = layer_norm(DEEPNORM_ALPHA * x + (1.0 + gate_m) * y, ln_mix_g[layer], ln_mix_b[layer])
        u = x * (1.0 + scale_f) + shift_f
        y = moe_ffn(u, moe_router_w[layer], moe_router_b[layer], moe_w_gu[layer], moe_b_gu[layer],
                    moe_w_dn[layer], moe_b_dn[layer])
        x = layer_norm(DEEPNORM_ALPHA * x + (1.0 + gate_f) * y, ln_ffn_g[layer], ln_ffn_b[layer])
    return x
```

```python
import math
import numpy as np
from contextlib import ExitStack
import concourse.bass as bass
import concourse.mybir as mybir
from concourse.bass_utils import run_bass_kernel_spmd

F32 = mybir.dt.float32
BF16 = mybir.dt.bfloat16
I32 = mybir.dt.int32
AF = mybir.ActivationFunctionType
ALU = mybir.AluOpType
AX = mybir.AxisListType

D = 1024
DEPTH = 4
NE = 32
TOPK = 4
CH = 512
ALPHA = (2.0 * DEPTH) ** 0.25
LN_EPS = 1e-5
RMS_EPS = 1e-6
TWO_PI_HI = 6.28125
TWO_PI_LO = 2.0 * math.pi - 6.28125


class Buf:
    __slots__ = ("w", "r", "g")

    def __init__(self):
        self.w = []
        self.r = []
        self.g = None


class T:
    __slots__ = ("t", "b")

    def __init__(self, t):
        self.t = t
        self.b = Buf()


class Sched:
    ENG = ("pe", "act", "dve", "pool", "sp")

    def __init__(self, nc, es, ndma=12):
        self.nc = nc
        self.es = es
        self.eng = {"pe": nc.tensor, "act": nc.scalar, "dve": nc.vector, "pool": nc.gpsimd, "sp": nc.sync}
        self.sem = {}
        self.cnt = {}
        self.seen = {e: {} for e in self.ENG}
        for e in self.ENG:
            self.sem[e] = es.enter_context(nc.semaphore("s_" + e))
            self.cnt[e] = 0
        self.dq = {}
        for q in ("sp", "pool", "act"):
            lst = []
            for i in range(ndma if q != "act" else 4):
                key = "d_%s%d" % (q, i)
                self.sem[key] = es.enter_context(nc.semaphore(key))
                self.cnt[key] = 0
                lst.append(key)
            self.dq[q] = [lst, 0]
        self.uid = 0

    def sb(self, shape, dt, name=None):
        self.uid += 1
        return T(self.es_cur.enter_context(self.nc.sbuf_tensor("%s_%d" % (name or "t", self.uid), list(shape), dt)))

    def ps(self, shape, dt, name=None):
        self.uid += 1
        return T(self.es_cur.enter_context(self.nc.psum_tensor("%s_%d" % (name or "p", self.uid), list(shape), dt)))

    def _wait(self, e, evs):
        seen = self.seen[e]
        for key, val in evs:
            if key == "pe" and e == "pe":
                continue
            if seen.get(key, 0) >= val:
                continue
            self.eng[e].wait_ge(self.sem[key], val)
            seen[key] = val

    @staticmethod
    def _deps(reads, writes, group=None):
        evs = []
        for b in reads:
            b = b.b if isinstance(b, T) else b
            evs.extend(b.w)
        for b in writes:
            b = b.b if isinstance(b, T) else b
            if group is None or b.g != group:
                evs.extend(b.w)
            evs.extend(b.r)
        return evs

    @staticmethod
    def _update(ev, reads, writes, group=None):
        for b in reads:
            b = b.b if isinstance(b, T) else b
            b.r.append(ev)
            if len(b.r) > 24:
                last = {}
                for k, v in b.r:
                    if last.get(k, 0) < v:
                        last[k] = v
                b.r = list(last.items())
        for b in writes:
            b = b.b if isinstance(b, T) else b
            if group is not None and b.g == group:
                b.w.append(ev)
            else:
                b.w = [ev]
                b.g = group
            b.r = []

    def op(self, e, fn, reads=(), writes=()):
        self._wait(e, self._deps(reads, writes))
        ins = fn(self.eng[e])
        self.cnt[e] += 1
        ins.then_inc(self.sem[e], 1)
        self.seen[e][e] = max(self.seen[e].get(e, 0), 0)
        self._update((e, self.cnt[e]), reads, writes)

    def mm(self, out, lhsT, rhs, start, stop, reads, writes, **kw):
        self.op("pe", lambda pe: pe.matmul(out, lhsT=lhsT, rhs=rhs, start=start, stop=stop, **kw), reads, writes)

    def dma(self, q, out, in_, reads=(), writes=(), group=None, **kw):
        lst, idx = self.dq[q]
        key = lst[idx % len(lst)]
        self.dq[q][1] = idx + 1
        evs = self._deps(reads, writes, group)
        if self.cnt[key] > 0:
            evs.append((key, self.cnt[key]))
        self._wait(q, evs)
        ins = self.eng[q].dma_start(out=out, in_=in_, **kw)
        self.cnt[key] += 16
        ins.then_inc(self.sem[key], 16)
        self._update((key, self.cnt[key]), reads, writes, group)

    def idma(self, out, out_off, in_, in_off, reads=(), writes=(), group=None):
        q = "pool"
        lst, idx = self.dq[q]
        key = lst[idx % len(lst)]
        self.dq[q][1] = idx + 1
        evs = self._deps(reads, writes, group)
        if self.cnt[key] > 0:
            evs.append((key, self.cnt[key]))
        self._wait(q, evs)
        ins = self.eng[q].indirect_dma_start(out=out, out_offset=out_off, in_=in_, in_offset=in_off)
        self.cnt[key] += 16
        ins.then_inc(self.sem[key], 16)
        self._update((key, self.cnt[key]), reads, writes, group)

    def barrier(self):
        evs = [(k, v) for k, v in self.cnt.items() if v > 0]
        for e in self.ENG:
            self._wait(e, evs)

    def phase(self):
        return _Phase(self)


class _Phase:
    def __init__(self, s):
        self.s = s

    def __enter__(self):
        self.es = ExitStack()
        self.es.__enter__()
        self.s.es_cur = self.es
        return self.s

    def __exit__(self, *a):
        self.s.barrier()
        self.es.__exit__(*a)
        return False


def bcast(ap, n=128):
    return ap.partition_broadcast(n)


def build(S=4096, layers=DEPTH, dbg=None):
    NT = S // 128
    NG = S // 512
    NCH = (S * TOPK) // CH + NE
    NSLOT = NCH * CH
    nc = bass.Bass("TRN2", target_bir_lowering=False)

    def din(name, shape, dt=F32):
        return nc.dram_tensor(name, list(shape), dt, kind="ExternalInput").ap()

    def dscr(name, shape, dt=F32):
        return nc.dram_tensor(name, list(shape), dt, kind="Internal").ap()

    n_even = (layers + 1) // 2
    n_odd = layers // 2
    x_in = din("x", [S, D])
    c_in = din("c", [128, 8])
    pos_in = din("pos", [32, S], I32)
    invf_in = din("invf", [32, 1])
    ada_w = din("ada_w", [layers, D, 6 * D])
    ada_b = din("ada_b", [DEPTH, 6 * D])
    ln_mix_g = din("ln_mix_g", [DEPTH, D]); ln_mix_b = din("ln_mix_b", [DEPTH, D])
    ln_ffn_g = din("ln_ffn_g", [DEPTH, D]); ln_ffn_b = din("ln_ffn_b", [DEPTH, D])
    ev_w_in = din("ev_w_in", [2, D, 2560])
    ev_conv = din("ev_conv", [2, 128, 4, 3])
    ev_sgT = din("ev_sgT", [2, 8, 128, 128])
    ev_sg_b = din("ev_sg_b", [2, 8, 128])
    ev_vn_g = din("ev_vn_g", [2, 512]); ev_vn_b = din("ev_vn_b", [2, 512])
    ev_w_out = din("ev_w_out", [2, D, D])
    od_w_in = din("od_w_in", [2, D, 1440])
    od_dw = din("od_dw", [2, 128, 4, 31])
    od_dw_b = din("od_dw_b", [2, 128, 4])
    od_cn_g = din("od_cn_g", [2, 128, 4]); od_cn_b = din("od_cn_b", [2, 128, 4])
    od_qn_g = din("od_qn_g", [2, 128, 2])
    od_w_uq = din("od_w_uq", [2, 256, 768])
    od_kvn_g = din("od_kvn_g", [2, 128, 1])
    od_w_ukv = din("od_w_ukv", [2, 128, 1024])
    od_w_out = din("od_w_out", [2, D, D])
    r_w = din("moe_router_w", [DEPTH, D, NE])
    r_b = din("moe_router_b", [DEPTH, NE])
    w_gu = din("moe_w_gu", [layers, NE, D, 2 * D])
    b_guT = din("moe_b_guT", [DEPTH, NE, 128, 16])
    w_dn = din("moe_w_dn", [layers, NE, D, D])
    b_dn = din("moe_b_dn", [DEPTH, NE, D])
    out = nc.dram_tensor("out", [S, D], F32, kind="ExternalOutput").ap()

    xres = dscr("xres", [S, D])
    delta = dscr("delta", [S, D])
    uT_d = dscr("uT_d", [8, 128, S], BF16)
    u_d = dscr("u_d", [S, D], BF16)
    xs_d = dscr("xs_d", [NSLOT, D], BF16)
    ys_d = dscr("ys_d", [NSLOT, D])
    mod_d = dscr("mod_d", [DEPTH, 6 * D])
    rope_d = dscr("rope_d", [2, 32, S])
    od_scr = {}
    if layers > 1:
        od_scr = {"qT": dscr("qT_d", [8, 96, S], BF16), "kT": dscr("kT_d", [8, 96, S], BF16), "v": dscr("v_d", [S, 8, 65], BF16),
                  "yc": dscr("yc_d", [4, 128, S], BF16), "yd": dscr("yd_d", [S, 512], BF16)}
    dbg_out = {}
    if dbg:
        for name, shape in dbg.items():
            dbg_out[name] = nc.dram_tensor("dbg_" + name, list(shape), F32, kind="ExternalOutput").ap()

    tokb = lambda: [Buf() for _ in range(NT)]
    b_xres = tokb(); b_delta = tokb(); b_uT = tokb(); b_u = tokb()
    b_xs = [Buf() for _ in range(NCH)]; b_ys = [Buf() for _ in range(NCH)]
    b_xs_all = Buf(); b_ys_all = Buf()
    b_mod = Buf(); b_rope = Buf()

    with ExitStack() as es_top:
        s = Sched(nc, es_top)
        s.es_cur = es_top
        ident_b = s.sb([128, 128], BF16, "identb")
        ident_f = s.sb([128, 128], F32, "identf")
        ones_f = s.sb([128, 128], F32, "onesf")
        ones_b = s.sb([128, 128], BF16, "onesb")
        su_f = s.sb([128, 128], F32, "suf")
        iota_p = s.sb([128, 1], F32, "iotap")
        posk_f = s.sb([128, NT, 4], F32, "poskf")
        posk_i = s.sb([128, NT, 4], I32, "poski")
        gatek = s.sb([128, NT, 4], F32, "gatek")
        widx = s.sb([128, NCH, 8], I32, "widx")
        bgidx = s.sb([128, NCH], I32, "bgidx")
        bdidx = s.sb([128, NCH], I32, "bdidx")
        base_pk = s.sb([128, 8], F32, "basepk")

        def mk_consts():
            s.op("pool", lambda e: e.memset(ident_b.t[:], 0.0), [], [ident_b])
            s.op("pool", lambda e: e.affine_select(out=ident_b.t[:], in_=ident_b.t[:], pattern=[[-1, 128]],
                                                   compare_op=ALU.not_equal, fill=1.0, base=0, channel_multiplier=1),
                 [ident_b], [ident_b])
            s.op("pool", lambda e: e.memset(ident_f.t[:], 0.0), [], [ident_f])
            s.op("pool", lambda e: e.affine_select(out=ident_f.t[:], in_=ident_f.t[:], pattern=[[-1, 128]],
                                                   compare_op=ALU.not_equal, fill=1.0, base=0, channel_multiplier=1),
                 [ident_f], [ident_f])
            s.op("pool", lambda e: e.memset(ones_f.t[:], 1.0), [], [ones_f])
            s.op("pool", lambda e: e.memset(ones_b.t[:], 1.0), [], [ones_b])
            s.op("pool", lambda e: e.memset(su_f.t[:], 1.0), [], [su_f])
            s.op("pool", lambda e: e.affine_select(out=su_f.t[:], in_=su_f.t[:], pattern=[[1, 128]],
                                                   compare_op=ALU.is_gt, fill=0.0, base=0, channel_multiplier=-1),
                 [su_f], [su_f])
            s.op("pool", lambda e: e.iota(iota_p.t[:], pattern=[[0, 1]], base=0, channel_multiplier=1,
                                          allow_small_or_imprecise_dtypes=True), [], [iota_p])
            s.op("pool", lambda e: e.iota(base_pk.t[:], pattern=[[128, 8]], base=0, channel_multiplier=1,
                                          allow_small_or_imprecise_dtypes=True), [], [base_pk])

        mk_consts()

        def prologue():
            with s.phase():
                ct = s.sb([128, 8], F32, "ct")
                cond = s.sb([128, 8], F32, "cond")
                condb = s.sb([128, 8, 128], F32, "condb")
                s.dma("sp", ct.t[:], c_in, [], [ct])
                s.op("act", lambda e: e.activation(out=cond.t[:], in_=ct.t[:], func=AF.Silu), [ct], [cond])
                for k in range(8):
                    s.op("dve", lambda e, k=k: e.tensor_scalar(condb.t[:, k, :], ones_f.t[:], cond.t[:, k:k + 1], None,
                                                               ALU.mult), [ones_f, cond], [condb])
                wst = [s.sb([128, 3072], F32, "adaw") for _ in range(3)]
                pacc = [s.ps([128, 512], F32, "pada") for _ in range(6)]
                modt = s.sb([1, 6 * D], F32, "modt")
                adab = s.sb([1, 6 * D], F32, "adab")
                it = 0
                for l in range(layers):
                    s.dma("sp", adab.t[:], ada_b[l:l + 1, :], [], [adab])
                    for half in range(2):
                        for k in range(8):
                            w = wst[it % 3]; it += 1
                            s.dma("sp", w.t[:], ada_w[l, k * 128:(k + 1) * 128, half * 3072:(half + 1) * 3072], [], [w])
                            for n in range(6):
                                s.mm(pacc[n].t[:], condb.t[:, k, :], w.t[:, n * 512:(n + 1) * 512], k == 0, k == 7,
                                     [condb, w], [pacc[n]])
                        for n in range(6):
                            c0 = half * 3072 + n * 512
                            s.op("dve", lambda e, n=n, c0=c0: e.tensor_tensor(modt.t[0:1, c0:c0 + 512], pacc[n].t[0:1, :],
                                                                              adab.t[0:1, c0:c0 + 512], ALU.add),
                                 [pacc[n], adab], [modt])
                    s.dma("sp", mod_d[l:l + 1, :], modt.t[:], [modt], [b_mod])
            if n_odd > 0:
              with s.phase():
                    pi_ = s.sb([32, S], I32, "posi")
                    ang = s.sb([32, S], F32, "ang")
                    q = s.sb([32, S], F32, "q")
                    qi = s.sb([32, S], I32, "qi")
                    r = s.sb([32, S], F32, "r")
                    m = s.sb([32, S], F32, "m")
                    invf = s.sb([32, 1], F32, "invf")
                    s.dma("sp", pi_.t[:], pos_in, [], [pi_])
                    s.dma("sp", invf.t[:], invf_in, [], [invf])
                    s.op("dve", lambda e: e.tensor_copy(out=ang.t[:], in_=pi_.t[:]), [pi_], [ang])
                    s.op("dve", lambda e: e.tensor_scalar(ang.t[:], ang.t[:], invf.t[:, 0:1], None, ALU.mult), [ang, invf], [ang])
                    s.op("dve", lambda e: e.tensor_scalar(q.t[:], ang.t[:], 1.0 / (2 * math.pi), None, ALU.mult), [ang], [q])
                    s.op("dve", lambda e: e.tensor_copy(out=qi.t[:], in_=q.t[:]), [q], [qi])
                    s.op("dve", lambda e: e.tensor_copy(out=q.t[:], in_=qi.t[:]), [qi], [q])
                    s.op("dve", lambda e: e.scalar_tensor_tensor(r.t[:], q.t[:], -TWO_PI_HI, ang.t[:], ALU.mult, ALU.add), [q, ang], [r])
                    s.op("dve", lambda e: e.scalar_tensor_tensor(r.t[:], q.t[:], -TWO_PI_LO, r.t[:], ALU.mult, ALU.add), [q, r], [r])

                    def wrap(t):
                        s.op("dve", lambda e: e.tensor_scalar(m.t[:], t.t[:], math.pi, -2 * math.pi, ALU.is_gt, ALU.mult), [t], [m])
                        s.op("dve", lambda e: e.tensor_tensor(t.t[:], t.t[:], m.t[:], ALU.add), [t, m], [t])
                        s.op("dve", lambda e: e.tensor_scalar(m.t[:], t.t[:], -math.pi, 2 * math.pi, ALU.is_lt, ALU.mult), [t], [m])
                        s.op("dve", lambda e: e.tensor_tensor(t.t[:], t.t[:], m.t[:], ALU.add), [t, m], [t])
                        s.op("dve", lambda e: e.tensor_scalar(t.t[:], t.t[:], math.pi, -math.pi, ALU.min, ALU.max), [t], [t])

                    wrap(r)
                    sn = s.sb([32, S], F32, "sn")
                    s.op("act", lambda e: e.activation(out=sn.t[:], in_=r.t[:], func=AF.Sin), [r], [sn])
                    s.dma("sp", rope_d[1], sn.t[:], [sn], [b_rope])
                    s.op("dve", lambda e: e.tensor_scalar(r.t[:], r.t[:], math.pi / 2, None, ALU.add), [r], [r])
                    wrap(r)
                    cs = s.sb([32, S], F32, "cs")
                    s.op("act", lambda e: e.activation(out=cs.t[:], in_=r.t[:], func=AF.Sin), [r], [cs])
                    s.dma("sp", rope_d[0], cs.t[:], [cs], [b_rope])

        def load_mod(l, j, plus1, name, scale=None):
            t = s.sb([128, D], F32, name)
            s.dma("sp", t.t[:], bcast(mod_d[l:l + 1, j * D:(j + 1) * D]), [b_mod], [t])
            if plus1 and scale is not None:
                s.op("pool", lambda e: e.tensor_scalar(t.t[:], t.t[:], 1.0, float(scale), ALU.add, ALU.mult), [t], [t])
            elif plus1:
                s.op("pool", lambda e: e.tensor_scalar(t.t[:], t.t[:], 1.0, None, ALU.add), [t], [t])
            return t

        def load_row(src_row, n, name):
            t = s.sb([128, n], F32, name)
            s.dma("sp", t.t[:], bcast(src_row), [], [t])
            return t

        def transposes_store_uT(ub, i, ptr, uTs):
            for k in range(8):
                s.op("pe", lambda e, k=k: e.transpose(ptr.t[:, k, :], ub.t[:, k * 128:(k + 1) * 128], ident_b.t[:]),
                     [ub, ident_b], [ptr])
            s.op("act", lambda e: e.copy(out=uTs.t[:], in_=ptr.t[:]), [ptr], [uTs])
            s.dma("sp", uT_d[:, :, i * 128:(i + 1) * 128].rearrange("k p t -> p k t"), uTs.t[:], [uTs], [b_uT[i]])

        def premod(l):
            with s.phase():
                sc1 = load_mod(l, 1, True, "sc1")
                sh = load_mod(l, 0, False, "sh")
                xt = [s.sb([128, D], F32, "xt") for _ in range(2)]
                ub = [s.sb([128, D], BF16, "ub") for _ in range(2)]
                ptr = [s.ps([128, 8, 128], BF16, "ptr") for _ in range(2)]
                uTs = [s.sb([128, 8, 128], BF16, "uTs") for _ in range(2)]
                for i in range(NT):
                    x = xt[i % 2]; u = ub[i % 2]
                    s.dma("sp", x.t[:], x_in[i * 128:(i + 1) * 128, :], [], [x])
                    s.op("dve", lambda e: e.tensor_tensor(x.t[:], x.t[:], sc1.t[:], ALU.mult), [x, sc1], [x])
                    s.op("dve", lambda e: e.tensor_tensor(u.t[:], x.t[:], sh.t[:], ALU.add), [x, sh], [u])
                    transposes_store_uT(u, i, ptr[i % 2], uTs[i % 2])

        def even_mixer(l):
            li = l // 2
            with s.phase():
                win = s.sb([128, 8, 2560], BF16, "win")
                wout = s.sb([128, 8, D], BF16, "wout")
                wv = ev_w_in[li].rearrange("(k p) f -> p k f", p=128)
                gpw = load_mod(l, 2, True, "gpw", 1.0 / ALPHA)
                wstg = [s.sb([128, D], F32, "wstg") for _ in range(2)]
                for k in range(8):
                    s.dma("pool", win.t[:, k, 0:1280], wv[:, k, 0:1280], [], [win], group="w")
                    s.dma("pool", win.t[:, k, 1280:2560], wv[:, k, 1280:2560], [], [win], group="w")
                for k in range(8):
                    s.dma("sp", wstg[k % 2].t[:], ev_w_out[li, k * 128:(k + 1) * 128, :], [], [wstg[k % 2]])
                    s.op("dve", lambda e, k=k: e.tensor_tensor(wout.t[:, k, :], wstg[k % 2].t[:], gpw.t[:], ALU.mult), [wstg[k % 2], gpw], [wout])
                sgf = s.sb([128, 8, 128], F32, "sgf")
                sgm = s.sb([128, 8, 128], BF16, "sgm")
                s.dma("sp", sgf.t[:], ev_sgT[li].rearrange("h j i -> j h i"), [], [sgf])
                for h in range(8):
                    s.op("pool", lambda e, h=h: e.affine_select(out=sgf.t[:, h, :], in_=sgf.t[:, h, :], pattern=[[1, 128]],
                                                                compare_op=ALU.is_ge, fill=0.0, base=0, channel_multiplier=-1),
                         [sgf], [sgf])
                s.op("pool", lambda e: e.tensor_copy(out=sgm.t[:], in_=sgf.t[:]), [sgf], [sgm])
                sgb = s.sb([128, 4, 128], F32, "sgb")
                for h in range(8):
                    s.dma("sp", sgb.t[(h % 2) * 64:(h % 2) * 64 + 64, h // 2, :], bcast(ev_sg_b[li, h:h + 1, :], 64), [], [sgb], group="w")
                cw = s.sb([128, 4, 3], F32, "cw")
                s.dma("sp", cw.t[:], ev_conv[li], [], [cw])
                vng = load_row(ev_vn_g[li:li + 1, :], 512, "vng")
                vnb = load_row(ev_vn_b[li:li + 1, :], 512, "vnb")
                halo = s.sb([128, 4, 2], F32, "halo")
                s.op("pool", lambda e: e.memset(halo.t[:], 0.0), [], [halo])

                uTg = [s.sb([128, 8, 512], BF16, "uTg") for _ in range(2)]
                pp = [s.ps([128, 512], F32, "pp") for _ in range(6)]
                psg = [s.ps([128, 128], F32, "psg") for _ in range(2)]
                cg = [s.sb([128, 512], F32, "cg") for _ in range(2)]
                cx = [s.sb([128, 514], F32, "cx") for _ in range(2)]
                acc = [s.sb([128, 512], F32, "acc") for _ in range(2)]
                yT = [s.sb([128, 8, 512], BF16, "yT") for _ in range(2)]
                zuT = [s.sb([128, 4, 512], F32, "zuT") for _ in range(2)]
                zv = [s.sb([128, 512], F32, "zv") for _ in range(2)]
                zvn = [s.sb([128, 512], BF16, "zvn") for _ in range(2)]
                st6 = [s.sb([128, 6], F32, "st6") for _ in range(2)]
                mv = [s.sb([128, 2], F32, "mv") for _ in range(2)]
                rstd = [s.sb([128, 1], F32, "rstd") for _ in range(2)]
                tmp = [s.sb([128, 128], F32, "tmp") for _ in range(2)]
                dl = [s.sb([128, D], F32, "dl") for _ in range(2)]
                ppi = 0
                for g in range(NG):
                    u = uTg[g % 2]; y = yT[g % 2]; zu = zuT[g % 2]
                    s.dma("sp", u.t[:], uT_d[:, :, g * 512:(g + 1) * 512].rearrange("k p t -> p k t"),
                          [b_uT[4 * g + j] for j in range(4)], [u])

                    def proj(f):
                        nonlocal ppi
                        p = pp[ppi % 6]; ppi += 1
                        for k in range(8):
                            s.mm(p.t[:], win.t[:, k, f * 128:(f + 1) * 128], u.t[:, k, :], k == 0, k == 7, [win, u], [p])
                        return p

                    for cc in range(4):
                        c_ = cg[cc % 2]; x_ = cx[cc % 2]; a_ = acc[cc % 2]
                        p_c = proj(4 + cc)
                        s.op("act", lambda e: e.copy(out=c_.t[:], in_=p_c.t[:]), [p_c], [c_])
                        p_x = proj(8 + cc)
                        s.op("pool", lambda e: e.tensor_copy(out=x_.t[:, 0:2], in_=halo.t[:, cc, :]), [halo], [x_])
                        s.op("dve", lambda e: e.tensor_tensor(x_.t[:, 2:514], p_x.t[:], c_.t[:], ALU.mult), [p_x, c_], [x_])
                        s.op("pool", lambda e: e.tensor_copy(out=halo.t[:, cc, :], in_=x_.t[:, 512:514]), [x_], [halo])
                        s.op("dve", lambda e: e.tensor_scalar(a_.t[:], x_.t[:, 0:512], cw.t[:, cc, 0:1], None, ALU.mult), [x_, cw], [a_])
                        s.op("dve", lambda e: e.scalar_tensor_tensor(a_.t[:], x_.t[:, 1:513], cw.t[:, cc, 1:2], a_.t[:], ALU.mult, ALU.add),
                             [x_, cw, a_], [a_])
                        s.op("dve", lambda e: e.scalar_tensor_tensor(a_.t[:], x_.t[:, 2:514], cw.t[:, cc, 2:3], a_.t[:], ALU.mult, ALU.add),
                             [x_, cw, a_], [a_])
                        p_b = proj(cc)
                        s.op("dve", lambda e: e.tensor_tensor(y.t[:, cc, :], p_b.t[:], a_.t[:], ALU.mult), [p_b, a_], [y])
                        p_u = proj(12 + cc)
                        s.op("act", lambda e: e.activation(out=zu.t[:, cc, :], in_=p_u.t[:], func=AF.Gelu), [p_u], [zu])
                    for tt in range(4):
                        i = 4 * g + tt
                        z = zv[tt % 2]; zn = zvn[tt % 2]; s6 = st6[tt % 2]; m_ = mv[tt % 2]; rs = rstd[tt % 2]
                        p = pp[ppi % 6]; ppi += 1
                        for k in range(8):
                            s.mm(p.t[:], u.t[:, k, tt * 128:(tt + 1) * 128], win.t[:, k, 2048:2560], k == 0, k == 7, [win, u], [p])
                        s.op("act", lambda e: e.activation(out=z.t[:], in_=p.t[:], func=AF.Gelu), [p], [z])
                        s.op("dve", lambda e: e.bn_stats(s6.t[:], z.t[:]), [z], [s6])
                        s.op("dve", lambda e: e.bn_aggr(m_.t[:], s6.t[:]), [s6], [m_])
                        s.op("act", lambda e: e.activation(out=rs.t[:], in_=m_.t[:, 1:2], func=AF.Sqrt, bias=eps_t.t[:, 0:1]), [m_, eps_t], [rs])
                        s.op("dve", lambda e: e.reciprocal(rs.t[:], rs.t[:]), [rs], [rs])
                        s.op("dve", lambda e: e.tensor_scalar(z.t[:], z.t[:], m_.t[:, 0:1], rs.t[:, 0:1], ALU.subtract, ALU.mult), [z, m_, rs], [z])
                        s.op("pool", lambda e: e.tensor_tensor(z.t[:], z.t[:], vng.t[:], ALU.mult), [z, vng], [z])
                        s.op("pool", lambda e: e.tensor_tensor(zn.t[:], z.t[:], vnb.t[:], ALU.add), [z, vnb], [zn])
                        for cc in range(4):
                            for hh in range(2):
                                h = 2 * cc + hh
                                pg = psg[(cc * 2 + hh) % 2]
                                t_ = tmp[(cc * 2 + hh) % 2]
                                s.mm(pg.t[:], zn.t[:, cc * 128:(cc + 1) * 128], sgm.t[:, h, :], True, True, [zn, sgm], [pg])
                                lo, hi = hh * 64, hh * 64 + 64
                                s.op("dve", lambda e: e.tensor_tensor(t_.t[lo:hi, :], pg.t[lo:hi, :], sgb.t[lo:hi, cc, :], ALU.add), [pg, sgb], [t_])
                                s.op("dve", lambda e: e.tensor_tensor(y.t[lo:hi, 4 + cc, tt * 128:(tt + 1) * 128], t_.t[lo:hi, :],
                                                                      zu.t[lo:hi, cc, tt * 128:(tt + 1) * 128], ALU.mult), [t_, zu], [y])
                        d_ = dl[tt % 2]
                        for nh in range(2):
                            p = pp[ppi % 6]; ppi += 1
                            for k in range(8):
                                s.mm(p.t[:], y.t[:, k, tt * 128:(tt + 1) * 128], wout.t[:, k, nh * 512:(nh + 1) * 512], k == 0, k == 7, [y, wout], [p])
                            s.op("act", lambda e: e.copy(out=d_.t[:, nh * 512:(nh + 1) * 512], in_=p.t[:]), [p], [d_])
                        s.dma("sp", delta[i * 128:(i + 1) * 128, :], d_.t[:], [d_], [b_delta[i]])

        def odd_mixer(l):
            li = l // 2
            SCALE = 1.0 / math.sqrt(96.0)
            qT_d = od_scr["qT"]; kT_d = od_scr["kT"]; v_d = od_scr["v"]; yc_d = od_scr["yc"]; yd_d = od_scr["yd"]
            b_q = Buf(); b_k = Buf(); b_v = Buf(); b_yc = Buf(); b_yd = Buf()
            with s.phase():
                win = s.sb([128, 8, 1440], BF16, "win")
                wv_ = od_w_in[li].rearrange("(k p) f -> p k f", p=128)
                for k in range(8):
                    s.dma("pool", win.t[:, k, :], wv_[:, k, :], [], [win], group="w")
                winr = s.sb([128, 8, 32], F32, "winr")
                s.dma("sp", winr.t[:], wv_[:, :, 1408:1440], [], [winr])
                win_sw = s.sb([128, 8, 96], BF16, "winsw")
                s.op("pool", lambda e: e.memset(win_sw.t[:], 0.0), [], [win_sw])
                s.op("dve", lambda e: e.tensor_scalar(win_sw.t[:, :, 64:80], winr.t[:, :, 16:32], -1.0, None, ALU.mult), [winr, win_sw], [win_sw])
                s.op("dve", lambda e: e.tensor_copy(out=win_sw.t[:, :, 80:96], in_=winr.t[:, :, 0:16]), [winr, win_sw], [win_sw])
                wuq = s.sb([128, 2, 768], BF16, "wuq")
                wuqf = s.sb([128, 2, 768], F32, "wuqf")
                uqv = od_w_uq[li].rearrange("(k p) f -> p k f", p=128)
                s.dma("pool", wuq.t[:], uqv, [], [wuq])
                s.dma("sp", wuqf.t[:], uqv, [], [wuqf])
                wuq_sw = s.sb([128, 2, 8, 96], BF16, "wuqsw")
                s.op("pool", lambda e: e.memset(wuq_sw.t[:], 0.0), [], [wuq_sw])
                wuqf4 = wuqf.t[:].rearrange("p k (h e) -> p k h e", e=96)
                for kc in range(2):
                    s.op("dve", lambda e, kc=kc: e.tensor_scalar(wuq_sw.t[:, kc, :, 64:80], wuqf4[:, kc, :, 80:96], -1.0, None, ALU.mult), [wuqf, wuq_sw], [wuq_sw])
                    s.op("dve", lambda e, kc=kc: e.tensor_copy(out=wuq_sw.t[:, kc, :, 80:96], in_=wuqf4[:, kc, :, 64:80]), [wuqf, wuq_sw], [wuq_sw])
                wk = s.sb([128, 8, 64], BF16, "wk")
                wvv = s.sb([128, 8, 64], BF16, "wvv")
                ukv = od_w_ukv[li].rearrange("r (h e) -> r h e", e=128)
                s.dma("pool", wk.t[:], ukv[:, :, 0:64], [], [wk])
                s.dma("pool", wvv.t[:], ukv[:, :, 64:128], [], [wvv])
                cwd = s.sb([128, 4, 31], F32, "cwd")
                s.dma("sp", cwd.t[:], od_dw[li], [], [cwd])
                dwb = s.sb([128, 4], F32, "dwb"); cng = s.sb([128, 4], F32, "cng"); cnb = s.sb([128, 4], F32, "cnb")
                qng = s.sb([128, 2], F32, "qng"); kvng = s.sb([128, 1], F32, "kvng")
                s.dma("sp", dwb.t[:], od_dw_b[li], [], [dwb]); s.dma("sp", cng.t[:], od_cn_g[li], [], [cng])
                s.dma("sp", cnb.t[:], od_cn_b[li], [], [cnb]); s.dma("sp", qng.t[:], od_qn_g[li], [], [qng])
                s.dma("sp", kvng.t[:], od_kvn_g[li], [], [kvng])
                diag = s.sb([128, 4, 31, 128], BF16, "diag")
                for cc in range(4):
                    for k in range(31):
                        eng = "dve" if (cc * 31 + k) % 2 == 0 else "pool"
                        s.op(eng, lambda e, cc=cc, k=k: e.tensor_scalar(diag.t[:, cc, k, :], ident_f.t[:], cwd.t[:, cc, k:k + 1], None, ALU.mult),
                             [ident_f, cwd], [diag])
                rms_eps = s.sb([128, 1], F32, "rmseps")
                s.op("pool", lambda e: e.memset(rms_eps.t[:], RMS_EPS), [], [rms_eps])

                uTg = [s.sb([128, 8, 512], BF16, "uTg") for _ in range(2)]
                glu = [s.sb([128, 4, 542], BF16, "glu") for _ in range(2)]
                s.op("pool", lambda e: e.memset(glu[1].t[:, :, 512:542], 0.0), [], [glu[1]])
                sgm = [s.sb([128, 512], F32, "sgm") for _ in range(2)]
                hbuf = s.sb([128, 4, 512], F32, "hbuf")
                hsq = s.sb([128, 4, 512], F32, "hsq")
                mean = s.sb([128, 512], F32, "mean"); var = s.sb([128, 512], F32, "var"); rstd = s.sb([128, 512], F32, "rstd")
                tb = [s.sb([128, 512], F32, "tb") for _ in range(2)]
                ycT = [s.sb([128, 4, 512], BF16, "ycT") for _ in range(2)]
                sq = [s.sb([128, 512], F32, "sq") for _ in range(2)]
                rq = s.sb([128, 512], F32, "rq")
                cqn = s.sb([128, 2, 512], BF16, "cqn")
                ckvn = s.sb([128, 512], BF16, "ckvn")
                cs = s.sb([128, 2, 512], F32, "cs")
                t1 = [s.sb([128, 512], F32, "t1") for _ in range(2)]
                t2 = [s.sb([128, 512], F32, "t2") for _ in range(2)]
                krT = s.sb([128, 512], BF16, "krT")
                qT = [s.sb([128, 8, 512], BF16, "qT") for _ in range(2)]
                kT = [s.sb([128, 8, 512], BF16, "kT") for _ in range(2)]
                vt = [s.sb([128, 8, 65], BF16, "vt") for _ in range(2)]
                for v_ in vt:
                    s.op("pool", lambda e, v_=v_: e.memset(v_.t[:, :, 64:65], 1.0), [], [v_])
                pp = [s.ps([128, 512], F32, "pp") for _ in range(8)]
                ppi = 0
                for g in range(NG):
                    u = uTg[g % 2]; gl = glu[g % 2]; glp = glu[(g + 1) % 2]
                    tsl = slice(g * 512, (g + 1) * 512)
                    s.dma("sp", u.t[:], uT_d[:, :, tsl].rearrange("k p t -> p k t"), [b_uT[4 * g + j] for j in range(4)], [u])
                    s.dma("sp", cs.t[64:96, 0, :], rope_d[0, :, tsl], [b_rope], [cs], group=("cs", g))
                    s.dma("sp", cs.t[64:96, 1, :], rope_d[1, :, tsl], [b_rope], [cs], group=("cs", g))

                    def proj(c0, m, wt=win):
                        nonlocal ppi
                        p = pp[ppi % 8]; ppi += 1
                        for k in range(8):
                            s.mm(p.t[0:m, :], wt.t[:, k, c0:c0 + m], u.t[:, k, :], k == 0, k == 7, [wt, u], [p])
                        return p

                    s.op("pool", lambda e: e.tensor_copy(out=gl.t[:, :, 0:30], in_=glp.t[:, :, 512:542]), [glp], [gl])
                    for cc in range(4):
                        sg_ = sgm[cc % 2]
                        p_b = proj(512 + cc * 128, 128)
                        s.op("act", lambda e: e.activation(out=sg_.t[:], in_=p_b.t[:], func=AF.Sigmoid), [p_b], [sg_])
                        p_a = proj(cc * 128, 128)
                        s.op("dve", lambda e: e.tensor_tensor(gl.t[:, cc, 30:542], p_a.t[:], sg_.t[:], ALU.mult), [p_a, sg_], [gl])
                    for cc in range(4):
                        p = pp[ppi % 8]; ppi += 1
                        for k in range(31):
                            s.mm(p.t[:], diag.t[:, cc, k, :], gl.t[:, cc, k:k + 512], k == 0, k == 30, [diag, gl], [p])
                        s.op("act", lambda e: e.activation(out=hbuf.t[:, cc, :], in_=p.t[:], func=AF.Identity, bias=dwb.t[:, cc:cc + 1]), [p, dwb], [hbuf])
                        s.op("act", lambda e: e.activation(out=hsq.t[:, cc, :], in_=p.t[:], func=AF.Square, bias=dwb.t[:, cc:cc + 1]), [p, dwb], [hsq])
                    p1 = pp[ppi % 8]; ppi += 1
                    p2 = pp[ppi % 8]; ppi += 1
                    for cc in range(4):
                        s.mm(p1.t[:], ones_f.t[:], hbuf.t[:, cc, :], cc == 0, cc == 3, [ones_f, hbuf], [p1])
                    for cc in range(4):
                        s.mm(p2.t[:], ones_f.t[:], hsq.t[:, cc, :], cc == 0, cc == 3, [ones_f, hsq], [p2])
                    s.op("dve", lambda e: e.tensor_scalar(mean.t[:], p1.t[:], 1.0 / 512, None, ALU.mult), [p1], [mean])
                    s.op("pool", lambda e: e.tensor_tensor(var.t[:], mean.t[:], mean.t[:], ALU.mult), [mean], [var])
                    s.op("dve", lambda e: e.scalar_tensor_tensor(var.t[:], p2.t[:], 1.0 / 512, var.t[:], ALU.mult, ALU.subtract), [p2, var], [var])
                    s.op("act", lambda e: e.activation(out=rstd.t[:], in_=var.t[:], func=AF.Sqrt, bias=eps_t.t[:, 0:1]), [var, eps_t], [rstd])
                    s.op("dve", lambda e: e.reciprocal(rstd.t[:], rstd.t[:]), [rstd], [rstd])
                    yc = ycT[g % 2]
                    for cc in range(4):
                        t_ = tb[cc % 2]
                        s.op("dve", lambda e: e.tensor_tensor(t_.t[:], hbuf.t[:, cc, :], mean.t[:], ALU.subtract), [hbuf, mean], [t_])
                        s.op("pool", lambda e: e.tensor_tensor(t_.t[:], t_.t[:], rstd.t[:], ALU.mult), [t_, rstd], [t_])
                        s.op("dve", lambda e: e.tensor_scalar(t_.t[:], t_.t[:], cng.t[:, cc:cc + 1], cnb.t[:, cc:cc + 1], ALU.mult, ALU.add), [t_, cng, cnb], [t_])
                        s.op("act", lambda e: e.activation(out=yc.t[:, cc, :], in_=t_.t[:], func=AF.Silu), [t_], [yc])
                    s.dma("sp", yc_d[:, :, tsl].rearrange("c p t -> p c t"), yc.t[:], [yc], [b_yc], group="st")
                    pq = [proj(1024, 128), proj(1152, 128)]
                    for c2 in range(2):
                        s.op("act", lambda e, c2=c2: e.activation(out=sq[c2].t[:], in_=pq[c2].t[:], func=AF.Square), [pq[c2]], [sq[c2]])
                    ps_ = pp[ppi % 8]; ppi += 1
                    for c2 in range(2):
                        s.mm(ps_.t[:], ones_f.t[:], sq[c2].t[:], c2 == 0, c2 == 1, [ones_f, sq[c2]], [ps_])
                    s.op("act", lambda e: e.activation(out=rq.t[:], in_=ps_.t[:], func=AF.Sqrt, bias=rms_eps.t[:, 0:1], scale=1.0 / 256), [ps_, rms_eps], [rq])
                    s.op("dve", lambda e: e.reciprocal(rq.t[:], rq.t[:]), [rq], [rq])
                    for c2 in range(2):
                        s.op("dve", lambda e, c2=c2: e.scalar_tensor_tensor(cqn.t[:, c2, :], pq[c2].t[:], qng.t[:, c2:c2 + 1], rq.t[:], ALU.mult, ALU.mult),
                             [pq[c2], qng, rq], [cqn])
                    pkv = proj(1280, 128)
                    s.op("act", lambda e: e.activation(out=sq[0].t[:], in_=pkv.t[:], func=AF.Square), [pkv], [sq[0]])
                    ps_ = pp[ppi % 8]; ppi += 1
                    s.mm(ps_.t[:], ones_f.t[:], sq[0].t[:], True, True, [ones_f, sq[0]], [ps_])
                    s.op("act", lambda e: e.activation(out=rq.t[:], in_=ps_.t[:], func=AF.Sqrt, bias=rms_eps.t[:, 0:1], scale=1.0 / 128), [ps_, rms_eps], [rq])
                    s.op("dve", lambda e: e.reciprocal(rq.t[:], rq.t[:]), [rq], [rq])
                    s.op("dve", lambda e: e.scalar_tensor_tensor(ckvn.t[:], pkv.t[:], kvng.t[:, 0:1], rq.t[:], ALU.mult, ALU.mult), [pkv, kvng, rq], [ckvn])
                    pk1 = proj(1344, 96)
                    pk2 = proj(0, 96, win_sw)
                    R = slice(64, 96)
                    s.op("dve", lambda e: e.tensor_tensor(t1[0].t[R, :], pk1.t[R, :], cs.t[R, 0, :], ALU.mult), [pk1, cs], [t1[0]])
                    s.op("dve", lambda e: e.tensor_tensor(t2[0].t[R, :], pk2.t[R, :], cs.t[R, 1, :], ALU.mult), [pk2, cs], [t2[0]])
                    s.op("pool", lambda e: e.tensor_tensor(krT.t[R, :], t1[0].t[R, :], t2[0].t[R, :], ALU.add), [t1[0], t2[0]], [krT])
                    q_ = qT[g % 2]; k_ = kT[g % 2]
                    for h in range(8):
                        a1 = t1[h % 2]; a2 = t2[h % 2]
                        pq1 = pp[ppi % 8]; ppi += 1
                        pq2 = pp[ppi % 8]; ppi += 1
                        for kc in range(2):
                            s.mm(pq1.t[0:96, :], wuq.t[:, kc, h * 96:(h + 1) * 96], cqn.t[:, kc, :], kc == 0, kc == 1, [wuq, cqn], [pq1])
                        for kc in range(2):
                            s.mm(pq2.t[0:96, :], wuq_sw.t[:, kc, h, :], cqn.t[:, kc, :], kc == 0, kc == 1, [wuq_sw, cqn], [pq2])
                        s.op("act", lambda e: e.copy(out=q_.t[0:64, h, :], in_=pq1.t[0:64, :]), [pq1], [q_])
                        s.op("dve", lambda e: e.tensor_tensor(a1.t[R, :], pq1.t[R, :], cs.t[R, 0, :], ALU.mult), [pq1, cs], [a1])
                        s.op("dve", lambda e: e.tensor_tensor(a2.t[R, :], pq2.t[R, :], cs.t[R, 1, :], ALU.mult), [pq2, cs], [a2])
                        s.op("pool", lambda e: e.tensor_tensor(q_.t[R, h, :], a1.t[R, :], a2.t[R, :], ALU.add), [a1, a2], [q_])
                        pk = pp[ppi % 8]; ppi += 1
                        s.mm(pk.t[0:64, :], wk.t[:, h, :], ckvn.t[:], True, True, [wk, ckvn], [pk])
                        s.op("act", lambda e: e.copy(out=k_.t[0:64, h, :], in_=pk.t[0:64, :]), [pk], [k_])
                        s.op("pool", lambda e: e.tensor_copy(out=k_.t[R, h, :], in_=krT.t[R, :]), [krT], [k_])
                    s.dma("sp", qT_d[:, :, tsl].rearrange("h p t -> p h t"), q_.t[0:96, :, :], [q_], [b_q], group="st")
                    s.dma("sp", kT_d[:, :, tsl].rearrange("h p t -> p h t"), k_.t[0:96, :, :], [k_], [b_k], group="st")
                    for tt in range(4):
                        i = 4 * g + tt
                        v_ = vt[tt % 2]
                        pv = pp[ppi % 8]; ppi += 1
                        s.mm(pv.t[:], ckvn.t[:, tt * 128:(tt + 1) * 128], wvv.t[:].rearrange("p h e -> p (h e)"), True, True, [ckvn, wvv], [pv])
                        s.op("act", lambda e: e.copy(out=v_.t[:, :, 0:64], in_=pv.t[:].rearrange("p (h e) -> p h e", e=64)), [pv], [v_])
                        s.dma("sp", v_d[i * 128:(i + 1) * 128, :, :], v_.t[:], [v_], [b_v], group="st")
            with s.phase():
                masks = s.sb([128, 4, 512], BF16, "masks")
                s.op("pool", lambda e: e.memset(masks.t[:], 1.0), [], [masks])
                for j in range(4):
                    s.op("pool", lambda e, j=j: e.affine_select(out=masks.t[:, j, :], in_=masks.t[:, j, :], pattern=[[1, 512]], compare_op=ALU.is_ge,
                                                                fill=0.0, base=-128 * j, channel_multiplier=-1), [masks], [masks])
                kTh = [s.sb([128, S], BF16, "kTh") for _ in range(2)]
                vh = [s.sb([128, NT, 65], BF16, "vh") for _ in range(2)]
                qg = [s.sb([128, 512], BF16, "qg") for _ in range(2)]
                PT = [s.sb([128, 512], BF16, "PT") for _ in range(3)]
                psT = [s.ps([128, 512], F32, "psT") for _ in range(3)]
                pacc = [s.ps([128, 65], F32, "pacc") for _ in range(4)]
                rec = [s.sb([128, 1], F32, "rec") for _ in range(2)]
                yd = [s.sb([128, 64], BF16, "yd") for _ in range(2)]
                it = 0
                for h in range(8):
                    kt = kTh[h % 2]; vv = vh[h % 2]
                    s.dma("sp", kt.t[0:96, :], kT_d[h], [b_k], [kt])
                    s.dma("sp", vv.t[:], v_d[:, h, :].rearrange("(t p) e -> p t e", p=128), [b_v], [vv])
                    for G in range(NG):
                        q_ = qg[G % 2]
                        s.dma("sp", q_.t[0:96, :], qT_d[h, :, G * 512:(G + 1) * 512], [b_q], [q_])
                        nkb = 4 * G + 4

                        def qk(kb):
                            nonlocal it
                            ps_ = psT[it % 3]; it += 1
                            s.mm(ps_.t[:], kt.t[0:96, kb * 128:(kb + 1) * 128], q_.t[0:96, :], True, True, [kt, q_], [ps_])
                            return ps_

                        pend = [qk(0)]
                        if nkb > 1:
                            pend.append(qk(1))
                        for kb in range(nkb):
                            ps_ = pend.pop(0)
                            if kb + 2 < nkb:
                                pend.append(qk(kb + 2))
                            pt = PT[kb % 3]
                            j = kb - 4 * G
                            c0 = max(j, 0) * 128
                            s.op("act", lambda e: e.activation(out=pt.t[:, c0:512], in_=ps_.t[:, c0:512], func=AF.Exp, scale=SCALE), [ps_], [pt])
                            if j >= 0:
                                s.op("dve", lambda e: e.tensor_tensor(pt.t[:, c0:c0 + 128], pt.t[:, c0:c0 + 128], masks.t[:, 0, 0:128], ALU.mult), [pt, masks], [pt])
                            for qs in range(4):
                                last_kb = 4 * G + qs
                                if kb > last_kb:
                                    continue
                                s.mm(pacc[qs].t[:], pt.t[:, qs * 128:(qs + 1) * 128], vv.t[:, kb, :], kb == 0, kb == last_kb, [pt, vv], [pacc[qs]])
                                if kb == last_kb:
                                    r_ = rec[qs % 2]; y_ = yd[qs % 2]
                                    i = 4 * G + qs
                                    s.op("dve", lambda e: e.reciprocal(r_.t[:], pacc[qs].t[:, 64:65]), [pacc[qs]], [r_])
                                    s.op("dve", lambda e: e.tensor_scalar(y_.t[:], pacc[qs].t[:, 0:64], r_.t[:, 0:1], None, ALU.mult), [pacc[qs], r_], [y_])
                                    s.dma("sp", yd_d[i * 128:(i + 1) * 128, h * 64:(h + 1) * 64], y_.t[:], [y_], [b_yd], group="st")
            with s.phase():
                wout = s.sb([128, 8, D], BF16, "wout")
                gpw = load_mod(l, 2, True, "gpw", 1.0 / ALPHA)
                wstg = [s.sb([128, D], F32, "wstg") for _ in range(2)]
                for k in range(8):
                    s.dma("sp", wstg[k % 2].t[:], od_w_out[li, k * 128:(k + 1) * 128, :], [], [wstg[k % 2]])
                    s.op("dve", lambda e, k=k: e.tensor_tensor(wout.t[:, k, :], wstg[k % 2].t[:], gpw.t[:], ALU.mult), [wstg[k % 2], gpw], [wout])
                yT = [s.sb([128, 8, 128], BF16, "yT") for _ in range(2)]
                ydt = [s.sb([128, 512], BF16, "ydt") for _ in range(2)]
                ptr = [s.ps([128, 4, 128], BF16, "ptr") for _ in range(2)]
                pp = [s.ps([128, 512], F32, "pp") for _ in range(4)]
                dl = [s.sb([128, D], F32, "dl") for _ in range(2)]
                for i in range(NT):
                    y = yT[i % 2]; yd_ = ydt[i % 2]; pt = ptr[i % 2]; d_ = dl[i % 2]
                    s.dma("sp", y.t[:, 0:4, :], yc_d[:, :, i * 128:(i + 1) * 128].rearrange("c p t -> p c t"), [b_yc], [y])
                    s.dma("sp", yd_.t[:], yd_d[i * 128:(i + 1) * 128, :], [b_yd], [yd_])
                    for c in range(4):
                        s.op("pe", lambda e, c=c: e.transpose(pt.t[:, c, :], yd_.t[:, c * 128:(c + 1) * 128], ident_b.t[:]), [yd_, ident_b], [pt])
                    s.op("act", lambda e: e.copy(out=y.t[:, 4:8, :], in_=pt.t[:]), [pt], [y])
                    for nh in range(2):
                        p = pp[(2 * i + nh) % 4]
                        for k in range(8):
                            s.mm(p.t[:], y.t[:, k, :], wout.t[:, k, nh * 512:(nh + 1) * 512], k == 0, k == 7, [y, wout], [p])
                        s.op("act", lambda e: e.copy(out=d_.t[:, nh * 512:(nh + 1) * 512], in_=p.t[:]), [p], [d_])
                    s.dma("sp", delta[i * 128:(i + 1) * 128, :], d_.t[:], [d_], [b_delta[i]])

        def norm_pass(l, kind, x_src, last):
            with s.phase():
                mix = kind == "mix"
                need_u = not (last and not mix)
                if mix:
                    lng = load_row(ln_mix_g[l:l + 1, :], D, "lng"); lnb = load_row(ln_mix_b[l:l + 1, :], D, "lnb")
                    sc1 = load_mod(l, 4, True, "sc1"); sh = load_mod(l, 3, False, "sh")
                else:
                    lng = load_row(ln_ffn_g[l:l + 1, :], D, "lng"); lnb = load_row(ln_ffn_b[l:l + 1, :], D, "lnb")
                    if need_u:
                        sc1 = load_mod(l + 1, 1, True, "sc1"); sh = load_mod(l + 1, 0, False, "sh")
                if need_u:
                    B2 = sh
                    tmpb = s.sb([128, D], F32, "tmpb")
                    s.op("dve", lambda e: e.tensor_tensor(tmpb.t[:], lnb.t[:], sc1.t[:], ALU.mult), [lnb, sc1], [tmpb])
                    s.op("dve", lambda e: e.tensor_tensor(B2.t[:], tmpb.t[:], sh.t[:], ALU.add), [tmpb, sh], [B2])
                    G2 = sc1
                    s.op("dve", lambda e: e.tensor_tensor(G2.t[:], sc1.t[:], lng.t[:], ALU.mult), [sc1, lng], [G2])
                eps2 = s.sb([128, 1], F32, "eps2")
                s.op("pool", lambda e: e.memset(eps2.t[:], LN_EPS / (ALPHA * ALPHA)), [], [eps2])
                NB = 4
                xt = [s.sb([128, D], F32, "xt") for _ in range(NB)]
                st12 = [s.sb([128, 12], F32, "st12") for _ in range(NB)]
                mv = [s.sb([128, 2], F32, "mv") for _ in range(NB)]
                rstd = [s.sb([128, 1], F32, "rstd") for _ in range(NB)]
                nbias = [s.sb([128, 1], F32, "nbias") for _ in range(NB)]
                nt = [s.sb([128, D], F32, "nt") for _ in range(3)]
                xo = [s.sb([128, D], F32, "xo") for _ in range(3)]
                ub = [s.sb([128, D], BF16, "ub") for _ in range(3)]
                if mix:
                    yt = [s.sb([128, D], F32, "yt") for _ in range(NB)]
                    uf = [s.sb([128, D], F32, "uf") for _ in range(3)]
                    ptf = [s.ps([128, 4, 128], F32, "ptf") for _ in range(4)]
                    uTf = [s.sb([128, 8, 128], F32, "uTf") for _ in range(3)]
                    rw = s.sb([128, 8, NE], F32, "rw")
                    s.dma("sp", rw.t[:], r_w[l].rearrange("(k p) e -> p k e", p=128), [], [rw])
                    rb = load_row(r_b[l:l + 1, :], NE, "rb")
                    plog = [s.ps([128, NE], F32, "plog") for _ in range(2)]
                    prk = [s.ps([128, NE], F32, "prk") for _ in range(2)]
                    lg = [s.sb([128, NE], F32, "lg") for _ in range(3)]
                    t8 = [s.sb([128, 8], F32, "t8") for _ in range(2)]
                    nv0 = [s.sb([128, 1], F32, "nv0") for _ in range(2)]
                    ex4 = [s.sb([128, 4], F32, "ex4") for _ in range(2)]
                    sm = [s.sb([128, 1], F32, "sm") for _ in range(2)]
                    msk = [s.sb([128, NE], F32, "msk") for _ in range(2)]
                    msum = s.sb([128, NE], F32, "msum")
                    oh = s.sb([128, NT, 4, NE], F32, "oh")
                    prod = s.sb([128, 4, NE], F32, "prod")
                    s.op("pool", lambda e: e.memset(msum.t[:], 0.0), [], [msum])
                else:
                    yk = [s.sb([128, D], F32, "yk") for _ in range(12)]
                    if need_u:
                        ptr = [s.ps([128, 8, 128], BF16, "ptr") for _ in range(2)]
                        uTs = [s.sb([128, 8, 128], BF16, "uTs") for _ in range(2)]
                        uf = [s.sb([128, D], F32, "uf") for _ in range(3)]

                def stageA(i):
                    x = xt[i % NB]; s12 = st12[i % NB]; m_ = mv[i % NB]; rs = rstd[i % NB]; nb_ = nbias[i % NB]
                    s.dma("sp", x.t[:], x_src[i * 128:(i + 1) * 128, :], [b_xres[i]] if x_src is xres else [], [x])
                    if mix:
                        y = yt[i % NB]
                        s.dma("sp", y.t[:], delta[i * 128:(i + 1) * 128, :], [b_delta[i]], [y])
                        s.op("dve", lambda e: e.tensor_tensor(x.t[:], x.t[:], y.t[:], ALU.add), [x, y], [x])
                    else:
                        ys_ = [yk[(i % 3) * 4 + k] for k in range(4)]
                        for k in range(4):
                            s.idma(ys_[k].t[:], None, ys_d, bass.IndirectOffsetOnAxis(ap=posk_i.t[:, i, k:k + 1], axis=0),
                                   [b_ys_all, posk_i], [ys_[k]])
                        for k in range(4):
                            s.op("act", lambda e, k=k: e.activation(out=ys_[k].t[:], in_=ys_[k].t[:], func=AF.Copy, scale=gatek.t[:, i, k:k + 1]), [ys_[k], gatek], [ys_[k]])
                        s.op("dve", lambda e: e.tensor_tensor(ys_[0].t[:], ys_[0].t[:], ys_[1].t[:], ALU.add), [ys_[0], ys_[1]], [ys_[0]])
                        s.op("dve", lambda e: e.tensor_tensor(ys_[2].t[:], ys_[2].t[:], ys_[3].t[:], ALU.add), [ys_[2], ys_[3]], [ys_[2]])
                        s.op("dve", lambda e: e.tensor_tensor(ys_[0].t[:], ys_[0].t[:], ys_[2].t[:], ALU.add), [ys_[0], ys_[2]], [ys_[0]])
                        s.op("dve", lambda e: e.tensor_tensor(x.t[:], x.t[:], ys_[0].t[:], ALU.add), [x, ys_[0]], [x])
                    s.op("dve", lambda e: e.bn_stats(s12.t[:, 0:6], x.t[:, 0:512]), [x], [s12])
                    s.op("dve", lambda e: e.bn_stats(s12.t[:, 6:12], x.t[:, 512:1024]), [x], [s12])
                    s.op("dve", lambda e: e.bn_aggr(m_.t[:], s12.t[:]), [s12], [m_])
                    s.op("act", lambda e: e.activation(out=rs.t[:], in_=m_.t[:, 1:2], func=AF.Ln, bias=eps2.t[:, 0:1]), [m_, eps2], [rs])
                    s.op("act", lambda e: e.activation(out=rs.t[:], in_=rs.t[:], func=AF.Exp, scale=-0.5), [rs], [rs])
                    s.op("dve", lambda e: e.scalar_tensor_tensor(nb_.t[:], m_.t[:, 0:1], -1.0, rs.t[:], ALU.mult, ALU.mult), [m_, rs], [nb_])

                def stageB(i):
                    x = xt[i % NB]; rs = rstd[i % NB]; nb_ = nbias[i % NB]
                    n_ = nt[i % 3]; o_ = xo[i % 3]
                    s.op("act", lambda e: e.activation(out=n_.t[:], in_=x.t[:], func=AF.Identity, scale=rs.t[:, 0:1], bias=nb_.t[:, 0:1]), [x, rs, nb_], [n_])
                    s.op("pool", lambda e: e.tensor_tensor(o_.t[:], n_.t[:], lng.t[:], ALU.mult), [n_, lng], [o_])
                    s.op("pool", lambda e: e.tensor_tensor(o_.t[:], o_.t[:], lnb.t[:], ALU.add), [o_, lnb], [o_])
                    if not need_u:
                        s.dma("sp", out[i * 128:(i + 1) * 128, :], o_.t[:], [o_], [b_xres[i]])
                        return
                    s.dma("sp", xres[i * 128:(i + 1) * 128, :], o_.t[:], [o_], [b_xres[i]])
                    u = ub[i % 3]; f = uf[i % 3]
                    s.op("dve", lambda e: e.tensor_tensor(f.t[:], n_.t[:], G2.t[:], ALU.mult), [n_, G2], [f])
                    if not mix:
                        s.op("dve", lambda e: e.tensor_tensor(u.t[:], f.t[:], B2.t[:], ALU.add), [f, B2], [u])
                        transposes_store_uT(u, i, ptr[i % 2], uTs[i % 2])
                        return
                    s.op("dve", lambda e: e.tensor_tensor(f.t[:], f.t[:], B2.t[:], ALU.add), [f, B2], [f])
                    s.op("act", lambda e: e.copy(out=u.t[:], in_=f.t[:]), [f], [u])
                    s.dma("sp", u_d[i * 128:(i + 1) * 128, :], u.t[:], [u], [b_u[i]])
                    uT = uTf[i % 3]
                    for hf in range(2):
                        pt = ptf[(2 * i + hf) % 4]
                        for kk in range(4):
                            k = hf * 4 + kk
                            s.op("pe", lambda e, k=k, kk=kk: e.transpose(pt.t[:, kk, :], f.t[:, k * 128:(k + 1) * 128], ident_f.t[:]),
                                 [f, ident_f], [pt])
                        s.op("act", lambda e: e.copy(out=uT.t[:, hf * 4:hf * 4 + 4, :], in_=pt.t[:]), [pt], [uT])
                    pl = plog[i % 2]; lgt = lg[i % 3]
                    for k in range(8):
                        s.mm(pl.t[:], uT.t[:, k, :], rw.t[:, k, :], k == 0, k == 7, [uT, rw], [pl])
                    s.op("dve", lambda e: e.tensor_tensor(lgt.t[:], pl.t[:], rb.t[:], ALU.add), [pl, rb], [lgt])

                def stageC(i):
                    lgt = lg[i % 3]; t8_ = t8[i % 2]; mk = msk[i % 2]
                    s.op("dve", lambda e: e.max(out=t8_.t[:], in_=lgt.t[:]), [lgt], [t8_])
                    pr = prk[i % 2]
                    s.op("dve", lambda e: e.tensor_scalar(mk.t[:], lgt.t[:], t8_.t[:, 3:4], None, ALU.is_ge), [lgt, t8_], [mk])
                    s.mm(pr.t[:], su_f.t[:], mk.t[:], True, False, [su_f, mk], [pr])
                    s.mm(pr.t[:], ones_f.t[:], msum.t[:], False, True, [ones_f, msum], [pr])
                    for k in range(4):
                        s.op("dve", lambda e, k=k: e.tensor_scalar(oh.t[:, i, k, :], lgt.t[:], t8_.t[:, k:k + 1], None, ALU.is_equal), [lgt, t8_], [oh])
                    n0 = nv0[i % 2]; e4 = ex4[i % 2]; sm_ = sm[i % 2]
                    s.op("dve", lambda e: e.tensor_scalar(n0.t[:], t8_.t[:, 0:1], -1.0, None, ALU.mult), [t8_], [n0])
                    s.op("act", lambda e: e.activation(out=e4.t[:], in_=t8_.t[:, 0:4], func=AF.Exp, bias=n0.t[:, 0:1]), [t8_, n0], [e4])
                    for k in range(4):
                        s.op("dve", lambda e, k=k: e.tensor_tensor(prod.t[:, k, :], oh.t[:, i, k, :], pr.t[:], ALU.mult), [oh, pr], [prod])
                    s.op("dve", lambda e: e.tensor_reduce(out=posk_f.t[:, i, :], in_=prod.t[:], axis=AX.X, op=ALU.add), [prod], [posk_f])
                    s.op("dve", lambda e: e.tensor_tensor(msum.t[:], msum.t[:], mk.t[:], ALU.add), [msum, mk], [msum])
                    s.op("dve", lambda e: e.tensor_reduce(out=sm_.t[:], in_=e4.t[:], axis=AX.X, op=ALU.add), [e4], [sm_])
                    s.op("dve", lambda e: e.reciprocal(sm_.t[:], sm_.t[:]), [sm_], [sm_])
                    s.op("dve", lambda e: e.tensor_scalar(gatek.t[:, i, :], e4.t[:], sm_.t[:, 0:1], None, ALU.mult), [e4, sm_], [gatek])

                for step in range(NT + 2):
                    if step < NT:
                        stageA(step)
                    if 1 <= step <= NT:
                        stageB(step - 1)
                    if mix and step >= 2:
                        stageC(step - 2)
                if kind == "mix":
                    pc = prk[0]
                    s.mm(pc.t[:], ones_f.t[:], msum.t[:], True, True, [ones_f, msum], [pc])
                    cnt = s.sb([128, NE], F32, "cnt")
                    nch = s.sb([128, NE], F32, "nch")
                    tmpc = s.sb([128, NE], F32, "tmpc")
                    cs_a = s.sb([128, NE], F32, "csa")
                    cs_b = s.sb([128, NE], F32, "csb")
                    s.op("dve", lambda e: e.tensor_copy(out=cnt.t[:], in_=pc.t[:]), [pc], [cnt])
                    s.op("dve", lambda e: e.tensor_scalar(nch.t[:], cnt.t[:], 0.5, None, ALU.is_gt), [cnt], [nch])
                    for j in range(1, S // CH):
                        s.op("dve", lambda e, j=j: e.tensor_scalar(tmpc.t[:], cnt.t[:], CH * j + 0.5, None, ALU.is_gt), [cnt], [tmpc])
                        s.op("dve", lambda e: e.tensor_tensor(nch.t[:], nch.t[:], tmpc.t[:], ALU.add), [nch, tmpc], [nch])
                    s.op("dve", lambda e: e.tensor_copy(out=cs_a.t[:], in_=nch.t[:]), [nch], [cs_a])
                    a, b = cs_a, cs_b
                    sh_ = 1
                    while sh_ < NE:
                        s.op("dve", lambda e, a=a, b=b, sh_=sh_: e.tensor_copy(out=b.t[:, 0:sh_], in_=a.t[:, 0:sh_]), [a], [b])
                        s.op("dve", lambda e, a=a, b=b, sh_=sh_: e.tensor_tensor(b.t[:, sh_:NE], a.t[:, sh_:NE], a.t[:, 0:NE - sh_], ALU.add), [a], [b])
                        a, b = b, a
                        sh_ *= 2
                    cend = a
                    base = s.sb([128, NE], F32, "base")
                    s.op("dve", lambda e: e.tensor_tensor(base.t[:], cend.t[:], nch.t[:], ALU.subtract), [cend, nch], [base])
                    s.op("dve", lambda e: e.tensor_scalar(base.t[:], base.t[:], float(CH), None, ALU.mult), [base], [base])
                    prod2 = s.sb([128, NT, 4, NE], F32, "prod2")
                    for i in range(NT):
                        for k in range(4):
                            s.op("pool", lambda e, i=i, k=k: e.tensor_tensor(prod2.t[:, i, k, :], oh.t[:, i, k, :], base.t[:], ALU.mult), [oh, base], [prod2])
                    bsel = s.sb([128, NT, 4], F32, "bsel")
                    s.op("dve", lambda e: e.tensor_reduce(out=bsel.t[:], in_=prod2.t[:], axis=AX.X, op=ALU.add), [prod2], [bsel])
                    s.op("dve", lambda e: e.tensor_tensor(posk_f.t[:], posk_f.t[:], bsel.t[:], ALU.add), [posk_f, bsel], [posk_f])
                    s.op("dve", lambda e: e.tensor_copy(out=posk_i.t[:], in_=posk_f.t[:]), [posk_f], [posk_i])
                    cmpt = s.sb([128, NE], F32, "cmpt")
                    cef = s.sb([128, NCH], F32, "cef")
                    wif = s.sb([128, NCH, 8], F32, "wif")
                    tf = s.sb([128, NCH], F32, "tf")
                    for c in range(NCH):
                        s.op("dve", lambda e, c=c: e.tensor_scalar(cmpt.t[:], cend.t[:], float(c), None, ALU.is_le), [cend], [cmpt])
                        s.op("dve", lambda e, c=c: e.tensor_reduce(out=cef.t[:, c:c + 1], in_=cmpt.t[:], axis=AX.X, op=ALU.add), [cmpt], [cef])
                    s.op("dve", lambda e: e.tensor_scalar(cef.t[:], cef.t[:], float(NE - 1), float(l * NE), ALU.min, ALU.add), [cef], [cef])
                    s.op("dve", lambda e: e.tensor_copy(out=bdidx.t[:], in_=cef.t[:]), [cef], [bdidx])
                    s.op("dve", lambda e: e.tensor_scalar(tf.t[:], cef.t[:], 128.0, iota_p.t[:, 0:1], ALU.mult, ALU.add), [cef, iota_p], [tf])
                    s.op("dve", lambda e: e.tensor_copy(out=bgidx.t[:], in_=tf.t[:]), [tf], [bgidx])
                    s.op("dve", lambda e: e.tensor_scalar(tf.t[:], cef.t[:], 1024.0, None, ALU.mult), [cef], [tf])
                    for c in range(NCH):
                        s.op("dve", lambda e, c=c: e.tensor_scalar(wif.t[:, c, :], base_pk.t[:], tf.t[:, c:c + 1], None, ALU.add), [base_pk, tf], [wif])
                    s.op("dve", lambda e: e.tensor_copy(out=widx.t[:], in_=wif.t[:]), [wif], [widx])
                    if "posk" in dbg_out:
                        s.dma("sp", dbg_out["posk"].rearrange("(i p) k -> p i k", p=128), posk_f.t[:], [posk_f], [])
                        s.dma("sp", dbg_out["gatek"].rearrange("(i p) k -> p i k", p=128), gatek.t[:], [gatek], [])
                        s.dma("sp", dbg_out["ctab"], cef.t[:, 0:NCH], [cef], [])

        def scatter_pass():
            with s.phase():
                ub = [s.sb([128, D], BF16, "ub") for _ in range(3)]
                for i in range(NT):
                    u = ub[i % 3]
                    s.dma("sp", u.t[:], u_d[i * 128:(i + 1) * 128, :], [b_u[i]], [u])
                    for k in range(4):
                        s.idma(xs_d, bass.IndirectOffsetOnAxis(ap=posk_i.t[:, i, k:k + 1], axis=0), u.t[:], None,
                               [u, posk_i], [b_xs_all], group="sc")

        def expert_pass(l):
            with s.phase():
                wgu = [s.sb([128, 8, 2 * D], BF16, "wgu") for _ in range(2)]
                wdn = [s.sb([128, 8, D], BF16, "wdn") for _ in range(2)]
                bgu = [s.sb([128, 16], F32, "bgu") for _ in range(2)]
                bdn = [s.sb([128, D], BF16, "bdn") for _ in range(2)]
                xsb = [[s.sb([128, D], BF16, "xsb") for _ in range(4)] for _ in range(2)]
                xT = [s.sb([128, 8, 512], BF16, "xT") for _ in range(2)]
                ptr = [s.ps([128, 8, 128], BF16, "ptr") for _ in range(2)]
                pg = [s.ps([128, 512], F32, "pg") for _ in range(4)]
                pd = [s.ps([128, 512], F32, "pd") for _ in range(2)]
                gc = [s.sb([128, 512], F32, "gc") for _ in range(2)]
                sg = [s.sb([128, 512], F32, "sg") for _ in range(2)]
                uc = [s.sb([128, 512], F32, "uc") for _ in range(2)]
                actT = [s.sb([128, 8, 512], BF16, "actT") for _ in range(2)]
                ysb = [s.sb([128, D], F32, "ysb") for _ in range(2)]
                gpf = load_mod(l, 5, True, "gpf", 1.0 / ALPHA)
                wguv = w_gu.rearrange("l e r f -> (l e r) f")
                wdnv = w_dn.rearrange("l e r f -> (l e r) f")
                bguv = b_guT.rearrange("l e p c -> (l e p) c")
                bdnv = b_dn.rearrange("l e f -> (l e) f")
                IO = bass.IndirectOffsetOnAxis

                def prefetch(c):
                    wg = wgu[c % 2]; wd = wdn[c % 2]; bg = bgu[c % 2]; bd = bdn[c % 2]
                    for k in range(8):
                        s.idma(wg.t[:, k, :], None, wguv, IO(ap=widx.t[:, c, k:k + 1], axis=0), [widx], [wg], group=("wg", c))
                    s.idma(bg.t[:], None, bguv, IO(ap=bgidx.t[:, c:c + 1], axis=0), [bgidx], [bg])
                    s.op("dve", lambda e: e.tensor_scalar(bg.t[:, 8:16], bg.t[:, 8:16], 1.0, None, ALU.add), [bg], [bg])
                    for k in range(8):
                        s.idma(wd.t[:, k, :], None, wdnv, IO(ap=widx.t[:, c, k:k + 1], axis=0), [widx], [wd], group=("wd", c))
                    s.idma(bd.t[:], None, bdnv, IO(ap=bdidx.t[:, c:c + 1], axis=0), [bdidx], [bd])
                    for sb_ in range(4):
                        xb = xsb[c % 2][sb_]
                        r0 = c * CH + sb_ * 128
                        s.dma("sp", xb.t[:], xs_d[r0:r0 + 128, :], [b_xs_all], [xb])

                def compute(c):
                    wg = wgu[c % 2]; wd = wdn[c % 2]; bg = bgu[c % 2]; bd = bdn[c % 2]
                    xt_ = xT[c % 2]; at = actT[c % 2]
                    for sb_ in range(4):
                        xb = xsb[c % 2][sb_]; pt = ptr[sb_ % 2]
                        for k in range(8):
                            s.op("pe", lambda e, k=k: e.transpose(pt.t[:, k, :], xb.t[:, k * 128:(k + 1) * 128], ident_b.t[:]), [xb, ident_b], [pt])
                        s.op("act", lambda e: e.copy(out=xt_.t[:, :, sb_ * 128:(sb_ + 1) * 128], in_=pt.t[:]), [pt], [xt_])
                    for j in range(8):
                        p_g = pg[(2 * j) % 4]; p_u = pg[(2 * j + 1) % 4]
                        for k in range(8):
                            s.mm(p_g.t[:], wg.t[:, k, j * 128:(j + 1) * 128], xt_.t[:, k, :], k == 0, k == 7, [wg, xt_], [p_g])
                        for k in range(8):
                            s.mm(p_u.t[:], wg.t[:, k, D + j * 128:D + (j + 1) * 128], xt_.t[:, k, :], k == 0, k == 7, [wg, xt_], [p_u])
                        g_ = gc[j % 2]; s_ = sg[j % 2]; u_ = uc[j % 2]
                        s.op("dve", lambda e: e.tensor_scalar(g_.t[:], p_g.t[:], bg.t[:, j:j + 1], 7.0, ALU.add, ALU.min), [p_g, bg], [g_])
                        s.op("act", lambda e: e.activation(out=s_.t[:], in_=g_.t[:], func=AF.Sigmoid, scale=1.702), [g_], [s_])
                        s.op("dve", lambda e: e.tensor_scalar(u_.t[:], p_u.t[:], bg.t[:, 8 + j:9 + j], 8.0, ALU.add, ALU.min), [p_u, bg], [u_])
                        s.op("dve", lambda e: e.tensor_tensor(g_.t[:], g_.t[:], s_.t[:], ALU.mult), [g_, s_], [g_])
                        s.op("dve", lambda e: e.scalar_tensor_tensor(at.t[:, j, :], u_.t[:], -6.0, g_.t[:], ALU.max, ALU.mult), [g_, u_], [at])
                    for sb_ in range(4):
                        yb = ysb[sb_ % 2]
                        for nh in range(2):
                            p = pd[nh]
                            for j in range(8):
                                s.mm(p.t[:], at.t[:, j, sb_ * 128:(sb_ + 1) * 128], wd.t[:, j, nh * 512:(nh + 1) * 512], j == 0, False, [at, wd], [p])
                            s.mm(p.t[:], ones_b.t[0:1, :], bd.t[0:1, nh * 512:(nh + 1) * 512], False, True, [ones_b, bd], [p])
                            s.op("dve", lambda e: e.tensor_tensor(yb.t[:, nh * 512:(nh + 1) * 512], p.t[:], gpf.t[:, nh * 512:(nh + 1) * 512], ALU.mult), [p, gpf], [yb])
                        r0 = c * CH + sb_ * 128
                        s.dma("act", ys_d[r0:r0 + 128, :], yb.t[:], [yb], [b_ys_all], group=("ys", l))

                prefetch(0)
                for c in range(NCH):
                    if c + 1 < NCH:
                        prefetch(c + 1)
                    compute(c)

        eps_t = s.sb([128, 1], F32, "eps")
        s.op("pool", lambda e: e.memset(eps_t.t[:], LN_EPS), [], [eps_t])

        prologue()
        premod(0)
        x_src = x_in
        stop_after = (dbg or {}).get("_stop", None)
        for l in range(layers):
            if l % 2 == 0:
                even_mixer(l)
            else:
                odd_mixer(l)
            norm_pass(l, "mix", x_src, False)
            x_src = xres
            scatter_pass()
            expert_pass(l)
            norm_pass(l, "ffn", x_src, l == layers - 1)
        s.barrier()
    return nc


def prep_inputs(inputs, S=4096, ncores=8, layers=DEPTH):
    f = lambda a: np.ascontiguousarray(np.asarray(a))
    shared = {}
    for k in ("ada_w", "ada_b", "ln_mix_g", "ln_mix_b", "ln_ffn_g", "ln_ffn_b", "ev_w_in", "ev_sg_b", "ev_vn_g", "ev_vn_b",
              "ev_w_out", "od_w_in", "od_w_uq", "od_w_ukv", "od_w_out", "moe_router_w", "moe_router_b", "moe_w_gu",
              "moe_w_dn", "moe_b_dn"):
        shared[k] = f(inputs[k][:layers]) if k in ("moe_w_gu", "moe_w_dn", "ada_w") else f(inputs[k])
    shared["ev_conv"] = f(np.asarray(inputs["ev_conv_w"]).transpose(0, 2, 1).reshape(2, 4, 128, 3).transpose(0, 2, 1, 3))
    shared["ev_sgT"] = f(np.asarray(inputs["ev_sg_w"]).transpose(0, 1, 3, 2))
    shared["od_dw"] = f(np.asarray(inputs["od_dw_w"]).transpose(0, 2, 1).reshape(2, 4, 128, 31).transpose(0, 2, 1, 3))
    col = lambda a, n: f(np.asarray(a).reshape(2, n, 128).transpose(0, 2, 1))
    shared["od_dw_b"] = col(inputs["od_dw_b"], 4)
    shared["od_cn_g"] = col(inputs["od_cn_g"], 4)
    shared["od_cn_b"] = col(inputs["od_cn_b"], 4)
    shared["od_qn_g"] = col(inputs["od_qn_g"], 2)
    shared["od_kvn_g"] = col(inputs["od_kvn_g"], 1)
    shared["moe_b_guT"] = f(np.asarray(inputs["moe_b_gu"]).reshape(DEPTH, NE, 16, 128).transpose(0, 1, 3, 2))
    invf = (10000.0 ** (-np.arange(0, 32, 2, dtype=np.float32) / 32)).astype(np.float32)
    shared["invf"] = f(np.concatenate([invf, invf]).reshape(32, 1))
    maps = []
    x = np.asarray(inputs["x"]); c = np.asarray(inputs["c"]); pos = np.asarray(inputs["positions"])
    for b in range(ncores):
        m = dict(shared)
        m["x"] = f(x[b, :S])
        m["c"] = f(c[b].reshape(8, 128).T)
        m["pos"] = f(np.broadcast_to(pos[b, :S].astype(np.int32)[None, :], (32, S)))
        maps.append(m)
    return maps


_NC_CACHE = {}


def kernel(**inputs):
    S = 4096
    if "nc" not in _NC_CACHE:
        _NC_CACHE["nc"] = build(S)
    nc = _NC_CACHE["nc"]
    maps = prep_inputs(inputs, S, 8)
    res = run_bass_kernel_spmd(nc, maps, core_ids=list(range(8)))
    return np.stack([np.asarray(r["out"]) for r in res.results], axis=0).astype(np.float32)
```

```python
import math
import numpy as np
from contextlib import ExitStack
import concourse.bass as bass
import concourse.mybir as mybir
from concourse.bass_utils import run_bass_kernel_spmd

F32 = mybir.dt.float32
BF16 = mybir.dt.bfloat16
I32 = mybir.dt.int32
AF = mybir.ActivationFunctionType
ALU = mybir.AluOpType
AX = mybir.AxisListType

D = 1024
DEPTH = 4
NE = 32
TOPK = 4
CH = 512
ALPHA = (2.0 * DEPTH) ** 0.25
LN_EPS = 1e-5
RMS_EPS = 1e-6
TWO_PI_HI = 6.28125
TWO_PI_LO = 2.0 * math.pi - 6.28125


class Buf:
    __slots__ = ("w", "r", "g")

    def __init__(self):
        self.w = []
        self.r = []
        self.g = None


class T:
    __slots__ = ("t", "b")

    def __init__(self, t):
        self.t = t
        self.b = Buf()


class Sched:
    ENG = ("pe", "act", "dve", "pool", "sp")

    def __init__(self, nc, es, ndma=12):
        self.nc = nc
        self.es = es
        self.eng = {"pe": nc.tensor, "act": nc.scalar, "dve": nc.vector, "pool": nc.gpsimd, "sp": nc.sync}
        self.sem = {}
        self.cnt = {}
        self.seen = {e: {} for e in self.ENG}
        for e in self.ENG:
            self.sem[e] = es.enter_context(nc.semaphore("s_" + e))
            self.cnt[e] = 0
        self.dq = {}
        for q in ("sp", "pool", "act"):
            lst = []
            for i in range(ndma if q != "act" else 8):
                key = "d_%s%d" % (q, i)
                self.sem[key] = es.enter_context(nc.semaphore(key))
                self.cnt[key] = 0
                lst.append(key)
            self.dq[q] = [lst, 0]
        self.uid = 0

    def sb(self, shape, dt, name=None):
        self.uid += 1
        return T(self.es_cur.enter_context(self.nc.sbuf_tensor("%s_%d" % (name or "t", self.uid), list(shape), dt)))

    def ps(self, shape, dt, name=None):
        self.uid += 1
        return T(self.es_cur.enter_context(self.nc.psum_tensor("%s_%d" % (name or "p", self.uid), list(shape), dt)))

    def _wait(self, e, evs):
        seen = self.seen[e]
        for key, val in evs:
            if key == "pe" and e == "pe":
                continue
            if seen.get(key, 0) >= val:
                continue
            self.eng[e].wait_ge(self.sem[key], val)
            seen[key] = val

    @staticmethod
    def _deps(reads, writes, group=None):
        evs = []
        for b in reads:
            b = b.b if isinstance(b, T) else b
            evs.extend(b.w)
        for b in writes:
            b = b.b if isinstance(b, T) else b
            if group is None or b.g != group:
                evs.extend(b.w)
            evs.extend(b.r)
        return evs

    @staticmethod
    def _update(ev, reads, writes, group=None):
        for b in reads:
            b = b.b if isinstance(b, T) else b
            b.r.append(ev)
            if len(b.r) > 24:
                last = {}
                for k, v in b.r:
                    if last.get(k, 0) < v:
                        last[k] = v
                b.r = list(last.items())
        for b in writes:
            b = b.b if isinstance(b, T) else b
            if group is not None and b.g == group:
                b.w.append(ev)
            else:
                b.w = [ev]
                b.g = group
            b.r = []

    def op(self, e, fn, reads=(), writes=()):
        self._wait(e, self._deps(reads, writes))
        ins = fn(self.eng[e])
        self.cnt[e] += 1
        ins.then_inc(self.sem[e], 1)
        self.seen[e][e] = max(self.seen[e].get(e, 0), 0)
        self._update((e, self.cnt[e]), reads, writes)

    def mm(self, out, lhsT, rhs, start, stop, reads, writes, **kw):
        self.op("pe", lambda pe: pe.matmul(out, lhsT=lhsT, rhs=rhs, start=start, stop=stop, **kw), reads, writes)

    def dma(self, q, out, in_, reads=(), writes=(), group=None, **kw):
        lst, idx = self.dq[q]
        key = lst[idx % len(lst)]
        self.dq[q][1] = idx + 1
        evs = self._deps(reads, writes, group)
        if self.cnt[key] > 0:
            evs.append((key, self.cnt[key]))
        self._wait(q, evs)
        ins = self.eng[q].dma_start(out=out, in_=in_, **kw)
        self.cnt[key] += 16
        ins.then_inc(self.sem[key], 16)
        self._update((key, self.cnt[key]), reads, writes, group)

    def idma(self, out, out_off, in_, in_off, reads=(), writes=(), group=None):
        q = "pool"
        lst, idx = self.dq[q]
        key = lst[idx % len(lst)]
        self.dq[q][1] = idx + 1
        evs = self._deps(reads, writes, group)
        if self.cnt[key] > 0:
            evs.append((key, self.cnt[key]))
        self._wait(q, evs)
        ins = self.eng[q].indirect_dma_start(out=out, out_offset=out_off, in_=in_, in_offset=in_off)
        self.cnt[key] += 16
        ins.then_inc(self.sem[key], 16)
        self._update((key, self.cnt[key]), reads, writes, group)

    def barrier(self):
        evs = [(k, v) for k, v in self.cnt.items() if v > 0]
        for e in self.ENG:
            self._wait(e, evs)

    def phase(self):
        return _Phase(self)


class _Phase:
    def __init__(self, s):
        self.s = s

    def __enter__(self):
        self.es = ExitStack()
        self.es.__enter__()
        self.s.es_cur = self.es
        return self.s

    def __exit__(self, *a):
        self.s.barrier()
        self.es.__exit__(*a)
        return False


def bcast(ap, n=128):
    return ap.partition_broadcast(n)


def build(S=4096, layers=DEPTH, dbg=None):
    NT = S // 128
    NG = S // 512
    NCH = (S * TOPK) // CH + NE
    NSLOT = NCH * CH
    nc = bass.Bass("TRN2", target_bir_lowering=False)

    def din(name, shape, dt=F32):
        return nc.dram_tensor(name, list(shape), dt, kind="ExternalInput").ap()

    def dscr(name, shape, dt=F32):
        return nc.dram_tensor(name, list(shape), dt, kind="Internal").ap()

    n_even = (layers + 1) // 2
    n_odd = layers // 2
    x_in = din("x", [S, D])
    c_in = din("c", [128, 8])
    pos_in = din("pos", [32, S], I32)
    invf_in = din("invf", [32, 1])
    ada_w = din("ada_w", [layers, D, 6 * D])
    ada_b = din("ada_b", [DEPTH, 6 * D])
    ln_mix_g = din("ln_mix_g", [DEPTH, D]); ln_mix_b = din("ln_mix_b", [DEPTH, D])
    ln_ffn_g = din("ln_ffn_g", [DEPTH, D]); ln_ffn_b = din("ln_ffn_b", [DEPTH, D])
    ev_w_in = din("ev_w_in", [2, D, 2560])
    ev_conv = din("ev_conv", [2, 128, 4, 3])
    ev_sgT = din("ev_sgT", [2, 8, 128, 128])
    ev_sg_b = din("ev_sg_b", [2, 8, 128])
    ev_vn_g = din("ev_vn_g", [2, 512]); ev_vn_b = din("ev_vn_b", [2, 512])
    ev_w_out = din("ev_w_out", [2, D, D])
    od_w_in = din("od_w_in", [2, D, 1440])
    od_dw = din("od_dw", [2, 128, 4, 31])
    od_dw_b = din("od_dw_b", [2, 128, 4])
    od_cn_g = din("od_cn_g", [2, 128, 4]); od_cn_b = din("od_cn_b", [2, 128, 4])
    od_qn_g = din("od_qn_g", [2, 128, 2])
    od_w_uq = din("od_w_uq", [2, 256, 768])
    od_kvn_g = din("od_kvn_g", [2, 128, 1])
    od_w_ukv = din("od_w_ukv", [2, 128, 1024])
    od_w_out = din("od_w_out", [2, D, D])
    r_w = din("moe_router_w", [DEPTH, D, NE])
    r_b = din("moe_router_b", [DEPTH, NE])
    w_gu = din("moe_w_gu", [layers, NE, D, 2 * D])
    b_guT = din("moe_b_guT", [DEPTH, NE, 128, 16])
    w_dn = din("moe_w_dn", [layers, NE, D, D])
    b_dn = din("moe_b_dn", [DEPTH, NE, D])
    out = nc.dram_tensor("out", [S, D], F32, kind="ExternalOutput").ap()

    xres = dscr("xres", [S, D])
    delta = dscr("delta", [S, D])
    uT_d = dscr("uT_d", [8, 128, S], BF16)
    u_d = dscr("u_d", [S, D], BF16)
    xs_d = dscr("xs_d", [NSLOT, D], BF16)
    ys_d = dscr("ys_d", [NSLOT, D])
    mod_d = dscr("mod_d", [DEPTH, 6 * D])
    rope_d = dscr("rope_d", [2, 32, S])
    od_scr = {}
    if layers > 1:
        od_scr = {"qT": dscr("qT_d", [8, 96, S], BF16), "kT": dscr("kT_d", [8, 96, S], BF16), "v": dscr("v_d", [S, 8, 65], BF16),
                  "yc": dscr("yc_d", [4, 128, S], BF16), "yd": dscr("yd_d", [S, 512], BF16)}
    dbg_out = {}
    if dbg:
        for name, shape in dbg.items():
            dbg_out[name] = nc.dram_tensor("dbg_" + name, list(shape), F32, kind="ExternalOutput").ap()

    tokb = lambda: [Buf() for _ in range(NT)]
    b_xres = tokb(); b_delta = tokb(); b_uT = tokb(); b_u = tokb()
    b_xs = [Buf() for _ in range(NCH)]; b_ys = [Buf() for _ in range(NCH)]
    b_xs_all = Buf(); b_ys_all = Buf()
    b_mod = Buf(); b_rope = Buf()

    with ExitStack() as es_top:
        s = Sched(nc, es_top)
        s.es_cur = es_top
        ident_b = s.sb([128, 128], BF16, "identb")
        ident_f = s.sb([128, 128], F32, "identf")
        ones_f = s.sb([128, 128], F32, "onesf")
        ones_b = s.sb([128, 128], BF16, "onesb")
        su_f = s.sb([128, 128], F32, "suf")
        iota_p = s.sb([128, 1], F32, "iotap")
        posk_f = s.sb([128, NT, 4], F32, "poskf")
        posk_i = s.sb([128, NT, 4], I32, "poski")
        gatek = s.sb([128, NT, 4], F32, "gatek")
        widx = s.sb([128, NCH, 8], I32, "widx")
        bgidx = s.sb([128, NCH], I32, "bgidx")
        bdidx = s.sb([128, NCH], I32, "bdidx")
        base_pk = s.sb([128, 8], F32, "basepk")

        def mk_consts():
            s.op("pool", lambda e: e.memset(ident_b.t[:], 0.0), [], [ident_b])
            s.op("pool", lambda e: e.affine_select(out=ident_b.t[:], in_=ident_b.t[:], pattern=[[-1, 128]],
                                                   compare_op=ALU.not_equal, fill=1.0, base=0, channel_multiplier=1),
                 [ident_b], [ident_b])
            s.op("pool", lambda e: e.memset(ident_f.t[:], 0.0), [], [ident_f])
            s.op("pool", lambda e: e.affine_select(out=ident_f.t[:], in_=ident_f.t[:], pattern=[[-1, 128]],
                                                   compare_op=ALU.not_equal, fill=1.0, base=0, channel_multiplier=1),
                 [ident_f], [ident_f])
            s.op("pool", lambda e: e.memset(ones_f.t[:], 1.0), [], [ones_f])
            s.op("pool", lambda e: e.memset(ones_b.t[:], 1.0), [], [ones_b])
            s.op("pool", lambda e: e.memset(su_f.t[:], 1.0), [], [su_f])
            s.op("pool", lambda e: e.affine_select(out=su_f.t[:], in_=su_f.t[:], pattern=[[1, 128]],
                                                   compare_op=ALU.is_gt, fill=0.0, base=0, channel_multiplier=-1),
                 [su_f], [su_f])
            s.op("pool", lambda e: e.iota(iota_p.t[:], pattern=[[0, 1]], base=0, channel_multiplier=1,
                                          allow_small_or_imprecise_dtypes=True), [], [iota_p])
            s.op("pool", lambda e: e.iota(base_pk.t[:], pattern=[[128, 8]], base=0, channel_multiplier=1,
                                          allow_small_or_imprecise_dtypes=True), [], [base_pk])

        mk_consts()

        def prologue():
            with s.phase():
                ct = s.sb([128, 8], F32, "ct")
                cond = s.sb([128, 8], F32, "cond")
                condb = s.sb([128, 8, 128], F32, "condb")
                s.dma("sp", ct.t[:], c_in, [], [ct])
                s.op("act", lambda e: e.activation(out=cond.t[:], in_=ct.t[:], func=AF.Silu), [ct], [cond])
                for k in range(8):
                    s.op("dve", lambda e, k=k: e.tensor_scalar(condb.t[:, k, :], ones_f.t[:], cond.t[:, k:k + 1], None,
                                                               ALU.mult), [ones_f, cond], [condb])
                wst = [s.sb([128, 3072], F32, "adaw") for _ in range(3)]
                pacc = [s.ps([128, 512], F32, "pada") for _ in range(6)]
                modt = s.sb([1, 6 * D], F32, "modt")
                adab = s.sb([1, 6 * D], F32, "adab")
                it = 0
                for l in range(layers):
                    s.dma("sp", adab.t[:], ada_b[l:l + 1, :], [], [adab])
                    for half in range(2):
                        for k in range(8):
                            w = wst[it % 3]; it += 1
                            s.dma("sp", w.t[:], ada_w[l, k * 128:(k + 1) * 128, half * 3072:(half + 1) * 3072], [], [w])
                            for n in range(6):
                                s.mm(pacc[n].t[:], condb.t[:, k, :], w.t[:, n * 512:(n + 1) * 512], k == 0, k == 7,
                                     [condb, w], [pacc[n]])
                        for n in range(6):
                            c0 = half * 3072 + n * 512
                            s.op("dve", lambda e, n=n, c0=c0: e.tensor_tensor(modt.t[0:1, c0:c0 + 512], pacc[n].t[0:1, :],
                                                                              adab.t[0:1, c0:c0 + 512], ALU.add),
                                 [pacc[n], adab], [modt])
                    s.dma("sp", mod_d[l:l + 1, :], modt.t[:], [modt], [b_mod])
            if n_odd > 0:
              with s.phase():
                    pi_ = s.sb([32, S], I32, "posi")
                    ang = s.sb([32, S], F32, "ang")
                    q = s.sb([32, S], F32, "q")
                    qi = s.sb([32, S], I32, "qi")
                    r = s.sb([32, S], F32, "r")
                    m = s.sb([32, S], F32, "m")
                    invf = s.sb([32, 1], F32, "invf")
                    s.dma("sp", pi_.t[:], pos_in, [], [pi_])
                    s.dma("sp", invf.t[:], invf_in, [], [invf])
                    s.op("dve", lambda e: e.tensor_copy(out=ang.t[:], in_=pi_.t[:]), [pi_], [ang])
                    s.op("dve", lambda e: e.tensor_scalar(ang.t[:], ang.t[:], invf.t[:, 0:1], None, ALU.mult), [ang, invf], [ang])
                    s.op("dve", lambda e: e.tensor_scalar(q.t[:], ang.t[:], 1.0 / (2 * math.pi), None, ALU.mult), [ang], [q])
                    s.op("dve", lambda e: e.tensor_copy(out=qi.t[:], in_=q.t[:]), [q], [qi])
                    s.op("dve", lambda e: e.tensor_copy(out=q.t[:], in_=qi.t[:]), [qi], [q])
                    s.op("dve", lambda e: e.scalar_tensor_tensor(r.t[:], q.t[:], -TWO_PI_HI, ang.t[:], ALU.mult, ALU.add), [q, ang], [r])
                    s.op("dve", lambda e: e.scalar_tensor_tensor(r.t[:], q.t[:], -TWO_PI_LO, r.t[:], ALU.mult, ALU.add), [q, r], [r])

                    def wrap(t):
                        s.op("dve", lambda e: e.tensor_scalar(m.t[:], t.t[:], math.pi, -2 * math.pi, ALU.is_gt, ALU.mult), [t], [m])
                        s.op("dve", lambda e: e.tensor_tensor(t.t[:], t.t[:], m.t[:], ALU.add), [t, m], [t])
                        s.op("dve", lambda e: e.tensor_scalar(m.t[:], t.t[:], -math.pi, 2 * math.pi, ALU.is_lt, ALU.mult), [t], [m])
                        s.op("dve", lambda e: e.tensor_tensor(t.t[:], t.t[:], m.t[:], ALU.add), [t, m], [t])
                        s.op("dve", lambda e: e.tensor_scalar(t.t[:], t.t[:], math.pi, -math.pi, ALU.min, ALU.max), [t], [t])

                    wrap(r)
                    sn = s.sb([32, S], F32, "sn")
                    s.op("act", lambda e: e.activation(out=sn.t[:], in_=r.t[:], func=AF.Sin), [r], [sn])
                    s.dma("sp", rope_d[1], sn.t[:], [sn], [b_rope])
                    s.op("dve", lambda e: e.tensor_scalar(r.t[:], r.t[:], math.pi / 2, None, ALU.add), [r], [r])
                    wrap(r)
                    cs = s.sb([32, S], F32, "cs")
                    s.op("act", lambda e: e.activation(out=cs.t[:], in_=r.t[:], func=AF.Sin), [r], [cs])
                    s.dma("sp", rope_d[0], cs.t[:], [cs], [b_rope])

        def load_mod(l, j, plus1, name, scale=None):
            t = s.sb([128, D], F32, name)
            s.dma("sp", t.t[:], bcast(mod_d[l:l + 1, j * D:(j + 1) * D]), [b_mod], [t])
            if plus1 and scale is not None:
                s.op("pool", lambda e: e.tensor_scalar(t.t[:], t.t[:], 1.0, float(scale), ALU.add, ALU.mult), [t], [t])
            elif plus1:
                s.op("pool", lambda e: e.tensor_scalar(t.t[:], t.t[:], 1.0, None, ALU.add), [t], [t])
            return t

        def load_row(src_row, n, name):
            t = s.sb([128, n], F32, name)
            s.dma("sp", t.t[:], bcast(src_row), [], [t])
            return t

        def transposes_store_uT(ub, i, ptr, uTs):
            for k in range(8):
                s.op("pe", lambda e, k=k: e.transpose(ptr.t[:, k, :], ub.t[:, k * 128:(k + 1) * 128], ident_b.t[:]),
                     [ub, ident_b], [ptr])
            s.op("act", lambda e: e.copy(out=uTs.t[:], in_=ptr.t[:]), [ptr], [uTs])
            s.dma("act", uT_d[:, :, i * 128:(i + 1) * 128].rearrange("k p t -> p k t"), uTs.t[:], [uTs], [b_uT[i]])

        def premod(l):
            with s.phase():
                sc1 = load_mod(l, 1, True, "sc1")
                sh = load_mod(l, 0, False, "sh")
                xt = [s.sb([128, D], F32, "xt") for _ in range(2)]
                ub = [s.sb([128, D], BF16, "ub") for _ in range(2)]
                ptr = [s.ps([128, 8, 128], BF16, "ptr") for _ in range(2)]
                uTs = [s.sb([128, 8, 128], BF16, "uTs") for _ in range(2)]
                for i in range(NT):
                    x = xt[i % 2]; u = ub[i % 2]
                    s.dma("sp", x.t[:], x_in[i * 128:(i + 1) * 128, :], [], [x])
                    s.op("dve", lambda e: e.tensor_tensor(x.t[:], x.t[:], sc1.t[:], ALU.mult), [x, sc1], [x])
                    s.op("dve", lambda e: e.tensor_tensor(u.t[:], x.t[:], sh.t[:], ALU.add), [x, sh], [u])
                    transposes_store_uT(u, i, ptr[i % 2], uTs[i % 2])

        def even_mixer(l):
            li = l // 2
            with s.phase():
                win = s.sb([128, 8, 2560], BF16, "win")
                wout = s.sb([128, 8, D], BF16, "wout")
                wv = ev_w_in[li].rearrange("(k p) f -> p k f", p=128)
                gpw = load_mod(l, 2, True, "gpw", 1.0 / ALPHA)
                wstg = [s.sb([128, D], F32, "wstg") for _ in range(2)]
                for k in range(8):
                    s.dma("pool", win.t[:, k, 0:1280], wv[:, k, 0:1280], [], [win], group="w")
                    s.dma("pool", win.t[:, k, 1280:2560], wv[:, k, 1280:2560], [], [win], group="w")
                for k in range(8):
                    s.dma("sp", wstg[k % 2].t[:], ev_w_out[li, k * 128:(k + 1) * 128, :], [], [wstg[k % 2]])
                    s.op("dve", lambda e, k=k: e.tensor_tensor(wout.t[:, k, :], wstg[k % 2].t[:], gpw.t[:], ALU.mult), [wstg[k % 2], gpw], [wout])
                sgf = s.sb([128, 8, 128], F32, "sgf")
                sgm = s.sb([128, 8, 128], BF16, "sgm")
                s.dma("sp", sgf.t[:], ev_sgT[li].rearrange("h j i -> j h i"), [], [sgf])
                for h in range(8):
                    s.op("pool", lambda e, h=h: e.affine_select(out=sgf.t[:, h, :], in_=sgf.t[:, h, :], pattern=[[1, 128]],
                                                                compare_op=ALU.is_ge, fill=0.0, base=0, channel_multiplier=-1),
                         [sgf], [sgf])
                s.op("pool", lambda e: e.tensor_copy(out=sgm.t[:], in_=sgf.t[:]), [sgf], [sgm])
                sgb = s.sb([128, 4, 128], F32, "sgb")
                for h in range(8):
                    s.dma("sp", sgb.t[(h % 2) * 64:(h % 2) * 64 + 64, h // 2, :], bcast(ev_sg_b[li, h:h + 1, :], 64), [], [sgb], group="w")
                cw = s.sb([128, 4, 3], F32, "cw")
                s.dma("sp", cw.t[:], ev_conv[li], [], [cw])
                vng = load_row(ev_vn_g[li:li + 1, :], 512, "vng")
                vnb = load_row(ev_vn_b[li:li + 1, :], 512, "vnb")
                halo = s.sb([128, 4, 2], F32, "halo")
                s.op("pool", lambda e: e.memset(halo.t[:], 0.0), [], [halo])

                uTg = [s.sb([128, 8, 512], BF16, "uTg") for _ in range(2)]
                pp = [s.ps([128, 512], F32, "pp") for _ in range(6)]
                psg = [s.ps([128, 128], F32, "psg") for _ in range(2)]
                cg = [s.sb([128, 512], F32, "cg") for _ in range(2)]
                cx = [s.sb([128, 514], F32, "cx") for _ in range(2)]
                acc = [s.sb([128, 512], F32, "acc") for _ in range(2)]
                yT = [s.sb([128, 8, 512], BF16, "yT") for _ in range(2)]
                zuT = [s.sb([128, 4, 512], F32, "zuT") for _ in range(2)]
                zv = [s.sb([128, 512], F32, "zv") for _ in range(2)]
                zvn = [s.sb([128, 512], BF16, "zvn") for _ in range(2)]
                st6 = [s.sb([128, 6], F32, "st6") for _ in range(2)]
                mv = [s.sb([128, 2], F32, "mv") for _ in range(2)]
                rstd = [s.sb([128, 1], F32, "rstd") for _ in range(2)]
                tmp = [s.sb([128, 128], F32, "tmp") for _ in range(2)]
                dl = [s.sb([128, D], F32, "dl") for _ in range(2)]
                ppi = 0
                for g in range(NG):
                    u = uTg[g % 2]; y = yT[g % 2]; zu = zuT[g % 2]
                    s.dma("sp", u.t[:], uT_d[:, :, g * 512:(g + 1) * 512].rearrange("k p t -> p k t"),
                          [b_uT[4 * g + j] for j in range(4)], [u])

                    def proj(f):
                        nonlocal ppi
                        p = pp[ppi % 6]; ppi += 1
                        for k in range(8):
                            s.mm(p.t[:], win.t[:, k, f * 128:(f + 1) * 128], u.t[:, k, :], k == 0, k == 7, [win, u], [p])
                        return p

                    for cc in range(4):
                        c_ = cg[cc % 2]; x_ = cx[cc % 2]; a_ = acc[cc % 2]
                        p_c = proj(4 + cc)
                        s.op("act", lambda e: e.copy(out=c_.t[:], in_=p_c.t[:]), [p_c], [c_])
                        p_x = proj(8 + cc)
                        s.op("pool", lambda e: e.tensor_copy(out=x_.t[:, 0:2], in_=halo.t[:, cc, :]), [halo], [x_])
                        s.op("dve", lambda e: e.tensor_tensor(x_.t[:, 2:514], p_x.t[:], c_.t[:], ALU.mult), [p_x, c_], [x_])
                        s.op("pool", lambda e: e.tensor_copy(out=halo.t[:, cc, :], in_=x_.t[:, 512:514]), [x_], [halo])
                        s.op("dve", lambda e: e.tensor_scalar(a_.t[:], x_.t[:, 0:512], cw.t[:, cc, 0:1], None, ALU.mult), [x_, cw], [a_])
                        s.op("dve", lambda e: e.scalar_tensor_tensor(a_.t[:], x_.t[:, 1:513], cw.t[:, cc, 1:2], a_.t[:], ALU.mult, ALU.add),
                             [x_, cw, a_], [a_])
                        s.op("dve", lambda e: e.scalar_tensor_tensor(a_.t[:], x_.t[:, 2:514], cw.t[:, cc, 2:3], a_.t[:], ALU.mult, ALU.add),
                             [x_, cw, a_], [a_])
                        p_b = proj(cc)
                        s.op("dve", lambda e: e.tensor_tensor(y.t[:, cc, :], p_b.t[:], a_.t[:], ALU.mult), [p_b, a_], [y])
                        p_u = proj(12 + cc)
                        s.op("act", lambda e: e.activation(out=zu.t[:, cc, :], in_=p_u.t[:], func=AF.Gelu), [p_u], [zu])
                    for tt in range(4):
                        i = 4 * g + tt
                        z = zv[tt % 2]; zn = zvn[tt % 2]; s6 = st6[tt % 2]; m_ = mv[tt % 2]; rs = rstd[tt % 2]
                        p = pp[ppi % 6]; ppi += 1
                        for k in range(8):
                            s.mm(p.t[:], u.t[:, k, tt * 128:(tt + 1) * 128], win.t[:, k, 2048:2560], k == 0, k == 7, [win, u], [p])
                        s.op("act", lambda e: e.activation(out=z.t[:], in_=p.t[:], func=AF.Gelu), [p], [z])
                        s.op("dve", lambda e: e.bn_stats(s6.t[:], z.t[:]), [z], [s6])
                        s.op("dve", lambda e: e.bn_aggr(m_.t[:], s6.t[:]), [s6], [m_])
                        s.op("act", lambda e: e.activation(out=rs.t[:], in_=m_.t[:, 1:2], func=AF.Sqrt, bias=eps_t.t[:, 0:1]), [m_, eps_t], [rs])
                        s.op("dve", lambda e: e.reciprocal(rs.t[:], rs.t[:]), [rs], [rs])
                        s.op("dve", lambda e: e.tensor_scalar(z.t[:], z.t[:], m_.t[:, 0:1], rs.t[:, 0:1], ALU.subtract, ALU.mult), [z, m_, rs], [z])
                        s.op("pool", lambda e: e.tensor_tensor(z.t[:], z.t[:], vng.t[:], ALU.mult), [z, vng], [z])
                        s.op("pool", lambda e: e.tensor_tensor(zn.t[:], z.t[:], vnb.t[:], ALU.add), [z, vnb], [zn])
                        for cc in range(4):
                            for hh in range(2):
                                h = 2 * cc + hh
                                pg = psg[(cc * 2 + hh) % 2]
                                t_ = tmp[(cc * 2 + hh) % 2]
                                s.mm(pg.t[:], zn.t[:, cc * 128:(cc + 1) * 128], sgm.t[:, h, :], True, True, [zn, sgm], [pg])
                                lo, hi = hh * 64, hh * 64 + 64
                                s.op("dve", lambda e: e.tensor_tensor(t_.t[lo:hi, :], pg.t[lo:hi, :], sgb.t[lo:hi, cc, :], ALU.add), [pg, sgb], [t_])
                                s.op("dve", lambda e: e.tensor_tensor(y.t[lo:hi, 4 + cc, tt * 128:(tt + 1) * 128], t_.t[lo:hi, :],
                                                                      zu.t[lo:hi, cc, tt * 128:(tt + 1) * 128], ALU.mult), [t_, zu], [y])
                        d_ = dl[tt % 2]
                        for nh in range(2):
                            p = pp[ppi % 6]; ppi += 1
                            for k in range(8):
                                s.mm(p.t[:], y.t[:, k, tt * 128:(tt + 1) * 128], wout.t[:, k, nh * 512:(nh + 1) * 512], k == 0, k == 7, [y, wout], [p])
                            s.op("act", lambda e: e.copy(out=d_.t[:, nh * 512:(nh + 1) * 512], in_=p.t[:]), [p], [d_])
                        s.dma("act", delta[i * 128:(i + 1) * 128, :], d_.t[:], [d_], [b_delta[i]])

        def odd_mixer(l):
            li = l // 2
            SCALE = 1.0 / math.sqrt(96.0)
            qT_d = od_scr["qT"]; kT_d = od_scr["kT"]; v_d = od_scr["v"]; yc_d = od_scr["yc"]; yd_d = od_scr["yd"]
            b_q = Buf(); b_k = Buf(); b_v = Buf(); b_yc = Buf(); b_yd = Buf()
            with s.phase():
                win = s.sb([128, 8, 1440], BF16, "win")
                wv_ = od_w_in[li].rearrange("(k p) f -> p k f", p=128)
                for k in range(8):
                    s.dma("pool", win.t[:, k, :], wv_[:, k, :], [], [win], group="w")
                winr = s.sb([128, 8, 32], F32, "winr")
                s.dma("sp", winr.t[:], wv_[:, :, 1408:1440], [], [winr])
                win_sw = s.sb([128, 8, 96], BF16, "winsw")
                s.op("pool", lambda e: e.memset(win_sw.t[:], 0.0), [], [win_sw])
                s.op("dve", lambda e: e.tensor_scalar(win_sw.t[:, :, 64:80], winr.t[:, :, 16:32], -1.0, None, ALU.mult), [winr, win_sw], [win_sw])
                s.op("dve", lambda e: e.tensor_copy(out=win_sw.t[:, :, 80:96], in_=winr.t[:, :, 0:16]), [winr, win_sw], [win_sw])
                wuq = s.sb([128, 2, 768], BF16, "wuq")
                wuqf = s.sb([128, 2, 768], F32, "wuqf")
                uqv = od_w_uq[li].rearrange("(k p) f -> p k f", p=128)
                s.dma("pool", wuq.t[:], uqv, [], [wuq])
                s.dma("sp", wuqf.t[:], uqv, [], [wuqf])
                wuq_sw = s.sb([128, 2, 8, 96], BF16, "wuqsw")
                s.op("pool", lambda e: e.memset(wuq_sw.t[:], 0.0), [], [wuq_sw])
                wuqf4 = wuqf.t[:].rearrange("p k (h e) -> p k h e", e=96)
                for kc in range(2):
                    s.op("dve", lambda e, kc=kc: e.tensor_scalar(wuq_sw.t[:, kc, :, 64:80], wuqf4[:, kc, :, 80:96], -1.0, None, ALU.mult), [wuqf, wuq_sw], [wuq_sw])
                    s.op("dve", lambda e, kc=kc: e.tensor_copy(out=wuq_sw.t[:, kc, :, 80:96], in_=wuqf4[:, kc, :, 64:80]), [wuqf, wuq_sw], [wuq_sw])
                wk = s.sb([128, 8, 64], BF16, "wk")
                wvv = s.sb([128, 8, 64], BF16, "wvv")
                ukv = od_w_ukv[li].rearrange("r (h e) -> r h e", e=128)
                s.dma("pool", wk.t[:], ukv[:, :, 0:64], [], [wk])
                s.dma("pool", wvv.t[:], ukv[:, :, 64:128], [], [wvv])
                cwd = s.sb([128, 4, 31], F32, "cwd")
                s.dma("sp", cwd.t[:], od_dw[li], [], [cwd])
                dwb = s.sb([128, 4], F32, "dwb"); cng = s.sb([128, 4], F32, "cng"); cnb = s.sb([128, 4], F32, "cnb")
                qng = s.sb([128, 2], F32, "qng"); kvng = s.sb([128, 1], F32, "kvng")
                s.dma("sp", dwb.t[:], od_dw_b[li], [], [dwb]); s.dma("sp", cng.t[:], od_cn_g[li], [], [cng])
                s.dma("sp", cnb.t[:], od_cn_b[li], [], [cnb]); s.dma("sp", qng.t[:], od_qn_g[li], [], [qng])
                s.dma("sp", kvng.t[:], od_kvn_g[li], [], [kvng])
                diag = s.sb([128, 4, 31, 128], BF16, "diag")
                for cc in range(4):
                    for k in range(31):
                        eng = "dve" if (cc * 31 + k) % 2 == 0 else "pool"
                        s.op(eng, lambda e, cc=cc, k=k: e.tensor_scalar(diag.t[:, cc, k, :], ident_f.t[:], cwd.t[:, cc, k:k + 1], None, ALU.mult),
                             [ident_f, cwd], [diag])
                rms_eps = s.sb([128, 1], F32, "rmseps")
                s.op("pool", lambda e: e.memset(rms_eps.t[:], RMS_EPS), [], [rms_eps])

                uTg = [s.sb([128, 8, 512], BF16, "uTg") for _ in range(2)]
                glu = [s.sb([128, 4, 542], BF16, "glu") for _ in range(2)]
                s.op("pool", lambda e: e.memset(glu[1].t[:, :, 512:542], 0.0), [], [glu[1]])
                sgm = [s.sb([128, 512], F32, "sgm") for _ in range(2)]
                hbuf = s.sb([128, 4, 512], F32, "hbuf")
                hsq = s.sb([128, 4, 512], F32, "hsq")
                mean = s.sb([128, 512], F32, "mean"); var = s.sb([128, 512], F32, "var"); rstd = s.sb([128, 512], F32, "rstd")
                tb = [s.sb([128, 512], F32, "tb") for _ in range(2)]
                ycT = [s.sb([128, 4, 512], BF16, "ycT") for _ in range(2)]
                sq = [s.sb([128, 512], F32, "sq") for _ in range(2)]
                rq = s.sb([128, 512], F32, "rq")
                cqn = s.sb([128, 2, 512], BF16, "cqn")
                ckvn = s.sb([128, 512], BF16, "ckvn")
                cs = s.sb([128, 2, 512], F32, "cs")
                t1 = [s.sb([128, 512], F32, "t1") for _ in range(2)]
                t2 = [s.sb([128, 512], F32, "t2") for _ in range(2)]
                krT = s.sb([128, 512], BF16, "krT")
                qT = [s.sb([128, 8, 512], BF16, "qT") for _ in range(2)]
                kT = [s.sb([128, 8, 512], BF16, "kT") for _ in range(2)]
                vt = [s.sb([128, 8, 65], BF16, "vt") for _ in range(2)]
                for v_ in vt:
                    s.op("pool", lambda e, v_=v_: e.memset(v_.t[:, :, 64:65], 1.0), [], [v_])
                pp = [s.ps([128, 512], F32, "pp") for _ in range(8)]
                ppi = 0
                for g in range(NG):
                    u = uTg[g % 2]; gl = glu[g % 2]; glp = glu[(g + 1) % 2]
                    tsl = slice(g * 512, (g + 1) * 512)
                    s.dma("sp", u.t[:], uT_d[:, :, tsl].rearrange("k p t -> p k t"), [b_uT[4 * g + j] for j in range(4)], [u])
                    s.dma("sp", cs.t[64:96, 0, :], rope_d[0, :, tsl], [b_rope], [cs], group=("cs", g))
                    s.dma("sp", cs.t[64:96, 1, :], rope_d[1, :, tsl], [b_rope], [cs], group=("cs", g))

                    def proj(c0, m, wt=win):
                        nonlocal ppi
                        p = pp[ppi % 8]; ppi += 1
                        for k in range(8):
                            s.mm(p.t[0:m, :], wt.t[:, k, c0:c0 + m], u.t[:, k, :], k == 0, k == 7, [wt, u], [p])
                        return p

                    s.op("pool", lambda e: e.tensor_copy(out=gl.t[:, :, 0:30], in_=glp.t[:, :, 512:542]), [glp], [gl])
                    for cc in range(4):
                        sg_ = sgm[cc % 2]
                        p_b = proj(512 + cc * 128, 128)
                        s.op("act", lambda e: e.activation(out=sg_.t[:], in_=p_b.t[:], func=AF.Sigmoid), [p_b], [sg_])
                        p_a = proj(cc * 128, 128)
                        s.op("dve", lambda e: e.tensor_tensor(gl.t[:, cc, 30:542], p_a.t[:], sg_.t[:], ALU.mult), [p_a, sg_], [gl])
                    for cc in range(4):
                        p = pp[ppi % 8]; ppi += 1
                        for k in range(31):
                            s.mm(p.t[:], diag.t[:, cc, k, :], gl.t[:, cc, k:k + 512], k == 0, k == 30, [diag, gl], [p])
                        s.op("act", lambda e: e.activation(out=hbuf.t[:, cc, :], in_=p.t[:], func=AF.Identity, bias=dwb.t[:, cc:cc + 1]), [p, dwb], [hbuf])
                        s.op("act", lambda e: e.activation(out=hsq.t[:, cc, :], in_=p.t[:], func=AF.Square, bias=dwb.t[:, cc:cc + 1]), [p, dwb], [hsq])
                    p1 = pp[ppi % 8]; ppi += 1
                    p2 = pp[ppi % 8]; ppi += 1
                    for cc in range(4):
                        s.mm(p1.t[:], ones_f.t[:], hbuf.t[:, cc, :], cc == 0, cc == 3, [ones_f, hbuf], [p1])
                    for cc in range(4):
                        s.mm(p2.t[:], ones_f.t[:], hsq.t[:, cc, :], cc == 0, cc == 3, [ones_f, hsq], [p2])
                    s.op("dve", lambda e: e.tensor_scalar(mean.t[:], p1.t[:], 1.0 / 512, None, ALU.mult), [p1], [mean])
                    s.op("pool", lambda e: e.tensor_tensor(var.t[:], mean.t[:], mean.t[:], ALU.mult), [mean], [var])
                    s.op("dve", lambda e: e.scalar_tensor_tensor(var.t[:], p2.t[:], 1.0 / 512, var.t[:], ALU.mult, ALU.subtract), [p2, var], [var])
                    s.op("act", lambda e: e.activation(out=rstd.t[:], in_=var.t[:], func=AF.Sqrt, bias=eps_t.t[:, 0:1]), [var, eps_t], [rstd])
                    s.op("dve", lambda e: e.reciprocal(rstd.t[:], rstd.t[:]), [rstd], [rstd])
                    yc = ycT[g % 2]
                    for cc in range(4):
                        t_ = tb[cc % 2]
                        s.op("dve", lambda e: e.tensor_tensor(t_.t[:], hbuf.t[:, cc, :], mean.t[:], ALU.subtract), [hbuf, mean], [t_])
                        s.op("pool", lambda e: e.tensor_tensor(t_.t[:], t_.t[:], rstd.t[:], ALU.mult), [t_, rstd], [t_])
                        s.op("dve", lambda e: e.tensor_scalar(t_.t[:], t_.t[:], cng.t[:, cc:cc + 1], cnb.t[:, cc:cc + 1], ALU.mult, ALU.add), [t_, cng, cnb], [t_])
                        s.op("act", lambda e: e.activation(out=yc.t[:, cc, :], in_=t_.t[:], func=AF.Silu), [t_], [yc])
                    s.dma("act", yc_d[:, :, tsl].rearrange("c p t -> p c t"), yc.t[:], [yc], [b_yc], group="st")
                    pq = [proj(1024, 128), proj(1152, 128)]
                    for c2 in range(2):
                        s.op("act", lambda e, c2=c2: e.activation(out=sq[c2].t[:], in_=pq[c2].t[:], func=AF.Square), [pq[c2]], [sq[c2]])
                    ps_ = pp[ppi % 8]; ppi += 1
                    for c2 in range(2):
                        s.mm(ps_.t[:], ones_f.t[:], sq[c2].t[:], c2 == 0, c2 == 1, [ones_f, sq[c2]], [ps_])
                    s.op("act", lambda e: e.activation(out=rq.t[:], in_=ps_.t[:], func=AF.Sqrt, bias=rms_eps.t[:, 0:1], scale=1.0 / 256), [ps_, rms_eps], [rq])
                    s.op("dve", lambda e: e.reciprocal(rq.t[:], rq.t[:]), [rq], [rq])
                    for c2 in range(2):
                        s.op("dve", lambda e, c2=c2: e.scalar_tensor_tensor(cqn.t[:, c2, :], pq[c2].t[:], qng.t[:, c2:c2 + 1], rq.t[:], ALU.mult, ALU.mult),
                             [pq[c2], qng, rq], [cqn])
                    pkv = proj(1280, 128)
                    s.op("act", lambda e: e.activation(out=sq[0].t[:], in_=pkv.t[:], func=AF.Square), [pkv], [sq[0]])
                    ps_ = pp[ppi % 8]; ppi += 1
                    s.mm(ps_.t[:], ones_f.t[:], sq[0].t[:], True, True, [ones_f, sq[0]], [ps_])
                    s.op("act", lambda e: e.activation(out=rq.t[:], in_=ps_.t[:], func=AF.Sqrt, bias=rms_eps.t[:, 0:1], scale=1.0 / 128), [ps_, rms_eps], [rq])
                    s.op("dve", lambda e: e.reciprocal(rq.t[:], rq.t[:]), [rq], [rq])
                    s.op("dve", lambda e: e.scalar_tensor_tensor(ckvn.t[:], pkv.t[:], kvng.t[:, 0:1], rq.t[:], ALU.mult, ALU.mult), [pkv, kvng, rq], [ckvn])
                    pk1 = proj(1344, 96)
                    pk2 = proj(0, 96, win_sw)
                    R = slice(64, 96)
                    s.op("dve", lambda e: e.tensor_tensor(t1[0].t[R, :], pk1.t[R, :], cs.t[R, 0, :], ALU.mult), [pk1, cs], [t1[0]])
                    s.op("dve", lambda e: e.tensor_tensor(t2[0].t[R, :], pk2.t[R, :], cs.t[R, 1, :], ALU.mult), [pk2, cs], [t2[0]])
                    s.op("pool", lambda e: e.tensor_tensor(krT.t[R, :], t1[0].t[R, :], t2[0].t[R, :], ALU.add), [t1[0], t2[0]], [krT])
                    q_ = qT[g % 2]; k_ = kT[g % 2]
                    for h in range(8):
                        a1 = t1[h % 2]; a2 = t2[h % 2]
                        pq1 = pp[ppi % 8]; ppi += 1
                        pq2 = pp[ppi % 8]; ppi += 1
                        for kc in range(2):
                            s.mm(pq1.t[0:96, :], wuq.t[:, kc, h * 96:(h + 1) * 96], cqn.t[:, kc, :], kc == 0, kc == 1, [wuq, cqn], [pq1])
                        for kc in range(2):
                            s.mm(pq2.t[0:96, :], wuq_sw.t[:, kc, h, :], cqn.t[:, kc, :], kc == 0, kc == 1, [wuq_sw, cqn], [pq2])
                        s.op("act", lambda e: e.copy(out=q_.t[0:64, h, :], in_=pq1.t[0:64, :]), [pq1], [q_])
                        s.op("dve", lambda e: e.tensor_tensor(a1.t[R, :], pq1.t[R, :], cs.t[R, 0, :], ALU.mult), [pq1, cs], [a1])
                        s.op("dve", lambda e: e.tensor_tensor(a2.t[R, :], pq2.t[R, :], cs.t[R, 1, :], ALU.mult), [pq2, cs], [a2])
                        s.op("pool", lambda e: e.tensor_tensor(q_.t[R, h, :], a1.t[R, :], a2.t[R, :], ALU.add), [a1, a2], [q_])
                        pk = pp[ppi % 8]; ppi += 1
                        s.mm(pk.t[0:64, :], wk.t[:, h, :], ckvn.t[:], True, True, [wk, ckvn], [pk])
                        s.op("act", lambda e: e.copy(out=k_.t[0:64, h, :], in_=pk.t[0:64, :]), [pk], [k_])
                        s.op("pool", lambda e: e.tensor_copy(out=k_.t[R, h, :], in_=krT.t[R, :]), [krT], [k_])
                    s.dma("pool", qT_d[:, :, tsl].rearrange("h p t -> p h t"), q_.t[0:96, :, :], [q_], [b_q], group="st")
                    s.dma("pool", kT_d[:, :, tsl].rearrange("h p t -> p h t"), k_.t[0:96, :, :], [k_], [b_k], group="st")
                    for tt in range(4):
                        i = 4 * g + tt
                        v_ = vt[tt % 2]
                        pv = pp[ppi % 8]; ppi += 1
                        s.mm(pv.t[:], ckvn.t[:, tt * 128:(tt + 1) * 128], wvv.t[:].rearrange("p h e -> p (h e)"), True, True, [ckvn, wvv], [pv])
                        s.op("act", lambda e: e.copy(out=v_.t[:, :, 0:64], in_=pv.t[:].rearrange("p (h e) -> p h e", e=64)), [pv], [v_])
                        s.dma("act", v_d[i * 128:(i + 1) * 128, :, :], v_.t[:], [v_], [b_v], group="st")
            with s.phase():
                masks = s.sb([128, 4, 512], BF16, "masks")
                s.op("pool", lambda e: e.memset(masks.t[:], 1.0), [], [masks])
                for j in range(4):
                    s.op("pool", lambda e, j=j: e.affine_select(out=masks.t[:, j, :], in_=masks.t[:, j, :], pattern=[[1, 512]], compare_op=ALU.is_ge,
                                                                fill=0.0, base=-128 * j, channel_multiplier=-1), [masks], [masks])
                kTh = [s.sb([128, S], BF16, "kTh") for _ in range(2)]
                vh = [s.sb([128, NT, 65], BF16, "vh") for _ in range(2)]
                qg = [s.sb([128, 512], BF16, "qg") for _ in range(2)]
                PT = [s.sb([128, 512], BF16, "PT") for _ in range(3)]
                psT = [s.ps([128, 512], F32, "psT") for _ in range(3)]
                pacc = [s.ps([128, 65], F32, "pacc") for _ in range(4)]
                rec = [s.sb([128, 1], F32, "rec") for _ in range(2)]
                yd = [s.sb([128, 64], BF16, "yd") for _ in range(2)]
                it = 0
                qg = [s.sb([128, 512], BF16, "qg3") for _ in range(3)]

                def load_head(h):
                    s.dma("sp", kTh[h % 2].t[0:96, :], kT_d[h], [b_k], [kTh[h % 2]])
                    s.dma("sp", vh[h % 2].t[:], v_d[:, h, :].rearrange("(t p) e -> p t e", p=128), [b_v], [vh[h % 2]])

                def load_q(n):
                    h_, G_ = divmod(n, NG)
                    s.dma("sp", qg[n % 3].t[0:96, :], qT_d[h_, :, G_ * 512:(G_ + 1) * 512], [b_q], [qg[n % 3]])

                load_head(0)
                load_q(0)
                for h in range(8):
                    kt = kTh[h % 2]; vv = vh[h % 2]
                    if h + 1 < 8:
                        load_head(h + 1)
                    for G in range(NG):
                        n = h * NG + G
                        q_ = qg[n % 3]
                        if n + 1 < 8 * NG:
                            load_q(n + 1)
                        nkb = 4 * G + 4

                        def qk(kb):
                            nonlocal it
                            ps_ = psT[it % 3]; it += 1
                            s.mm(ps_.t[:], kt.t[0:96, kb * 128:(kb + 1) * 128], q_.t[0:96, :], True, True, [kt, q_], [ps_])
                            return ps_

                        pend = [qk(0)]
                        if nkb > 1:
                            pend.append(qk(1))
                        for kb in range(nkb):
                            ps_ = pend.pop(0)
                            if kb + 2 < nkb:
                                pend.append(qk(kb + 2))
                            pt = PT[kb % 3]
                            j = kb - 4 * G
                            c0 = max(j, 0) * 128
                            s.op("act", lambda e: e.activation(out=pt.t[:, c0:512], in_=ps_.t[:, c0:512], func=AF.Exp, scale=SCALE), [ps_], [pt])
                            if j >= 0:
                                s.op("dve", lambda e: e.tensor_tensor(pt.t[:, c0:c0 + 128], pt.t[:, c0:c0 + 128], masks.t[:, 0, 0:128], ALU.mult), [pt, masks], [pt])
                            for qs in range(4):
                                last_kb = 4 * G + qs
                                if kb > last_kb:
                                    continue
                                s.mm(pacc[qs].t[:], pt.t[:, qs * 128:(qs + 1) * 128], vv.t[:, kb, :], kb == 0, kb == last_kb, [pt, vv], [pacc[qs]])
                                if kb == last_kb:
                                    r_ = rec[qs % 2]; y_ = yd[qs % 2]
                                    i = 4 * G + qs
                                    s.op("dve", lambda e: e.reciprocal(r_.t[:], pacc[qs].t[:, 64:65]), [pacc[qs]], [r_])
                                    s.op("dve", lambda e: e.tensor_scalar(y_.t[:], pacc[qs].t[:, 0:64], r_.t[:, 0:1], None, ALU.mult), [pacc[qs], r_], [y_])
                                    s.dma("pool", yd_d[i * 128:(i + 1) * 128, h * 64:(h + 1) * 64], y_.t[:], [y_], [b_yd], group="st")
            with s.phase():
                wout = s.sb([128, 8, D], BF16, "wout")
                gpw = load_mod(l, 2, True, "gpw", 1.0 / ALPHA)
                wstg = [s.sb([128, D], F32, "wstg") for _ in range(2)]
                for k in range(8):
                    s.dma("sp", wstg[k % 2].t[:], od_w_out[li, k * 128:(k + 1) * 128, :], [], [wstg[k % 2]])
                    s.op("dve", lambda e, k=k: e.tensor_tensor(wout.t[:, k, :], wstg[k % 2].t[:], gpw.t[:], ALU.mult), [wstg[k % 2], gpw], [wout])
                yT = [s.sb([128, 8, 128], BF16, "yT") for _ in range(2)]
                ydt = [s.sb([128, 512], BF16, "ydt") for _ in range(2)]
                ptr = [s.ps([128, 4, 128], BF16, "ptr") for _ in range(2)]
                pp = [s.ps([128, 512], F32, "pp") for _ in range(4)]
                dl = [s.sb([128, D], F32, "dl") for _ in range(2)]
                for i in range(NT):
                    y = yT[i % 2]; yd_ = ydt[i % 2]; pt = ptr[i % 2]; d_ = dl[i % 2]
                    s.dma("sp", y.t[:, 0:4, :], yc_d[:, :, i * 128:(i + 1) * 128].rearrange("c p t -> p c t"), [b_yc], [y])
                    s.dma("sp", yd_.t[:], yd_d[i * 128:(i + 1) * 128, :], [b_yd], [yd_])
                    for c in range(4):
                        s.op("pe", lambda e, c=c: e.transpose(pt.t[:, c, :], yd_.t[:, c * 128:(c + 1) * 128], ident_b.t[:]), [yd_, ident_b], [pt])
                    s.op("act", lambda e: e.copy(out=y.t[:, 4:8, :], in_=pt.t[:]), [pt], [y])
                    for nh in range(2):
                        p = pp[(2 * i + nh) % 4]
                        for k in range(8):
                            s.mm(p.t[:], y.t[:, k, :], wout.t[:, k, nh * 512:(nh + 1) * 512], k == 0, k == 7, [y, wout], [p])
                        s.op("act", lambda e: e.copy(out=d_.t[:, nh * 512:(nh + 1) * 512], in_=p.t[:]), [p], [d_])
                    s.dma("act", delta[i * 128:(i + 1) * 128, :], d_.t[:], [d_], [b_delta[i]])

        def norm_pass(l, kind, x_src, last):
            with s.phase():
                mix = kind == "mix"
                need_u = not (last and not mix)
                if mix:
                    lng = load_row(ln_mix_g[l:l + 1, :], D, "lng"); lnb = load_row(ln_mix_b[l:l + 1, :], D, "lnb")
                    sc1 = load_mod(l, 4, True, "sc1"); sh = load_mod(l, 3, False, "sh")
                else:
                    lng = load_row(ln_ffn_g[l:l + 1, :], D, "lng"); lnb = load_row(ln_ffn_b[l:l + 1, :], D, "lnb")
                    if need_u:
                        sc1 = load_mod(l + 1, 1, True, "sc1"); sh = load_mod(l + 1, 0, False, "sh")
                if need_u:
                    B2 = sh
                    tmpb = s.sb([128, D], F32, "tmpb")
                    s.op("dve", lambda e: e.tensor_tensor(tmpb.t[:], lnb.t[:], sc1.t[:], ALU.mult), [lnb, sc1], [tmpb])
                    s.op("dve", lambda e: e.tensor_tensor(B2.t[:], tmpb.t[:], sh.t[:], ALU.add), [tmpb, sh], [B2])
                    G2 = sc1
                    s.op("dve", lambda e: e.tensor_tensor(G2.t[:], sc1.t[:], lng.t[:], ALU.mult), [sc1, lng], [G2])
                eps2 = s.sb([128, 1], F32, "eps2")
                s.op("pool", lambda e: e.memset(eps2.t[:], LN_EPS / (ALPHA * ALPHA)), [], [eps2])
                NB = 4
                xt = [s.sb([128, D], F32, "xt") for _ in range(NB)]
                st12 = [s.sb([128, 12], F32, "st12") for _ in range(NB)]
                mv = [s.sb([128, 2], F32, "mv") for _ in range(NB)]
                rstd = [s.sb([128, 1], F32, "rstd") for _ in range(NB)]
                nbias = [s.sb([128, 1], F32, "nbias") for _ in range(NB)]
                nt = [s.sb([128, D], F32, "nt") for _ in range(3)]
                xo = [s.sb([128, D], F32, "xo") for _ in range(3)]
                ub = [s.sb([128, D], BF16, "ub") for _ in range(3)]
                if mix:
                    yt = [s.sb([128, D], F32, "yt") for _ in range(NB)]
                    uf = [s.sb([128, D], F32, "uf") for _ in range(3)]
                    ptf = [s.ps([128, 4, 128], F32, "ptf") for _ in range(4)]
                    uTf = [s.sb([128, 8, 128], F32, "uTf") for _ in range(3)]
                    rw = s.sb([128, 8, NE], F32, "rw")
                    s.dma("sp", rw.t[:], r_w[l].rearrange("(k p) e -> p k e", p=128), [], [rw])
                    rb = load_row(r_b[l:l + 1, :], NE, "rb")
                    plog = [s.ps([128, NE], F32, "plog") for _ in range(2)]
                    prk = [s.ps([128, NE], F32, "prk") for _ in range(2)]
                    lg = [s.sb([128, NE], F32, "lg") for _ in range(3)]
                    t8 = [s.sb([128, 8], F32, "t8") for _ in range(2)]
                    nv0 = [s.sb([128, 1], F32, "nv0") for _ in range(2)]
                    ex4 = [s.sb([128, 4], F32, "ex4") for _ in range(2)]
                    sm = [s.sb([128, 1], F32, "sm") for _ in range(2)]
                    msk = [s.sb([128, NE], F32, "msk") for _ in range(2)]
                    msum = s.sb([128, NE], F32, "msum")
                    oh = s.sb([128, NT, 4, NE], F32, "oh")
                    prod = s.sb([128, 4, NE], F32, "prod")
                    s.op("pool", lambda e: e.memset(msum.t[:], 0.0), [], [msum])
                else:
                    yk = [s.sb([128, D], F32, "yk") for _ in range(12)]
                    if need_u:
                        ptr = [s.ps([128, 8, 128], BF16, "ptr") for _ in range(2)]
                        uTs = [s.sb([128, 8, 128], BF16, "uTs") for _ in range(2)]
                        uf = [s.sb([128, D], F32, "uf") for _ in range(3)]

                def stageL(i):
                    x = xt[i % NB]
                    s.dma("sp", x.t[:], x_src[i * 128:(i + 1) * 128, :], [b_xres[i]] if x_src is xres else [], [x])
                    if mix:
                        y = yt[i % NB]
                        s.dma("sp", y.t[:], delta[i * 128:(i + 1) * 128, :], [b_delta[i]], [y])
                    else:
                        ys_ = [yk[(i % 3) * 4 + k] for k in range(4)]
                        for k in range(4):
                            s.idma(ys_[k].t[:], None, ys_d, bass.IndirectOffsetOnAxis(ap=posk_i.t[:, i, k:k + 1], axis=0),
                                   [b_ys_all, posk_i], [ys_[k]])

                def stageA(i):
                    x = xt[i % NB]; s12 = st12[i % NB]; m_ = mv[i % NB]; rs = rstd[i % NB]; nb_ = nbias[i % NB]
                    if mix:
                        y = yt[i % NB]
                        s.op("dve", lambda e: e.tensor_tensor(x.t[:], x.t[:], y.t[:], ALU.add), [x, y], [x])
                    else:
                        ys_ = [yk[(i % 3) * 4 + k] for k in range(4)]
                        for k in range(4):
                            s.op("act", lambda e, k=k: e.activation(out=ys_[k].t[:], in_=ys_[k].t[:], func=AF.Copy, scale=gatek.t[:, i, k:k + 1]), [ys_[k], gatek], [ys_[k]])
                        s.op("dve", lambda e: e.tensor_tensor(ys_[0].t[:], ys_[0].t[:], ys_[1].t[:], ALU.add), [ys_[0], ys_[1]], [ys_[0]])
                        s.op("dve", lambda e: e.tensor_tensor(ys_[2].t[:], ys_[2].t[:], ys_[3].t[:], ALU.add), [ys_[2], ys_[3]], [ys_[2]])
                        s.op("dve", lambda e: e.tensor_tensor(ys_[0].t[:], ys_[0].t[:], ys_[2].t[:], ALU.add), [ys_[0], ys_[2]], [ys_[0]])
                        s.op("dve", lambda e: e.tensor_tensor(x.t[:], x.t[:], ys_[0].t[:], ALU.add), [x, ys_[0]], [x])
                    s.op("dve", lambda e: e.bn_stats(s12.t[:, 0:6], x.t[:, 0:512]), [x], [s12])
                    s.op("dve", lambda e: e.bn_stats(s12.t[:, 6:12], x.t[:, 512:1024]), [x], [s12])
                    s.op("dve", lambda e: e.bn_aggr(m_.t[:], s12.t[:]), [s12], [m_])
                    s.op("act", lambda e: e.activation(out=rs.t[:], in_=m_.t[:, 1:2], func=AF.Ln, bias=eps2.t[:, 0:1]), [m_, eps2], [rs])
                    s.op("act", lambda e: e.activation(out=rs.t[:], in_=rs.t[:], func=AF.Exp, scale=-0.5), [rs], [rs])
                    s.op("dve", lambda e: e.scalar_tensor_tensor(nb_.t[:], m_.t[:, 0:1], -1.0, rs.t[:], ALU.mult, ALU.mult), [m_, rs], [nb_])

                def stageB(i):
                    x = xt[i % NB]; rs = rstd[i % NB]; nb_ = nbias[i % NB]
                    n_ = nt[i % 3]; o_ = xo[i % 3]
                    s.op("act", lambda e: e.activation(out=n_.t[:], in_=x.t[:], func=AF.Identity, scale=rs.t[:, 0:1], bias=nb_.t[:, 0:1]), [x, rs, nb_], [n_])
                    s.op("pool", lambda e: e.tensor_tensor(o_.t[:], n_.t[:], lng.t[:], ALU.mult), [n_, lng], [o_])
                    s.op("pool", lambda e: e.tensor_tensor(o_.t[:], o_.t[:], lnb.t[:], ALU.add), [o_, lnb], [o_])
                    if not need_u:
                        s.dma("pool", out[i * 128:(i + 1) * 128, :], o_.t[:], [o_], [b_xres[i]])
                        return
                    s.dma("pool", xres[i * 128:(i + 1) * 128, :], o_.t[:], [o_], [b_xres[i]])
                    u = ub[i % 3]; f = uf[i % 3]
                    s.op("dve", lambda e: e.tensor_tensor(f.t[:], n_.t[:], G2.t[:], ALU.mult), [n_, G2], [f])
                    if not mix:
                        s.op("dve", lambda e: e.tensor_tensor(u.t[:], f.t[:], B2.t[:], ALU.add), [f, B2], [u])
                        transposes_store_uT(u, i, ptr[i % 2], uTs[i % 2])
                        return
                    s.op("dve", lambda e: e.tensor_tensor(f.t[:], f.t[:], B2.t[:], ALU.add), [f, B2], [f])
                    s.op("act", lambda e: e.copy(out=u.t[:], in_=f.t[:]), [f], [u])
                    s.dma("act", u_d[i * 128:(i + 1) * 128, :], u.t[:], [u], [b_u[i]])
                    uT = uTf[i % 3]
                    for hf in range(2):
                        pt = ptf[(2 * i + hf) % 4]
                        for kk in range(4):
                            k = hf * 4 + kk
                            s.op("pe", lambda e, k=k, kk=kk: e.transpose(pt.t[:, kk, :], f.t[:, k * 128:(k + 1) * 128], ident_f.t[:]),
                                 [f, ident_f], [pt])
                        s.op("act", lambda e: e.copy(out=uT.t[:, hf * 4:hf * 4 + 4, :], in_=pt.t[:]), [pt], [uT])
                    pl = plog[i % 2]; lgt = lg[i % 3]
                    for k in range(8):
                        s.mm(pl.t[:], uT.t[:, k, :], rw.t[:, k, :], k == 0, k == 7, [uT, rw], [pl])
                    s.op("dve", lambda e: e.tensor_tensor(lgt.t[:], pl.t[:], rb.t[:], ALU.add), [pl, rb], [lgt])

                def stageC(i):
                    lgt = lg[i % 3]; t8_ = t8[i % 2]; mk = msk[i % 2]
                    s.op("dve", lambda e: e.max(out=t8_.t[:], in_=lgt.t[:]), [lgt], [t8_])
                    pr = prk[i % 2]
                    s.op("dve", lambda e: e.tensor_scalar(mk.t[:], lgt.t[:], t8_.t[:, 3:4], None, ALU.is_ge), [lgt, t8_], [mk])
                    s.mm(pr.t[:], su_f.t[:], mk.t[:], True, False, [su_f, mk], [pr])
                    s.mm(pr.t[:], ones_f.t[:], msum.t[:], False, True, [ones_f, msum], [pr])
                    for k in range(4):
                        s.op("dve", lambda e, k=k: e.tensor_scalar(oh.t[:, i, k, :], lgt.t[:], t8_.t[:, k:k + 1], None, ALU.is_equal), [lgt, t8_], [oh])
                    n0 = nv0[i % 2]; e4 = ex4[i % 2]; sm_ = sm[i % 2]
                    s.op("dve", lambda e: e.tensor_scalar(n0.t[:], t8_.t[:, 0:1], -1.0, None, ALU.mult), [t8_], [n0])
                    s.op("act", lambda e: e.activation(out=e4.t[:], in_=t8_.t[:, 0:4], func=AF.Exp, bias=n0.t[:, 0:1]), [t8_, n0], [e4])
                    for k in range(4):
                        s.op("dve", lambda e, k=k: e.tensor_tensor(prod.t[:, k, :], oh.t[:, i, k, :], pr.t[:], ALU.mult), [oh, pr], [prod])
                    s.op("dve", lambda e: e.tensor_reduce(out=posk_f.t[:, i, :], in_=prod.t[:], axis=AX.X, op=ALU.add), [prod], [posk_f])
                    s.op("dve", lambda e: e.tensor_tensor(msum.t[:], msum.t[:], mk.t[:], ALU.add), [msum, mk], [msum])
                    s.op("dve", lambda e: e.tensor_reduce(out=sm_.t[:], in_=e4.t[:], axis=AX.X, op=ALU.add), [e4], [sm_])
                    s.op("dve", lambda e: e.reciprocal(sm_.t[:], sm_.t[:]), [sm_], [sm_])
                    s.op("dve", lambda e: e.tensor_scalar(gatek.t[:, i, :], e4.t[:], sm_.t[:, 0:1], None, ALU.mult), [e4, sm_], [gatek])

                yk_sets = 3
                for step in range(NT + 3):
                    if step < NT:
                        stageL(step)
                    if mix and 3 <= step:
                        stageC(step - 3)
                    if 2 <= step <= NT + 1:
                        stageB(step - 2)
                    if 1 <= step <= NT:
                        stageA(step - 1)
                if kind == "mix":
                    pc = prk[0]
                    s.mm(pc.t[:], ones_f.t[:], msum.t[:], True, True, [ones_f, msum], [pc])
                    cnt = s.sb([128, NE], F32, "cnt")
                    nch = s.sb([128, NE], F32, "nch")
                    tmpc = s.sb([128, NE], F32, "tmpc")
                    cs_a = s.sb([128, NE], F32, "csa")
                    cs_b = s.sb([128, NE], F32, "csb")
                    s.op("dve", lambda e: e.tensor_copy(out=cnt.t[:], in_=pc.t[:]), [pc], [cnt])
                    s.op("dve", lambda e: e.tensor_scalar(nch.t[:], cnt.t[:], 0.5, None, ALU.is_gt), [cnt], [nch])
                    for j in range(1, S // CH):
                        s.op("dve", lambda e, j=j: e.tensor_scalar(tmpc.t[:], cnt.t[:], CH * j + 0.5, None, ALU.is_gt), [cnt], [tmpc])
                        s.op("dve", lambda e: e.tensor_tensor(nch.t[:], nch.t[:], tmpc.t[:], ALU.add), [nch, tmpc], [nch])
                    s.op("dve", lambda e: e.tensor_copy(out=cs_a.t[:], in_=nch.t[:]), [nch], [cs_a])
                    a, b = cs_a, cs_b
                    sh_ = 1
                    while sh_ < NE:
                        s.op("dve", lambda e, a=a, b=b, sh_=sh_: e.tensor_copy(out=b.t[:, 0:sh_], in_=a.t[:, 0:sh_]), [a], [b])
                        s.op("dve", lambda e, a=a, b=b, sh_=sh_: e.tensor_tensor(b.t[:, sh_:NE], a.t[:, sh_:NE], a.t[:, 0:NE - sh_], ALU.add), [a], [b])
                        a, b = b, a
                        sh_ *= 2
                    cend = a
                    base = s.sb([128, NE], F32, "base")
                    s.op("dve", lambda e: e.tensor_tensor(base.t[:], cend.t[:], nch.t[:], ALU.subtract), [cend, nch], [base])
                    s.op("dve", lambda e: e.tensor_scalar(base.t[:], base.t[:], float(CH), None, ALU.mult), [base], [base])
                    prod2 = s.sb([128, NT, 4, NE], F32, "prod2")
                    for i in range(NT):
                        for k in range(4):
                            s.op("pool", lambda e, i=i, k=k: e.tensor_tensor(prod2.t[:, i, k, :], oh.t[:, i, k, :], base.t[:], ALU.mult), [oh, base], [prod2])
                    bsel = s.sb([128, NT, 4], F32, "bsel")
                    s.op("dve", lambda e: e.tensor_reduce(out=bsel.t[:], in_=prod2.t[:], axis=AX.X, op=ALU.add), [prod2], [bsel])
                    s.op("dve", lambda e: e.tensor_tensor(posk_f.t[:], posk_f.t[:], bsel.t[:], ALU.add), [posk_f, bsel], [posk_f])
                    s.op("dve", lambda e: e.tensor_copy(out=posk_i.t[:], in_=posk_f.t[:]), [posk_f], [posk_i])
                    cmpt = s.sb([128, NE], F32, "cmpt")
                    cef = s.sb([128, NCH], F32, "cef")
                    wif = s.sb([128, NCH, 8], F32, "wif")
                    tf = s.sb([128, NCH], F32, "tf")
                    for c in range(NCH):
                        s.op("dve", lambda e, c=c: e.tensor_scalar(cmpt.t[:], cend.t[:], float(c), None, ALU.is_le), [cend], [cmpt])
                        s.op("dve", lambda e, c=c: e.tensor_reduce(out=cef.t[:, c:c + 1], in_=cmpt.t[:], axis=AX.X, op=ALU.add), [cmpt], [cef])
                    s.op("dve", lambda e: e.tensor_scalar(cef.t[:], cef.t[:], float(NE - 1), float(l * NE), ALU.min, ALU.add), [cef], [cef])
                    s.op("dve", lambda e: e.tensor_copy(out=bdidx.t[:], in_=cef.t[:]), [cef], [bdidx])
                    s.op("dve", lambda e: e.tensor_scalar(tf.t[:], cef.t[:], 128.0, iota_p.t[:, 0:1], ALU.mult, ALU.add), [cef, iota_p], [tf])
                    s.op("dve", lambda e: e.tensor_copy(out=bgidx.t[:], in_=tf.t[:]), [tf], [bgidx])
                    s.op("dve", lambda e: e.tensor_scalar(tf.t[:], cef.t[:], 1024.0, None, ALU.mult), [cef], [tf])
                    for c in range(NCH):
                        s.op("dve", lambda e, c=c: e.tensor_scalar(wif.t[:, c, :], base_pk.t[:], tf.t[:, c:c + 1], None, ALU.add), [base_pk, tf], [wif])
                    s.op("dve", lambda e: e.tensor_copy(out=widx.t[:], in_=wif.t[:]), [wif], [widx])
                    if "posk" in dbg_out:
                        s.dma("sp", dbg_out["posk"].rearrange("(i p) k -> p i k", p=128), posk_f.t[:], [posk_f], [])
                        s.dma("sp", dbg_out["gatek"].rearrange("(i p) k -> p i k", p=128), gatek.t[:], [gatek], [])
                        s.dma("sp", dbg_out["ctab"], cef.t[:, 0:NCH], [cef], [])

        def scatter_pass():
            with s.phase():
                ub = [s.sb([128, D], BF16, "ub") for _ in range(3)]
                for i in range(NT):
                    u = ub[i % 3]
                    s.dma("sp", u.t[:], u_d[i * 128:(i + 1) * 128, :], [b_u[i]], [u])
                    for k in range(4):
                        s.idma(xs_d, bass.IndirectOffsetOnAxis(ap=posk_i.t[:, i, k:k + 1], axis=0), u.t[:], None,
                               [u, posk_i], [b_xs_all], group="sc")

        def expert_pass(l):
            with s.phase():
                wgu = [s.sb([128, 8, 2 * D], BF16, "wgu") for _ in range(2)]
                wdn = [s.sb([128, 8, D], BF16, "wdn") for _ in range(2)]
                bgu = [s.sb([128, 16], F32, "bgu") for _ in range(2)]
                bdn = [s.sb([128, D], BF16, "bdn") for _ in range(2)]
                xsb = [[s.sb([128, D], BF16, "xsb") for _ in range(4)] for _ in range(2)]
                xT = [s.sb([128, 8, 512], BF16, "xT") for _ in range(2)]
                ptr = [s.ps([128, 8, 128], BF16, "ptr") for _ in range(2)]
                pg = [s.ps([128, 512], F32, "pg") for _ in range(4)]
                pd = [s.ps([128, 512], F32, "pd") for _ in range(2)]
                gc = [s.sb([128, 512], F32, "gc") for _ in range(2)]
                sg = [s.sb([128, 512], F32, "sg") for _ in range(2)]
                uc = [s.sb([128, 512], F32, "uc") for _ in range(2)]
                actT = [s.sb([128, 8, 512], BF16, "actT") for _ in range(2)]
                ysb = [s.sb([128, D], F32, "ysb") for _ in range(2)]
                gpf = load_mod(l, 5, True, "gpf", 1.0 / ALPHA)
                wguv = w_gu.rearrange("l e r f -> (l e r) f")
                wdnv = w_dn.rearrange("l e r f -> (l e r) f")
                bguv = b_guT.rearrange("l e p c -> (l e p) c")
                bdnv = b_dn.rearrange("l e f -> (l e) f")
                IO = bass.IndirectOffsetOnAxis

                def prefetch(c):
                    wg = wgu[c % 2]; wd = wdn[c % 2]; bg = bgu[c % 2]; bd = bdn[c % 2]
                    for k in range(8):
                        s.idma(wg.t[:, k, :], None, wguv, IO(ap=widx.t[:, c, k:k + 1], axis=0), [widx], [wg], group=("wg", c))
                    s.idma(bg.t[:], None, bguv, IO(ap=bgidx.t[:, c:c + 1], axis=0), [bgidx], [bg])
                    s.op("dve", lambda e: e.tensor_scalar(bg.t[:, 8:16], bg.t[:, 8:16], 1.0, None, ALU.add), [bg], [bg])
                    for k in range(8):
                        s.idma(wd.t[:, k, :], None, wdnv, IO(ap=widx.t[:, c, k:k + 1], axis=0), [widx], [wd], group=("wd", c))
                    s.idma(bd.t[:], None, bdnv, IO(ap=bdidx.t[:, c:c + 1], axis=0), [bdidx], [bd])
                    for sb_ in range(4):
                        xb = xsb[c % 2][sb_]
                        r0 = c * CH + sb_ * 128
                        s.dma("sp", xb.t[:], xs_d[r0:r0 + 128, :], [b_xs_all], [xb])

                def compute(c):
                    wg = wgu[c % 2]; wd = wdn[c % 2]; bg = bgu[c % 2]; bd = bdn[c % 2]
                    xt_ = xT[c % 2]; at = actT[c % 2]
                    for sb_ in range(4):
                        xb = xsb[c % 2][sb_]; pt = ptr[sb_ % 2]
                        for k in range(8):
                            s.op("pe", lambda e, k=k: e.transpose(pt.t[:, k, :], xb.t[:, k * 128:(k + 1) * 128], ident_b.t[:]), [xb, ident_b], [pt])
                        s.op("act", lambda e: e.copy(out=xt_.t[:, :, sb_ * 128:(sb_ + 1) * 128], in_=pt.t[:]), [pt], [xt_])
                    for j in range(8):
                        p_g = pg[(2 * j) % 4]; p_u = pg[(2 * j + 1) % 4]
                        for k in range(8):
                            s.mm(p_g.t[:], wg.t[:, k, j * 128:(j + 1) * 128], xt_.t[:, k, :], k == 0, k == 7, [wg, xt_], [p_g])
                        for k in range(8):
                            s.mm(p_u.t[:], wg.t[:, k, D + j * 128:D + (j + 1) * 128], xt_.t[:, k, :], k == 0, k == 7, [wg, xt_], [p_u])
                        g_ = gc[j % 2]; s_ = sg[j % 2]; u_ = uc[j % 2]
                        s.op("dve", lambda e: e.tensor_scalar(g_.t[:], p_g.t[:], bg.t[:, j:j + 1], 7.0, ALU.add, ALU.min), [p_g, bg], [g_])
                        s.op("act", lambda e: e.activation(out=s_.t[:], in_=g_.t[:], func=AF.Sigmoid, scale=1.702), [g_], [s_])
                        s.op("dve", lambda e: e.tensor_scalar(u_.t[:], p_u.t[:], bg.t[:, 8 + j:9 + j], 8.0, ALU.add, ALU.min), [p_u, bg], [u_])
                        s.op("dve", lambda e: e.tensor_tensor(g_.t[:], g_.t[:], s_.t[:], ALU.mult), [g_, s_], [g_])
                        s.op("dve", lambda e: e.scalar_tensor_tensor(at.t[:, j, :], u_.t[:], -6.0, g_.t[:], ALU.max, ALU.mult), [g_, u_], [at])
                    for sb_ in range(4):
                        yb = ysb[sb_ % 2]
                        for nh in range(2):
                            p = pd[nh]
                            for j in range(8):
                                s.mm(p.t[:], at.t[:, j, sb_ * 128:(sb_ + 1) * 128], wd.t[:, j, nh * 512:(nh + 1) * 512], j == 0, False, [at, wd], [p])
                            s.mm(p.t[:], ones_b.t[0:1, :], bd.t[0:1, nh * 512:(nh + 1) * 512], False, True, [ones_b, bd], [p])
                            s.op("dve", lambda e: e.tensor_tensor(yb.t[:, nh * 512:(nh + 1) * 512], p.t[:], gpf.t[:, nh * 512:(nh + 1) * 512], ALU.mult), [p, gpf], [yb])
                        r0 = c * CH + sb_ * 128
                        s.dma("sp", ys_d[r0:r0 + 128, :], yb.t[:], [yb], [b_ys_all], group=("ys", l))

                prefetch(0)
                for c in range(NCH):
                    if c + 1 < NCH:
                        prefetch(c + 1)
                    compute(c)

        eps_t = s.sb([128, 1], F32, "eps")
        s.op("pool", lambda e: e.memset(eps_t.t[:], LN_EPS), [], [eps_t])

        prologue()
        premod(0)
        x_src = x_in
        stop_after = (dbg or {}).get("_stop", None)
        for l in range(layers):
            if l % 2 == 0:
                even_mixer(l)
            else:
                odd_mixer(l)
            norm_pass(l, "mix", x_src, False)
            x_src = xres
            scatter_pass()
            expert_pass(l)
            norm_pass(l, "ffn", x_src, l == layers - 1)
        s.barrier()
    return nc


def prep_inputs(inputs, S=4096, ncores=8, layers=DEPTH):
    f = lambda a: np.ascontiguousarray(np.asarray(a))
    shared = {}
    for k in ("ada_w", "ada_b", "ln_mix_g", "ln_mix_b", "ln_ffn_g", "ln_ffn_b", "ev_w_in", "ev_sg_b", "ev_vn_g", "ev_vn_b",
              "ev_w_out", "od_w_in", "od_w_uq", "od_w_ukv", "od_w_out", "moe_router_w", "moe_router_b", "moe_w_gu",
              "moe_w_dn", "moe_b_dn"):
        shared[k] = f(inputs[k][:layers]) if k in ("moe_w_gu", "moe_w_dn", "ada_w") else f(inputs[k])
    shared["ev_conv"] = f(np.asarray(inputs["ev_conv_w"]).transpose(0, 2, 1).reshape(2, 4, 128, 3).transpose(0, 2, 1, 3))
    shared["ev_sgT"] = f(np.asarray(inputs["ev_sg_w"]).transpose(0, 1, 3, 2))
    shared["od_dw"] = f(np.asarray(inputs["od_dw_w"]).transpose(0, 2, 1).reshape(2, 4, 128, 31).transpose(0, 2, 1, 3))
    col = lambda a, n: f(np.asarray(a).reshape(2, n, 128).transpose(0, 2, 1))
    shared["od_dw_b"] = col(inputs["od_dw_b"], 4)
    shared["od_cn_g"] = col(inputs["od_cn_g"], 4)
    shared["od_cn_b"] = col(inputs["od_cn_b"], 4)
    shared["od_qn_g"] = col(inputs["od_qn_g"], 2)
    shared["od_kvn_g"] = col(inputs["od_kvn_g"], 1)
    shared["moe_b_guT"] = f(np.asarray(inputs["moe_b_gu"]).reshape(DEPTH, NE, 16, 128).transpose(0, 1, 3, 2))
    invf = (10000.0 ** (-np.arange(0, 32, 2, dtype=np.float32) / 32)).astype(np.float32)
    shared["invf"] = f(np.concatenate([invf, invf]).reshape(32, 1))
    maps = []
    x = np.asarray(inputs["x"]); c = np.asarray(inputs["c"]); pos = np.asarray(inputs["positions"])
    for b in range(ncores):
        m = dict(shared)
        m["x"] = f(x[b, :S])
        m["c"] = f(c[b].reshape(8, 128).T)
        m["pos"] = f(np.broadcast_to(pos[b, :S].astype(np.int32)[None, :], (32, S)))
        maps.append(m)
    return maps


_NC_CACHE = {}


def kernel(**inputs):
    S = 4096
    if "nc" not in _NC_CACHE:
        _NC_CACHE["nc"] = build(S)
    nc = _NC_CACHE["nc"]
    maps = prep_inputs(inputs, S, 8)
    res = run_bass_kernel_spmd(nc, maps, core_ids=list(range(8)))
    return np.stack([np.asarray(r["out"]) for r in res.results], axis=0).astype(np.float32)
```

```python
import math
import numpy as np
from contextlib import ExitStack
import concourse.bass as bass
import concourse.mybir as mybir
from concourse.bass_utils import run_bass_kernel_spmd

F32 = mybir.dt.float32
BF16 = mybir.dt.bfloat16
I32 = mybir.dt.int32
AF = mybir.ActivationFunctionType
ALU = mybir.AluOpType
AX = mybir.AxisListType

D = 1024
DEPTH = 4
NE = 32
TOPK = 4
CH = 384
ALPHA = (2.0 * DEPTH) ** 0.25
LN_EPS = 1e-5
RMS_EPS = 1e-6
TWO_PI_HI = 6.28125
TWO_PI_LO = 2.0 * math.pi - 6.28125


class Buf:
    __slots__ = ("w", "r", "g")

    def __init__(self):
        self.w = []
        self.r = []
        self.g = None


class T:
    __slots__ = ("t", "b")

    def __init__(self, t):
        self.t = t
        self.b = Buf()


class Sched:
    ENG = ("pe", "act", "dve", "pool", "sp")

    def __init__(self, nc, es, ndma=12):
        self.nc = nc
        self.es = es
        self.eng = {"pe": nc.tensor, "act": nc.scalar, "dve": nc.vector, "pool": nc.gpsimd, "sp": nc.sync}
        self.sem = {}
        self.cnt = {}
        self.seen = {e: {} for e in self.ENG}
        for e in self.ENG:
            self.sem[e] = es.enter_context(nc.semaphore("s_" + e))
            self.cnt[e] = 0
        self.dq = {}
        for q in ("sp", "pool", "act"):
            lst = []
            for i in range(ndma if q != "act" else 8):
                key = "d_%s%d" % (q, i)
                self.sem[key] = es.enter_context(nc.semaphore(key))
                self.cnt[key] = 0
                lst.append(key)
            self.dq[q] = [lst, 0]
        self.uid = 0

    def sb(self, shape, dt, name=None):
        self.uid += 1
        return T(self.es_cur.enter_context(self.nc.sbuf_tensor("%s_%d" % (name or "t", self.uid), list(shape), dt)))

    def ps(self, shape, dt, name=None):
        self.uid += 1
        return T(self.es_cur.enter_context(self.nc.psum_tensor("%s_%d" % (name or "p", self.uid), list(shape), dt)))

    def _wait(self, e, evs):
        seen = self.seen[e]
        for key, val in evs:
            if key == "pe" and e == "pe":
                continue
            if seen.get(key, 0) >= val:
                continue
            self.eng[e].wait_ge(self.sem[key], val)
            seen[key] = val

    @staticmethod
    def _deps(reads, writes, group=None):
        evs = []
        for b in reads:
            b = b.b if isinstance(b, T) else b
            evs.extend(b.w)
        for b in writes:
            b = b.b if isinstance(b, T) else b
            if group is None or b.g != group:
                evs.extend(b.w)
            evs.extend(b.r)
        return evs

    @staticmethod
    def _update(ev, reads, writes, group=None):
        for b in reads:
            b = b.b if isinstance(b, T) else b
            b.r.append(ev)
            if len(b.r) > 24:
                last = {}
                for k, v in b.r:
                    if last.get(k, 0) < v:
                        last[k] = v
                b.r = list(last.items())
        for b in writes:
            b = b.b if isinstance(b, T) else b
            if group is not None and b.g == group:
                b.w.append(ev)
            else:
                b.w = [ev]
                b.g = group
            b.r = []

    def op(self, e, fn, reads=(), writes=()):
        self._wait(e, self._deps(reads, writes))
        ins = fn(self.eng[e])
        self.cnt[e] += 1
        ins.then_inc(self.sem[e], 1)
        self.seen[e][e] = max(self.seen[e].get(e, 0), 0)
        self._update((e, self.cnt[e]), reads, writes)

    def mm(self, out, lhsT, rhs, start, stop, reads, writes, **kw):
        self.op("pe", lambda pe: pe.matmul(out, lhsT=lhsT, rhs=rhs, start=start, stop=stop, **kw), reads, writes)

    def dma(self, q, out, in_, reads=(), writes=(), group=None, **kw):
        lst, idx = self.dq[q]
        key = lst[idx % len(lst)]
        self.dq[q][1] = idx + 1
        evs = self._deps(reads, writes, group)
        if self.cnt[key] > 0:
            evs.append((key, self.cnt[key]))
        self._wait(q, evs)
        ins = self.eng[q].dma_start(out=out, in_=in_, **kw)
        self.cnt[key] += 16
        ins.then_inc(self.sem[key], 16)
        self._update((key, self.cnt[key]), reads, writes, group)

    def idma(self, out, out_off, in_, in_off, reads=(), writes=(), group=None):
        q = "pool"
        lst, idx = self.dq[q]
        key = lst[idx % len(lst)]
        self.dq[q][1] = idx + 1
        evs = self._deps(reads, writes, group)
        if self.cnt[key] > 0:
            evs.append((key, self.cnt[key]))
        self._wait(q, evs)
        ins = self.eng[q].indirect_dma_start(out=out, out_offset=out_off, in_=in_, in_offset=in_off)
        self.cnt[key] += 16
        ins.then_inc(self.sem[key], 16)
        self._update((key, self.cnt[key]), reads, writes, group)

    def barrier(self):
        evs = [(k, v) for k, v in self.cnt.items() if v > 0]
        for e in self.ENG:
            self._wait(e, evs)

    def phase(self):
        return _Phase(self)


class _Phase:
    def __init__(self, s):
        self.s = s

    def __enter__(self):
        self.es = ExitStack()
        self.es.__enter__()
        self.s.es_cur = self.es
        return self.s

    def __exit__(self, *a):
        self.s.barrier()
        self.es.__exit__(*a)
        return False


def bcast(ap, n=128):
    return ap.partition_broadcast(n)


def build(S=4096, layers=DEPTH, dbg=None):
    NT = S // 128
    NG = S // 512
    NCH = (S * TOPK) // CH + NE
    NSLOT = NCH * CH
    nc = bass.Bass("TRN2", target_bir_lowering=False)

    def din(name, shape, dt=F32):
        return nc.dram_tensor(name, list(shape), dt, kind="ExternalInput").ap()

    def dscr(name, shape, dt=F32):
        return nc.dram_tensor(name, list(shape), dt, kind="Internal").ap()

    n_even = (layers + 1) // 2
    n_odd = layers // 2
    x_in = din("x", [S, D])
    c_in = din("c", [128, 8])
    pos_in = din("pos", [32, S], I32)
    invf_in = din("invf", [32, 1])
    ada_w = din("ada_w", [layers, D, 6 * D])
    ada_b = din("ada_b", [DEPTH, 6 * D])
    ln_mix_g = din("ln_mix_g", [DEPTH, D]); ln_mix_b = din("ln_mix_b", [DEPTH, D])
    ln_ffn_g = din("ln_ffn_g", [DEPTH, D]); ln_ffn_b = din("ln_ffn_b", [DEPTH, D])
    ev_w_in = din("ev_w_in", [2, D, 2560])
    ev_conv = din("ev_conv", [2, 128, 4, 3])
    ev_sgT = din("ev_sgT", [2, 8, 128, 128])
    ev_sg_b = din("ev_sg_b", [2, 8, 128])
    ev_vn_g = din("ev_vn_g", [2, 512]); ev_vn_b = din("ev_vn_b", [2, 512])
    ev_w_out = din("ev_w_out", [2, D, D])
    od_w_in = din("od_w_in", [2, D, 1440])
    od_dw = din("od_dw", [2, 128, 4, 31])
    od_dw_b = din("od_dw_b", [2, 128, 4])
    od_cn_g = din("od_cn_g", [2, 128, 4]); od_cn_b = din("od_cn_b", [2, 128, 4])
    od_qn_g = din("od_qn_g", [2, 128, 2])
    od_w_uq = din("od_w_uq", [2, 256, 768])
    od_kvn_g = din("od_kvn_g", [2, 128, 1])
    od_w_ukv = din("od_w_ukv", [2, 128, 1024])
    od_w_out = din("od_w_out", [2, D, D])
    r_w = din("moe_router_w", [DEPTH, D, NE])
    r_b = din("moe_router_b", [DEPTH, NE])
    w_gu = din("moe_w_gu", [layers, NE, D, 2 * D])
    b_guT = din("moe_b_guT", [DEPTH, NE, 128, 16])
    w_dn = din("moe_w_dn", [layers, NE, D, D])
    b_dn = din("moe_b_dn", [DEPTH, NE, D])
    out = nc.dram_tensor("out", [S, D], F32, kind="ExternalOutput").ap()

    xres = dscr("xres", [S, D])
    delta = dscr("delta", [S, D])
    uT_d = dscr("uT_d", [8, 128, S], BF16)
    u_d = dscr("u_d", [S, D], BF16)
    xs_d = dscr("xs_d", [NSLOT, D], BF16)
    ys_d = dscr("ys_d", [NSLOT, D])
    mod_d = dscr("mod_d", [DEPTH, 6 * D])
    rope_d = dscr("rope_d", [2, 32, S])
    od_scr = {}
    if layers > 1:
        od_scr = {"qT": dscr("qT_d", [8, 96, S], BF16), "kT": dscr("kT_d", [8, 96, S], BF16), "v": dscr("v_d", [S, 8, 65], BF16),
                  "yc": dscr("yc_d", [4, 128, S], BF16), "yd": dscr("yd_d", [S, 512], BF16)}
    dbg_out = {}
    if dbg:
        for name, shape in dbg.items():
            dbg_out[name] = nc.dram_tensor("dbg_" + name, list(shape), F32, kind="ExternalOutput").ap()

    tokb = lambda: [Buf() for _ in range(NT)]
    b_xres = tokb(); b_delta = tokb(); b_uT = tokb(); b_u = tokb()
    b_xs = [Buf() for _ in range(NCH)]; b_ys = [Buf() for _ in range(NCH)]
    b_xs_all = Buf(); b_ys_all = Buf()
    b_mod = Buf(); b_rope = Buf()

    with ExitStack() as es_top:
        s = Sched(nc, es_top)
        s.es_cur = es_top
        ident_b = s.sb([128, 128], BF16, "identb")
        ident_f = s.sb([128, 128], F32, "identf")
        ones_f = s.sb([128, 128], F32, "onesf")
        ones_b = s.sb([128, 128], BF16, "onesb")
        su_f = s.sb([128, 128], F32, "suf")
        iota_p = s.sb([128, 1], F32, "iotap")
        posk_f = s.sb([128, NT, 4], F32, "poskf")
        posk_i = s.sb([128, NT, 4], I32, "poski")
        gatek = s.sb([128, NT, 4], F32, "gatek")
        widx = s.sb([128, NCH, 8], I32, "widx")
        bgidx = s.sb([128, NCH], I32, "bgidx")
        bdidx = s.sb([128, NCH], I32, "bdidx")
        base_pk = s.sb([128, 8], F32, "basepk")
        iota_c = s.sb([128, NCH], F32, "iotac")

        def mk_consts():
            s.op("pool", lambda e: e.memset(ident_b.t[:], 0.0), [], [ident_b])
            s.op("pool", lambda e: e.affine_select(out=ident_b.t[:], in_=ident_b.t[:], pattern=[[-1, 128]],
                                                   compare_op=ALU.not_equal, fill=1.0, base=0, channel_multiplier=1),
                 [ident_b], [ident_b])
            s.op("pool", lambda e: e.memset(ident_f.t[:], 0.0), [], [ident_f])
            s.op("pool", lambda e: e.affine_select(out=ident_f.t[:], in_=ident_f.t[:], pattern=[[-1, 128]],
                                                   compare_op=ALU.not_equal, fill=1.0, base=0, channel_multiplier=1),
                 [ident_f], [ident_f])
            s.op("pool", lambda e: e.memset(ones_f.t[:], 1.0), [], [ones_f])
            s.op("pool", lambda e: e.memset(ones_b.t[:], 1.0), [], [ones_b])
            s.op("pool", lambda e: e.memset(su_f.t[:], 1.0), [], [su_f])
            s.op("pool", lambda e: e.affine_select(out=su_f.t[:], in_=su_f.t[:], pattern=[[1, 128]],
                                                   compare_op=ALU.is_gt, fill=0.0, base=0, channel_multiplier=-1),
                 [su_f], [su_f])
            s.op("pool", lambda e: e.iota(iota_p.t[:], pattern=[[0, 1]], base=0, channel_multiplier=1,
                                          allow_small_or_imprecise_dtypes=True), [], [iota_p])
            s.op("pool", lambda e: e.iota(base_pk.t[:], pattern=[[128, 8]], base=0, channel_multiplier=1,
                                          allow_small_or_imprecise_dtypes=True), [], [base_pk])
            s.op("pool", lambda e: e.iota(iota_c.t[:], pattern=[[1, NCH]], base=0, channel_multiplier=0,
                                          allow_small_or_imprecise_dtypes=True), [], [iota_c])

        mk_consts()

        def prologue():
            with s.phase():
                ct = s.sb([128, 8], F32, "ct")
                cond = s.sb([128, 8], F32, "cond")
                condb = s.sb([128, 8, 128], F32, "condb")
                s.dma("sp", ct.t[:], c_in, [], [ct])
                s.op("act", lambda e: e.activation(out=cond.t[:], in_=ct.t[:], func=AF.Silu), [ct], [cond])
                for k in range(8):
                    s.op("dve", lambda e, k=k: e.tensor_scalar(condb.t[:, k, :], ones_f.t[:], cond.t[:, k:k + 1], None,
                                                               ALU.mult), [ones_f, cond], [condb])
                wst = [s.sb([128, 3072], F32, "adaw") for _ in range(3)]
                pacc = [s.ps([128, 512], F32, "pada") for _ in range(6)]
                modt = s.sb([1, 6 * D], F32, "modt")
                adab = s.sb([1, 6 * D], F32, "adab")
                it = 0
                for l in range(layers):
                    s.dma("sp", adab.t[:], ada_b[l:l + 1, :], [], [adab])
                    for half in range(2):
                        for k in range(8):
                            w = wst[it % 3]; it += 1
                            s.dma("sp", w.t[:], ada_w[l, k * 128:(k + 1) * 128, half * 3072:(half + 1) * 3072], [], [w])
                            for n in range(6):
                                s.mm(pacc[n].t[:], condb.t[:, k, :], w.t[:, n * 512:(n + 1) * 512], k == 0, k == 7,
                                     [condb, w], [pacc[n]])
                        for n in range(6):
                            c0 = half * 3072 + n * 512
                            s.op("dve", lambda e, n=n, c0=c0: e.tensor_tensor(modt.t[0:1, c0:c0 + 512], pacc[n].t[0:1, :],
                                                                              adab.t[0:1, c0:c0 + 512], ALU.add),
                                 [pacc[n], adab], [modt])
                    s.dma("sp", mod_d[l:l + 1, :], modt.t[:], [modt], [b_mod])
            if n_odd > 0:
              with s.phase():
                    pi_ = s.sb([32, S], I32, "posi")
                    ang = s.sb([32, S], F32, "ang")
                    q = s.sb([32, S], F32, "q")
                    qi = s.sb([32, S], I32, "qi")
                    r = s.sb([32, S], F32, "r")
                    m = s.sb([32, S], F32, "m")
                    invf = s.sb([32, 1], F32, "invf")
                    s.dma("sp", pi_.t[:], pos_in, [], [pi_])
                    s.dma("sp", invf.t[:], invf_in, [], [invf])
                    s.op("dve", lambda e: e.tensor_copy(out=ang.t[:], in_=pi_.t[:]), [pi_], [ang])
                    s.op("dve", lambda e: e.tensor_scalar(ang.t[:], ang.t[:], invf.t[:, 0:1], None, ALU.mult), [ang, invf], [ang])
                    s.op("dve", lambda e: e.tensor_scalar(q.t[:], ang.t[:], 1.0 / (2 * math.pi), None, ALU.mult), [ang], [q])
                    s.op("dve", lambda e: e.tensor_copy(out=qi.t[:], in_=q.t[:]), [q], [qi])
                    s.op("dve", lambda e: e.tensor_copy(out=q.t[:], in_=qi.t[:]), [qi], [q])
                    s.op("dve", lambda e: e.scalar_tensor_tensor(r.t[:], q.t[:], -TWO_PI_HI, ang.t[:], ALU.mult, ALU.add), [q, ang], [r])
                    s.op("dve", lambda e: e.scalar_tensor_tensor(r.t[:], q.t[:], -TWO_PI_LO, r.t[:], ALU.mult, ALU.add), [q, r], [r])

                    def wrap(t):
                        s.op("dve", lambda e: e.tensor_scalar(m.t[:], t.t[:], math.pi, -2 * math.pi, ALU.is_gt, ALU.mult), [t], [m])
                        s.op("dve", lambda e: e.tensor_tensor(t.t[:], t.t[:], m.t[:], ALU.add), [t, m], [t])
                        s.op("dve", lambda e: e.tensor_scalar(m.t[:], t.t[:], -math.pi, 2 * math.pi, ALU.is_lt, ALU.mult), [t], [m])
                        s.op("dve", lambda e: e.tensor_tensor(t.t[:], t.t[:], m.t[:], ALU.add), [t, m], [t])
                        s.op("dve", lambda e: e.tensor_scalar(t.t[:], t.t[:], math.pi, -math.pi, ALU.min, ALU.max), [t], [t])

                    wrap(r)
                    sn = s.sb([32, S], F32, "sn")
                    s.op("act", lambda e: e.activation(out=sn.t[:], in_=r.t[:], func=AF.Sin), [r], [sn])
                    s.dma("sp", rope_d[1], sn.t[:], [sn], [b_rope])
                    s.op("dve", lambda e: e.tensor_scalar(r.t[:], r.t[:], math.pi / 2, None, ALU.add), [r], [r])
                    wrap(r)
                    cs = s.sb([32, S], F32, "cs")
                    s.op("act", lambda e: e.activation(out=cs.t[:], in_=r.t[:], func=AF.Sin), [r], [cs])
                    s.dma("sp", rope_d[0], cs.t[:], [cs], [b_rope])

        def load_mod(l, j, plus1, name, scale=None):
            t = s.sb([128, D], F32, name)
            s.dma("sp", t.t[:], bcast(mod_d[l:l + 1, j * D:(j + 1) * D]), [b_mod], [t])
            if plus1 and scale is not None:
                s.op("pool", lambda e: e.tensor_scalar(t.t[:], t.t[:], 1.0, float(scale), ALU.add, ALU.mult), [t], [t])
            elif plus1:
                s.op("pool", lambda e: e.tensor_scalar(t.t[:], t.t[:], 1.0, None, ALU.add), [t], [t])
            return t

        def load_row(src_row, n, name):
            t = s.sb([128, n], F32, name)
            s.dma("sp", t.t[:], bcast(src_row), [], [t])
            return t

        def transposes_store_uT(ub, i, ptr, uTs):
            for k in range(8):
                s.op("pe", lambda e, k=k: e.transpose(ptr.t[:, k, :], ub.t[:, k * 128:(k + 1) * 128], ident_b.t[:]),
                     [ub, ident_b], [ptr])
            s.op("act", lambda e: e.copy(out=uTs.t[:], in_=ptr.t[:]), [ptr], [uTs])
            s.dma("act", uT_d[:, :, i * 128:(i + 1) * 128].rearrange("k p t -> p k t"), uTs.t[:], [uTs], [b_uT[i]])

        def premod(l):
            with s.phase():
                sc1 = load_mod(l, 1, True, "sc1")
                sh = load_mod(l, 0, False, "sh")
                xt = [s.sb([128, D], F32, "xt") for _ in range(2)]
                ub = [s.sb([128, D], BF16, "ub") for _ in range(2)]
                ptr = [s.ps([128, 8, 128], BF16, "ptr") for _ in range(2)]
                uTs = [s.sb([128, 8, 128], BF16, "uTs") for _ in range(2)]
                for i in range(NT):
                    x = xt[i % 2]; u = ub[i % 2]
                    s.dma("sp", x.t[:], x_in[i * 128:(i + 1) * 128, :], [], [x])
                    s.op("dve", lambda e: e.tensor_tensor(x.t[:], x.t[:], sc1.t[:], ALU.mult), [x, sc1], [x])
                    s.op("dve", lambda e: e.tensor_tensor(u.t[:], x.t[:], sh.t[:], ALU.add), [x, sh], [u])
                    transposes_store_uT(u, i, ptr[i % 2], uTs[i % 2])

        def even_mixer(l):
            li = l // 2
            with s.phase():
                win = s.sb([128, 8, 2560], BF16, "win")
                wout = s.sb([128, 8, D], BF16, "wout")
                wv = ev_w_in[li].rearrange("(k p) f -> p k f", p=128)
                gpw = load_mod(l, 2, True, "gpw", 1.0 / ALPHA)
                wstg = [s.sb([128, D], F32, "wstg") for _ in range(2)]
                for k in range(8):
                    s.dma("pool", win.t[:, k, 0:1280], wv[:, k, 0:1280], [], [win], group="w")
                    s.dma("pool", win.t[:, k, 1280:2560], wv[:, k, 1280:2560], [], [win], group="w")
                for k in range(8):
                    s.dma("sp", wstg[k % 2].t[:], ev_w_out[li, k * 128:(k + 1) * 128, :], [], [wstg[k % 2]])
                    s.op("dve", lambda e, k=k: e.tensor_tensor(wout.t[:, k, :], wstg[k % 2].t[:], gpw.t[:], ALU.mult), [wstg[k % 2], gpw], [wout])
                sgf = s.sb([128, 8, 128], F32, "sgf")
                sgm = s.sb([128, 8, 128], BF16, "sgm")
                s.dma("sp", sgf.t[:], ev_sgT[li].rearrange("h j i -> j h i"), [], [sgf])
                for h in range(8):
                    s.op("pool", lambda e, h=h: e.affine_select(out=sgf.t[:, h, :], in_=sgf.t[:, h, :], pattern=[[1, 128]],
                                                                compare_op=ALU.is_ge, fill=0.0, base=0, channel_multiplier=-1),
                         [sgf], [sgf])
                s.op("pool", lambda e: e.tensor_copy(out=sgm.t[:], in_=sgf.t[:]), [sgf], [sgm])
                sgb = s.sb([128, 4, 128], F32, "sgb")
                for h in range(8):
                    s.dma("sp", sgb.t[(h % 2) * 64:(h % 2) * 64 + 64, h // 2, :], bcast(ev_sg_b[li, h:h + 1, :], 64), [], [sgb], group="w")
                cw = s.sb([128, 4, 3], F32, "cw")
                s.dma("sp", cw.t[:], ev_conv[li], [], [cw])
                vng = load_row(ev_vn_g[li:li + 1, :], 512, "vng")
                vnb = load_row(ev_vn_b[li:li + 1, :], 512, "vnb")
                halo = s.sb([128, 4, 2], F32, "halo")
                s.op("pool", lambda e: e.memset(halo.t[:], 0.0), [], [halo])

                uTg = [s.sb([128, 8, 512], BF16, "uTg") for _ in range(2)]
                pp = [s.ps([128, 512], F32, "pp") for _ in range(6)]
                psg = [s.ps([128, 128], F32, "psg") for _ in range(2)]
                cg = [s.sb([128, 512], F32, "cg") for _ in range(2)]
                cx = [s.sb([128, 514], F32, "cx") for _ in range(2)]
                acc = [s.sb([128, 512], F32, "acc") for _ in range(2)]
                yT = [s.sb([128, 8, 512], BF16, "yT") for _ in range(2)]
                zuT = [s.sb([128, 4, 512], F32, "zuT") for _ in range(2)]
                zv = [s.sb([128, 512], F32, "zv") for _ in range(2)]
                zvn = [s.sb([128, 512], BF16, "zvn") for _ in range(2)]
                st6 = [s.sb([128, 6], F32, "st6") for _ in range(2)]
                mv = [s.sb([128, 2], F32, "mv") for _ in range(2)]
                rstd = [s.sb([128, 1], F32, "rstd") for _ in range(2)]
                tmp = [s.sb([128, 128], F32, "tmp") for _ in range(2)]
                dl = [s.sb([128, D], F32, "dl") for _ in range(2)]
                ppi = 0
                for g in range(NG):
                    u = uTg[g % 2]; y = yT[g % 2]; zu = zuT[g % 2]
                    s.dma("sp", u.t[:], uT_d[:, :, g * 512:(g + 1) * 512].rearrange("k p t -> p k t"),
                          [b_uT[4 * g + j] for j in range(4)], [u])

                    def proj(f):
                        nonlocal ppi
                        p = pp[ppi % 6]; ppi += 1
                        for k in range(8):
                            s.mm(p.t[:], win.t[:, k, f * 128:(f + 1) * 128], u.t[:, k, :], k == 0, k == 7, [win, u], [p])
                        return p

                    for cc in range(4):
                        c_ = cg[cc % 2]; x_ = cx[cc % 2]; a_ = acc[cc % 2]
                        p_c = proj(4 + cc)
                        s.op("act", lambda e: e.copy(out=c_.t[:], in_=p_c.t[:]), [p_c], [c_])
                        p_x = proj(8 + cc)
                        s.op("pool", lambda e: e.tensor_copy(out=x_.t[:, 0:2], in_=halo.t[:, cc, :]), [halo], [x_])
                        s.op("dve", lambda e: e.tensor_tensor(x_.t[:, 2:514], p_x.t[:], c_.t[:], ALU.mult), [p_x, c_], [x_])
                        s.op("pool", lambda e: e.tensor_copy(out=halo.t[:, cc, :], in_=x_.t[:, 512:514]), [x_], [halo])
                        s.op("dve", lambda e: e.tensor_scalar(a_.t[:], x_.t[:, 0:512], cw.t[:, cc, 0:1], None, ALU.mult), [x_, cw], [a_])
                        s.op("dve", lambda e: e.scalar_tensor_tensor(a_.t[:], x_.t[:, 1:513], cw.t[:, cc, 1:2], a_.t[:], ALU.mult, ALU.add),
                             [x_, cw, a_], [a_])
                        s.op("dve", lambda e: e.scalar_tensor_tensor(a_.t[:], x_.t[:, 2:514], cw.t[:, cc, 2:3], a_.t[:], ALU.mult, ALU.add),
                             [x_, cw, a_], [a_])
                        p_b = proj(cc)
                        s.op("dve", lambda e: e.tensor_tensor(y.t[:, cc, :], p_b.t[:], a_.t[:], ALU.mult), [p_b, a_], [y])
                        p_u = proj(12 + cc)
                        s.op("act", lambda e: e.activation(out=zu.t[:, cc, :], in_=p_u.t[:], func=AF.Gelu), [p_u], [zu])
                    for tt in range(4):
                        i = 4 * g + tt
                        z = zv[tt % 2]; zn = zvn[tt % 2]; s6 = st6[tt % 2]; m_ = mv[tt % 2]; rs = rstd[tt % 2]
                        p = pp[ppi % 6]; ppi += 1
                        for k in range(8):
                            s.mm(p.t[:], u.t[:, k, tt * 128:(tt + 1) * 128], win.t[:, k, 2048:2560], k == 0, k == 7, [win, u], [p])
                        s.op("act", lambda e: e.activation(out=z.t[:], in_=p.t[:], func=AF.Gelu), [p], [z])
                        s.op("dve", lambda e: e.bn_stats(s6.t[:], z.t[:]), [z], [s6])
                        s.op("dve", lambda e: e.bn_aggr(m_.t[:], s6.t[:]), [s6], [m_])
                        s.op("act", lambda e: e.activation(out=rs.t[:], in_=m_.t[:, 1:2], func=AF.Sqrt, bias=eps_t.t[:, 0:1]), [m_, eps_t], [rs])
                        s.op("dve", lambda e: e.reciprocal(rs.t[:], rs.t[:]), [rs], [rs])
                        s.op("dve", lambda e: e.tensor_scalar(z.t[:], z.t[:], m_.t[:, 0:1], rs.t[:, 0:1], ALU.subtract, ALU.mult), [z, m_, rs], [z])
                        s.op("pool", lambda e: e.tensor_tensor(z.t[:], z.t[:], vng.t[:], ALU.mult), [z, vng], [z])
                        s.op("pool", lambda e: e.tensor_tensor(zn.t[:], z.t[:], vnb.t[:], ALU.add), [z, vnb], [zn])
                        for cc in range(4):
                            for hh in range(2):
                                h = 2 * cc + hh
                                pg = psg[(cc * 2 + hh) % 2]
                                t_ = tmp[(cc * 2 + hh) % 2]
                                s.mm(pg.t[:], zn.t[:, cc * 128:(cc + 1) * 128], sgm.t[:, h, :], True, True, [zn, sgm], [pg])
                                lo, hi = hh * 64, hh * 64 + 64
                                s.op("dve", lambda e: e.tensor_tensor(t_.t[lo:hi, :], pg.t[lo:hi, :], sgb.t[lo:hi, cc, :], ALU.add), [pg, sgb], [t_])
                                s.op("dve", lambda e: e.tensor_tensor(y.t[lo:hi, 4 + cc, tt * 128:(tt + 1) * 128], t_.t[lo:hi, :],
                                                                      zu.t[lo:hi, cc, tt * 128:(tt + 1) * 128], ALU.mult), [t_, zu], [y])
                        d_ = dl[tt % 2]
                        for nh in range(2):
                            p = pp[ppi % 6]; ppi += 1
                            for k in range(8):
                                s.mm(p.t[:], y.t[:, k, tt * 128:(tt + 1) * 128], wout.t[:, k, nh * 512:(nh + 1) * 512], k == 0, k == 7, [y, wout], [p])
                            s.op("act", lambda e: e.copy(out=d_.t[:, nh * 512:(nh + 1) * 512], in_=p.t[:]), [p], [d_])
                        s.dma("act", delta[i * 128:(i + 1) * 128, :], d_.t[:], [d_], [b_delta[i]])

        def odd_mixer(l):
            li = l // 2
            SCALE = 1.0 / math.sqrt(96.0)
            qT_d = od_scr["qT"]; kT_d = od_scr["kT"]; v_d = od_scr["v"]; yc_d = od_scr["yc"]; yd_d = od_scr["yd"]
            b_q = Buf(); b_k = Buf(); b_v = Buf(); b_yc = Buf(); b_yd = Buf()
            with s.phase():
                win = s.sb([128, 8, 1440], BF16, "win")
                wv_ = od_w_in[li].rearrange("(k p) f -> p k f", p=128)
                for k in range(8):
                    s.dma("pool", win.t[:, k, :], wv_[:, k, :], [], [win], group="w")
                winr = s.sb([128, 8, 32], F32, "winr")
                s.dma("sp", winr.t[:], wv_[:, :, 1408:1440], [], [winr])
                win_sw = s.sb([128, 8, 96], BF16, "winsw")
                s.op("pool", lambda e: e.memset(win_sw.t[:], 0.0), [], [win_sw])
                s.op("dve", lambda e: e.tensor_scalar(win_sw.t[:, :, 64:80], winr.t[:, :, 16:32], -1.0, None, ALU.mult), [winr, win_sw], [win_sw])
                s.op("dve", lambda e: e.tensor_copy(out=win_sw.t[:, :, 80:96], in_=winr.t[:, :, 0:16]), [winr, win_sw], [win_sw])
                wuq = s.sb([128, 2, 768], BF16, "wuq")
                wuqf = s.sb([128, 2, 768], F32, "wuqf")
                uqv = od_w_uq[li].rearrange("(k p) f -> p k f", p=128)
                s.dma("pool", wuq.t[:], uqv, [], [wuq])
                s.dma("sp", wuqf.t[:], uqv, [], [wuqf])
                wuq_sw = s.sb([128, 2, 8, 96], BF16, "wuqsw")
                s.op("pool", lambda e: e.memset(wuq_sw.t[:], 0.0), [], [wuq_sw])
                wuqf4 = wuqf.t[:].rearrange("p k (h e) -> p k h e", e=96)
                for kc in range(2):
                    s.op("dve", lambda e, kc=kc: e.tensor_scalar(wuq_sw.t[:, kc, :, 64:80], wuqf4[:, kc, :, 80:96], -1.0, None, ALU.mult), [wuqf, wuq_sw], [wuq_sw])
                    s.op("dve", lambda e, kc=kc: e.tensor_copy(out=wuq_sw.t[:, kc, :, 80:96], in_=wuqf4[:, kc, :, 64:80]), [wuqf, wuq_sw], [wuq_sw])
                wk = s.sb([128, 8, 64], BF16, "wk")
                wvv = s.sb([128, 8, 64], BF16, "wvv")
                ukv = od_w_ukv[li].rearrange("r (h e) -> r h e", e=128)
                s.dma("pool", wk.t[:], ukv[:, :, 0:64], [], [wk])
                s.dma("pool", wvv.t[:], ukv[:, :, 64:128], [], [wvv])
                cwd = s.sb([128, 4, 31], F32, "cwd")
                s.dma("sp", cwd.t[:], od_dw[li], [], [cwd])
                dwb = s.sb([128, 4], F32, "dwb"); cng = s.sb([128, 4], F32, "cng"); cnb = s.sb([128, 4], F32, "cnb")
                qng = s.sb([128, 2], F32, "qng"); kvng = s.sb([128, 1], F32, "kvng")
                s.dma("sp", dwb.t[:], od_dw_b[li], [], [dwb]); s.dma("sp", cng.t[:], od_cn_g[li], [], [cng])
                s.dma("sp", cnb.t[:], od_cn_b[li], [], [cnb]); s.dma("sp", qng.t[:], od_qn_g[li], [], [qng])
                s.dma("sp", kvng.t[:], od_kvn_g[li], [], [kvng])
                diag = s.sb([128, 4, 31, 128], BF16, "diag")
                for cc in range(4):
                    for k in range(31):
                        eng = "dve" if (cc * 31 + k) % 2 == 0 else "pool"
                        s.op(eng, lambda e, cc=cc, k=k: e.tensor_scalar(diag.t[:, cc, k, :], ident_f.t[:], cwd.t[:, cc, k:k + 1], None, ALU.mult),
                             [ident_f, cwd], [diag])
                rms_eps = s.sb([128, 1], F32, "rmseps")
                s.op("pool", lambda e: e.memset(rms_eps.t[:], RMS_EPS), [], [rms_eps])

                uTg = [s.sb([128, 8, 512], BF16, "uTg") for _ in range(2)]
                glu = [s.sb([128, 4, 542], BF16, "glu") for _ in range(2)]
                s.op("pool", lambda e: e.memset(glu[1].t[:, :, 512:542], 0.0), [], [glu[1]])
                sgm = [s.sb([128, 512], F32, "sgm") for _ in range(2)]
                hbuf = s.sb([128, 4, 512], F32, "hbuf")
                hsq = s.sb([128, 4, 512], F32, "hsq")
                mean = s.sb([128, 512], F32, "mean"); var = s.sb([128, 512], F32, "var"); rstd = s.sb([128, 512], F32, "rstd")
                tb = [s.sb([128, 512], F32, "tb") for _ in range(2)]
                ycT = [s.sb([128, 4, 512], BF16, "ycT") for _ in range(2)]
                sq = [s.sb([128, 512], F32, "sq") for _ in range(2)]
                rq = s.sb([128, 512], F32, "rq")
                cqn = s.sb([128, 2, 512], BF16, "cqn")
                ckvn = s.sb([128, 512], BF16, "ckvn")
                cs = s.sb([128, 2, 512], F32, "cs")
                t1 = [s.sb([128, 512], F32, "t1") for _ in range(2)]
                t2 = [s.sb([128, 512], F32, "t2") for _ in range(2)]
                krT = s.sb([128, 512], BF16, "krT")
                qT = [s.sb([128, 8, 512], BF16, "qT") for _ in range(2)]
                kT = [s.sb([128, 8, 512], BF16, "kT") for _ in range(2)]
                vt = [s.sb([128, 8, 65], BF16, "vt") for _ in range(2)]
                for v_ in vt:
                    s.op("pool", lambda e, v_=v_: e.memset(v_.t[:, :, 64:65], 1.0), [], [v_])
                pp = [s.ps([128, 512], F32, "pp") for _ in range(8)]
                ppi = 0
                for g in range(NG):
                    u = uTg[g % 2]; gl = glu[g % 2]; glp = glu[(g + 1) % 2]
                    tsl = slice(g * 512, (g + 1) * 512)
                    s.dma("sp", u.t[:], uT_d[:, :, tsl].rearrange("k p t -> p k t"), [b_uT[4 * g + j] for j in range(4)], [u])
                    s.dma("sp", cs.t[64:96, 0, :], rope_d[0, :, tsl], [b_rope], [cs], group=("cs", g))
                    s.dma("sp", cs.t[64:96, 1, :], rope_d[1, :, tsl], [b_rope], [cs], group=("cs", g))

                    def proj(c0, m, wt=win):
                        nonlocal ppi
                        p = pp[ppi % 8]; ppi += 1
                        for k in range(8):
                            s.mm(p.t[0:m, :], wt.t[:, k, c0:c0 + m], u.t[:, k, :], k == 0, k == 7, [wt, u], [p])
                        return p

                    s.op("pool", lambda e: e.tensor_copy(out=gl.t[:, :, 0:30], in_=glp.t[:, :, 512:542]), [glp], [gl])
                    for cc in range(4):
                        sg_ = sgm[cc % 2]
                        p_b = proj(512 + cc * 128, 128)
                        s.op("act", lambda e: e.activation(out=sg_.t[:], in_=p_b.t[:], func=AF.Sigmoid), [p_b], [sg_])
                        p_a = proj(cc * 128, 128)
                        s.op("dve", lambda e: e.tensor_tensor(gl.t[:, cc, 30:542], p_a.t[:], sg_.t[:], ALU.mult), [p_a, sg_], [gl])
                    for cc in range(4):
                        p = pp[ppi % 8]; ppi += 1
                        for k in range(31):
                            s.mm(p.t[:], diag.t[:, cc, k, :], gl.t[:, cc, k:k + 512], k == 0, k == 30, [diag, gl], [p])
                        s.op("act", lambda e: e.activation(out=hbuf.t[:, cc, :], in_=p.t[:], func=AF.Identity, bias=dwb.t[:, cc:cc + 1]), [p, dwb], [hbuf])
                        s.op("act", lambda e: e.activation(out=hsq.t[:, cc, :], in_=p.t[:], func=AF.Square, bias=dwb.t[:, cc:cc + 1]), [p, dwb], [hsq])
                    p1 = pp[ppi % 8]; ppi += 1
                    p2 = pp[ppi % 8]; ppi += 1
                    for cc in range(4):
                        s.mm(p1.t[:], ones_f.t[:], hbuf.t[:, cc, :], cc == 0, cc == 3, [ones_f, hbuf], [p1])
                    for cc in range(4):
                        s.mm(p2.t[:], ones_f.t[:], hsq.t[:, cc, :], cc == 0, cc == 3, [ones_f, hsq], [p2])
                    s.op("dve", lambda e: e.tensor_scalar(mean.t[:], p1.t[:], 1.0 / 512, None, ALU.mult), [p1], [mean])
                    s.op("pool", lambda e: e.tensor_tensor(var.t[:], mean.t[:], mean.t[:], ALU.mult), [mean], [var])
                    s.op("dve", lambda e: e.scalar_tensor_tensor(var.t[:], p2.t[:], 1.0 / 512, var.t[:], ALU.mult, ALU.subtract), [p2, var], [var])
                    s.op("act", lambda e: e.activation(out=rstd.t[:], in_=var.t[:], func=AF.Sqrt, bias=eps_t.t[:, 0:1]), [var, eps_t], [rstd])
                    s.op("dve", lambda e: e.reciprocal(rstd.t[:], rstd.t[:]), [rstd], [rstd])
                    yc = ycT[g % 2]
                    for cc in range(4):
                        t_ = tb[cc % 2]
                        s.op("dve", lambda e: e.tensor_tensor(t_.t[:], hbuf.t[:, cc, :], mean.t[:], ALU.subtract), [hbuf, mean], [t_])
                        s.op("pool", lambda e: e.tensor_tensor(t_.t[:], t_.t[:], rstd.t[:], ALU.mult), [t_, rstd], [t_])
                        s.op("dve", lambda e: e.tensor_scalar(t_.t[:], t_.t[:], cng.t[:, cc:cc + 1], cnb.t[:, cc:cc + 1], ALU.mult, ALU.add), [t_, cng, cnb], [t_])
                        s.op("act", lambda e: e.activation(out=yc.t[:, cc, :], in_=t_.t[:], func=AF.Silu), [t_], [yc])
                    s.dma("act", yc_d[:, :, tsl].rearrange("c p t -> p c t"), yc.t[:], [yc], [b_yc], group="st")
                    pq = [proj(1024, 128), proj(1152, 128)]
                    for c2 in range(2):
                        s.op("act", lambda e, c2=c2: e.activation(out=sq[c2].t[:], in_=pq[c2].t[:], func=AF.Square), [pq[c2]], [sq[c2]])
                    ps_ = pp[ppi % 8]; ppi += 1
                    for c2 in range(2):
                        s.mm(ps_.t[:], ones_f.t[:], sq[c2].t[:], c2 == 0, c2 == 1, [ones_f, sq[c2]], [ps_])
                    s.op("act", lambda e: e.activation(out=rq.t[:], in_=ps_.t[:], func=AF.Sqrt, bias=rms_eps.t[:, 0:1], scale=1.0 / 256), [ps_, rms_eps], [rq])
                    s.op("dve", lambda e: e.reciprocal(rq.t[:], rq.t[:]), [rq], [rq])
                    for c2 in range(2):
                        s.op("dve", lambda e, c2=c2: e.scalar_tensor_tensor(cqn.t[:, c2, :], pq[c2].t[:], qng.t[:, c2:c2 + 1], rq.t[:], ALU.mult, ALU.mult),
                             [pq[c2], qng, rq], [cqn])
                    pkv = proj(1280, 128)
                    s.op("act", lambda e: e.activation(out=sq[0].t[:], in_=pkv.t[:], func=AF.Square), [pkv], [sq[0]])
                    ps_ = pp[ppi % 8]; ppi += 1
                    s.mm(ps_.t[:], ones_f.t[:], sq[0].t[:], True, True, [ones_f, sq[0]], [ps_])
                    s.op("act", lambda e: e.activation(out=rq.t[:], in_=ps_.t[:], func=AF.Sqrt, bias=rms_eps.t[:, 0:1], scale=1.0 / 128), [ps_, rms_eps], [rq])
                    s.op("dve", lambda e: e.reciprocal(rq.t[:], rq.t[:]), [rq], [rq])
                    s.op("dve", lambda e: e.scalar_tensor_tensor(ckvn.t[:], pkv.t[:], kvng.t[:, 0:1], rq.t[:], ALU.mult, ALU.mult), [pkv, kvng, rq], [ckvn])
                    pk1 = proj(1344, 96)
                    pk2 = proj(0, 96, win_sw)
                    R = slice(64, 96)
                    s.op("dve", lambda e: e.tensor_tensor(t1[0].t[R, :], pk1.t[R, :], cs.t[R, 0, :], ALU.mult), [pk1, cs], [t1[0]])
                    s.op("dve", lambda e: e.tensor_tensor(t2[0].t[R, :], pk2.t[R, :], cs.t[R, 1, :], ALU.mult), [pk2, cs], [t2[0]])
                    s.op("pool", lambda e: e.tensor_tensor(krT.t[R, :], t1[0].t[R, :], t2[0].t[R, :], ALU.add), [t1[0], t2[0]], [krT])
                    q_ = qT[g % 2]; k_ = kT[g % 2]
                    for h in range(8):
                        a1 = t1[h % 2]; a2 = t2[h % 2]
                        pq1 = pp[ppi % 8]; ppi += 1
                        pq2 = pp[ppi % 8]; ppi += 1
                        for kc in range(2):
                            s.mm(pq1.t[0:96, :], wuq.t[:, kc, h * 96:(h + 1) * 96], cqn.t[:, kc, :], kc == 0, kc == 1, [wuq, cqn], [pq1])
                        for kc in range(2):
                            s.mm(pq2.t[0:96, :], wuq_sw.t[:, kc, h, :], cqn.t[:, kc, :], kc == 0, kc == 1, [wuq_sw, cqn], [pq2])
                        s.op("act", lambda e: e.copy(out=q_.t[0:64, h, :], in_=pq1.t[0:64, :]), [pq1], [q_])
                        s.op("dve", lambda e: e.tensor_tensor(a1.t[R, :], pq1.t[R, :], cs.t[R, 0, :], ALU.mult), [pq1, cs], [a1])
                        s.op("dve", lambda e: e.tensor_tensor(a2.t[R, :], pq2.t[R, :], cs.t[R, 1, :], ALU.mult), [pq2, cs], [a2])
                        s.op("pool", lambda e: e.tensor_tensor(q_.t[R, h, :], a1.t[R, :], a2.t[R, :], ALU.add), [a1, a2], [q_])
                        pk = pp[ppi % 8]; ppi += 1
                        s.mm(pk.t[0:64, :], wk.t[:, h, :], ckvn.t[:], True, True, [wk, ckvn], [pk])
                        s.op("act", lambda e: e.copy(out=k_.t[0:64, h, :], in_=pk.t[0:64, :]), [pk], [k_])
                        s.op("pool", lambda e: e.tensor_copy(out=k_.t[R, h, :], in_=krT.t[R, :]), [krT], [k_])
                    s.dma("sp", qT_d[:, :, tsl].rearrange("h p t -> p h t"), q_.t[0:96, :, :], [q_], [b_q], group="st")
                    s.dma("sp", kT_d[:, :, tsl].rearrange("h p t -> p h t"), k_.t[0:96, :, :], [k_], [b_k], group="st")
                    for tt in range(4):
                        i = 4 * g + tt
                        v_ = vt[tt % 2]
                        pv = pp[ppi % 8]; ppi += 1
                        s.mm(pv.t[:], ckvn.t[:, tt * 128:(tt + 1) * 128], wvv.t[:].rearrange("p h e -> p (h e)"), True, True, [ckvn, wvv], [pv])
                        s.op("act", lambda e: e.copy(out=v_.t[:, :, 0:64], in_=pv.t[:].rearrange("p (h e) -> p h e", e=64)), [pv], [v_])
                        s.dma("act", v_d[i * 128:(i + 1) * 128, :, :], v_.t[:], [v_], [b_v], group="st")
            with s.phase():
                masks = s.sb([128, 4, 512], BF16, "masks")
                s.op("pool", lambda e: e.memset(masks.t[:], 1.0), [], [masks])
                for j in range(4):
                    s.op("pool", lambda e, j=j: e.affine_select(out=masks.t[:, j, :], in_=masks.t[:, j, :], pattern=[[1, 512]], compare_op=ALU.is_ge,
                                                                fill=0.0, base=-128 * j, channel_multiplier=-1), [masks], [masks])
                kTh = [s.sb([128, S], BF16, "kTh") for _ in range(2)]
                vh = [s.sb([128, NT, 65], BF16, "vh") for _ in range(2)]
                qg = [s.sb([128, 512], BF16, "qg") for _ in range(2)]
                PT = [s.sb([128, 512], BF16, "PT") for _ in range(3)]
                psT = [s.ps([128, 512], F32, "psT") for _ in range(3)]
                pacc = [s.ps([128, 65], F32, "pacc") for _ in range(4)]
                rec = [s.sb([128, 1], F32, "rec") for _ in range(2)]
                yd = [s.sb([128, 64], BF16, "yd") for _ in range(2)]
                it = 0
                qg = [s.sb([128, 512], BF16, "qg3") for _ in range(3)]

                def load_head(h):
                    s.dma("sp", kTh[h % 2].t[0:96, :], kT_d[h], [b_k], [kTh[h % 2]])
                    vsrc = v_d[:, h, :].rearrange("(t p) e -> p t e", p=128)
                    for qq in range(4):
                        t0_, t1_ = qq * NT // 4, (qq + 1) * NT // 4
                        if t1_ > t0_:
                            s.dma("sp", vh[h % 2].t[:, t0_:t1_, :], vsrc[:, t0_:t1_, :], [b_v], [vh[h % 2]], group=("vh", h))

                def load_q(n):
                    h_, G_ = divmod(n, NG)
                    s.dma("sp", qg[n % 3].t[0:96, :], qT_d[h_, :, G_ * 512:(G_ + 1) * 512], [b_q], [qg[n % 3]])

                load_head(0)
                load_q(0)
                for h in range(8):
                    kt = kTh[h % 2]; vv = vh[h % 2]
                    if h + 1 < 8:
                        load_head(h + 1)
                    for G in range(NG):
                        n = h * NG + G
                        q_ = qg[n % 3]
                        if n + 1 < 8 * NG:
                            load_q(n + 1)
                        nkb = 4 * G + 4

                        def qk(kb):
                            nonlocal it
                            ps_ = psT[it % 3]; it += 1
                            s.mm(ps_.t[:], kt.t[0:96, kb * 128:(kb + 1) * 128], q_.t[0:96, :], True, True, [kt, q_], [ps_])
                            return ps_

                        pend = [qk(0)]
                        if nkb > 1:
                            pend.append(qk(1))
                        for kb in range(nkb):
                            ps_ = pend.pop(0)
                            if kb + 2 < nkb:
                                pend.append(qk(kb + 2))
                            pt = PT[kb % 3]
                            j = kb - 4 * G
                            c0 = max(j, 0) * 128
                            s.op("act", lambda e: e.activation(out=pt.t[:, c0:512], in_=ps_.t[:, c0:512], func=AF.Exp, scale=SCALE), [ps_], [pt])
                            if j >= 0:
                                s.op("dve", lambda e: e.tensor_tensor(pt.t[:, c0:c0 + 128], pt.t[:, c0:c0 + 128], masks.t[:, 0, 0:128], ALU.mult), [pt, masks], [pt])
                            for qs in range(4):
                                last_kb = 4 * G + qs
                                if kb > last_kb:
                                    continue
                                s.mm(pacc[qs].t[:], pt.t[:, qs * 128:(qs + 1) * 128], vv.t[:, kb, :], kb == 0, kb == last_kb, [pt, vv], [pacc[qs]])
                                if kb == last_kb:
                                    r_ = rec[qs % 2]; y_ = yd[qs % 2]
                                    i = 4 * G + qs
                                    s.op("dve", lambda e: e.reciprocal(r_.t[:], pacc[qs].t[:, 64:65]), [pacc[qs]], [r_])
                                    s.op("dve", lambda e: e.tensor_scalar(y_.t[:], pacc[qs].t[:, 0:64], r_.t[:, 0:1], None, ALU.mult), [pacc[qs], r_], [y_])
                                    s.dma("sp", yd_d[i * 128:(i + 1) * 128, h * 64:(h + 1) * 64], y_.t[:], [y_], [b_yd], group="st")
            with s.phase():
                wout = s.sb([128, 8, D], BF16, "wout")
                gpw = load_mod(l, 2, True, "gpw", 1.0 / ALPHA)
                wstg = [s.sb([128, D], F32, "wstg") for _ in range(2)]
                for k in range(8):
                    s.dma("sp", wstg[k % 2].t[:], od_w_out[li, k * 128:(k + 1) * 128, :], [], [wstg[k % 2]])
                    s.op("dve", lambda e, k=k: e.tensor_tensor(wout.t[:, k, :], wstg[k % 2].t[:], gpw.t[:], ALU.mult), [wstg[k % 2], gpw], [wout])
                yT = [s.sb([128, 8, 128], BF16, "yT") for _ in range(2)]
                ydt = [s.sb([128, 512], BF16, "ydt") for _ in range(2)]
                ptr = [s.ps([128, 4, 128], BF16, "ptr") for _ in range(2)]
                pp = [s.ps([128, 512], F32, "pp") for _ in range(4)]
                dl = [s.sb([128, D], F32, "dl") for _ in range(2)]
                for i in range(NT):
                    y = yT[i % 2]; yd_ = ydt[i % 2]; pt = ptr[i % 2]; d_ = dl[i % 2]
                    s.dma("sp", y.t[:, 0:4, :], yc_d[:, :, i * 128:(i + 1) * 128].rearrange("c p t -> p c t"), [b_yc], [y])
                    s.dma("sp", yd_.t[:], yd_d[i * 128:(i + 1) * 128, :], [b_yd], [yd_])
                    for c in range(4):
                        s.op("pe", lambda e, c=c: e.transpose(pt.t[:, c, :], yd_.t[:, c * 128:(c + 1) * 128], ident_b.t[:]), [yd_, ident_b], [pt])
                    s.op("act", lambda e: e.copy(out=y.t[:, 4:8, :], in_=pt.t[:]), [pt], [y])
                    for nh in range(2):
                        p = pp[(2 * i + nh) % 4]
                        for k in range(8):
                            s.mm(p.t[:], y.t[:, k, :], wout.t[:, k, nh * 512:(nh + 1) * 512], k == 0, k == 7, [y, wout], [p])
                        s.op("act", lambda e: e.copy(out=d_.t[:, nh * 512:(nh + 1) * 512], in_=p.t[:]), [p], [d_])
                    s.dma("act", delta[i * 128:(i + 1) * 128, :], d_.t[:], [d_], [b_delta[i]])

        def norm_pass(l, kind, x_src, last):
            with s.phase():
                mix = kind == "mix"
                need_u = not (last and not mix)
                if mix:
                    lng = load_row(ln_mix_g[l:l + 1, :], D, "lng"); lnb = load_row(ln_mix_b[l:l + 1, :], D, "lnb")
                    sc1 = load_mod(l, 4, True, "sc1"); sh = load_mod(l, 3, False, "sh")
                else:
                    lng = load_row(ln_ffn_g[l:l + 1, :], D, "lng"); lnb = load_row(ln_ffn_b[l:l + 1, :], D, "lnb")
                    if need_u:
                        sc1 = load_mod(l + 1, 1, True, "sc1"); sh = load_mod(l + 1, 0, False, "sh")
                if need_u:
                    B2 = sh
                    tmpb = s.sb([128, D], F32, "tmpb")
                    s.op("dve", lambda e: e.tensor_tensor(tmpb.t[:], lnb.t[:], sc1.t[:], ALU.mult), [lnb, sc1], [tmpb])
                    s.op("dve", lambda e: e.tensor_tensor(B2.t[:], tmpb.t[:], sh.t[:], ALU.add), [tmpb, sh], [B2])
                    G2 = sc1
                    s.op("dve", lambda e: e.tensor_tensor(G2.t[:], sc1.t[:], lng.t[:], ALU.mult), [sc1, lng], [G2])
                eps2 = s.sb([128, 1], F32, "eps2")
                s.op("pool", lambda e: e.memset(eps2.t[:], LN_EPS / (ALPHA * ALPHA)), [], [eps2])
                NB = 4
                xt = [s.sb([128, D], F32, "xt") for _ in range(NB)]
                st12 = [s.sb([128, 12], F32, "st12") for _ in range(NB)]
                mv = [s.sb([128, 2], F32, "mv") for _ in range(NB)]
                rstd = [s.sb([128, 1], F32, "rstd") for _ in range(NB)]
                nbias = [s.sb([128, 1], F32, "nbias") for _ in range(NB)]
                nt = [s.sb([128, D], F32, "nt") for _ in range(3)]
                xo = [s.sb([128, D], F32, "xo") for _ in range(3)]
                ub = [s.sb([128, D], BF16, "ub") for _ in range(3)]
                if mix:
                    yt = [s.sb([128, D], F32, "yt") for _ in range(NB)]
                    uf = [s.sb([128, D], F32, "uf") for _ in range(3)]
                    ptf = [s.ps([128, 4, 128], F32, "ptf") for _ in range(4)]
                    uTf = [s.sb([128, 8, 128], F32, "uTf") for _ in range(3)]
                    rw = s.sb([128, 8, NE], F32, "rw")
                    s.dma("sp", rw.t[:], r_w[l].rearrange("(k p) e -> p k e", p=128), [], [rw])
                    rb = load_row(r_b[l:l + 1, :], NE, "rb")
                    plog = [s.ps([128, NE], F32, "plog") for _ in range(2)]
                    prk = [s.ps([128, NE], F32, "prk") for _ in range(2)]
                    lg = [s.sb([128, NE], F32, "lg") for _ in range(3)]
                    t8 = [s.sb([128, 8], F32, "t8") for _ in range(2)]
                    nv0 = [s.sb([128, 1], F32, "nv0") for _ in range(2)]
                    ex4 = [s.sb([128, 4], F32, "ex4") for _ in range(2)]
                    sm = [s.sb([128, 1], F32, "sm") for _ in range(2)]
                    msk = [s.sb([128, NE], F32, "msk") for _ in range(2)]
                    msum = s.sb([128, NE], F32, "msum")
                    oh = s.sb([128, NT, 4, NE], F32, "oh")
                    prod = s.sb([128, 4, NE], F32, "prod")
                    s.op("pool", lambda e: e.memset(msum.t[:], 0.0), [], [msum])
                else:
                    yk = [s.sb([128, D], F32, "yk") for _ in range(12)]
                    if need_u:
                        ptr = [s.ps([128, 8, 128], BF16, "ptr") for _ in range(2)]
                        uTs = [s.sb([128, 8, 128], BF16, "uTs") for _ in range(2)]
                        uf = [s.sb([128, D], F32, "uf") for _ in range(3)]

                def stageL(i):
                    x = xt[i % NB]
                    s.dma("sp", x.t[:], x_src[i * 128:(i + 1) * 128, :], [b_xres[i]] if x_src is xres else [], [x])
                    if mix:
                        y = yt[i % NB]
                        s.dma("sp", y.t[:], delta[i * 128:(i + 1) * 128, :], [b_delta[i]], [y])
                    else:
                        ys_ = [yk[(i % 3) * 4 + k] for k in range(4)]
                        for k in range(4):
                            s.idma(ys_[k].t[:], None, ys_d, bass.IndirectOffsetOnAxis(ap=posk_i.t[:, i, k:k + 1], axis=0),
                                   [b_ys_all, posk_i], [ys_[k]])

                def stageA(i):
                    x = xt[i % NB]; s12 = st12[i % NB]; m_ = mv[i % NB]; rs = rstd[i % NB]; nb_ = nbias[i % NB]
                    if mix:
                        y = yt[i % NB]
                        s.op("dve", lambda e: e.tensor_tensor(x.t[:], x.t[:], y.t[:], ALU.add), [x, y], [x])
                    else:
                        ys_ = [yk[(i % 3) * 4 + k] for k in range(4)]
                        for k in range(4):
                            s.op("act", lambda e, k=k: e.activation(out=ys_[k].t[:], in_=ys_[k].t[:], func=AF.Copy, scale=gatek.t[:, i, k:k + 1]), [ys_[k], gatek], [ys_[k]])
                        s.op("dve", lambda e: e.tensor_tensor(ys_[0].t[:], ys_[0].t[:], ys_[1].t[:], ALU.add), [ys_[0], ys_[1]], [ys_[0]])
                        s.op("dve", lambda e: e.tensor_tensor(ys_[2].t[:], ys_[2].t[:], ys_[3].t[:], ALU.add), [ys_[2], ys_[3]], [ys_[2]])
                        s.op("dve", lambda e: e.tensor_tensor(ys_[0].t[:], ys_[0].t[:], ys_[2].t[:], ALU.add), [ys_[0], ys_[2]], [ys_[0]])
                        s.op("dve", lambda e: e.tensor_tensor(x.t[:], x.t[:], ys_[0].t[:], ALU.add), [x, ys_[0]], [x])
                    s.op("dve", lambda e: e.bn_stats(s12.t[:, 0:6], x.t[:, 0:512]), [x], [s12])
                    s.op("dve", lambda e: e.bn_stats(s12.t[:, 6:12], x.t[:, 512:1024]), [x], [s12])
                    s.op("dve", lambda e: e.bn_aggr(m_.t[:], s12.t[:]), [s12], [m_])
                    s.op("act", lambda e: e.activation(out=rs.t[:], in_=m_.t[:, 1:2], func=AF.Ln, bias=eps2.t[:, 0:1]), [m_, eps2], [rs])
                    s.op("act", lambda e: e.activation(out=rs.t[:], in_=rs.t[:], func=AF.Exp, scale=-0.5), [rs], [rs])
                    s.op("dve", lambda e: e.scalar_tensor_tensor(nb_.t[:], m_.t[:, 0:1], -1.0, rs.t[:], ALU.mult, ALU.mult), [m_, rs], [nb_])

                def stageB(i):
                    x = xt[i % NB]; rs = rstd[i % NB]; nb_ = nbias[i % NB]
                    n_ = nt[i % 3]; o_ = xo[i % 3]
                    s.op("act", lambda e: e.activation(out=n_.t[:], in_=x.t[:], func=AF.Identity, scale=rs.t[:, 0:1], bias=nb_.t[:, 0:1]), [x, rs, nb_], [n_])
                    s.op("dve", lambda e: e.tensor_tensor(o_.t[:], n_.t[:], lng.t[:], ALU.mult), [n_, lng], [o_])
                    s.op("dve", lambda e: e.tensor_tensor(o_.t[:], o_.t[:], lnb.t[:], ALU.add), [o_, lnb], [o_])
                    if not need_u:
                        s.dma("sp", out[i * 128:(i + 1) * 128, :], o_.t[:], [o_], [b_xres[i]])
                        return
                    s.dma("sp", xres[i * 128:(i + 1) * 128, :], o_.t[:], [o_], [b_xres[i]])
                    u = ub[i % 3]; f = uf[i % 3]
                    s.op("dve", lambda e: e.tensor_tensor(f.t[:], n_.t[:], G2.t[:], ALU.mult), [n_, G2], [f])
                    if not mix:
                        s.op("dve", lambda e: e.tensor_tensor(u.t[:], f.t[:], B2.t[:], ALU.add), [f, B2], [u])
                        transposes_store_uT(u, i, ptr[i % 2], uTs[i % 2])
                        return
                    s.op("dve", lambda e: e.tensor_tensor(f.t[:], f.t[:], B2.t[:], ALU.add), [f, B2], [f])
                    s.op("act", lambda e: e.copy(out=u.t[:], in_=f.t[:]), [f], [u])
                    s.dma("act", u_d[i * 128:(i + 1) * 128, :], u.t[:], [u], [b_u[i]])
                    uT = uTf[i % 3]
                    for hf in range(2):
                        pt = ptf[(2 * i + hf) % 4]
                        for kk in range(4):
                            k = hf * 4 + kk
                            s.op("pe", lambda e, k=k, kk=kk: e.transpose(pt.t[:, kk, :], f.t[:, k * 128:(k + 1) * 128], ident_f.t[:]),
                                 [f, ident_f], [pt])
                        s.op("act", lambda e: e.copy(out=uT.t[:, hf * 4:hf * 4 + 4, :], in_=pt.t[:]), [pt], [uT])
                    pl = plog[i % 2]
                    for k in range(8):
                        s.mm(pl.t[:], uT.t[:, k, :], rw.t[:, k, :], k == 0, k == 7, [uT, rw], [pl])

                def stageC(i):
                    lgt = lg[i % 3]; t8_ = t8[i % 2]; mk = msk[i % 2]; pl = plog[i % 2]
                    s.op("dve", lambda e: e.tensor_tensor(lgt.t[:], pl.t[:], rb.t[:], ALU.add), [pl, rb], [lgt])
                    s.op("dve", lambda e: e.max(out=t8_.t[:], in_=lgt.t[:]), [lgt], [t8_])
                    pr = prk[i % 2]
                    s.op("dve", lambda e: e.tensor_scalar(mk.t[:], lgt.t[:], t8_.t[:, 3:4], None, ALU.is_ge), [lgt, t8_], [mk])
                    s.mm(pr.t[:], su_f.t[:], mk.t[:], True, False, [su_f, mk], [pr])
                    s.mm(pr.t[:], ones_f.t[:], msum.t[:], False, True, [ones_f, msum], [pr])
                    for k in range(4):
                        s.op("dve", lambda e, k=k: e.tensor_scalar(oh.t[:, i, k, :], lgt.t[:], t8_.t[:, k:k + 1], None, ALU.is_equal), [lgt, t8_], [oh])
                    n0 = nv0[i % 2]; e4 = ex4[i % 2]; sm_ = sm[i % 2]
                    s.op("dve", lambda e: e.tensor_scalar(n0.t[:], t8_.t[:, 0:1], -1.0, None, ALU.mult), [t8_], [n0])
                    s.op("act", lambda e: e.activation(out=e4.t[:], in_=t8_.t[:, 0:4], func=AF.Exp, bias=n0.t[:, 0:1]), [t8_, n0], [e4])
                    for k in range(4):
                        s.op("dve", lambda e, k=k: e.tensor_tensor(prod.t[:, k, :], oh.t[:, i, k, :], pr.t[:], ALU.mult), [oh, pr], [prod])
                    s.op("dve", lambda e: e.tensor_reduce(out=posk_f.t[:, i, :], in_=prod.t[:], axis=AX.X, op=ALU.add), [prod], [posk_f])
                    s.op("dve", lambda e: e.tensor_tensor(msum.t[:], msum.t[:], mk.t[:], ALU.add), [msum, mk], [msum])
                    s.op("dve", lambda e: e.tensor_reduce(out=sm_.t[:], in_=e4.t[:], axis=AX.X, op=ALU.add), [e4], [sm_])
                    s.op("dve", lambda e: e.reciprocal(sm_.t[:], sm_.t[:]), [sm_], [sm_])
                    s.op("dve", lambda e: e.tensor_scalar(gatek.t[:, i, :], e4.t[:], sm_.t[:, 0:1], None, ALU.mult), [e4, sm_], [gatek])

                yk_sets = 3
                for step in range(NT + 3):
                    if step < NT:
                        stageL(step)
                    if mix and 3 <= step:
                        stageC(step - 3)
                    if 2 <= step <= NT + 1:
                        stageB(step - 2)
                    if 1 <= step <= NT:
                        stageA(step - 1)
                if kind == "mix":
                    pc = prk[0]
                    s.mm(pc.t[:], ones_f.t[:], msum.t[:], True, True, [ones_f, msum], [pc])
                    cnt = s.sb([128, NE], F32, "cnt")
                    nch = s.sb([128, NE], F32, "nch")
                    tmpc = s.sb([128, NE], F32, "tmpc")
                    cs_a = s.sb([128, NE], F32, "csa")
                    cs_b = s.sb([128, NE], F32, "csb")
                    s.op("dve", lambda e: e.tensor_copy(out=cnt.t[:], in_=pc.t[:]), [pc], [cnt])
                    s.op("dve", lambda e: e.tensor_scalar(nch.t[:], cnt.t[:], 0.5, None, ALU.is_gt), [cnt], [nch])
                    for j in range(1, (S + CH - 1) // CH):
                        s.op("dve", lambda e, j=j: e.tensor_scalar(tmpc.t[:], cnt.t[:], CH * j + 0.5, None, ALU.is_gt), [cnt], [tmpc])
                        s.op("dve", lambda e: e.tensor_tensor(nch.t[:], nch.t[:], tmpc.t[:], ALU.add), [nch, tmpc], [nch])
                    s.op("dve", lambda e: e.tensor_copy(out=cs_a.t[:], in_=nch.t[:]), [nch], [cs_a])
                    a, b = cs_a, cs_b
                    sh_ = 1
                    while sh_ < NE:
                        s.op("dve", lambda e, a=a, b=b, sh_=sh_: e.tensor_copy(out=b.t[:, 0:sh_], in_=a.t[:, 0:sh_]), [a], [b])
                        s.op("dve", lambda e, a=a, b=b, sh_=sh_: e.tensor_tensor(b.t[:, sh_:NE], a.t[:, sh_:NE], a.t[:, 0:NE - sh_], ALU.add), [a], [b])
                        a, b = b, a
                        sh_ *= 2
                    cend = a
                    base = s.sb([128, NE], F32, "base")
                    s.op("dve", lambda e: e.tensor_tensor(base.t[:], cend.t[:], nch.t[:], ALU.subtract), [cend, nch], [base])
                    s.op("dve", lambda e: e.tensor_scalar(base.t[:], base.t[:], float(CH), None, ALU.mult), [base], [base])
                    prod2 = s.sb([128, NT, 4, NE], F32, "prod2")
                    s.op("dve", lambda e: e.tensor_tensor(prod2.t[:].rearrange("p i k e -> p (i k) e"), oh.t[:].rearrange("p i k e -> p (i k) e"),
                                                          base.t[:].unsqueeze(1).to_broadcast([128, NT * 4, NE]), ALU.mult), [oh, base], [prod2])
                    bsel = s.sb([128, NT, 4], F32, "bsel")
                    s.op("dve", lambda e: e.tensor_reduce(out=bsel.t[:], in_=prod2.t[:], axis=AX.X, op=ALU.add), [prod2], [bsel])
                    s.op("dve", lambda e: e.tensor_tensor(posk_f.t[:], posk_f.t[:], bsel.t[:], ALU.add), [posk_f, bsel], [posk_f])
                    s.op("dve", lambda e: e.tensor_copy(out=posk_i.t[:], in_=posk_f.t[:]), [posk_f], [posk_i])
                    cmp3 = s.sb([128, NCH, NE], F32, "cmp3")
                    cef = s.sb([128, NCH], F32, "cef")
                    wif = s.sb([128, NCH, 8], F32, "wif")
                    tf = s.sb([128, NCH], F32, "tf")
                    s.op("dve", lambda e: e.tensor_tensor(cmp3.t[:], cend.t[:].unsqueeze(1).to_broadcast([128, NCH, NE]),
                                                          iota_c.t[:].unsqueeze(2).to_broadcast([128, NCH, NE]), ALU.is_le), [cend, iota_c], [cmp3])
                    s.op("dve", lambda e: e.tensor_reduce(out=cef.t[:], in_=cmp3.t[:], axis=AX.X, op=ALU.add), [cmp3], [cef])
                    s.op("dve", lambda e: e.tensor_scalar(cef.t[:], cef.t[:], float(NE - 1), float(l * NE), ALU.min, ALU.add), [cef], [cef])
                    s.op("dve", lambda e: e.tensor_copy(out=bdidx.t[:], in_=cef.t[:]), [cef], [bdidx])
                    s.op("dve", lambda e: e.tensor_scalar(tf.t[:], cef.t[:], 128.0, iota_p.t[:, 0:1], ALU.mult, ALU.add), [cef, iota_p], [tf])
                    s.op("dve", lambda e: e.tensor_copy(out=bgidx.t[:], in_=tf.t[:]), [tf], [bgidx])
                    s.op("dve", lambda e: e.tensor_scalar(tf.t[:], cef.t[:], 1024.0, None, ALU.mult), [cef], [tf])
                    s.op("dve", lambda e: e.tensor_tensor(wif.t[:], tf.t[:].unsqueeze(2).to_broadcast([128, NCH, 8]),
                                                          base_pk.t[:].unsqueeze(1).to_broadcast([128, NCH, 8]), ALU.add), [tf, base_pk], [wif])
                    s.op("dve", lambda e: e.tensor_copy(out=widx.t[:], in_=wif.t[:]), [wif], [widx])
                    if "posk" in dbg_out:
                        s.dma("sp", dbg_out["posk"].rearrange("(i p) k -> p i k", p=128), posk_f.t[:], [posk_f], [])
                        s.dma("sp", dbg_out["gatek"].rearrange("(i p) k -> p i k", p=128), gatek.t[:], [gatek], [])
                        s.dma("sp", dbg_out["ctab"], cef.t[:, 0:NCH], [cef], [])

        def scatter_pass():
            with s.phase():
                ub = [s.sb([128, D], BF16, "ub") for _ in range(3)]
                for i in range(NT):
                    u = ub[i % 3]
                    s.dma("sp", u.t[:], u_d[i * 128:(i + 1) * 128, :], [b_u[i]], [u])
                    for k in range(4):
                        s.idma(xs_d, bass.IndirectOffsetOnAxis(ap=posk_i.t[:, i, k:k + 1], axis=0), u.t[:], None,
                               [u, posk_i], [b_xs_all], group="sc")

        def expert_pass(l):
            with s.phase():
                wgu = [s.sb([128, 8, 2 * D], BF16, "wgu") for _ in range(3)]
                wdn = [s.sb([128, 8, D], BF16, "wdn") for _ in range(2)]
                bgu = [s.sb([128, 16], F32, "bgu") for _ in range(3)]
                bdn = [s.sb([128, D], BF16, "bdn") for _ in range(2)]
                SBK = CH // 128
                xsb = [[s.sb([128, D], BF16, "xsb") for _ in range(SBK)] for _ in range(2)]
                xT = [s.sb([128, 8, CH], BF16, "xT") for _ in range(2)]
                ptr = [s.ps([128, 8, 128], BF16, "ptr") for _ in range(2)]
                pg = [s.ps([128, 512], F32, "pg") for _ in range(4)]
                pd = [s.ps([128, 512], F32, "pd") for _ in range(2)]
                gc = [s.sb([128, CH], F32, "gc") for _ in range(2)]
                sg = [s.sb([128, CH], F32, "sg") for _ in range(2)]
                uc = [s.sb([128, CH], F32, "uc") for _ in range(2)]
                actT = [s.sb([128, 8, CH], BF16, "actT") for _ in range(2)]
                ysb = [s.sb([128, D], F32, "ysb") for _ in range(2)]
                gpf = load_mod(l, 5, True, "gpf", 1.0 / ALPHA)
                wguv = w_gu.rearrange("l e r f -> (l e r) f")
                wdnv = w_dn.rearrange("l e r f -> (l e r) f")
                bguv = b_guT.rearrange("l e p c -> (l e p) c")
                bdnv = b_dn.rearrange("l e f -> (l e) f")
                IO = bass.IndirectOffsetOnAxis

                def pre_g(c):
                    wg = wgu[c % 3]; bg = bgu[c % 3]
                    for k in range(8):
                        s.idma(wg.t[:, k, :], None, wguv, IO(ap=widx.t[:, c, k:k + 1], axis=0), [widx], [wg], group=("wg", c))
                    s.idma(bg.t[:], None, bguv, IO(ap=bgidx.t[:, c:c + 1], axis=0), [bgidx], [bg])
                    s.op("dve", lambda e: e.tensor_scalar(bg.t[:, 8:16], bg.t[:, 8:16], 1.0, None, ALU.add), [bg], [bg])

                def pre_d(c):
                    wd = wdn[c % 2]; bd = bdn[c % 2]
                    for k in range(8):
                        s.idma(wd.t[:, k, :], None, wdnv, IO(ap=widx.t[:, c, k:k + 1], axis=0), [widx], [wd], group=("wd", c))
                    s.idma(bd.t[:], None, bdnv, IO(ap=bdidx.t[:, c:c + 1], axis=0), [bdidx], [bd])
                    for sb_ in range(SBK):
                        xb = xsb[c % 2][sb_]
                        r0 = c * CH + sb_ * 128
                        s.dma("sp", xb.t[:], xs_d[r0:r0 + 128, :], [b_xs_all], [xb])

                def tr(c):
                    xt_ = xT[c % 2]
                    for sb_ in range(SBK):
                        xb = xsb[c % 2][sb_]; pt = ptr[sb_ % 2]
                        for k in range(8):
                            s.op("pe", lambda e, k=k: e.transpose(pt.t[:, k, :], xb.t[:, k * 128:(k + 1) * 128], ident_b.t[:]), [xb, ident_b], [pt])
                        s.op("act", lambda e: e.copy(out=xt_.t[:, :, sb_ * 128:(sb_ + 1) * 128], in_=pt.t[:]), [pt], [xt_])

                def gu(c):
                    wg = wgu[c % 3]; bg = bgu[c % 3]
                    xt_ = xT[c % 2]; at = actT[c % 2]
                    for j in range(8):
                        p_g = pg[(2 * j) % 4]; p_u = pg[(2 * j + 1) % 4]
                        for k in range(8):
                            s.mm(p_g.t[:, 0:CH], wg.t[:, k, j * 128:(j + 1) * 128], xt_.t[:, k, :], k == 0, k == 7, [wg, xt_], [p_g])
                        for k in range(8):
                            s.mm(p_u.t[:, 0:CH], wg.t[:, k, D + j * 128:D + (j + 1) * 128], xt_.t[:, k, :], k == 0, k == 7, [wg, xt_], [p_u])
                        g_ = gc[j % 2]; s_ = sg[j % 2]; u_ = uc[j % 2]
                        s.op("dve", lambda e: e.tensor_scalar(g_.t[:], p_g.t[:, 0:CH], bg.t[:, j:j + 1], 7.0, ALU.add, ALU.min), [p_g, bg], [g_])
                        s.op("act", lambda e: e.activation(out=s_.t[:], in_=g_.t[:], func=AF.Sigmoid, scale=1.702), [g_], [s_])
                        s.op("dve", lambda e: e.tensor_scalar(u_.t[:], p_u.t[:, 0:CH], bg.t[:, 8 + j:9 + j], 8.0, ALU.add, ALU.min), [p_u, bg], [u_])
                        s.op("dve", lambda e: e.tensor_tensor(g_.t[:], g_.t[:], s_.t[:], ALU.mult), [g_, s_], [g_])
                        s.op("dve", lambda e: e.scalar_tensor_tensor(at.t[:, j, :], u_.t[:], -6.0, g_.t[:], ALU.max, ALU.mult), [g_, u_], [at])

                def dn(c):
                    wd = wdn[c % 2]; bd = bdn[c % 2]; at = actT[c % 2]
                    for sb_ in range(SBK):
                        yb = ysb[sb_ % 2]
                        for nh in range(2):
                            p = pd[nh]
                            for j in range(8):
                                s.mm(p.t[:], at.t[:, j, sb_ * 128:(sb_ + 1) * 128], wd.t[:, j, nh * 512:(nh + 1) * 512], j == 0, False, [at, wd], [p])
                            s.mm(p.t[:], ones_b.t[0:1, :], bd.t[0:1, nh * 512:(nh + 1) * 512], False, True, [ones_b, bd], [p])
                            s.op("dve", lambda e: e.tensor_tensor(yb.t[:, nh * 512:(nh + 1) * 512], p.t[:], gpf.t[:, nh * 512:(nh + 1) * 512], ALU.mult), [p, gpf], [yb])
                        r0 = c * CH + sb_ * 128
                        s.dma("sp", ys_d[r0:r0 + 128, :], yb.t[:], [yb], [b_ys_all], group=("ys", l))

                pre_g(0)
                pre_d(0)
                if NCH > 1:
                    pre_g(1)
                tr(0)
                for c in range(NCH):
                    if c + 2 < NCH:
                        pre_g(c + 2)
                    if c + 1 < NCH:
                        pre_d(c + 1)
                    gu(c)
                    if c + 1 < NCH:
                        tr(c + 1)
                    dn(c)

        eps_t = s.sb([128, 1], F32, "eps")
        s.op("pool", lambda e: e.memset(eps_t.t[:], LN_EPS), [], [eps_t])

        prologue()
        premod(0)
        x_src = x_in
        stop_after = (dbg or {}).get("_stop", None)
        for l in range(layers):
            if l % 2 == 0:
                even_mixer(l)
            else:
                odd_mixer(l)
            norm_pass(l, "mix", x_src, False)
            x_src = xres
            scatter_pass()
            expert_pass(l)
            norm_pass(l, "ffn", x_src, l == layers - 1)
        s.barrier()
    return nc


def prep_inputs(inputs, S=4096, ncores=8, layers=DEPTH):
    f = lambda a: np.ascontiguousarray(np.asarray(a))
    shared = {}
    for k in ("ada_w", "ada_b", "ln_mix_g", "ln_mix_b", "ln_ffn_g", "ln_ffn_b", "ev_w_in", "ev_sg_b", "ev_vn_g", "ev_vn_b",
              "ev_w_out", "od_w_in", "od_w_uq", "od_w_ukv", "od_w_out", "moe_router_w", "moe_router_b", "moe_w_gu",
              "moe_w_dn", "moe_b_dn"):
        shared[k] = f(inputs[k][:layers]) if k in ("moe_w_gu", "moe_w_dn", "ada_w") else f(inputs[k])
    shared["ev_conv"] = f(np.asarray(inputs["ev_conv_w"]).transpose(0, 2, 1).reshape(2, 4, 128, 3).transpose(0, 2, 1, 3))
    shared["ev_sgT"] = f(np.asarray(inputs["ev_sg_w"]).transpose(0, 1, 3, 2))
    shared["od_dw"] = f(np.asarray(inputs["od_dw_w"]).transpose(0, 2, 1).reshape(2, 4, 128, 31).transpose(0, 2, 1, 3))
    col = lambda a, n: f(np.asarray(a).reshape(2, n, 128).transpose(0, 2, 1))
    shared["od_dw_b"] = col(inputs["od_dw_b"], 4)
    shared["od_cn_g"] = col(inputs["od_cn_g"], 4)
    shared["od_cn_b"] = col(inputs["od_cn_b"], 4)
    shared["od_qn_g"] = col(inputs["od_qn_g"], 2)
    shared["od_kvn_g"] = col(inputs["od_kvn_g"], 1)
    shared["moe_b_guT"] = f(np.asarray(inputs["moe_b_gu"]).reshape(DEPTH, NE, 16, 128).transpose(0, 1, 3, 2))
    invf = (10000.0 ** (-np.arange(0, 32, 2, dtype=np.float32) / 32)).astype(np.float32)
    shared["invf"] = f(np.concatenate([invf, invf]).reshape(32, 1))
    maps = []
    x = np.asarray(inputs["x"]); c = np.asarray(inputs["c"]); pos = np.asarray(inputs["positions"])
    for b in range(ncores):
        m = dict(shared)
        m["x"] = f(x[b, :S])
        m["c"] = f(c[b].reshape(8, 128).T)
        m["pos"] = f(np.broadcast_to(pos[b, :S].astype(np.int32)[None, :], (32, S)))
        maps.append(m)
    return maps


_NC_CACHE = {}


def kernel(**inputs):
    S = 4096
    if "nc" not in _NC_CACHE:
        _NC_CACHE["nc"] = build(S)
    nc = _NC_CACHE["nc"]
    maps = prep_inputs(inputs, S, 8)
    res = run_bass_kernel_spmd(nc, maps, core_ids=list(range(8)))
    return np.stack([np.asarray(r["out"]) for r in res.results], axis=0).astype(np.float32)
```

```python
import math
import numpy as np
from contextlib import ExitStack
import concourse.bass as bass
import concourse.mybir as mybir
from concourse.bass_utils import run_bass_kernel_spmd

F32 = mybir.dt.float32
BF16 = mybir.dt.bfloat16
I32 = mybir.dt.int32
AF = mybir.ActivationFunctionType
ALU = mybir.AluOpType
AX = mybir.AxisListType

D = 1024
DEPTH = 4
NE = 32
TOPK = 4
CH = 384
ALPHA = (2.0 * DEPTH) ** 0.25
LN_EPS = 1e-5
RMS_EPS = 1e-6
TWO_PI_HI = 6.28125
TWO_PI_LO = 2.0 * math.pi - 6.28125


class Buf:
    __slots__ = ("w", "r", "g")

    def __init__(self):
        self.w = []
        self.r = []
        self.g = None


class T:
    __slots__ = ("t", "b")

    def __init__(self, t):
        self.t = t
        self.b = Buf()


class Sched:
    ENG = ("pe", "act", "dve", "pool", "sp")

    def __init__(self, nc, es, ndma=12):
        self.nc = nc
        self.es = es
        self.eng = {"pe": nc.tensor, "act": nc.scalar, "dve": nc.vector, "pool": nc.gpsimd, "sp": nc.sync}
        self.sem = {}
        self.cnt = {}
        self.seen = {e: {} for e in self.ENG}
        for e in self.ENG:
            self.sem[e] = es.enter_context(nc.semaphore("s_" + e))
            self.cnt[e] = 0
        self.dq = {}
        for q in ("sp", "pool", "act"):
            lst = []
            for i in range(ndma if q != "act" else 8):
                key = "d_%s%d" % (q, i)
                self.sem[key] = es.enter_context(nc.semaphore(key))
                self.cnt[key] = 0
                lst.append(key)
            self.dq[q] = [lst, 0]
        self.uid = 0

    def sb(self, shape, dt, name=None):
        self.uid += 1
        return T(self.es_cur.enter_context(self.nc.sbuf_tensor("%s_%d" % (name or "t", self.uid), list(shape), dt)))

    def ps(self, shape, dt, name=None):
        self.uid += 1
        return T(self.es_cur.enter_context(self.nc.psum_tensor("%s_%d" % (name or "p", self.uid), list(shape), dt)))

    def _wait(self, e, evs):
        seen = self.seen[e]
        for key, val in evs:
            if key == "pe" and e == "pe":
                continue
            if seen.get(key, 0) >= val:
                continue
            self.eng[e].wait_ge(self.sem[key], val)
            seen[key] = val

    @staticmethod
    def _deps(reads, writes, group=None):
        evs = []
        for b in reads:
            b = b.b if isinstance(b, T) else b
            evs.extend(b.w)
        for b in writes:
            b = b.b if isinstance(b, T) else b
            if group is None or b.g != group:
                evs.extend(b.w)
            evs.extend(b.r)
        return evs

    @staticmethod
    def _update(ev, reads, writes, group=None):
        for b in reads:
            b = b.b if isinstance(b, T) else b
            b.r.append(ev)
            if len(b.r) > 24:
                last = {}
                for k, v in b.r:
                    if last.get(k, 0) < v:
                        last[k] = v
                b.r = list(last.items())
        for b in writes:
            b = b.b if isinstance(b, T) else b
            if group is not None and b.g == group:
                b.w.append(ev)
            else:
                b.w = [ev]
                b.g = group
            b.r = []

    def op(self, e, fn, reads=(), writes=()):
        self._wait(e, self._deps(reads, writes))
        ins = fn(self.eng[e])
        self.cnt[e] += 1
        ins.then_inc(self.sem[e], 1)
        self.seen[e][e] = max(self.seen[e].get(e, 0), 0)
        self._update((e, self.cnt[e]), reads, writes)

    def mm(self, out, lhsT, rhs, start, stop, reads, writes, **kw):
        self.op("pe", lambda pe: pe.matmul(out, lhsT=lhsT, rhs=rhs, start=start, stop=stop, **kw), reads, writes)

    def dma(self, q, out, in_, reads=(), writes=(), group=None, **kw):
        lst, idx = self.dq[q]
        key = lst[idx % len(lst)]
        self.dq[q][1] = idx + 1
        evs = self._deps(reads, writes, group)
        if self.cnt[key] > 0:
            evs.append((key, self.cnt[key]))
        self._wait(q, evs)
        ins = self.eng[q].dma_start(out=out, in_=in_, **kw)
        self.cnt[key] += 16
        ins.then_inc(self.sem[key], 16)
        self._update((key, self.cnt[key]), reads, writes, group)

    def idma(self, out, out_off, in_, in_off, reads=(), writes=(), group=None):
        q = "pool"
        lst, idx = self.dq[q]
        key = lst[idx % len(lst)]
        self.dq[q][1] = idx + 1
        evs = self._deps(reads, writes, group)
        if self.cnt[key] > 0:
            evs.append((key, self.cnt[key]))
        self._wait(q, evs)
        ins = self.eng[q].indirect_dma_start(out=out, out_offset=out_off, in_=in_, in_offset=in_off)
        self.cnt[key] += 16
        ins.then_inc(self.sem[key], 16)
        self._update((key, self.cnt[key]), reads, writes, group)

    def barrier(self):
        evs = [(k, v) for k, v in self.cnt.items() if v > 0]
        for e in self.ENG:
            self._wait(e, evs)

    def phase(self):
        return _Phase(self)


class _Phase:
    def __init__(self, s):
        self.s = s

    def __enter__(self):
        self.es = ExitStack()
        self.es.__enter__()
        self.s.es_cur = self.es
        return self.s

    def __exit__(self, *a):
        self.s.barrier()
        self.es.__exit__(*a)
        return False


def bcast(ap, n=128):
    return ap.partition_broadcast(n)


def build(S=4096, layers=DEPTH, dbg=None):
    NT = S // 128
    NG = S // 512
    NCH = (S * TOPK) // CH + NE
    NSLOT = NCH * CH
    nc = bass.Bass("TRN2", target_bir_lowering=False)

    def din(name, shape, dt=F32):
        return nc.dram_tensor(name, list(shape), dt, kind="ExternalInput").ap()

    def dscr(name, shape, dt=F32):
        return nc.dram_tensor(name, list(shape), dt, kind="Internal").ap()

    n_even = (layers + 1) // 2
    n_odd = layers // 2
    x_in = din("x", [S, D])
    c_in = din("c", [128, 8])
    pos_in = din("pos", [32, S], I32)
    invf_in = din("invf", [32, 1])
    ada_w = din("ada_w", [layers, D, 6 * D])
    ada_b = din("ada_b", [DEPTH, 6 * D])
    ln_mix_g = din("ln_mix_g", [DEPTH, D]); ln_mix_b = din("ln_mix_b", [DEPTH, D])
    ln_ffn_g = din("ln_ffn_g", [DEPTH, D]); ln_ffn_b = din("ln_ffn_b", [DEPTH, D])
    ev_w_in = din("ev_w_in", [2, D, 2560])
    ev_conv = din("ev_conv", [2, 128, 4, 3])
    ev_sgT = din("ev_sgT", [2, 8, 128, 128])
    ev_sg_b = din("ev_sg_b", [2, 8, 128])
    ev_vn_g = din("ev_vn_g", [2, 512]); ev_vn_b = din("ev_vn_b", [2, 512])
    ev_w_out = din("ev_w_out", [2, D, D])
    od_w_in = din("od_w_in", [2, D, 1440])
    od_dw = din("od_dw", [2, 128, 4, 31])
    od_dw_b = din("od_dw_b", [2, 128, 4])
    od_cn_g = din("od_cn_g", [2, 128, 4]); od_cn_b = din("od_cn_b", [2, 128, 4])
    od_qn_g = din("od_qn_g", [2, 128, 2])
    od_w_uq = din("od_w_uq", [2, 256, 768])
    od_kvn_g = din("od_kvn_g", [2, 128, 1])
    od_w_ukv = din("od_w_ukv", [2, 128, 1024])
    od_w_out = din("od_w_out", [2, D, D])
    r_w = din("moe_router_w", [DEPTH, D, NE])
    r_b = din("moe_router_b", [DEPTH, NE])
    w_gu = din("moe_w_gu", [layers, NE, D, 2 * D])
    b_guT = din("moe_b_guT", [DEPTH, NE, 128, 16])
    w_dn = din("moe_w_dn", [layers, NE, D, D])
    b_dn = din("moe_b_dn", [DEPTH, NE, D])
    out = nc.dram_tensor("out", [S, D], F32, kind="ExternalOutput").ap()

    xres = dscr("xres", [S, D])
    delta = dscr("delta", [S, D])
    uT_d = dscr("uT_d", [8, 128, S], BF16)
    u_d = dscr("u_d", [S, D], BF16)
    xs_d = dscr("xs_d", [NSLOT, D], BF16)
    ys_d = dscr("ys_d", [NSLOT, D])
    mod_d = dscr("mod_d", [DEPTH, 6 * D])
    rope_d = dscr("rope_d", [2, 32, S])
    od_scr = {}
    if layers > 1:
        od_scr = {"qT": dscr("qT_d", [8, 96, S], BF16), "kT": dscr("kT_d", [8, 96, S], BF16), "v": dscr("v_d", [S, 8, 65], BF16),
                  "yc": dscr("yc_d", [4, 128, S], BF16), "yd": dscr("yd_d", [S, 512], BF16)}
    dbg_out = {}
    if dbg:
        for name, shape in dbg.items():
            dbg_out[name] = nc.dram_tensor("dbg_" + name, list(shape), F32, kind="ExternalOutput").ap()

    tokb = lambda: [Buf() for _ in range(NT)]
    b_xres = tokb(); b_delta = tokb(); b_uT = tokb(); b_u = tokb()
    b_xs = [Buf() for _ in range(NCH)]; b_ys = [Buf() for _ in range(NCH)]
    b_xs_all = Buf(); b_ys_all = Buf()
    b_mod = Buf(); b_rope = Buf()

    with ExitStack() as es_top:
        s = Sched(nc, es_top)
        s.es_cur = es_top
        ident_b = s.sb([128, 128], BF16, "identb")
        ident_f = s.sb([128, 128], F32, "identf")
        ones_f = s.sb([128, 128], F32, "onesf")
        ones_b = s.sb([128, 128], BF16, "onesb")
        su_f = s.sb([128, 128], F32, "suf")
        iota_p = s.sb([128, 1], F32, "iotap")
        posk_f = s.sb([128, NT, 4], F32, "poskf")
        posk_i = s.sb([128, NT, 4], I32, "poski")
        gatek = s.sb([128, NT, 4], F32, "gatek")
        widx = s.sb([128, NCH, 8], I32, "widx")
        bgidx = s.sb([128, NCH], I32, "bgidx")
        bdidx = s.sb([128, NCH], I32, "bdidx")
        base_pk = s.sb([128, 8], F32, "basepk")
        iota_c = s.sb([128, NCH], F32, "iotac")

        def mk_consts():
            s.op("pool", lambda e: e.memset(ident_b.t[:], 0.0), [], [ident_b])
            s.op("pool", lambda e: e.affine_select(out=ident_b.t[:], in_=ident_b.t[:], pattern=[[-1, 128]],
                                                   compare_op=ALU.not_equal, fill=1.0, base=0, channel_multiplier=1),
                 [ident_b], [ident_b])
            s.op("pool", lambda e: e.memset(ident_f.t[:], 0.0), [], [ident_f])
            s.op("pool", lambda e: e.affine_select(out=ident_f.t[:], in_=ident_f.t[:], pattern=[[-1, 128]],
                                                   compare_op=ALU.not_equal, fill=1.0, base=0, channel_multiplier=1),
                 [ident_f], [ident_f])
            s.op("pool", lambda e: e.memset(ones_f.t[:], 1.0), [], [ones_f])
            s.op("pool", lambda e: e.memset(ones_b.t[:], 1.0), [], [ones_b])
            s.op("pool", lambda e: e.memset(su_f.t[:], 1.0), [], [su_f])
            s.op("pool", lambda e: e.affine_select(out=su_f.t[:], in_=su_f.t[:], pattern=[[1, 128]],
                                                   compare_op=ALU.is_gt, fill=0.0, base=0, channel_multiplier=-1),
                 [su_f], [su_f])
            s.op("pool", lambda e: e.iota(iota_p.t[:], pattern=[[0, 1]], base=0, channel_multiplier=1,
                                          allow_small_or_imprecise_dtypes=True), [], [iota_p])
            s.op("pool", lambda e: e.iota(base_pk.t[:], pattern=[[128, 8]], base=0, channel_multiplier=1,
                                          allow_small_or_imprecise_dtypes=True), [], [base_pk])
            s.op("pool", lambda e: e.iota(iota_c.t[:], pattern=[[1, NCH]], base=0, channel_multiplier=0,
                                          allow_small_or_imprecise_dtypes=True), [], [iota_c])

        mk_consts()

        def prologue():
            with s.phase():
                ct = s.sb([128, 8], F32, "ct")
                cond = s.sb([128, 8], F32, "cond")
                condb = s.sb([128, 8, 128], F32, "condb")
                s.dma("sp", ct.t[:], c_in, [], [ct])
                s.op("act", lambda e: e.activation(out=cond.t[:], in_=ct.t[:], func=AF.Silu), [ct], [cond])
                for k in range(8):
                    s.op("dve", lambda e, k=k: e.tensor_scalar(condb.t[:, k, :], ones_f.t[:], cond.t[:, k:k + 1], None,
                                                               ALU.mult), [ones_f, cond], [condb])
                wst = [s.sb([128, 3072], F32, "adaw") for _ in range(3)]
                pacc = [s.ps([128, 512], F32, "pada") for _ in range(6)]
                modt = s.sb([1, 6 * D], F32, "modt")
                adab = s.sb([1, 6 * D], F32, "adab")
                it = 0
                for l in range(layers):
                    s.dma("sp", adab.t[:], ada_b[l:l + 1, :], [], [adab])
                    for half in range(2):
                        for k in range(8):
                            w = wst[it % 3]; it += 1
                            s.dma("sp", w.t[:], ada_w[l, k * 128:(k + 1) * 128, half * 3072:(half + 1) * 3072], [], [w])
                            for n in range(6):
                                s.mm(pacc[n].t[:], condb.t[:, k, :], w.t[:, n * 512:(n + 1) * 512], k == 0, k == 7,
                                     [condb, w], [pacc[n]])
                        for n in range(6):
                            c0 = half * 3072 + n * 512
                            s.op("dve", lambda e, n=n, c0=c0: e.tensor_tensor(modt.t[0:1, c0:c0 + 512], pacc[n].t[0:1, :],
                                                                              adab.t[0:1, c0:c0 + 512], ALU.add),
                                 [pacc[n], adab], [modt])
                    s.dma("sp", mod_d[l:l + 1, :], modt.t[:], [modt], [b_mod])
            if n_odd > 0:
              with s.phase():
                    pi_ = s.sb([32, S], I32, "posi")
                    ang = s.sb([32, S], F32, "ang")
                    q = s.sb([32, S], F32, "q")
                    qi = s.sb([32, S], I32, "qi")
                    r = s.sb([32, S], F32, "r")
                    m = s.sb([32, S], F32, "m")
                    invf = s.sb([32, 1], F32, "invf")
                    s.dma("sp", pi_.t[:], pos_in, [], [pi_])
                    s.dma("sp", invf.t[:], invf_in, [], [invf])
                    s.op("dve", lambda e: e.tensor_copy(out=ang.t[:], in_=pi_.t[:]), [pi_], [ang])
                    s.op("dve", lambda e: e.tensor_scalar(ang.t[:], ang.t[:], invf.t[:, 0:1], None, ALU.mult), [ang, invf], [ang])
                    s.op("dve", lambda e: e.tensor_scalar(q.t[:], ang.t[:], 1.0 / (2 * math.pi), None, ALU.mult), [ang], [q])
                    s.op("dve", lambda e: e.tensor_copy(out=qi.t[:], in_=q.t[:]), [q], [qi])
                    s.op("dve", lambda e: e.tensor_copy(out=q.t[:], in_=qi.t[:]), [qi], [q])
                    s.op("dve", lambda e: e.scalar_tensor_tensor(r.t[:], q.t[:], -TWO_PI_HI, ang.t[:], ALU.mult, ALU.add), [q, ang], [r])
                    s.op("dve", lambda e: e.scalar_tensor_tensor(r.t[:], q.t[:], -TWO_PI_LO, r.t[:], ALU.mult, ALU.add), [q, r], [r])

                    def wrap(t):
                        s.op("dve", lambda e: e.tensor_scalar(m.t[:], t.t[:], math.pi, -2 * math.pi, ALU.is_gt, ALU.mult), [t], [m])
                        s.op("dve", lambda e: e.tensor_tensor(t.t[:], t.t[:], m.t[:], ALU.add), [t, m], [t])
                        s.op("dve", lambda e: e.tensor_scalar(m.t[:], t.t[:], -math.pi, 2 * math.pi, ALU.is_lt, ALU.mult), [t], [m])
                        s.op("dve", lambda e: e.tensor_tensor(t.t[:], t.t[:], m.t[:], ALU.add), [t, m], [t])
                        s.op("dve", lambda e: e.tensor_scalar(t.t[:], t.t[:], math.pi, -math.pi, ALU.min, ALU.max), [t], [t])

                    wrap(r)
                    sn = s.sb([32, S], F32, "sn")
                    s.op("act", lambda e: e.activation(out=sn.t[:], in_=r.t[:], func=AF.Sin), [r], [sn])
                    s.dma("sp", rope_d[1], sn.t[:], [sn], [b_rope])
                    s.op("dve", lambda e: e.tensor_scalar(r.t[:], r.t[:], math.pi / 2, None, ALU.add), [r], [r])
                    wrap(r)
                    cs = s.sb([32, S], F32, "cs")
                    s.op("act", lambda e: e.activation(out=cs.t[:], in_=r.t[:], func=AF.Sin), [r], [cs])
                    s.dma("sp", rope_d[0], cs.t[:], [cs], [b_rope])

        def load_mod(l, j, plus1, name, scale=None):
            t = s.sb([128, D], F32, name)
            s.dma("sp", t.t[:], bcast(mod_d[l:l + 1, j * D:(j + 1) * D]), [b_mod], [t])
            if plus1 and scale is not None:
                s.op("pool", lambda e: e.tensor_scalar(t.t[:], t.t[:], 1.0, float(scale), ALU.add, ALU.mult), [t], [t])
            elif plus1:
                s.op("pool", lambda e: e.tensor_scalar(t.t[:], t.t[:], 1.0, None, ALU.add), [t], [t])
            return t

        def load_row(src_row, n, name):
            t = s.sb([128, n], F32, name)
            s.dma("sp", t.t[:], bcast(src_row), [], [t])
            return t

        def transposes_store_uT(ub, i, ptr, uTs):
            for k in range(8):
                s.op("pe", lambda e, k=k: e.transpose(ptr.t[:, k, :], ub.t[:, k * 128:(k + 1) * 128], ident_b.t[:]),
                     [ub, ident_b], [ptr])
            s.op("act", lambda e: e.copy(out=uTs.t[:], in_=ptr.t[:]), [ptr], [uTs])
            s.dma("act", uT_d[:, :, i * 128:(i + 1) * 128].rearrange("k p t -> p k t"), uTs.t[:], [uTs], [b_uT[i]])

        def premod(l):
            with s.phase():
                sc1 = load_mod(l, 1, True, "sc1")
                sh = load_mod(l, 0, False, "sh")
                xt = [s.sb([128, D], F32, "xt") for _ in range(2)]
                ub = [s.sb([128, D], BF16, "ub") for _ in range(2)]
                ptr = [s.ps([128, 8, 128], BF16, "ptr") for _ in range(2)]
                uTs = [s.sb([128, 8, 128], BF16, "uTs") for _ in range(2)]
                for i in range(NT):
                    x = xt[i % 2]; u = ub[i % 2]
                    s.dma("sp", x.t[:], x_in[i * 128:(i + 1) * 128, :], [], [x])
                    s.op("dve", lambda e: e.tensor_tensor(x.t[:], x.t[:], sc1.t[:], ALU.mult), [x, sc1], [x])
                    s.op("dve", lambda e: e.tensor_tensor(u.t[:], x.t[:], sh.t[:], ALU.add), [x, sh], [u])
                    transposes_store_uT(u, i, ptr[i % 2], uTs[i % 2])

        def even_mixer(l):
            li = l // 2
            with s.phase():
                win = s.sb([128, 8, 2560], BF16, "win")
                wout = s.sb([128, 8, D], BF16, "wout")
                wv = ev_w_in[li].rearrange("(k p) f -> p k f", p=128)
                gpw = load_mod(l, 2, True, "gpw", 1.0 / ALPHA)
                wstg = [s.sb([128, D], F32, "wstg") for _ in range(2)]
                for k in range(8):
                    s.dma("pool", win.t[:, k, 0:1280], wv[:, k, 0:1280], [], [win], group="w")
                    s.dma("pool", win.t[:, k, 1280:2560], wv[:, k, 1280:2560], [], [win], group="w")
                for k in range(8):
                    s.dma("sp", wstg[k % 2].t[:], ev_w_out[li, k * 128:(k + 1) * 128, :], [], [wstg[k % 2]])
                    s.op("dve", lambda e, k=k: e.tensor_tensor(wout.t[:, k, :], wstg[k % 2].t[:], gpw.t[:], ALU.mult), [wstg[k % 2], gpw], [wout])
                sgf = s.sb([128, 8, 128], F32, "sgf")
                sgm = s.sb([128, 8, 128], BF16, "sgm")
                s.dma("sp", sgf.t[:], ev_sgT[li].rearrange("h j i -> j h i"), [], [sgf])
                for h in range(8):
                    s.op("pool", lambda e, h=h: e.affine_select(out=sgf.t[:, h, :], in_=sgf.t[:, h, :], pattern=[[1, 128]],
                                                                compare_op=ALU.is_ge, fill=0.0, base=0, channel_multiplier=-1),
                         [sgf], [sgf])
                s.op("pool", lambda e: e.tensor_copy(out=sgm.t[:], in_=sgf.t[:]), [sgf], [sgm])
                sgb = s.sb([128, 4, 128], F32, "sgb")
                for h in range(8):
                    s.dma("sp", sgb.t[(h % 2) * 64:(h % 2) * 64 + 64, h // 2, :], bcast(ev_sg_b[li, h:h + 1, :], 64), [], [sgb], group="w")
                cw = s.sb([128, 4, 3], F32, "cw")
                s.dma("sp", cw.t[:], ev_conv[li], [], [cw])
                vng = load_row(ev_vn_g[li:li + 1, :], 512, "vng")
                vnb = load_row(ev_vn_b[li:li + 1, :], 512, "vnb")
                halo = s.sb([128, 4, 2], F32, "halo")
                s.op("pool", lambda e: e.memset(halo.t[:], 0.0), [], [halo])

                uTg = [s.sb([128, 8, 512], BF16, "uTg") for _ in range(2)]
                pp = [s.ps([128, 512], F32, "pp") for _ in range(6)]
                psg = [s.ps([128, 128], F32, "psg") for _ in range(2)]
                cg = [s.sb([128, 512], F32, "cg") for _ in range(2)]
                cx = [s.sb([128, 514], F32, "cx") for _ in range(2)]
                acc = [s.sb([128, 512], F32, "acc") for _ in range(2)]
                yT = [s.sb([128, 8, 512], BF16, "yT") for _ in range(2)]
                zuT = [s.sb([128, 4, 512], F32, "zuT") for _ in range(2)]
                zv = [s.sb([128, 512], F32, "zv") for _ in range(2)]
                zvn = [s.sb([128, 512], BF16, "zvn") for _ in range(2)]
                st6 = [s.sb([128, 6], F32, "st6") for _ in range(2)]
                mv = [s.sb([128, 2], F32, "mv") for _ in range(2)]
                rstd = [s.sb([128, 1], F32, "rstd") for _ in range(2)]
                tmp = [s.sb([128, 128], F32, "tmp") for _ in range(2)]
                dl = [s.sb([128, D], F32, "dl") for _ in range(2)]
                ppi = 0
                for g in range(NG):
                    u = uTg[g % 2]; y = yT[g % 2]; zu = zuT[g % 2]
                    s.dma("sp", u.t[:], uT_d[:, :, g * 512:(g + 1) * 512].rearrange("k p t -> p k t"),
                          [b_uT[4 * g + j] for j in range(4)], [u])

                    def proj(f):
                        nonlocal ppi
                        p = pp[ppi % 6]; ppi += 1
                        for k in range(8):
                            s.mm(p.t[:], win.t[:, k, f * 128:(f + 1) * 128], u.t[:, k, :], k == 0, k == 7, [win, u], [p])
                        return p

                    for cc in range(4):
                        c_ = cg[cc % 2]; x_ = cx[cc % 2]; a_ = acc[cc % 2]
                        p_c = proj(4 + cc)
                        s.op("act", lambda e: e.copy(out=c_.t[:], in_=p_c.t[:]), [p_c], [c_])
                        p_x = proj(8 + cc)
                        s.op("pool", lambda e: e.tensor_copy(out=x_.t[:, 0:2], in_=halo.t[:, cc, :]), [halo], [x_])
                        s.op("dve", lambda e: e.tensor_tensor(x_.t[:, 2:514], p_x.t[:], c_.t[:], ALU.mult), [p_x, c_], [x_])
                        s.op("pool", lambda e: e.tensor_copy(out=halo.t[:, cc, :], in_=x_.t[:, 512:514]), [x_], [halo])
                        s.op("dve", lambda e: e.tensor_scalar(a_.t[:], x_.t[:, 0:512], cw.t[:, cc, 0:1], None, ALU.mult), [x_, cw], [a_])
                        s.op("dve", lambda e: e.scalar_tensor_tensor(a_.t[:], x_.t[:, 1:513], cw.t[:, cc, 1:2], a_.t[:], ALU.mult, ALU.add),
                             [x_, cw, a_], [a_])
                        s.op("dve", lambda e: e.scalar_tensor_tensor(a_.t[:], x_.t[:, 2:514], cw.t[:, cc, 2:3], a_.t[:], ALU.mult, ALU.add),
                             [x_, cw, a_], [a_])
                        p_b = proj(cc)
                        s.op("dve", lambda e: e.tensor_tensor(y.t[:, cc, :], p_b.t[:], a_.t[:], ALU.mult), [p_b, a_], [y])
                        p_u = proj(12 + cc)
                        s.op("act", lambda e: e.activation(out=zu.t[:, cc, :], in_=p_u.t[:], func=AF.Gelu), [p_u], [zu])
                    for tt in range(4):
                        i = 4 * g + tt
                        z = zv[tt % 2]; zn = zvn[tt % 2]; s6 = st6[tt % 2]; m_ = mv[tt % 2]; rs = rstd[tt % 2]
                        p = pp[ppi % 6]; ppi += 1
                        for k in range(8):
                            s.mm(p.t[:], u.t[:, k, tt * 128:(tt + 1) * 128], win.t[:, k, 2048:2560], k == 0, k == 7, [win, u], [p])
                        s.op("act", lambda e: e.activation(out=z.t[:], in_=p.t[:], func=AF.Gelu), [p], [z])
                        s.op("dve", lambda e: e.bn_stats(s6.t[:], z.t[:]), [z], [s6])
                        s.op("dve", lambda e: e.bn_aggr(m_.t[:], s6.t[:]), [s6], [m_])
                        s.op("act", lambda e: e.activation(out=rs.t[:], in_=m_.t[:, 1:2], func=AF.Sqrt, bias=eps_t.t[:, 0:1]), [m_, eps_t], [rs])
                        s.op("dve", lambda e: e.reciprocal(rs.t[:], rs.t[:]), [rs], [rs])
                        s.op("dve", lambda e: e.tensor_scalar(z.t[:], z.t[:], m_.t[:, 0:1], rs.t[:, 0:1], ALU.subtract, ALU.mult), [z, m_, rs], [z])
                        s.op("pool", lambda e: e.tensor_tensor(z.t[:], z.t[:], vng.t[:], ALU.mult), [z, vng], [z])
                        s.op("pool", lambda e: e.tensor_tensor(zn.t[:], z.t[:], vnb.t[:], ALU.add), [z, vnb], [zn])
                        for cc in range(4):
                            for hh in range(2):
                                h = 2 * cc + hh
                                pg = psg[(cc * 2 + hh) % 2]
                                t_ = tmp[(cc * 2 + hh) % 2]
                                s.mm(pg.t[:], zn.t[:, cc * 128:(cc + 1) * 128], sgm.t[:, h, :], True, True, [zn, sgm], [pg])
                                lo, hi = hh * 64, hh * 64 + 64
                                s.op("dve", lambda e: e.tensor_tensor(t_.t[lo:hi, :], pg.t[lo:hi, :], sgb.t[lo:hi, cc, :], ALU.add), [pg, sgb], [t_])
                                s.op("dve", lambda e: e.tensor_tensor(y.t[lo:hi, 4 + cc, tt * 128:(tt + 1) * 128], t_.t[lo:hi, :],
                                                                      zu.t[lo:hi, cc, tt * 128:(tt + 1) * 128], ALU.mult), [t_, zu], [y])
                        d_ = dl[tt % 2]
                        for nh in range(2):
                            p = pp[ppi % 6]; ppi += 1
                            for k in range(8):
                                s.mm(p.t[:], y.t[:, k, tt * 128:(tt + 1) * 128], wout.t[:, k, nh * 512:(nh + 1) * 512], k == 0, k == 7, [y, wout], [p])
                            s.op("act", lambda e: e.copy(out=d_.t[:, nh * 512:(nh + 1) * 512], in_=p.t[:]), [p], [d_])
                        s.dma("act", delta[i * 128:(i + 1) * 128, :], d_.t[:], [d_], [b_delta[i]])

        def odd_mixer(l):
            li = l // 2
            SCALE = 1.0 / math.sqrt(96.0)
            qT_d = od_scr["qT"]; kT_d = od_scr["kT"]; v_d = od_scr["v"]; yc_d = od_scr["yc"]; yd_d = od_scr["yd"]
            b_q = Buf(); b_k = Buf(); b_v = Buf(); b_yc = Buf(); b_yd = Buf()
            with s.phase():
                win = s.sb([128, 8, 1440], BF16, "win")
                wv_ = od_w_in[li].rearrange("(k p) f -> p k f", p=128)
                for k in range(8):
                    s.dma("pool", win.t[:, k, :], wv_[:, k, :], [], [win], group="w")
                winr = s.sb([128, 8, 32], F32, "winr")
                s.dma("sp", winr.t[:], wv_[:, :, 1408:1440], [], [winr])
                win_sw = s.sb([128, 8, 96], BF16, "winsw")
                s.op("pool", lambda e: e.memset(win_sw.t[:], 0.0), [], [win_sw])
                s.op("dve", lambda e: e.tensor_scalar(win_sw.t[:, :, 64:80], winr.t[:, :, 16:32], -1.0, None, ALU.mult), [winr, win_sw], [win_sw])
                s.op("dve", lambda e: e.tensor_copy(out=win_sw.t[:, :, 80:96], in_=winr.t[:, :, 0:16]), [winr, win_sw], [win_sw])
                wuq = s.sb([128, 2, 768], BF16, "wuq")
                wuqf = s.sb([128, 2, 768], F32, "wuqf")
                uqv = od_w_uq[li].rearrange("(k p) f -> p k f", p=128)
                s.dma("pool", wuq.t[:], uqv, [], [wuq])
                s.dma("sp", wuqf.t[:], uqv, [], [wuqf])
                wuq_sw = s.sb([128, 2, 8, 96], BF16, "wuqsw")
                s.op("pool", lambda e: e.memset(wuq_sw.t[:], 0.0), [], [wuq_sw])
                wuqf4 = wuqf.t[:].rearrange("p k (h e) -> p k h e", e=96)
                for kc in range(2):
                    s.op("dve", lambda e, kc=kc: e.tensor_scalar(wuq_sw.t[:, kc, :, 64:80], wuqf4[:, kc, :, 80:96], -1.0, None, ALU.mult), [wuqf, wuq_sw], [wuq_sw])
                    s.op("dve", lambda e, kc=kc: e.tensor_copy(out=wuq_sw.t[:, kc, :, 80:96], in_=wuqf4[:, kc, :, 64:80]), [wuqf, wuq_sw], [wuq_sw])
                wk = s.sb([128, 8, 64], BF16, "wk")
                wvv = s.sb([128, 8, 64], BF16, "wvv")
                ukv = od_w_ukv[li].rearrange("r (h e) -> r h e", e=128)
                s.dma("pool", wk.t[:], ukv[:, :, 0:64], [], [wk])
                s.dma("pool", wvv.t[:], ukv[:, :, 64:128], [], [wvv])
                cwd = s.sb([128, 4, 31], F32, "cwd")
                s.dma("sp", cwd.t[:], od_dw[li], [], [cwd])
                dwb = s.sb([128, 4], F32, "dwb"); cng = s.sb([128, 4], F32, "cng"); cnb = s.sb([128, 4], F32, "cnb")
                qng = s.sb([128, 2], F32, "qng"); kvng = s.sb([128, 1], F32, "kvng")
                s.dma("sp", dwb.t[:], od_dw_b[li], [], [dwb]); s.dma("sp", cng.t[:], od_cn_g[li], [], [cng])
                s.dma("sp", cnb.t[:], od_cn_b[li], [], [cnb]); s.dma("sp", qng.t[:], od_qn_g[li], [], [qng])
                s.dma("sp", kvng.t[:], od_kvn_g[li], [], [kvng])
                diag = s.sb([128, 4, 31, 128], BF16, "diag")
                for cc in range(4):
                    for k in range(31):
                        eng = "dve" if (cc * 31 + k) % 2 == 0 else "pool"
                        s.op(eng, lambda e, cc=cc, k=k: e.tensor_scalar(diag.t[:, cc, k, :], ident_f.t[:], cwd.t[:, cc, k:k + 1], None, ALU.mult),
                             [ident_f, cwd], [diag])
                rms_eps = s.sb([128, 1], F32, "rmseps")
                s.op("pool", lambda e: e.memset(rms_eps.t[:], RMS_EPS), [], [rms_eps])

                uTg = [s.sb([128, 8, 512], BF16, "uTg") for _ in range(2)]
                glu = [s.sb([128, 4, 542], BF16, "glu") for _ in range(2)]
                s.op("pool", lambda e: e.memset(glu[1].t[:, :, 512:542], 0.0), [], [glu[1]])
                sgm = [s.sb([128, 512], F32, "sgm") for _ in range(2)]
                hbuf = s.sb([128, 4, 512], F32, "hbuf")
                hsq = s.sb([128, 4, 512], F32, "hsq")
                mean = s.sb([128, 512], F32, "mean"); var = s.sb([128, 512], F32, "var"); rstd = s.sb([128, 512], F32, "rstd")
                tb = [s.sb([128, 512], F32, "tb") for _ in range(2)]
                ycT = [s.sb([128, 4, 512], BF16, "ycT") for _ in range(2)]
                sq = [s.sb([128, 512], F32, "sq") for _ in range(2)]
                rq = s.sb([128, 512], F32, "rq")
                cqn = s.sb([128, 2, 512], BF16, "cqn")
                ckvn = s.sb([128, 512], BF16, "ckvn")
                cs = s.sb([128, 2, 512], F32, "cs")
                t1 = [s.sb([128, 512], F32, "t1") for _ in range(2)]
                t2 = [s.sb([128, 512], F32, "t2") for _ in range(2)]
                krT = s.sb([128, 512], BF16, "krT")
                qT = [s.sb([128, 8, 512], BF16, "qT") for _ in range(2)]
                kT = [s.sb([128, 8, 512], BF16, "kT") for _ in range(2)]
                vt = [s.sb([128, 8, 65], BF16, "vt") for _ in range(2)]
                for v_ in vt:
                    s.op("pool", lambda e, v_=v_: e.memset(v_.t[:, :, 64:65], 1.0), [], [v_])
                pp = [s.ps([128, 512], F32, "pp") for _ in range(8)]
                ppi = 0
                for g in range(NG):
                    u = uTg[g % 2]; gl = glu[g % 2]; glp = glu[(g + 1) % 2]
                    tsl = slice(g * 512, (g + 1) * 512)
                    s.dma("sp", u.t[:], uT_d[:, :, tsl].rearrange("k p t -> p k t"), [b_uT[4 * g + j] for j in range(4)], [u])
                    s.dma("sp", cs.t[64:96, 0, :], rope_d[0, :, tsl], [b_rope], [cs], group=("cs", g))
                    s.dma("sp", cs.t[64:96, 1, :], rope_d[1, :, tsl], [b_rope], [cs], group=("cs", g))

                    def proj(c0, m, wt=win):
                        nonlocal ppi
                        p = pp[ppi % 8]; ppi += 1
                        for k in range(8):
                            s.mm(p.t[0:m, :], wt.t[:, k, c0:c0 + m], u.t[:, k, :], k == 0, k == 7, [wt, u], [p])
                        return p

                    s.op("pool", lambda e: e.tensor_copy(out=gl.t[:, :, 0:30], in_=glp.t[:, :, 512:542]), [glp], [gl])
                    for cc in range(4):
                        sg_ = sgm[cc % 2]
                        p_b = proj(512 + cc * 128, 128)
                        s.op("act", lambda e: e.activation(out=sg_.t[:], in_=p_b.t[:], func=AF.Sigmoid), [p_b], [sg_])
                        p_a = proj(cc * 128, 128)
                        s.op("dve", lambda e: e.tensor_tensor(gl.t[:, cc, 30:542], p_a.t[:], sg_.t[:], ALU.mult), [p_a, sg_], [gl])
                    for cc in range(4):
                        p = pp[ppi % 8]; ppi += 1
                        for k in range(31):
                            s.mm(p.t[:], diag.t[:, cc, k, :], gl.t[:, cc, k:k + 512], k == 0, k == 30, [diag, gl], [p])
                        s.op("act", lambda e: e.activation(out=hbuf.t[:, cc, :], in_=p.t[:], func=AF.Identity, bias=dwb.t[:, cc:cc + 1]), [p, dwb], [hbuf])
                        s.op("act", lambda e: e.activation(out=hsq.t[:, cc, :], in_=p.t[:], func=AF.Square, bias=dwb.t[:, cc:cc + 1]), [p, dwb], [hsq])
                    p1 = pp[ppi % 8]; ppi += 1
                    p2 = pp[ppi % 8]; ppi += 1
                    for cc in range(4):
                        s.mm(p1.t[:], ones_f.t[:], hbuf.t[:, cc, :], cc == 0, cc == 3, [ones_f, hbuf], [p1])
                    for cc in range(4):
                        s.mm(p2.t[:], ones_f.t[:], hsq.t[:, cc, :], cc == 0, cc == 3, [ones_f, hsq], [p2])
                    s.op("dve", lambda e: e.tensor_scalar(mean.t[:], p1.t[:], 1.0 / 512, None, ALU.mult), [p1], [mean])
                    s.op("pool", lambda e: e.tensor_tensor(var.t[:], mean.t[:], mean.t[:], ALU.mult), [mean], [var])
                    s.op("dve", lambda e: e.scalar_tensor_tensor(var.t[:], p2.t[:], 1.0 / 512, var.t[:], ALU.mult, ALU.subtract), [p2, var], [var])
                    s.op("act", lambda e: e.activation(out=rstd.t[:], in_=var.t[:], func=AF.Sqrt, bias=eps_t.t[:, 0:1]), [var, eps_t], [rstd])
                    s.op("dve", lambda e: e.reciprocal(rstd.t[:], rstd.t[:]), [rstd], [rstd])
                    yc = ycT[g % 2]
                    for cc in range(4):
                        t_ = tb[cc % 2]
                        s.op("dve", lambda e: e.tensor_tensor(t_.t[:], hbuf.t[:, cc, :], mean.t[:], ALU.subtract), [hbuf, mean], [t_])
                        s.op("pool", lambda e: e.tensor_tensor(t_.t[:], t_.t[:], rstd.t[:], ALU.mult), [t_, rstd], [t_])
                        s.op("dve", lambda e: e.tensor_scalar(t_.t[:], t_.t[:], cng.t[:, cc:cc + 1], cnb.t[:, cc:cc + 1], ALU.mult, ALU.add), [t_, cng, cnb], [t_])
                        s.op("act", lambda e: e.activation(out=yc.t[:, cc, :], in_=t_.t[:], func=AF.Silu), [t_], [yc])
                    s.dma("act", yc_d[:, :, tsl].rearrange("c p t -> p c t"), yc.t[:], [yc], [b_yc], group="st")
                    pq = [proj(1024, 128), proj(1152, 128)]
                    for c2 in range(2):
                        s.op("act", lambda e, c2=c2: e.activation(out=sq[c2].t[:], in_=pq[c2].t[:], func=AF.Square), [pq[c2]], [sq[c2]])
                    ps_ = pp[ppi % 8]; ppi += 1
                    for c2 in range(2):
                        s.mm(ps_.t[:], ones_f.t[:], sq[c2].t[:], c2 == 0, c2 == 1, [ones_f, sq[c2]], [ps_])
                    s.op("act", lambda e: e.activation(out=rq.t[:], in_=ps_.t[:], func=AF.Sqrt, bias=rms_eps.t[:, 0:1], scale=1.0 / 256), [ps_, rms_eps], [rq])
                    s.op("dve", lambda e: e.reciprocal(rq.t[:], rq.t[:]), [rq], [rq])
                    for c2 in range(2):
                        s.op("dve", lambda e, c2=c2: e.scalar_tensor_tensor(cqn.t[:, c2, :], pq[c2].t[:], qng.t[:, c2:c2 + 1], rq.t[:], ALU.mult, ALU.mult),
                             [pq[c2], qng, rq], [cqn])
                    pkv = proj(1280, 128)
                    s.op("act", lambda e: e.activation(out=sq[0].t[:], in_=pkv.t[:], func=AF.Square), [pkv], [sq[0]])
                    ps_ = pp[ppi % 8]; ppi += 1
                    s.mm(ps_.t[:], ones_f.t[:], sq[0].t[:], True, True, [ones_f, sq[0]], [ps_])
                    s.op("act", lambda e: e.activation(out=rq.t[:], in_=ps_.t[:], func=AF.Sqrt, bias=rms_eps.t[:, 0:1], scale=1.0 / 128), [ps_, rms_eps], [rq])
                    s.op("dve", lambda e: e.reciprocal(rq.t[:], rq.t[:]), [rq], [rq])
                    s.op("dve", lambda e: e.scalar_tensor_tensor(ckvn.t[:], pkv.t[:], kvng.t[:, 0:1], rq.t[:], ALU.mult, ALU.mult), [pkv, kvng, rq], [ckvn])
                    pk1 = proj(1344, 96)
                    pk2 = proj(0, 96, win_sw)
                    R = slice(64, 96)
                    s.op("dve", lambda e: e.tensor_tensor(t1[0].t[R, :], pk1.t[R, :], cs.t[R, 0, :], ALU.mult), [pk1, cs], [t1[0]])
                    s.op("dve", lambda e: e.tensor_tensor(t2[0].t[R, :], pk2.t[R, :], cs.t[R, 1, :], ALU.mult), [pk2, cs], [t2[0]])
                    s.op("pool", lambda e: e.tensor_tensor(krT.t[R, :], t1[0].t[R, :], t2[0].t[R, :], ALU.add), [t1[0], t2[0]], [krT])
                    q_ = qT[g % 2]; k_ = kT[g % 2]
                    for h in range(8):
                        a1 = t1[h % 2]; a2 = t2[h % 2]
                        pq1 = pp[ppi % 8]; ppi += 1
                        pq2 = pp[ppi % 8]; ppi += 1
                        for kc in range(2):
                            s.mm(pq1.t[0:96, :], wuq.t[:, kc, h * 96:(h + 1) * 96], cqn.t[:, kc, :], kc == 0, kc == 1, [wuq, cqn], [pq1])
                        for kc in range(2):
                            s.mm(pq2.t[0:96, :], wuq_sw.t[:, kc, h, :], cqn.t[:, kc, :], kc == 0, kc == 1, [wuq_sw, cqn], [pq2])
                        s.op("act", lambda e: e.copy(out=q_.t[0:64, h, :], in_=pq1.t[0:64, :]), [pq1], [q_])
                        s.op("dve", lambda e: e.tensor_tensor(a1.t[R, :], pq1.t[R, :], cs.t[R, 0, :], ALU.mult), [pq1, cs], [a1])
                        s.op("dve", lambda e: e.tensor_tensor(a2.t[R, :], pq2.t[R, :], cs.t[R, 1, :], ALU.mult), [pq2, cs], [a2])
                        s.op("pool", lambda e: e.tensor_tensor(q_.t[R, h, :], a1.t[R, :], a2.t[R, :], ALU.add), [a1, a2], [q_])
                        pk = pp[ppi % 8]; ppi += 1
                        s.mm(pk.t[0:64, :], wk.t[:, h, :], ckvn.t[:], True, True, [wk, ckvn], [pk])
                        s.op("act", lambda e: e.copy(out=k_.t[0:64, h, :], in_=pk.t[0:64, :]), [pk], [k_])
                        s.op("pool", lambda e: e.tensor_copy(out=k_.t[R, h, :], in_=krT.t[R, :]), [krT], [k_])
                    s.dma("sp", qT_d[:, :, tsl].rearrange("h p t -> p h t"), q_.t[0:96, :, :], [q_], [b_q], group="st")
                    s.dma("sp", kT_d[:, :, tsl].rearrange("h p t -> p h t"), k_.t[0:96, :, :], [k_], [b_k], group="st")
                    for tt in range(4):
                        i = 4 * g + tt
                        v_ = vt[tt % 2]
                        pv = pp[ppi % 8]; ppi += 1
                        s.mm(pv.t[:], ckvn.t[:, tt * 128:(tt + 1) * 128], wvv.t[:].rearrange("p h e -> p (h e)"), True, True, [ckvn, wvv], [pv])
                        s.op("act", lambda e: e.copy(out=v_.t[:, :, 0:64], in_=pv.t[:].rearrange("p (h e) -> p h e", e=64)), [pv], [v_])
                        s.dma("act", v_d[i * 128:(i + 1) * 128, :, :], v_.t[:], [v_], [b_v], group="st")
            with s.phase():
                masks = s.sb([128, 4, 512], BF16, "masks")
                s.op("pool", lambda e: e.memset(masks.t[:], 1.0), [], [masks])
                for j in range(4):
                    s.op("pool", lambda e, j=j: e.affine_select(out=masks.t[:, j, :], in_=masks.t[:, j, :], pattern=[[1, 512]], compare_op=ALU.is_ge,
                                                                fill=0.0, base=-128 * j, channel_multiplier=-1), [masks], [masks])
                kTh = [s.sb([128, S], BF16, "kTh") for _ in range(2)]
                vh = [s.sb([128, NT, 65], BF16, "vh") for _ in range(2)]
                qg = [s.sb([128, 512], BF16, "qg") for _ in range(2)]
                PT = [s.sb([128, 512], BF16, "PT") for _ in range(3)]
                psT = [s.ps([128, 512], F32, "psT") for _ in range(3)]
                pacc = [s.ps([128, 65], F32, "pacc") for _ in range(4)]
                rec = [s.sb([128, 1], F32, "rec") for _ in range(2)]
                yd = [s.sb([128, 64], BF16, "yd") for _ in range(2)]
                it = 0
                qg = [s.sb([128, 512], BF16, "qg3") for _ in range(3)]

                def load_head(h):
                    s.dma("sp", kTh[h % 2].t[0:96, :], kT_d[h], [b_k], [kTh[h % 2]])
                    vsrc = v_d[:, h, :].rearrange("(t p) e -> p t e", p=128)
                    for qq in range(4):
                        t0_, t1_ = qq * NT // 4, (qq + 1) * NT // 4
                        if t1_ > t0_:
                            s.dma("sp", vh[h % 2].t[:, t0_:t1_, :], vsrc[:, t0_:t1_, :], [b_v], [vh[h % 2]], group=("vh", h))

                def load_q(n):
                    h_, G_ = divmod(n, NG)
                    s.dma("sp", qg[n % 3].t[0:96, :], qT_d[h_, :, G_ * 512:(G_ + 1) * 512], [b_q], [qg[n % 3]])

                load_head(0)
                load_q(0)
                for h in range(8):
                    kt = kTh[h % 2]; vv = vh[h % 2]
                    if h + 1 < 8:
                        load_head(h + 1)
                    for G in range(NG):
                        n = h * NG + G
                        q_ = qg[n % 3]
                        if n + 1 < 8 * NG:
                            load_q(n + 1)
                        nkb = 4 * G + 4

                        def qk(kb):
                            nonlocal it
                            ps_ = psT[it % 3]; it += 1
                            s.mm(ps_.t[:], kt.t[0:96, kb * 128:(kb + 1) * 128], q_.t[0:96, :], True, True, [kt, q_], [ps_])
                            return ps_

                        pend = [qk(0)]
                        if nkb > 1:
                            pend.append(qk(1))
                        for kb in range(nkb):
                            ps_ = pend.pop(0)
                            if kb + 2 < nkb:
                                pend.append(qk(kb + 2))
                            pt = PT[kb % 3]
                            j = kb - 4 * G
                            c0 = max(j, 0) * 128
                            s.op("act", lambda e: e.activation(out=pt.t[:, c0:512], in_=ps_.t[:, c0:512], func=AF.Exp, scale=SCALE), [ps_], [pt])
                            if j >= 0:
                                s.op("dve", lambda e: e.tensor_tensor(pt.t[:, c0:c0 + 128], pt.t[:, c0:c0 + 128], masks.t[:, 0, 0:128], ALU.mult), [pt, masks], [pt])
                            for qs in range(4):
                                last_kb = 4 * G + qs
                                if kb > last_kb:
                                    continue
                                s.mm(pacc[qs].t[:], pt.t[:, qs * 128:(qs + 1) * 128], vv.t[:, kb, :], kb == 0, kb == last_kb, [pt, vv], [pacc[qs]])
                                if kb == last_kb:
                                    r_ = rec[qs % 2]; y_ = yd[qs % 2]
                                    i = 4 * G + qs
                                    s.op("dve", lambda e: e.reciprocal(r_.t[:], pacc[qs].t[:, 64:65]), [pacc[qs]], [r_])
                                    s.op("dve", lambda e: e.tensor_scalar(y_.t[:], pacc[qs].t[:, 0:64], r_.t[:, 0:1], None, ALU.mult), [pacc[qs], r_], [y_])
                                    s.dma("sp", yd_d[i * 128:(i + 1) * 128, h * 64:(h + 1) * 64], y_.t[:], [y_], [b_yd], group="st")
            with s.phase():
                wout = s.sb([128, 8, D], BF16, "wout")
                gpw = load_mod(l, 2, True, "gpw", 1.0 / ALPHA)
                wstg = [s.sb([128, D], F32, "wstg") for _ in range(2)]
                for k in range(8):
                    s.dma("sp", wstg[k % 2].t[:], od_w_out[li, k * 128:(k + 1) * 128, :], [], [wstg[k % 2]])
                    s.op("dve", lambda e, k=k: e.tensor_tensor(wout.t[:, k, :], wstg[k % 2].t[:], gpw.t[:], ALU.mult), [wstg[k % 2], gpw], [wout])
                yT = [s.sb([128, 8, 128], BF16, "yT") for _ in range(2)]
                ydt = [s.sb([128, 512], BF16, "ydt") for _ in range(2)]
                ptr = [s.ps([128, 4, 128], BF16, "ptr") for _ in range(2)]
                pp = [s.ps([128, 512], F32, "pp") for _ in range(4)]
                dl = [s.sb([128, D], F32, "dl") for _ in range(2)]
                for i in range(NT):
                    y = yT[i % 2]; yd_ = ydt[i % 2]; pt = ptr[i % 2]; d_ = dl[i % 2]
                    s.dma("sp", y.t[:, 0:4, :], yc_d[:, :, i * 128:(i + 1) * 128].rearrange("c p t -> p c t"), [b_yc], [y])
                    s.dma("sp", yd_.t[:], yd_d[i * 128:(i + 1) * 128, :], [b_yd], [yd_])
                    for c in range(4):
                        s.op("pe", lambda e, c=c: e.transpose(pt.t[:, c, :], yd_.t[:, c * 128:(c + 1) * 128], ident_b.t[:]), [yd_, ident_b], [pt])
                    s.op("act", lambda e: e.copy(out=y.t[:, 4:8, :], in_=pt.t[:]), [pt], [y])
                    for nh in range(2):
                        p = pp[(2 * i + nh) % 4]
                        for k in range(8):
                            s.mm(p.t[:], y.t[:, k, :], wout.t[:, k, nh * 512:(nh + 1) * 512], k == 0, k == 7, [y, wout], [p])
                        s.op("act", lambda e: e.copy(out=d_.t[:, nh * 512:(nh + 1) * 512], in_=p.t[:]), [p], [d_])
                    s.dma("act", delta[i * 128:(i + 1) * 128, :], d_.t[:], [d_], [b_delta[i]])

        def norm_pass(l, kind, x_src, last):
            with s.phase():
                mix = kind == "mix"
                need_u = not (last and not mix)
                if mix:
                    lng = load_row(ln_mix_g[l:l + 1, :], D, "lng"); lnb = load_row(ln_mix_b[l:l + 1, :], D, "lnb")
                    sc1 = load_mod(l, 4, True, "sc1"); sh = load_mod(l, 3, False, "sh")
                else:
                    lng = load_row(ln_ffn_g[l:l + 1, :], D, "lng"); lnb = load_row(ln_ffn_b[l:l + 1, :], D, "lnb")
                    if need_u:
                        sc1 = load_mod(l + 1, 1, True, "sc1"); sh = load_mod(l + 1, 0, False, "sh")
                if need_u:
                    B2 = sh
                    tmpb = s.sb([128, D], F32, "tmpb")
                    s.op("dve", lambda e: e.tensor_tensor(tmpb.t[:], lnb.t[:], sc1.t[:], ALU.mult), [lnb, sc1], [tmpb])
                    s.op("dve", lambda e: e.tensor_tensor(B2.t[:], tmpb.t[:], sh.t[:], ALU.add), [tmpb, sh], [B2])
                    G2 = sc1
                    s.op("dve", lambda e: e.tensor_tensor(G2.t[:], sc1.t[:], lng.t[:], ALU.mult), [sc1, lng], [G2])
                eps2 = s.sb([128, 1], F32, "eps2")
                s.op("pool", lambda e: e.memset(eps2.t[:], LN_EPS / (ALPHA * ALPHA)), [], [eps2])
                NB = 4
                xt = [s.sb([128, D], F32, "xt") for _ in range(NB)]
                st12 = [s.sb([128, 12], F32, "st12") for _ in range(NB)]
                mv = [s.sb([128, 2], F32, "mv") for _ in range(NB)]
                rstd = [s.sb([128, 1], F32, "rstd") for _ in range(NB)]
                nbias = [s.sb([128, 1], F32, "nbias") for _ in range(NB)]
                nt = [s.sb([128, D], F32, "nt") for _ in range(3)]
                xo = [s.sb([128, D], F32, "xo") for _ in range(3)]
                ub = [s.sb([128, D], BF16, "ub") for _ in range(3)]
                if mix:
                    yt = [s.sb([128, D], F32, "yt") for _ in range(NB)]
                    uf = [s.sb([128, D], F32, "uf") for _ in range(3)]
                    ptf = [s.ps([128, 4, 128], F32, "ptf") for _ in range(4)]
                    uTf = [s.sb([128, 8, 128], F32, "uTf") for _ in range(3)]
                    rw = s.sb([128, 8, NE], F32, "rw")
                    s.dma("sp", rw.t[:], r_w[l].rearrange("(k p) e -> p k e", p=128), [], [rw])
                    rb = load_row(r_b[l:l + 1, :], NE, "rb")
                    plog = [s.ps([128, NE], F32, "plog") for _ in range(2)]
                    prk = [s.ps([128, NE], F32, "prk") for _ in range(2)]
                    lg = [s.sb([128, NE], F32, "lg") for _ in range(3)]
                    t8 = [s.sb([128, 8], F32, "t8") for _ in range(2)]
                    nv0 = [s.sb([128, 1], F32, "nv0") for _ in range(2)]
                    ex4 = [s.sb([128, 4], F32, "ex4") for _ in range(2)]
                    sm = [s.sb([128, 1], F32, "sm") for _ in range(2)]
                    msk = [s.sb([128, NE], F32, "msk") for _ in range(2)]
                    msum = s.sb([128, NE], F32, "msum")
                    oh = s.sb([128, NT, 4, NE], F32, "oh")
                    prod = s.sb([128, 4, NE], F32, "prod")
                    s.op("pool", lambda e: e.memset(msum.t[:], 0.0), [], [msum])
                else:
                    yk = [s.sb([128, D], F32, "yk") for _ in range(12)]
                    if need_u:
                        ptr = [s.ps([128, 8, 128], BF16, "ptr") for _ in range(2)]
                        uTs = [s.sb([128, 8, 128], BF16, "uTs") for _ in range(2)]
                        uf = [s.sb([128, D], F32, "uf") for _ in range(3)]

                def stageL(i):
                    x = xt[i % NB]
                    s.dma("sp", x.t[:], x_src[i * 128:(i + 1) * 128, :], [b_xres[i]] if x_src is xres else [], [x])
                    if mix:
                        y = yt[i % NB]
                        s.dma("sp", y.t[:], delta[i * 128:(i + 1) * 128, :], [b_delta[i]], [y])
                    else:
                        ys_ = [yk[(i % 3) * 4 + k] for k in range(4)]
                        for k in range(4):
                            s.idma(ys_[k].t[:], None, ys_d, bass.IndirectOffsetOnAxis(ap=posk_i.t[:, i, k:k + 1], axis=0),
                                   [b_ys_all, posk_i], [ys_[k]])

                def stageA(i):
                    x = xt[i % NB]; s12 = st12[i % NB]; m_ = mv[i % NB]; rs = rstd[i % NB]; nb_ = nbias[i % NB]
                    if mix:
                        y = yt[i % NB]
                        s.op("dve", lambda e: e.tensor_tensor(x.t[:], x.t[:], y.t[:], ALU.add), [x, y], [x])
                    else:
                        ys_ = [yk[(i % 3) * 4 + k] for k in range(4)]
                        for k in range(4):
                            s.op("act", lambda e, k=k: e.activation(out=ys_[k].t[:], in_=ys_[k].t[:], func=AF.Copy, scale=gatek.t[:, i, k:k + 1]), [ys_[k], gatek], [ys_[k]])
                        s.op("dve", lambda e: e.tensor_tensor(ys_[0].t[:], ys_[0].t[:], ys_[1].t[:], ALU.add), [ys_[0], ys_[1]], [ys_[0]])
                        s.op("dve", lambda e: e.tensor_tensor(ys_[2].t[:], ys_[2].t[:], ys_[3].t[:], ALU.add), [ys_[2], ys_[3]], [ys_[2]])
                        s.op("dve", lambda e: e.tensor_tensor(ys_[0].t[:], ys_[0].t[:], ys_[2].t[:], ALU.add), [ys_[0], ys_[2]], [ys_[0]])
                        s.op("dve", lambda e: e.tensor_tensor(x.t[:], x.t[:], ys_[0].t[:], ALU.add), [x, ys_[0]], [x])
                    s.op("dve", lambda e: e.bn_stats(s12.t[:, 0:6], x.t[:, 0:512]), [x], [s12])
                    s.op("dve", lambda e: e.bn_stats(s12.t[:, 6:12], x.t[:, 512:1024]), [x], [s12])
                    s.op("dve", lambda e: e.bn_aggr(m_.t[:], s12.t[:]), [s12], [m_])
                    s.op("act", lambda e: e.activation(out=rs.t[:], in_=m_.t[:, 1:2], func=AF.Ln, bias=eps2.t[:, 0:1]), [m_, eps2], [rs])
                    s.op("act", lambda e: e.activation(out=rs.t[:], in_=rs.t[:], func=AF.Exp, scale=-0.5), [rs], [rs])
                    s.op("dve", lambda e: e.scalar_tensor_tensor(nb_.t[:], m_.t[:, 0:1], -1.0, rs.t[:], ALU.mult, ALU.mult), [m_, rs], [nb_])

                def stageB(i):
                    x = xt[i % NB]; rs = rstd[i % NB]; nb_ = nbias[i % NB]
                    n_ = nt[i % 3]; o_ = xo[i % 3]
                    s.op("act", lambda e: e.activation(out=n_.t[:], in_=x.t[:], func=AF.Identity, scale=rs.t[:, 0:1], bias=nb_.t[:, 0:1]), [x, rs, nb_], [n_])
                    s.op("dve", lambda e: e.tensor_tensor(o_.t[:], n_.t[:], lng.t[:], ALU.mult), [n_, lng], [o_])
                    s.op("dve", lambda e: e.tensor_tensor(o_.t[:], o_.t[:], lnb.t[:], ALU.add), [o_, lnb], [o_])
                    if not need_u:
                        s.dma("sp", out[i * 128:(i + 1) * 128, :], o_.t[:], [o_], [b_xres[i]])
                        return
                    s.dma("sp", xres[i * 128:(i + 1) * 128, :], o_.t[:], [o_], [b_xres[i]])
                    u = ub[i % 3]; f = uf[i % 3]
                    s.op("dve", lambda e: e.tensor_tensor(f.t[:], n_.t[:], G2.t[:], ALU.mult), [n_, G2], [f])
                    if not mix:
                        s.op("dve", lambda e: e.tensor_tensor(u.t[:], f.t[:], B2.t[:], ALU.add), [f, B2], [u])
                        transposes_store_uT(u, i, ptr[i % 2], uTs[i % 2])
                        return
                    s.op("dve", lambda e: e.tensor_tensor(f.t[:], f.t[:], B2.t[:], ALU.add), [f, B2], [f])
                    s.op("act", lambda e: e.copy(out=u.t[:], in_=f.t[:]), [f], [u])
                    s.dma("act", u_d[i * 128:(i + 1) * 128, :], u.t[:], [u], [b_u[i]])
                    uT = uTf[i % 3]
                    for hf in range(2):
                        pt = ptf[(2 * i + hf) % 4]
                        for kk in range(4):
                            k = hf * 4 + kk
                            s.op("pe", lambda e, k=k, kk=kk: e.transpose(pt.t[:, kk, :], f.t[:, k * 128:(k + 1) * 128], ident_f.t[:]),
                                 [f, ident_f], [pt])
                        s.op("act", lambda e: e.copy(out=uT.t[:, hf * 4:hf * 4 + 4, :], in_=pt.t[:]), [pt], [uT])
                    pl = plog[i % 2]
                    for k in range(8):
                        s.mm(pl.t[:], uT.t[:, k, :], rw.t[:, k, :], k == 0, k == 7, [uT, rw], [pl])

                def stageC(i):
                    lgt = lg[i % 3]; t8_ = t8[i % 2]; mk = msk[i % 2]; pl = plog[i % 2]
                    s.op("dve", lambda e: e.tensor_tensor(lgt.t[:], pl.t[:], rb.t[:], ALU.add), [pl, rb], [lgt])
                    s.op("dve", lambda e: e.max(out=t8_.t[:], in_=lgt.t[:]), [lgt], [t8_])
                    pr = prk[i % 2]
                    s.op("dve", lambda e: e.tensor_scalar(mk.t[:], lgt.t[:], t8_.t[:, 3:4], None, ALU.is_ge), [lgt, t8_], [mk])
                    s.mm(pr.t[:], su_f.t[:], mk.t[:], True, False, [su_f, mk], [pr])
                    s.mm(pr.t[:], ones_f.t[:], msum.t[:], False, True, [ones_f, msum], [pr])
                    for k in range(4):
                        s.op("dve", lambda e, k=k: e.tensor_scalar(oh.t[:, i, k, :], lgt.t[:], t8_.t[:, k:k + 1], None, ALU.is_equal), [lgt, t8_], [oh])
                    n0 = nv0[i % 2]; e4 = ex4[i % 2]; sm_ = sm[i % 2]
                    s.op("dve", lambda e: e.tensor_scalar(n0.t[:], t8_.t[:, 0:1], -1.0, None, ALU.mult), [t8_], [n0])
                    s.op("act", lambda e: e.activation(out=e4.t[:], in_=t8_.t[:, 0:4], func=AF.Exp, bias=n0.t[:, 0:1]), [t8_, n0], [e4])
                    for k in range(4):
                        s.op("dve", lambda e, k=k: e.tensor_tensor(prod.t[:, k, :], oh.t[:, i, k, :], pr.t[:], ALU.mult), [oh, pr], [prod])
                    s.op("dve", lambda e: e.tensor_reduce(out=posk_f.t[:, i, :], in_=prod.t[:], axis=AX.X, op=ALU.add), [prod], [posk_f])
                    s.op("dve", lambda e: e.tensor_tensor(msum.t[:], msum.t[:], mk.t[:], ALU.add), [msum, mk], [msum])
                    s.op("dve", lambda e: e.tensor_reduce(out=sm_.t[:], in_=e4.t[:], axis=AX.X, op=ALU.add), [e4], [sm_])
                    s.op("dve", lambda e: e.reciprocal(sm_.t[:], sm_.t[:]), [sm_], [sm_])
                    s.op("dve", lambda e: e.tensor_scalar(gatek.t[:, i, :], e4.t[:], sm_.t[:, 0:1], None, ALU.mult), [e4, sm_], [gatek])

                yk_sets = 3
                for step in range(NT + 3):
                    if step < NT:
                        stageL(step)
                    if 2 <= step <= NT + 1:
                        stageB(step - 2)
                    if 1 <= step <= NT:
                        stageA(step - 1)
                    if mix and 3 <= step:
                        stageC(step - 3)
                if kind == "mix":
                    pc = prk[0]
                    s.mm(pc.t[:], ones_f.t[:], msum.t[:], True, True, [ones_f, msum], [pc])
                    cnt = s.sb([128, NE], F32, "cnt")
                    nch = s.sb([128, NE], F32, "nch")
                    tmpc = s.sb([128, NE], F32, "tmpc")
                    cs_a = s.sb([128, NE], F32, "csa")
                    cs_b = s.sb([128, NE], F32, "csb")
                    s.op("dve", lambda e: e.tensor_copy(out=cnt.t[:], in_=pc.t[:]), [pc], [cnt])
                    s.op("dve", lambda e: e.tensor_scalar(nch.t[:], cnt.t[:], 0.5, None, ALU.is_gt), [cnt], [nch])
                    for j in range(1, (S + CH - 1) // CH):
                        s.op("dve", lambda e, j=j: e.tensor_scalar(tmpc.t[:], cnt.t[:], CH * j + 0.5, None, ALU.is_gt), [cnt], [tmpc])
                        s.op("dve", lambda e: e.tensor_tensor(nch.t[:], nch.t[:], tmpc.t[:], ALU.add), [nch, tmpc], [nch])
                    s.op("dve", lambda e: e.tensor_copy(out=cs_a.t[:], in_=nch.t[:]), [nch], [cs_a])
                    a, b = cs_a, cs_b
                    sh_ = 1
                    while sh_ < NE:
                        s.op("dve", lambda e, a=a, b=b, sh_=sh_: e.tensor_copy(out=b.t[:, 0:sh_], in_=a.t[:, 0:sh_]), [a], [b])
                        s.op("dve", lambda e, a=a, b=b, sh_=sh_: e.tensor_tensor(b.t[:, sh_:NE], a.t[:, sh_:NE], a.t[:, 0:NE - sh_], ALU.add), [a], [b])
                        a, b = b, a
                        sh_ *= 2
                    cend = a
                    base = s.sb([128, NE], F32, "base")
                    s.op("dve", lambda e: e.tensor_tensor(base.t[:], cend.t[:], nch.t[:], ALU.subtract), [cend, nch], [base])
                    s.op("dve", lambda e: e.tensor_scalar(base.t[:], base.t[:], float(CH), None, ALU.mult), [base], [base])
                    prod2 = s.sb([128, NT, 4, NE], F32, "prod2")
                    s.op("dve", lambda e: e.tensor_tensor(prod2.t[:].rearrange("p i k e -> p (i k) e"), oh.t[:].rearrange("p i k e -> p (i k) e"),
                                                          base.t[:].unsqueeze(1).to_broadcast([128, NT * 4, NE]), ALU.mult), [oh, base], [prod2])
                    bsel = s.sb([128, NT, 4], F32, "bsel")
                    s.op("dve", lambda e: e.tensor_reduce(out=bsel.t[:], in_=prod2.t[:], axis=AX.X, op=ALU.add), [prod2], [bsel])
                    s.op("dve", lambda e: e.tensor_tensor(posk_f.t[:], posk_f.t[:], bsel.t[:], ALU.add), [posk_f, bsel], [posk_f])
                    s.op("dve", lambda e: e.tensor_copy(out=posk_i.t[:], in_=posk_f.t[:]), [posk_f], [posk_i])
                    cmp3 = s.sb([128, NCH, NE], F32, "cmp3")
                    cef = s.sb([128, NCH], F32, "cef")
                    wif = s.sb([128, NCH, 8], F32, "wif")
                    tf = s.sb([128, NCH], F32, "tf")
                    s.op("dve", lambda e: e.tensor_tensor(cmp3.t[:], cend.t[:].unsqueeze(1).to_broadcast([128, NCH, NE]),
                                                          iota_c.t[:].unsqueeze(2).to_broadcast([128, NCH, NE]), ALU.is_le), [cend, iota_c], [cmp3])
                    s.op("dve", lambda e: e.tensor_reduce(out=cef.t[:], in_=cmp3.t[:], axis=AX.X, op=ALU.add), [cmp3], [cef])
                    s.op("dve", lambda e: e.tensor_scalar(cef.t[:], cef.t[:], float(NE - 1), float(l * NE), ALU.min, ALU.add), [cef], [cef])
                    s.op("dve", lambda e: e.tensor_copy(out=bdidx.t[:], in_=cef.t[:]), [cef], [bdidx])
                    s.op("dve", lambda e: e.tensor_scalar(tf.t[:], cef.t[:], 128.0, iota_p.t[:, 0:1], ALU.mult, ALU.add), [cef, iota_p], [tf])
                    s.op("dve", lambda e: e.tensor_copy(out=bgidx.t[:], in_=tf.t[:]), [tf], [bgidx])
                    s.op("dve", lambda e: e.tensor_scalar(tf.t[:], cef.t[:], 1024.0, None, ALU.mult), [cef], [tf])
                    s.op("dve", lambda e: e.tensor_tensor(wif.t[:], tf.t[:].unsqueeze(2).to_broadcast([128, NCH, 8]),
                                                          base_pk.t[:].unsqueeze(1).to_broadcast([128, NCH, 8]), ALU.add), [tf, base_pk], [wif])
                    s.op("dve", lambda e: e.tensor_copy(out=widx.t[:], in_=wif.t[:]), [wif], [widx])
                    if "posk" in dbg_out:
                        s.dma("sp", dbg_out["posk"].rearrange("(i p) k -> p i k", p=128), posk_f.t[:], [posk_f], [])
                        s.dma("sp", dbg_out["gatek"].rearrange("(i p) k -> p i k", p=128), gatek.t[:], [gatek], [])
                        s.dma("sp", dbg_out["ctab"], cef.t[:, 0:NCH], [cef], [])

        def scatter_pass():
            with s.phase():
                ub = [s.sb([128, D], BF16, "ub") for _ in range(3)]
                for i in range(NT):
                    u = ub[i % 3]
                    s.dma("sp", u.t[:], u_d[i * 128:(i + 1) * 128, :], [b_u[i]], [u])
                    for k in range(4):
                        s.idma(xs_d, bass.IndirectOffsetOnAxis(ap=posk_i.t[:, i, k:k + 1], axis=0), u.t[:], None,
                               [u, posk_i], [b_xs_all], group="sc")

        def expert_pass(l):
            with s.phase():
                wgu = [s.sb([128, 8, 2 * D], BF16, "wgu") for _ in range(3)]
                wdn = [s.sb([128, 8, D], BF16, "wdn") for _ in range(2)]
                bgu = [s.sb([128, 16], F32, "bgu") for _ in range(3)]
                bdn = [s.sb([128, D], BF16, "bdn") for _ in range(2)]
                SBK = CH // 128
                xsb = [[s.sb([128, D], BF16, "xsb") for _ in range(SBK)] for _ in range(2)]
                xT = [s.sb([128, 8, CH], BF16, "xT") for _ in range(2)]
                ptr = [s.ps([128, 8, 128], BF16, "ptr") for _ in range(2)]
                pg = [s.ps([128, 512], F32, "pg") for _ in range(4)]
                pd = [s.ps([128, 512], F32, "pd") for _ in range(2)]
                gc = [s.sb([128, CH], F32, "gc") for _ in range(2)]
                sg = [s.sb([128, CH], F32, "sg") for _ in range(2)]
                uc = [s.sb([128, CH], F32, "uc") for _ in range(2)]
                actT = [s.sb([128, 8, CH], BF16, "actT") for _ in range(2)]
                ysb = [s.sb([128, D], F32, "ysb") for _ in range(2)]
                gpf = load_mod(l, 5, True, "gpf", 1.0 / ALPHA)
                wguv = w_gu.rearrange("l e r f -> (l e r) f")
                wdnv = w_dn.rearrange("l e r f -> (l e r) f")
                bguv = b_guT.rearrange("l e p c -> (l e p) c")
                bdnv = b_dn.rearrange("l e f -> (l e) f")
                IO = bass.IndirectOffsetOnAxis

                def pre_g(c):
                    wg = wgu[c % 3]; bg = bgu[c % 3]
                    for k in range(8):
                        s.idma(wg.t[:, k, :], None, wguv, IO(ap=widx.t[:, c, k:k + 1], axis=0), [widx], [wg], group=("wg", c))
                    s.idma(bg.t[:], None, bguv, IO(ap=bgidx.t[:, c:c + 1], axis=0), [bgidx], [bg])
                    s.op("dve", lambda e: e.tensor_scalar(bg.t[:, 8:16], bg.t[:, 8:16], 1.0, None, ALU.add), [bg], [bg])

                def pre_d(c):
                    wd = wdn[c % 2]; bd = bdn[c % 2]
                    for k in range(8):
                        s.idma(wd.t[:, k, :], None, wdnv, IO(ap=widx.t[:, c, k:k + 1], axis=0), [widx], [wd], group=("wd", c))
                    s.idma(bd.t[:], None, bdnv, IO(ap=bdidx.t[:, c:c + 1], axis=0), [bdidx], [bd])
                    for sb_ in range(SBK):
                        xb = xsb[c % 2][sb_]
                        r0 = c * CH + sb_ * 128
                        s.dma("sp", xb.t[:], xs_d[r0:r0 + 128, :], [b_xs_all], [xb])

                def tr(c):
                    xt_ = xT[c % 2]
                    for sb_ in range(SBK):
                        xb = xsb[c % 2][sb_]; pt = ptr[sb_ % 2]
                        for k in range(8):
                            s.op("pe", lambda e, k=k: e.transpose(pt.t[:, k, :], xb.t[:, k * 128:(k + 1) * 128], ident_b.t[:]), [xb, ident_b], [pt])
                        s.op("act", lambda e: e.copy(out=xt_.t[:, :, sb_ * 128:(sb_ + 1) * 128], in_=pt.t[:]), [pt], [xt_])

                def gu(c):
                    wg = wgu[c % 3]; bg = bgu[c % 3]
                    xt_ = xT[c % 2]; at = actT[c % 2]
                    for j in range(8):
                        p_g = pg[(2 * j) % 4]; p_u = pg[(2 * j + 1) % 4]
                        for k in range(8):
                            s.mm(p_g.t[:, 0:CH], wg.t[:, k, j * 128:(j + 1) * 128], xt_.t[:, k, :], k == 0, k == 7, [wg, xt_], [p_g])
                        for k in range(8):
                            s.mm(p_u.t[:, 0:CH], wg.t[:, k, D + j * 128:D + (j + 1) * 128], xt_.t[:, k, :], k == 0, k == 7, [wg, xt_], [p_u])
                        g_ = gc[j % 2]; s_ = sg[j % 2]; u_ = uc[j % 2]
                        s.op("dve", lambda e: e.tensor_scalar(g_.t[:], p_g.t[:, 0:CH], bg.t[:, j:j + 1], 7.0, ALU.add, ALU.min), [p_g, bg], [g_])
                        s.op("act", lambda e: e.activation(out=s_.t[:], in_=g_.t[:], func=AF.Sigmoid, scale=1.702), [g_], [s_])
                        s.op("dve", lambda e: e.tensor_scalar(u_.t[:], p_u.t[:, 0:CH], bg.t[:, 8 + j:9 + j], 8.0, ALU.add, ALU.min), [p_u, bg], [u_])
                        s.op("dve", lambda e: e.tensor_tensor(g_.t[:], g_.t[:], s_.t[:], ALU.mult), [g_, s_], [g_])
                        s.op("dve", lambda e: e.scalar_tensor_tensor(at.t[:, j, :], u_.t[:], -6.0, g_.t[:], ALU.max, ALU.mult), [g_, u_], [at])

                def dn(c):
                    wd = wdn[c % 2]; bd = bdn[c % 2]; at = actT[c % 2]
                    for sb_ in range(SBK):
                        yb = ysb[sb_ % 2]
                        for nh in range(2):
                            p = pd[nh]
                            for j in range(8):
                                s.mm(p.t[:], at.t[:, j, sb_ * 128:(sb_ + 1) * 128], wd.t[:, j, nh * 512:(nh + 1) * 512], j == 0, False, [at, wd], [p])
                            s.mm(p.t[:], ones_b.t[0:1, :], bd.t[0:1, nh * 512:(nh + 1) * 512], False, True, [ones_b, bd], [p])
                            s.op("dve", lambda e: e.tensor_tensor(yb.t[:, nh * 512:(nh + 1) * 512], p.t[:], gpf.t[:, nh * 512:(nh + 1) * 512], ALU.mult), [p, gpf], [yb])
                        r0 = c * CH + sb_ * 128
                        s.dma("sp", ys_d[r0:r0 + 128, :], yb.t[:], [yb], [b_ys_all], group=("ys", l))

                pre_g(0)
                pre_d(0)
                if NCH > 1:
                    pre_g(1)
                tr(0)
                for c in range(NCH):
                    if c + 2 < NCH:
                        pre_g(c + 2)
                    if c + 1 < NCH:
                        pre_d(c + 1)
                    gu(c)
                    if c + 1 < NCH:
                        tr(c + 1)
                    dn(c)

        eps_t = s.sb([128, 1], F32, "eps")
        s.op("pool", lambda e: e.memset(eps_t.t[:], LN_EPS), [], [eps_t])

        prologue()
        premod(0)
        x_src = x_in
        stop_after = (dbg or {}).get("_stop", None)
        for l in range(layers):
            if l % 2 == 0:
                even_mixer(l)
            else:
                odd_mixer(l)
            norm_pass(l, "mix", x_src, False)
            x_src = xres
            scatter_pass()
            expert_pass(l)
            norm_pass(l, "ffn", x_src, l == layers - 1)
        s.barrier()
    return nc


def prep_inputs(inputs, S=4096, ncores=8, layers=DEPTH):
    f = lambda a: np.ascontiguousarray(np.asarray(a))
    shared = {}
    for k in ("ada_w", "ada_b", "ln_mix_g", "ln_mix_b", "ln_ffn_g", "ln_ffn_b", "ev_w_in", "ev_sg_b", "ev_vn_g", "ev_vn_b",
              "ev_w_out", "od_w_in", "od_w_uq", "od_w_ukv", "od_w_out", "moe_router_w", "moe_router_b", "moe_w_gu",
              "moe_w_dn", "moe_b_dn"):
        shared[k] = f(inputs[k][:layers]) if k in ("moe_w_gu", "moe_w_dn", "ada_w") else f(inputs[k])
    shared["ev_conv"] = f(np.asarray(inputs["ev_conv_w"]).transpose(0, 2, 1).reshape(2, 4, 128, 3).transpose(0, 2, 1, 3))
    shared["ev_sgT"] = f(np.asarray(inputs["ev_sg_w"]).transpose(0, 1, 3, 2))
    shared["od_dw"] = f(np.asarray(inputs["od_dw_w"]).transpose(0, 2, 1).reshape(2, 4, 128, 31).transpose(0, 2, 1, 3))
    col = lambda a, n: f(np.asarray(a).reshape(2, n, 128).transpose(0, 2, 1))
    shared["od_dw_b"] = col(inputs["od_dw_b"], 4)
    shared["od_cn_g"] = col(inputs["od_cn_g"], 4)
    shared["od_cn_b"] = col(inputs["od_cn_b"], 4)
    shared["od_qn_g"] = col(inputs["od_qn_g"], 2)
    shared["od_kvn_g"] = col(inputs["od_kvn_g"], 1)
    shared["moe_b_guT"] = f(np.asarray(inputs["moe_b_gu"]).reshape(DEPTH, NE, 16, 128).transpose(0, 1, 3, 2))
    invf = (10000.0 ** (-np.arange(0, 32, 2, dtype=np.float32) / 32)).astype(np.float32)
    shared["invf"] = f(np.concatenate([invf, invf]).reshape(32, 1))
    maps = []
    x = np.asarray(inputs["x"]); c = np.asarray(inputs["c"]); pos = np.asarray(inputs["positions"])
    for b in range(ncores):
        m = dict(shared)
        m["x"] = f(x[b, :S])
        m["c"] = f(c[b].reshape(8, 128).T)
        m["pos"] = f(np.broadcast_to(pos[b, :S].astype(np.int32)[None, :], (32, S)))
        maps.append(m)
    return maps


_NC_CACHE = {}


def kernel(**inputs):
    S = 4096
    if "nc" not in _NC_CACHE:
        _NC_CACHE["nc"] = build(S)
    nc = _NC_CACHE["nc"]
    maps = prep_inputs(inputs, S, 8)
    res = run_bass_kernel_spmd(nc, maps, core_ids=list(range(8)))
    return np.stack([np.asarray(r["out"]) for r in res.results], axis=0).astype(np.float32)
```

```python
import math
import numpy as np
from contextlib import ExitStack
import concourse.bass as bass
import concourse.mybir as mybir
from concourse.bass_utils import run_bass_kernel_spmd

F32 = mybir.dt.float32
BF16 = mybir.dt.bfloat16
I32 = mybir.dt.int32
AF = mybir.ActivationFunctionType
ALU = mybir.AluOpType
AX = mybir.AxisListType

D = 1024
DEPTH = 4
NE = 32
TOPK = 4
CH = 384
ALPHA = (2.0 * DEPTH) ** 0.25
LN_EPS = 1e-5
RMS_EPS = 1e-6
TWO_PI_HI = 6.28125
TWO_PI_LO = 2.0 * math.pi - 6.28125


class Buf:
    __slots__ = ("w", "r", "g")

    def __init__(self):
        self.w = []
        self.r = []
        self.g = None


class T:
    __slots__ = ("t", "b")

    def __init__(self, t):
        self.t = t
        self.b = Buf()


class Sched:
    ENG = ("pe", "act", "dve", "pool", "sp")

    def __init__(self, nc, es, ndma=12):
        self.nc = nc
        self.es = es
        self.eng = {"pe": nc.tensor, "act": nc.scalar, "dve": nc.vector, "pool": nc.gpsimd, "sp": nc.sync}
        self.sem = {}
        self.cnt = {}
        self.seen = {e: {} for e in self.ENG}
        for e in self.ENG:
            self.sem[e] = es.enter_context(nc.semaphore("s_" + e))
            self.cnt[e] = 0
        self.dq = {}
        for q in ("sp", "pool", "act"):
            lst = []
            for i in range(ndma if q != "act" else 8):
                key = "d_%s%d" % (q, i)
                self.sem[key] = es.enter_context(nc.semaphore(key))
                self.cnt[key] = 0
                lst.append(key)
            self.dq[q] = [lst, 0]
        self.uid = 0

    def sb(self, shape, dt, name=None):
        self.uid += 1
        return T(self.es_cur.enter_context(self.nc.sbuf_tensor("%s_%d" % (name or "t", self.uid), list(shape), dt)))

    def ps(self, shape, dt, name=None):
        self.uid += 1
        return T(self.es_cur.enter_context(self.nc.psum_tensor("%s_%d" % (name or "p", self.uid), list(shape), dt)))

    def _wait(self, e, evs):
        seen = self.seen[e]
        for key, val in evs:
            if key == "pe" and e == "pe":
                continue
            if seen.get(key, 0) >= val:
                continue
            self.eng[e].wait_ge(self.sem[key], val)
            seen[key] = val

    @staticmethod
    def _deps(reads, writes, group=None):
        evs = []
        for b in reads:
            b = b.b if isinstance(b, T) else b
            evs.extend(b.w)
        for b in writes:
            b = b.b if isinstance(b, T) else b
            if group is None or b.g != group:
                evs.extend(b.w)
            evs.extend(b.r)
        return evs

    @staticmethod
    def _update(ev, reads, writes, group=None):
        for b in reads:
            b = b.b if isinstance(b, T) else b
            b.r.append(ev)
            if len(b.r) > 24:
                last = {}
                for k, v in b.r:
                    if last.get(k, 0) < v:
                        last[k] = v
                b.r = list(last.items())
        for b in writes:
            b = b.b if isinstance(b, T) else b
            if group is not None and b.g == group:
                b.w.append(ev)
            else:
                b.w = [ev]
                b.g = group
            b.r = []

    def op(self, e, fn, reads=(), writes=()):
        self._wait(e, self._deps(reads, writes))
        ins = fn(self.eng[e])
        self.cnt[e] += 1
        ins.then_inc(self.sem[e], 1)
        self.seen[e][e] = max(self.seen[e].get(e, 0), 0)
        self._update((e, self.cnt[e]), reads, writes)

    def mm(self, out, lhsT, rhs, start, stop, reads, writes, **kw):
        self.op("pe", lambda pe: pe.matmul(out, lhsT=lhsT, rhs=rhs, start=start, stop=stop, **kw), reads, writes)

    def dma(self, q, out, in_, reads=(), writes=(), group=None, **kw):
        lst, idx = self.dq[q]
        key = lst[idx % len(lst)]
        self.dq[q][1] = idx + 1
        evs = self._deps(reads, writes, group)
        if self.cnt[key] > 0:
            evs.append((key, self.cnt[key]))
        self._wait(q, evs)
        ins = self.eng[q].dma_start(out=out, in_=in_, **kw)
        self.cnt[key] += 16
        ins.then_inc(self.sem[key], 16)
        self._update((key, self.cnt[key]), reads, writes, group)

    def idma(self, out, out_off, in_, in_off, reads=(), writes=(), group=None):
        q = "pool"
        lst, idx = self.dq[q]
        key = lst[idx % len(lst)]
        self.dq[q][1] = idx + 1
        evs = self._deps(reads, writes, group)
        if self.cnt[key] > 0:
            evs.append((key, self.cnt[key]))
        self._wait(q, evs)
        ins = self.eng[q].indirect_dma_start(out=out, out_offset=out_off, in_=in_, in_offset=in_off)
        self.cnt[key] += 16
        ins.then_inc(self.sem[key], 16)
        self._update((key, self.cnt[key]), reads, writes, group)

    def barrier(self):
        evs = [(k, v) for k, v in self.cnt.items() if v > 0]
        for e in self.ENG:
            self._wait(e, evs)

    def phase(self):
        return _Phase(self)


class _Phase:
    def __init__(self, s):
        self.s = s

    def __enter__(self):
        self.es = ExitStack()
        self.es.__enter__()
        self.s.es_cur = self.es
        return self.s

    def __exit__(self, *a):
        self.s.barrier()
        self.es.__exit__(*a)
        return False


def bcast(ap, n=128):
    return ap.partition_broadcast(n)


def build(S=4096, layers=DEPTH, dbg=None):
    NT = S // 128
    NG = S // 512
    NCH = (S * TOPK) // CH + NE
    NSLOT = NCH * CH
    nc = bass.Bass("TRN2", target_bir_lowering=False)

    def din(name, shape, dt=F32):
        return nc.dram_tensor(name, list(shape), dt, kind="ExternalInput").ap()

    def dscr(name, shape, dt=F32):
        return nc.dram_tensor(name, list(shape), dt, kind="Internal").ap()

    n_even = (layers + 1) // 2
    n_odd = layers // 2
    x_in = din("x", [S, D])
    c_in = din("c", [128, 8])
    pos_in = din("pos", [32, S], I32)
    invf_in = din("invf", [32, 1])
    ada_w = din("ada_w", [layers, D, 6 * D])
    ada_b = din("ada_b", [DEPTH, 6 * D])
    ln_mix_g = din("ln_mix_g", [DEPTH, D]); ln_mix_b = din("ln_mix_b", [DEPTH, D])
    ln_ffn_g = din("ln_ffn_g", [DEPTH, D]); ln_ffn_b = din("ln_ffn_b", [DEPTH, D])
    ev_w_in = din("ev_w_in", [2, D, 2560])
    ev_conv = din("ev_conv", [2, 128, 4, 3])
    ev_sgT = din("ev_sgT", [2, 8, 128, 128])
    ev_sg_b = din("ev_sg_b", [2, 8, 128])
    ev_vn_g = din("ev_vn_g", [2, 512]); ev_vn_b = din("ev_vn_b", [2, 512])
    ev_w_out = din("ev_w_out", [2, D, D])
    od_w_in = din("od_w_in", [2, D, 1440])
    od_dw = din("od_dw", [2, 128, 4, 31])
    od_dw_b = din("od_dw_b", [2, 128, 4])
    od_cn_g = din("od_cn_g", [2, 128, 4]); od_cn_b = din("od_cn_b", [2, 128, 4])
    od_qn_g = din("od_qn_g", [2, 128, 2])
    od_w_uq = din("od_w_uq", [2, 256, 768])
    od_kvn_g = din("od_kvn_g", [2, 128, 1])
    od_w_ukv = din("od_w_ukv", [2, 128, 1024])
    od_w_out = din("od_w_out", [2, D, D])
    r_w = din("moe_router_w", [DEPTH, D, NE])
    r_b = din("moe_router_b", [DEPTH, NE])
    w_gu = din("moe_w_gu", [layers, NE, D, 2 * D])
    b_guT = din("moe_b_guT", [DEPTH, NE, 128, 16])
    w_dn = din("moe_w_dn", [layers, NE, D, D])
    b_dn = din("moe_b_dn", [DEPTH, NE, D])
    out = nc.dram_tensor("out", [S, D], F32, kind="ExternalOutput").ap()

    xres = dscr("xres", [S, D])
    delta = dscr("delta", [S, D])
    uT_d = dscr("uT_d", [8, 128, S], BF16)
    u_d = dscr("u_d", [S, D], BF16)
    xs_d = dscr("xs_d", [NSLOT, D], BF16)
    ys_d = dscr("ys_d", [NSLOT, D])
    mod_d = dscr("mod_d", [DEPTH, 6 * D])
    rope_d = dscr("rope_d", [2, 32, S])
    od_scr = {}
    if layers > 1:
        od_scr = {"qT": dscr("qT_d", [8, 96, S], BF16), "kT": dscr("kT_d", [8, 96, S], BF16), "v": dscr("v_d", [S, 8, 65], BF16),
                  "yc": dscr("yc_d", [4, 128, S], BF16), "yd": dscr("yd_d", [S, 512], BF16)}
    dbg_out = {}
    if dbg:
        for name, shape in dbg.items():
            dbg_out[name] = nc.dram_tensor("dbg_" + name, list(shape), F32, kind="ExternalOutput").ap()

    tokb = lambda: [Buf() for _ in range(NT)]
    b_xres = tokb(); b_delta = tokb(); b_uT = tokb(); b_u = tokb()
    b_xs = [Buf() for _ in range(NCH)]; b_ys = [Buf() for _ in range(NCH)]
    b_xs_all = Buf(); b_ys_all = Buf()
    b_mod = Buf(); b_rope = Buf()

    with ExitStack() as es_top:
        s = Sched(nc, es_top)
        s.es_cur = es_top
        ident_b = s.sb([128, 128], BF16, "identb")
        ident_f = s.sb([128, 128], F32, "identf")
        ones_f = s.sb([128, 128], F32, "onesf")
        ones_b = s.sb([128, 128], BF16, "onesb")
        c1702 = s.sb([1, 128], BF16, "c1702")
        su_f = s.sb([128, 128], F32, "suf")
        iota_p = s.sb([128, 1], F32, "iotap")
        posk_f = s.sb([128, NT, 4], F32, "poskf")
        posk_i = s.sb([128, NT, 4], I32, "poski")
        gatek = s.sb([128, NT, 4], F32, "gatek")
        widx = s.sb([128, NCH, 8], I32, "widx")
        bgidx = s.sb([128, NCH], I32, "bgidx")
        bdidx = s.sb([128, NCH], I32, "bdidx")
        base_pk = s.sb([128, 8], F32, "basepk")
        iota_c = s.sb([128, NCH], F32, "iotac")

        def mk_consts():
            s.op("pool", lambda e: e.memset(ident_b.t[:], 0.0), [], [ident_b])
            s.op("pool", lambda e: e.affine_select(out=ident_b.t[:], in_=ident_b.t[:], pattern=[[-1, 128]],
                                                   compare_op=ALU.not_equal, fill=1.0, base=0, channel_multiplier=1),
                 [ident_b], [ident_b])
            s.op("pool", lambda e: e.memset(ident_f.t[:], 0.0), [], [ident_f])
            s.op("pool", lambda e: e.affine_select(out=ident_f.t[:], in_=ident_f.t[:], pattern=[[-1, 128]],
                                                   compare_op=ALU.not_equal, fill=1.0, base=0, channel_multiplier=1),
                 [ident_f], [ident_f])
            s.op("pool", lambda e: e.memset(ones_f.t[:], 1.0), [], [ones_f])
            s.op("pool", lambda e: e.memset(ones_b.t[:], 1.0), [], [ones_b])
            s.op("pool", lambda e: e.memset(c1702.t[:], 1.702), [], [c1702])
            s.op("pool", lambda e: e.memset(su_f.t[:], 1.0), [], [su_f])
            s.op("pool", lambda e: e.affine_select(out=su_f.t[:], in_=su_f.t[:], pattern=[[1, 128]],
                                                   compare_op=ALU.is_gt, fill=0.0, base=0, channel_multiplier=-1),
                 [su_f], [su_f])
            s.op("pool", lambda e: e.iota(iota_p.t[:], pattern=[[0, 1]], base=0, channel_multiplier=1,
                                          allow_small_or_imprecise_dtypes=True), [], [iota_p])
            s.op("pool", lambda e: e.iota(base_pk.t[:], pattern=[[128, 8]], base=0, channel_multiplier=1,
                                          allow_small_or_imprecise_dtypes=True), [], [base_pk])
            s.op("pool", lambda e: e.iota(iota_c.t[:], pattern=[[1, NCH]], base=0, channel_multiplier=0,
                                          allow_small_or_imprecise_dtypes=True), [], [iota_c])

        mk_consts()

        def prologue():
            with s.phase():
                ct = s.sb([128, 8], F32, "ct")
                cond = s.sb([128, 8], F32, "cond")
                condb = s.sb([128, 8, 128], F32, "condb")
                s.dma("sp", ct.t[:], c_in, [], [ct])
                s.op("act", lambda e: e.activation(out=cond.t[:], in_=ct.t[:], func=AF.Silu), [ct], [cond])
                for k in range(8):
                    s.op("dve", lambda e, k=k: e.tensor_scalar(condb.t[:, k, :], ones_f.t[:], cond.t[:, k:k + 1], None,
                                                               ALU.mult), [ones_f, cond], [condb])
                wst = [s.sb([128, 3072], F32, "adaw") for _ in range(3)]
                pacc = [s.ps([128, 512], F32, "pada") for _ in range(6)]
                modt = s.sb([1, 6 * D], F32, "modt")
                adab = s.sb([1, 6 * D], F32, "adab")
                it = 0
                for l in range(layers):
                    s.dma("sp", adab.t[:], ada_b[l:l + 1, :], [], [adab])
                    for half in range(2):
                        for k in range(8):
                            w = wst[it % 3]; it += 1
                            s.dma("sp", w.t[:], ada_w[l, k * 128:(k + 1) * 128, half * 3072:(half + 1) * 3072], [], [w])
                            for n in range(6):
                                s.mm(pacc[n].t[:], condb.t[:, k, :], w.t[:, n * 512:(n + 1) * 512], k == 0, k == 7,
                                     [condb, w], [pacc[n]])
                        for n in range(6):
                            c0 = half * 3072 + n * 512
                            s.op("dve", lambda e, n=n, c0=c0: e.tensor_tensor(modt.t[0:1, c0:c0 + 512], pacc[n].t[0:1, :],
                                                                              adab.t[0:1, c0:c0 + 512], ALU.add),
                                 [pacc[n], adab], [modt])
                    s.dma("sp", mod_d[l:l + 1, :], modt.t[:], [modt], [b_mod])
            if n_odd > 0:
              with s.phase():
                    pi_ = s.sb([32, S], I32, "posi")
                    ang = s.sb([32, S], F32, "ang")
                    q = s.sb([32, S], F32, "q")
                    qi = s.sb([32, S], I32, "qi")
                    r = s.sb([32, S], F32, "r")
                    m = s.sb([32, S], F32, "m")
                    invf = s.sb([32, 1], F32, "invf")
                    s.dma("sp", pi_.t[:], pos_in, [], [pi_])
                    s.dma("sp", invf.t[:], invf_in, [], [invf])
                    s.op("dve", lambda e: e.tensor_copy(out=ang.t[:], in_=pi_.t[:]), [pi_], [ang])
                    s.op("dve", lambda e: e.tensor_scalar(ang.t[:], ang.t[:], invf.t[:, 0:1], None, ALU.mult), [ang, invf], [ang])
                    s.op("dve", lambda e: e.tensor_scalar(q.t[:], ang.t[:], 1.0 / (2 * math.pi), None, ALU.mult), [ang], [q])
                    s.op("dve", lambda e: e.tensor_copy(out=qi.t[:], in_=q.t[:]), [q], [qi])
                    s.op("dve", lambda e: e.tensor_copy(out=q.t[:], in_=qi.t[:]), [qi], [q])
                    s.op("dve", lambda e: e.scalar_tensor_tensor(r.t[:], q.t[:], -TWO_PI_HI, ang.t[:], ALU.mult, ALU.add), [q, ang], [r])
                    s.op("dve", lambda e: e.scalar_tensor_tensor(r.t[:], q.t[:], -TWO_PI_LO, r.t[:], ALU.mult, ALU.add), [q, r], [r])

                    def wrap(t):
                        s.op("dve", lambda e: e.tensor_scalar(m.t[:], t.t[:], math.pi, -2 * math.pi, ALU.is_gt, ALU.mult), [t], [m])
                        s.op("dve", lambda e: e.tensor_tensor(t.t[:], t.t[:], m.t[:], ALU.add), [t, m], [t])
                        s.op("dve", lambda e: e.tensor_scalar(m.t[:], t.t[:], -math.pi, 2 * math.pi, ALU.is_lt, ALU.mult), [t], [m])
                        s.op("dve", lambda e: e.tensor_tensor(t.t[:], t.t[:], m.t[:], ALU.add), [t, m], [t])
                        s.op("dve", lambda e: e.tensor_scalar(t.t[:], t.t[:], math.pi, -math.pi, ALU.min, ALU.max), [t], [t])

                    wrap(r)
                    sn = s.sb([32, S], F32, "sn")
                    s.op("act", lambda e: e.activation(out=sn.t[:], in_=r.t[:], func=AF.Sin), [r], [sn])
                    s.dma("sp", rope_d[1], sn.t[:], [sn], [b_rope])
                    s.op("dve", lambda e: e.tensor_scalar(r.t[:], r.t[:], math.pi / 2, None, ALU.add), [r], [r])
                    wrap(r)
                    cs = s.sb([32, S], F32, "cs")
                    s.op("act", lambda e: e.activation(out=cs.t[:], in_=r.t[:], func=AF.Sin), [r], [cs])
                    s.dma("sp", rope_d[0], cs.t[:], [cs], [b_rope])

        def load_mod(l, j, plus1, name, scale=None):
            t = s.sb([128, D], F32, name)
            s.dma("sp", t.t[:], bcast(mod_d[l:l + 1, j * D:(j + 1) * D]), [b_mod], [t])
            if plus1 and scale is not None:
                s.op("pool", lambda e: e.tensor_scalar(t.t[:], t.t[:], 1.0, float(scale), ALU.add, ALU.mult), [t], [t])
            elif plus1:
                s.op("pool", lambda e: e.tensor_scalar(t.t[:], t.t[:], 1.0, None, ALU.add), [t], [t])
            return t

        def load_row(src_row, n, name):
            t = s.sb([128, n], F32, name)
            s.dma("sp", t.t[:], bcast(src_row), [], [t])
            return t

        def transposes_store_uT(ub, i, ptr, uTs):
            for k in range(8):
                s.op("pe", lambda e, k=k: e.transpose(ptr.t[:, k, :], ub.t[:, k * 128:(k + 1) * 128], ident_b.t[:]),
                     [ub, ident_b], [ptr])
            s.op("act", lambda e: e.copy(out=uTs.t[:], in_=ptr.t[:]), [ptr], [uTs])
            s.dma("act", uT_d[:, :, i * 128:(i + 1) * 128].rearrange("k p t -> p k t"), uTs.t[:], [uTs], [b_uT[i]])

        def premod(l):
            with s.phase():
                sc1 = load_mod(l, 1, True, "sc1")
                sh = load_mod(l, 0, False, "sh")
                xt = [s.sb([128, D], F32, "xt") for _ in range(2)]
                ub = [s.sb([128, D], BF16, "ub") for _ in range(2)]
                ptr = [s.ps([128, 8, 128], BF16, "ptr") for _ in range(2)]
                uTs = [s.sb([128, 8, 128], BF16, "uTs") for _ in range(2)]
                for i in range(NT):
                    x = xt[i % 2]; u = ub[i % 2]
                    s.dma("sp", x.t[:], x_in[i * 128:(i + 1) * 128, :], [], [x])
                    s.op("dve", lambda e: e.tensor_tensor(x.t[:], x.t[:], sc1.t[:], ALU.mult), [x, sc1], [x])
                    s.op("dve", lambda e: e.tensor_tensor(u.t[:], x.t[:], sh.t[:], ALU.add), [x, sh], [u])
                    transposes_store_uT(u, i, ptr[i % 2], uTs[i % 2])

        def even_mixer(l):
            li = l // 2
            with s.phase():
                win = s.sb([128, 8, 2560], BF16, "win")
                wout = s.sb([128, 8, D], BF16, "wout")
                wv = ev_w_in[li].rearrange("(k p) f -> p k f", p=128)
                gpw = load_mod(l, 2, True, "gpw", 1.0 / ALPHA)
                wstg = [s.sb([128, D], F32, "wstg") for _ in range(2)]
                for k in range(8):
                    s.dma("pool", win.t[:, k, 0:1280], wv[:, k, 0:1280], [], [win], group="w")
                    s.dma("pool", win.t[:, k, 1280:2560], wv[:, k, 1280:2560], [], [win], group="w")
                for k in range(8):
                    s.dma("sp", wstg[k % 2].t[:], ev_w_out[li, k * 128:(k + 1) * 128, :], [], [wstg[k % 2]])
                    s.op("dve", lambda e, k=k: e.tensor_tensor(wout.t[:, k, :], wstg[k % 2].t[:], gpw.t[:], ALU.mult), [wstg[k % 2], gpw], [wout])
                sgf = s.sb([128, 8, 128], F32, "sgf")
                sgm = s.sb([128, 8, 128], BF16, "sgm")
                s.dma("sp", sgf.t[:], ev_sgT[li].rearrange("h j i -> j h i"), [], [sgf])
                for h in range(8):
                    s.op("pool", lambda e, h=h: e.affine_select(out=sgf.t[:, h, :], in_=sgf.t[:, h, :], pattern=[[1, 128]],
                                                                compare_op=ALU.is_ge, fill=0.0, base=0, channel_multiplier=-1),
                         [sgf], [sgf])
                s.op("pool", lambda e: e.tensor_copy(out=sgm.t[:], in_=sgf.t[:]), [sgf], [sgm])
                sgb = s.sb([128, 4, 128], F32, "sgb")
                for h in range(8):
                    s.dma("sp", sgb.t[(h % 2) * 64:(h % 2) * 64 + 64, h // 2, :], bcast(ev_sg_b[li, h:h + 1, :], 64), [], [sgb], group="w")
                cw = s.sb([128, 4, 3], F32, "cw")
                s.dma("sp", cw.t[:], ev_conv[li], [], [cw])
                vng = load_row(ev_vn_g[li:li + 1, :], 512, "vng")
                vnb = load_row(ev_vn_b[li:li + 1, :], 512, "vnb")
                halo = s.sb([128, 4, 2], F32, "halo")
                s.op("pool", lambda e: e.memset(halo.t[:], 0.0), [], [halo])

                uTg = [s.sb([128, 8, 512], BF16, "uTg") for _ in range(2)]
                pp = [s.ps([128, 512], F32, "pp") for _ in range(6)]
                psg = [s.ps([128, 128], F32, "psg") for _ in range(2)]
                cg = [s.sb([128, 512], F32, "cg") for _ in range(2)]
                cx = [s.sb([128, 514], F32, "cx") for _ in range(2)]
                acc = [s.sb([128, 512], F32, "acc") for _ in range(2)]
                yT = [s.sb([128, 8, 512], BF16, "yT") for _ in range(2)]
                zuT = [s.sb([128, 4, 512], F32, "zuT") for _ in range(2)]
                zv = [s.sb([128, 512], F32, "zv") for _ in range(2)]
                zvn = [s.sb([128, 512], BF16, "zvn") for _ in range(2)]
                st6 = [s.sb([128, 6], F32, "st6") for _ in range(2)]
                mv = [s.sb([128, 2], F32, "mv") for _ in range(2)]
                rstd = [s.sb([128, 1], F32, "rstd") for _ in range(2)]
                tmp = [s.sb([128, 128], F32, "tmp") for _ in range(2)]
                dl = [s.sb([128, D], F32, "dl") for _ in range(2)]
                ppi = 0
                for g in range(NG):
                    u = uTg[g % 2]; y = yT[g % 2]; zu = zuT[g % 2]
                    s.dma("sp", u.t[:], uT_d[:, :, g * 512:(g + 1) * 512].rearrange("k p t -> p k t"),
                          [b_uT[4 * g + j] for j in range(4)], [u])

                    def proj(f):
                        nonlocal ppi
                        p = pp[ppi % 6]; ppi += 1
                        for k in range(8):
                            s.mm(p.t[:], win.t[:, k, f * 128:(f + 1) * 128], u.t[:, k, :], k == 0, k == 7, [win, u], [p])
                        return p

                    for cc in range(4):
                        c_ = cg[cc % 2]; x_ = cx[cc % 2]; a_ = acc[cc % 2]
                        p_c = proj(4 + cc)
                        s.op("act", lambda e: e.copy(out=c_.t[:], in_=p_c.t[:]), [p_c], [c_])
                        p_x = proj(8 + cc)
                        s.op("pool", lambda e: e.tensor_copy(out=x_.t[:, 0:2], in_=halo.t[:, cc, :]), [halo], [x_])
                        s.op("dve", lambda e: e.tensor_tensor(x_.t[:, 2:514], p_x.t[:], c_.t[:], ALU.mult), [p_x, c_], [x_])
                        s.op("pool", lambda e: e.tensor_copy(out=halo.t[:, cc, :], in_=x_.t[:, 512:514]), [x_], [halo])
                        s.op("dve", lambda e: e.tensor_scalar(a_.t[:], x_.t[:, 0:512], cw.t[:, cc, 0:1], None, ALU.mult), [x_, cw], [a_])
                        s.op("dve", lambda e: e.scalar_tensor_tensor(a_.t[:], x_.t[:, 1:513], cw.t[:, cc, 1:2], a_.t[:], ALU.mult, ALU.add),
                             [x_, cw, a_], [a_])
                        s.op("dve", lambda e: e.scalar_tensor_tensor(a_.t[:], x_.t[:, 2:514], cw.t[:, cc, 2:3], a_.t[:], ALU.mult, ALU.add),
                             [x_, cw, a_], [a_])
                        p_b = proj(cc)
                        s.op("dve", lambda e: e.tensor_tensor(y.t[:, cc, :], p_b.t[:], a_.t[:], ALU.mult), [p_b, a_], [y])
                        p_u = proj(12 + cc)
                        s.op("act", lambda e: e.activation(out=zu.t[:, cc, :], in_=p_u.t[:], func=AF.Gelu), [p_u], [zu])
                    for tt in range(4):
                        i = 4 * g + tt
                        z = zv[tt % 2]; zn = zvn[tt % 2]; s6 = st6[tt % 2]; m_ = mv[tt % 2]; rs = rstd[tt % 2]
                        p = pp[ppi % 6]; ppi += 1
                        for k in range(8):
                            s.mm(p.t[:], u.t[:, k, tt * 128:(tt + 1) * 128], win.t[:, k, 2048:2560], k == 0, k == 7, [win, u], [p])
                        s.op("act", lambda e: e.activation(out=z.t[:], in_=p.t[:], func=AF.Gelu), [p], [z])
                        s.op("dve", lambda e: e.bn_stats(s6.t[:], z.t[:]), [z], [s6])
                        s.op("dve", lambda e: e.bn_aggr(m_.t[:], s6.t[:]), [s6], [m_])
                        s.op("act", lambda e: e.activation(out=rs.t[:], in_=m_.t[:, 1:2], func=AF.Sqrt, bias=eps_t.t[:, 0:1]), [m_, eps_t], [rs])
                        s.op("dve", lambda e: e.reciprocal(rs.t[:], rs.t[:]), [rs], [rs])
                        s.op("dve", lambda e: e.tensor_scalar(z.t[:], z.t[:], m_.t[:, 0:1], rs.t[:, 0:1], ALU.subtract, ALU.mult), [z, m_, rs], [z])
                        s.op("pool", lambda e: e.tensor_tensor(z.t[:], z.t[:], vng.t[:], ALU.mult), [z, vng], [z])
                        s.op("pool", lambda e: e.tensor_tensor(zn.t[:], z.t[:], vnb.t[:], ALU.add), [z, vnb], [zn])
                        for cc in range(4):
                            for hh in range(2):
                                h = 2 * cc + hh
                                pg = psg[(cc * 2 + hh) % 2]
                                t_ = tmp[(cc * 2 + hh) % 2]
                                s.mm(pg.t[:], zn.t[:, cc * 128:(cc + 1) * 128], sgm.t[:, h, :], True, True, [zn, sgm], [pg])
                                lo, hi = hh * 64, hh * 64 + 64
                                s.op("dve", lambda e: e.tensor_tensor(t_.t[lo:hi, :], pg.t[lo:hi, :], sgb.t[lo:hi, cc, :], ALU.add), [pg, sgb], [t_])
                                s.op("dve", lambda e: e.tensor_tensor(y.t[lo:hi, 4 + cc, tt * 128:(tt + 1) * 128], t_.t[lo:hi, :],
                                                                      zu.t[lo:hi, cc, tt * 128:(tt + 1) * 128], ALU.mult), [t_, zu], [y])
                        d_ = dl[tt % 2]
                        for nh in range(2):
                            p = pp[ppi % 6]; ppi += 1
                            for k in range(8):
                                s.mm(p.t[:], y.t[:, k, tt * 128:(tt + 1) * 128], wout.t[:, k, nh * 512:(nh + 1) * 512], k == 0, k == 7, [y, wout], [p])
                            s.op("act", lambda e: e.copy(out=d_.t[:, nh * 512:(nh + 1) * 512], in_=p.t[:]), [p], [d_])
                        s.dma("act", delta[i * 128:(i + 1) * 128, :], d_.t[:], [d_], [b_delta[i]])

        def odd_mixer(l):
            li = l // 2
            SCALE = 1.0 / math.sqrt(96.0)
            qT_d = od_scr["qT"]; kT_d = od_scr["kT"]; v_d = od_scr["v"]; yc_d = od_scr["yc"]; yd_d = od_scr["yd"]
            b_q = Buf(); b_k = Buf(); b_v = Buf(); b_yc = Buf(); b_yd = Buf()
            with s.phase():
                win = s.sb([128, 8, 1440], BF16, "win")
                wv_ = od_w_in[li].rearrange("(k p) f -> p k f", p=128)
                for k in range(8):
                    s.dma("pool", win.t[:, k, :], wv_[:, k, :], [], [win], group="w")
                winr = s.sb([128, 8, 32], F32, "winr")
                s.dma("sp", winr.t[:], wv_[:, :, 1408:1440], [], [winr])
                win_sw = s.sb([128, 8, 96], BF16, "winsw")
                s.op("pool", lambda e: e.memset(win_sw.t[:], 0.0), [], [win_sw])
                s.op("dve", lambda e: e.tensor_scalar(win_sw.t[:, :, 64:80], winr.t[:, :, 16:32], -1.0, None, ALU.mult), [winr, win_sw], [win_sw])
                s.op("dve", lambda e: e.tensor_copy(out=win_sw.t[:, :, 80:96], in_=winr.t[:, :, 0:16]), [winr, win_sw], [win_sw])
                wuq = s.sb([128, 2, 768], BF16, "wuq")
                wuqf = s.sb([128, 2, 768], F32, "wuqf")
                uqv = od_w_uq[li].rearrange("(k p) f -> p k f", p=128)
                s.dma("pool", wuq.t[:], uqv, [], [wuq])
                s.dma("sp", wuqf.t[:], uqv, [], [wuqf])
                wuq_sw = s.sb([128, 2, 8, 96], BF16, "wuqsw")
                s.op("pool", lambda e: e.memset(wuq_sw.t[:], 0.0), [], [wuq_sw])
                wuqf4 = wuqf.t[:].rearrange("p k (h e) -> p k h e", e=96)
                for kc in range(2):
                    s.op("dve", lambda e, kc=kc: e.tensor_scalar(wuq_sw.t[:, kc, :, 64:80], wuqf4[:, kc, :, 80:96], -1.0, None, ALU.mult), [wuqf, wuq_sw], [wuq_sw])
                    s.op("dve", lambda e, kc=kc: e.tensor_copy(out=wuq_sw.t[:, kc, :, 80:96], in_=wuqf4[:, kc, :, 64:80]), [wuqf, wuq_sw], [wuq_sw])
                wk = s.sb([128, 8, 64], BF16, "wk")
                wvv = s.sb([128, 8, 64], BF16, "wvv")
                ukv = od_w_ukv[li].rearrange("r (h e) -> r h e", e=128)
                s.dma("pool", wk.t[:], ukv[:, :, 0:64], [], [wk])
                s.dma("pool", wvv.t[:], ukv[:, :, 64:128], [], [wvv])
                cwd = s.sb([128, 4, 31], F32, "cwd")
                s.dma("sp", cwd.t[:], od_dw[li], [], [cwd])
                dwb = s.sb([128, 4], F32, "dwb"); cng = s.sb([128, 4], F32, "cng"); cnb = s.sb([128, 4], F32, "cnb")
                qng = s.sb([128, 2], F32, "qng"); kvng = s.sb([128, 1], F32, "kvng")
                s.dma("sp", dwb.t[:], od_dw_b[li], [], [dwb]); s.dma("sp", cng.t[:], od_cn_g[li], [], [cng])
                s.dma("sp", cnb.t[:], od_cn_b[li], [], [cnb]); s.dma("sp", qng.t[:], od_qn_g[li], [], [qng])
                s.dma("sp", kvng.t[:], od_kvn_g[li], [], [kvng])
                diag = s.sb([128, 4, 31, 128], BF16, "diag")
                for cc in range(4):
                    for k in range(31):
                        eng = "dve" if (cc * 31 + k) % 2 == 0 else "pool"
                        s.op(eng, lambda e, cc=cc, k=k: e.tensor_scalar(diag.t[:, cc, k, :], ident_f.t[:], cwd.t[:, cc, k:k + 1], None, ALU.mult),
                             [ident_f, cwd], [diag])
                rms_eps = s.sb([128, 1], F32, "rmseps")
                s.op("pool", lambda e: e.memset(rms_eps.t[:], RMS_EPS), [], [rms_eps])

                uTg = [s.sb([128, 8, 512], BF16, "uTg") for _ in range(2)]
                glu = [s.sb([128, 4, 542], BF16, "glu") for _ in range(2)]
                s.op("pool", lambda e: e.memset(glu[1].t[:, :, 512:542], 0.0), [], [glu[1]])
                sgm = [s.sb([128, 512], F32, "sgm") for _ in range(2)]
                hbuf = s.sb([128, 4, 512], F32, "hbuf")
                hsq = s.sb([128, 4, 512], F32, "hsq")
                mean = s.sb([128, 512], F32, "mean"); var = s.sb([128, 512], F32, "var"); rstd = s.sb([128, 512], F32, "rstd")
                tb = [s.sb([128, 512], F32, "tb") for _ in range(2)]
                ycT = [s.sb([128, 4, 512], BF16, "ycT") for _ in range(2)]
                sq = [s.sb([128, 512], F32, "sq") for _ in range(2)]
                rq = s.sb([128, 512], F32, "rq")
                cqn = s.sb([128, 2, 512], BF16, "cqn")
                ckvn = s.sb([128, 512], BF16, "ckvn")
                cs = s.sb([128, 2, 512], F32, "cs")
                t1 = [s.sb([128, 512], F32, "t1") for _ in range(2)]
                t2 = [s.sb([128, 512], F32, "t2") for _ in range(2)]
                krT = s.sb([128, 512], BF16, "krT")
                qT = [s.sb([128, 8, 512], BF16, "qT") for _ in range(2)]
                kT = [s.sb([128, 8, 512], BF16, "kT") for _ in range(2)]
                vt = [s.sb([128, 8, 65], BF16, "vt") for _ in range(2)]
                for v_ in vt:
                    s.op("pool", lambda e, v_=v_: e.memset(v_.t[:, :, 64:65], 1.0), [], [v_])
                pp = [s.ps([128, 512], F32, "pp") for _ in range(8)]
                ppi = 0
                for g in range(NG):
                    u = uTg[g % 2]; gl = glu[g % 2]; glp = glu[(g + 1) % 2]
                    tsl = slice(g * 512, (g + 1) * 512)
                    s.dma("sp", u.t[:], uT_d[:, :, tsl].rearrange("k p t -> p k t"), [b_uT[4 * g + j] for j in range(4)], [u])
                    s.dma("sp", cs.t[64:96, 0, :], rope_d[0, :, tsl], [b_rope], [cs], group=("cs", g))
                    s.dma("sp", cs.t[64:96, 1, :], rope_d[1, :, tsl], [b_rope], [cs], group=("cs", g))

                    def proj(c0, m, wt=win):
                        nonlocal ppi
                        p = pp[ppi % 8]; ppi += 1
                        for k in range(8):
                            s.mm(p.t[0:m, :], wt.t[:, k, c0:c0 + m], u.t[:, k, :], k == 0, k == 7, [wt, u], [p])
                        return p

                    s.op("pool", lambda e: e.tensor_copy(out=gl.t[:, :, 0:30], in_=glp.t[:, :, 512:542]), [glp], [gl])
                    for cc in range(4):
                        sg_ = sgm[cc % 2]
                        p_b = proj(512 + cc * 128, 128)
                        s.op("act", lambda e: e.activation(out=sg_.t[:], in_=p_b.t[:], func=AF.Sigmoid), [p_b], [sg_])
                        p_a = proj(cc * 128, 128)
                        s.op("dve", lambda e: e.tensor_tensor(gl.t[:, cc, 30:542], p_a.t[:], sg_.t[:], ALU.mult), [p_a, sg_], [gl])
                    for cc in range(4):
                        p = pp[ppi % 8]; ppi += 1
                        for k in range(31):
                            s.mm(p.t[:], diag.t[:, cc, k, :], gl.t[:, cc, k:k + 512], k == 0, k == 30, [diag, gl], [p])
                        s.op("act", lambda e: e.activation(out=hbuf.t[:, cc, :], in_=p.t[:], func=AF.Identity, bias=dwb.t[:, cc:cc + 1]), [p, dwb], [hbuf])
                        s.op("act", lambda e: e.activation(out=hsq.t[:, cc, :], in_=p.t[:], func=AF.Square, bias=dwb.t[:, cc:cc + 1]), [p, dwb], [hsq])
                    p1 = pp[ppi % 8]; ppi += 1
                    p2 = pp[ppi % 8]; ppi += 1
                    for cc in range(4):
                        s.mm(p1.t[:], ones_f.t[:], hbuf.t[:, cc, :], cc == 0, cc == 3, [ones_f, hbuf], [p1])
                    for cc in range(4):
                        s.mm(p2.t[:], ones_f.t[:], hsq.t[:, cc, :], cc == 0, cc == 3, [ones_f, hsq], [p2])
                    s.op("dve", lambda e: e.tensor_scalar(mean.t[:], p1.t[:], 1.0 / 512, None, ALU.mult), [p1], [mean])
                    s.op("pool", lambda e: e.tensor_tensor(var.t[:], mean.t[:], mean.t[:], ALU.mult), [mean], [var])
                    s.op("dve", lambda e: e.scalar_tensor_tensor(var.t[:], p2.t[:], 1.0 / 512, var.t[:], ALU.mult, ALU.subtract), [p2, var], [var])
                    s.op("act", lambda e: e.activation(out=rstd.t[:], in_=var.t[:], func=AF.Sqrt, bias=eps_t.t[:, 0:1]), [var, eps_t], [rstd])
                    s.op("dve", lambda e: e.reciprocal(rstd.t[:], rstd.t[:]), [rstd], [rstd])
                    yc = ycT[g % 2]
                    for cc in range(4):
                        t_ = tb[cc % 2]
                        s.op("dve", lambda e: e.tensor_tensor(t_.t[:], hbuf.t[:, cc, :], mean.t[:], ALU.subtract), [hbuf, mean], [t_])
                        s.op("pool", lambda e: e.tensor_tensor(t_.t[:], t_.t[:], rstd.t[:], ALU.mult), [t_, rstd], [t_])
                        s.op("dve", lambda e: e.tensor_scalar(t_.t[:], t_.t[:], cng.t[:, cc:cc + 1], cnb.t[:, cc:cc + 1], ALU.mult, ALU.add), [t_, cng, cnb], [t_])
                        s.op("act", lambda e: e.activation(out=yc.t[:, cc, :], in_=t_.t[:], func=AF.Silu), [t_], [yc])
                    s.dma("act", yc_d[:, :, tsl].rearrange("c p t -> p c t"), yc.t[:], [yc], [b_yc], group="st")
                    pq = [proj(1024, 128), proj(1152, 128)]
                    for c2 in range(2):
                        s.op("act", lambda e, c2=c2: e.activation(out=sq[c2].t[:], in_=pq[c2].t[:], func=AF.Square), [pq[c2]], [sq[c2]])
                    ps_ = pp[ppi % 8]; ppi += 1
                    for c2 in range(2):
                        s.mm(ps_.t[:], ones_f.t[:], sq[c2].t[:], c2 == 0, c2 == 1, [ones_f, sq[c2]], [ps_])
                    s.op("act", lambda e: e.activation(out=rq.t[:], in_=ps_.t[:], func=AF.Sqrt, bias=rms_eps.t[:, 0:1], scale=1.0 / 256), [ps_, rms_eps], [rq])
                    s.op("dve", lambda e: e.reciprocal(rq.t[:], rq.t[:]), [rq], [rq])
                    for c2 in range(2):
                        s.op("dve", lambda e, c2=c2: e.scalar_tensor_tensor(cqn.t[:, c2, :], pq[c2].t[:], qng.t[:, c2:c2 + 1], rq.t[:], ALU.mult, ALU.mult),
                             [pq[c2], qng, rq], [cqn])
                    pkv = proj(1280, 128)
                    s.op("act", lambda e: e.activation(out=sq[0].t[:], in_=pkv.t[:], func=AF.Square), [pkv], [sq[0]])
                    ps_ = pp[ppi % 8]; ppi += 1
                    s.mm(ps_.t[:], ones_f.t[:], sq[0].t[:], True, True, [ones_f, sq[0]], [ps_])
                    s.op("act", lambda e: e.activation(out=rq.t[:], in_=ps_.t[:], func=AF.Sqrt, bias=rms_eps.t[:, 0:1], scale=1.0 / 128), [ps_, rms_eps], [rq])
                    s.op("dve", lambda e: e.reciprocal(rq.t[:], rq.t[:]), [rq], [rq])
                    s.op("dve", lambda e: e.scalar_tensor_tensor(ckvn.t[:], pkv.t[:], kvng.t[:, 0:1], rq.t[:], ALU.mult, ALU.mult), [pkv, kvng, rq], [ckvn])
                    pk1 = proj(1344, 96)
                    pk2 = proj(0, 96, win_sw)
                    R = slice(64, 96)
                    s.op("dve", lambda e: e.tensor_tensor(t1[0].t[R, :], pk1.t[R, :], cs.t[R, 0, :], ALU.mult), [pk1, cs], [t1[0]])
                    s.op("dve", lambda e: e.tensor_tensor(t2[0].t[R, :], pk2.t[R, :], cs.t[R, 1, :], ALU.mult), [pk2, cs], [t2[0]])
                    s.op("pool", lambda e: e.tensor_tensor(krT.t[R, :], t1[0].t[R, :], t2[0].t[R, :], ALU.add), [t1[0], t2[0]], [krT])
                    q_ = qT[g % 2]; k_ = kT[g % 2]
                    for h in range(8):
                        a1 = t1[h % 2]; a2 = t2[h % 2]
                        pq1 = pp[ppi % 8]; ppi += 1
                        pq2 = pp[ppi % 8]; ppi += 1
                        for kc in range(2):
                            s.mm(pq1.t[0:96, :], wuq.t[:, kc, h * 96:(h + 1) * 96], cqn.t[:, kc, :], kc == 0, kc == 1, [wuq, cqn], [pq1])
                        for kc in range(2):
                            s.mm(pq2.t[0:96, :], wuq_sw.t[:, kc, h, :], cqn.t[:, kc, :], kc == 0, kc == 1, [wuq_sw, cqn], [pq2])
                        s.op("act", lambda e: e.copy(out=q_.t[0:64, h, :], in_=pq1.t[0:64, :]), [pq1], [q_])
                        s.op("dve", lambda e: e.tensor_tensor(a1.t[R, :], pq1.t[R, :], cs.t[R, 0, :], ALU.mult), [pq1, cs], [a1])
                        s.op("dve", lambda e: e.tensor_tensor(a2.t[R, :], pq2.t[R, :], cs.t[R, 1, :], ALU.mult), [pq2, cs], [a2])
                        s.op("pool", lambda e: e.tensor_tensor(q_.t[R, h, :], a1.t[R, :], a2.t[R, :], ALU.add), [a1, a2], [q_])
                        pk = pp[ppi % 8]; ppi += 1
                        s.mm(pk.t[0:64, :], wk.t[:, h, :], ckvn.t[:], True, True, [wk, ckvn], [pk])
                        s.op("act", lambda e: e.copy(out=k_.t[0:64, h, :], in_=pk.t[0:64, :]), [pk], [k_])
                        s.op("pool", lambda e: e.tensor_copy(out=k_.t[R, h, :], in_=krT.t[R, :]), [krT], [k_])
                    s.dma("sp", qT_d[:, :, tsl].rearrange("h p t -> p h t"), q_.t[0:96, :, :], [q_], [b_q], group="st")
                    s.dma("sp", kT_d[:, :, tsl].rearrange("h p t -> p h t"), k_.t[0:96, :, :], [k_], [b_k], group="st")
                    for tt in range(4):
                        i = 4 * g + tt
                        v_ = vt[tt % 2]
                        pv = pp[ppi % 8]; ppi += 1
                        s.mm(pv.t[:], ckvn.t[:, tt * 128:(tt + 1) * 128], wvv.t[:].rearrange("p h e -> p (h e)"), True, True, [ckvn, wvv], [pv])
                        s.op("act", lambda e: e.copy(out=v_.t[:, :, 0:64], in_=pv.t[:].rearrange("p (h e) -> p h e", e=64)), [pv], [v_])
                        s.dma("act", v_d[i * 128:(i + 1) * 128, :, :], v_.t[:], [v_], [b_v], group="st")
            with s.phase():
                masks = s.sb([128, 4, 512], BF16, "masks")
                s.op("pool", lambda e: e.memset(masks.t[:], 1.0), [], [masks])
                for j in range(4):
                    s.op("pool", lambda e, j=j: e.affine_select(out=masks.t[:, j, :], in_=masks.t[:, j, :], pattern=[[1, 512]], compare_op=ALU.is_ge,
                                                                fill=0.0, base=-128 * j, channel_multiplier=-1), [masks], [masks])
                kTh = [s.sb([128, S], BF16, "kTh") for _ in range(2)]
                vh = [s.sb([128, NT, 65], BF16, "vh") for _ in range(2)]
                qg = [s.sb([128, 512], BF16, "qg") for _ in range(2)]
                PT = [s.sb([128, 512], BF16, "PT") for _ in range(3)]
                psT = [s.ps([128, 512], F32, "psT") for _ in range(3)]
                pacc = [s.ps([128, 65], F32, "pacc") for _ in range(4)]
                rec = [s.sb([128, 1], F32, "rec") for _ in range(2)]
                yd = [s.sb([128, 64], BF16, "yd") for _ in range(2)]
                it = 0
                qg = [s.sb([128, 512], BF16, "qg3") for _ in range(3)]

                def load_head(h):
                    s.dma("sp", kTh[h % 2].t[0:96, :], kT_d[h], [b_k], [kTh[h % 2]])
                    vsrc = v_d[:, h, :].rearrange("(t p) e -> p t e", p=128)
                    for qq in range(4):
                        t0_, t1_ = qq * NT // 4, (qq + 1) * NT // 4
                        if t1_ > t0_:
                            s.dma("sp", vh[h % 2].t[:, t0_:t1_, :], vsrc[:, t0_:t1_, :], [b_v], [vh[h % 2]], group=("vh", h))

                def load_q(n):
                    h_, G_ = divmod(n, NG)
                    s.dma("sp", qg[n % 3].t[0:96, :], qT_d[h_, :, G_ * 512:(G_ + 1) * 512], [b_q], [qg[n % 3]])

                load_head(0)
                load_q(0)
                for h in range(8):
                    kt = kTh[h % 2]; vv = vh[h % 2]
                    if h + 1 < 8:
                        load_head(h + 1)
                    for G in range(NG):
                        n = h * NG + G
                        q_ = qg[n % 3]
                        if n + 1 < 8 * NG:
                            load_q(n + 1)
                        nkb = 4 * G + 4

                        def qk(kb):
                            nonlocal it
                            ps_ = psT[it % 3]; it += 1
                            s.mm(ps_.t[:], kt.t[0:96, kb * 128:(kb + 1) * 128], q_.t[0:96, :], True, True, [kt, q_], [ps_])
                            return ps_

                        pend = [qk(0)]
                        if nkb > 1:
                            pend.append(qk(1))
                        for kb in range(nkb):
                            ps_ = pend.pop(0)
                            if kb + 2 < nkb:
                                pend.append(qk(kb + 2))
                            pt = PT[kb % 3]
                            j = kb - 4 * G
                            c0 = max(j, 0) * 128
                            s.op("act", lambda e: e.activation(out=pt.t[:, c0:512], in_=ps_.t[:, c0:512], func=AF.Exp, scale=SCALE), [ps_], [pt])
                            if j >= 0:
                                s.op("dve", lambda e: e.tensor_tensor(pt.t[:, c0:c0 + 128], pt.t[:, c0:c0 + 128], masks.t[:, 0, 0:128], ALU.mult), [pt, masks], [pt])
                            for qs in range(4):
                                last_kb = 4 * G + qs
                                if kb > last_kb:
                                    continue
                                s.mm(pacc[qs].t[:], pt.t[:, qs * 128:(qs + 1) * 128], vv.t[:, kb, :], kb == 0, kb == last_kb, [pt, vv], [pacc[qs]])
                                if kb == last_kb:
                                    r_ = rec[qs % 2]; y_ = yd[qs % 2]
                                    i = 4 * G + qs
                                    s.op("dve", lambda e: e.reciprocal(r_.t[:], pacc[qs].t[:, 64:65]), [pacc[qs]], [r_])
                                    s.op("dve", lambda e: e.tensor_scalar(y_.t[:], pacc[qs].t[:, 0:64], r_.t[:, 0:1], None, ALU.mult), [pacc[qs], r_], [y_])
                                    s.dma("sp", yd_d[i * 128:(i + 1) * 128, h * 64:(h + 1) * 64], y_.t[:], [y_], [b_yd], group="st")
            with s.phase():
                wout = s.sb([128, 8, D], BF16, "wout")
                gpw = load_mod(l, 2, True, "gpw", 1.0 / ALPHA)
                wstg = [s.sb([128, D], F32, "wstg") for _ in range(2)]
                for k in range(8):
                    s.dma("sp", wstg[k % 2].t[:], od_w_out[li, k * 128:(k + 1) * 128, :], [], [wstg[k % 2]])
                    s.op("dve", lambda e, k=k: e.tensor_tensor(wout.t[:, k, :], wstg[k % 2].t[:], gpw.t[:], ALU.mult), [wstg[k % 2], gpw], [wout])
                yT = [s.sb([128, 8, 128], BF16, "yT") for _ in range(2)]
                ydt = [s.sb([128, 512], BF16, "ydt") for _ in range(2)]
                ptr = [s.ps([128, 4, 128], BF16, "ptr") for _ in range(2)]
                pp = [s.ps([128, 512], F32, "pp") for _ in range(4)]
                dl = [s.sb([128, D], F32, "dl") for _ in range(2)]
                for i in range(NT):
                    y = yT[i % 2]; yd_ = ydt[i % 2]; pt = ptr[i % 2]; d_ = dl[i % 2]
                    s.dma("sp", y.t[:, 0:4, :], yc_d[:, :, i * 128:(i + 1) * 128].rearrange("c p t -> p c t"), [b_yc], [y])
                    s.dma("sp", yd_.t[:], yd_d[i * 128:(i + 1) * 128, :], [b_yd], [yd_])
                    for c in range(4):
                        s.op("pe", lambda e, c=c: e.transpose(pt.t[:, c, :], yd_.t[:, c * 128:(c + 1) * 128], ident_b.t[:]), [yd_, ident_b], [pt])
                    s.op("act", lambda e: e.copy(out=y.t[:, 4:8, :], in_=pt.t[:]), [pt], [y])
                    for nh in range(2):
                        p = pp[(2 * i + nh) % 4]
                        for k in range(8):
                            s.mm(p.t[:], y.t[:, k, :], wout.t[:, k, nh * 512:(nh + 1) * 512], k == 0, k == 7, [y, wout], [p])
                        s.op("act", lambda e: e.copy(out=d_.t[:, nh * 512:(nh + 1) * 512], in_=p.t[:]), [p], [d_])
                    s.dma("act", delta[i * 128:(i + 1) * 128, :], d_.t[:], [d_], [b_delta[i]])

        def norm_pass(l, kind, x_src, last):
            with s.phase():
                mix = kind == "mix"
                need_u = not (last and not mix)
                if mix:
                    lng = load_row(ln_mix_g[l:l + 1, :], D, "lng"); lnb = load_row(ln_mix_b[l:l + 1, :], D, "lnb")
                    sc1 = load_mod(l, 4, True, "sc1"); sh = load_mod(l, 3, False, "sh")
                else:
                    lng = load_row(ln_ffn_g[l:l + 1, :], D, "lng"); lnb = load_row(ln_ffn_b[l:l + 1, :], D, "lnb")
                    if need_u:
                        sc1 = load_mod(l + 1, 1, True, "sc1"); sh = load_mod(l + 1, 0, False, "sh")
                if need_u:
                    B2 = sh
                    tmpb = s.sb([128, D], F32, "tmpb")
                    s.op("dve", lambda e: e.tensor_tensor(tmpb.t[:], lnb.t[:], sc1.t[:], ALU.mult), [lnb, sc1], [tmpb])
                    s.op("dve", lambda e: e.tensor_tensor(B2.t[:], tmpb.t[:], sh.t[:], ALU.add), [tmpb, sh], [B2])
                    G2 = sc1
                    s.op("dve", lambda e: e.tensor_tensor(G2.t[:], sc1.t[:], lng.t[:], ALU.mult), [sc1, lng], [G2])
                eps2 = s.sb([128, 1], F32, "eps2")
                s.op("pool", lambda e: e.memset(eps2.t[:], LN_EPS / (ALPHA * ALPHA)), [], [eps2])
                NB = 4
                xt = [s.sb([128, D], F32, "xt") for _ in range(NB)]
                st12 = [s.sb([128, 12], F32, "st12") for _ in range(NB)]
                mv = [s.sb([128, 2], F32, "mv") for _ in range(NB)]
                rstd = [s.sb([128, 1], F32, "rstd") for _ in range(NB)]
                nbias = [s.sb([128, 1], F32, "nbias") for _ in range(NB)]
                nt = [s.sb([128, D], F32, "nt") for _ in range(3)]
                xo = [s.sb([128, D], F32, "xo") for _ in range(3)]
                ub = [s.sb([128, D], BF16, "ub") for _ in range(3)]
                if mix:
                    yt = [s.sb([128, D], F32, "yt") for _ in range(NB)]
                    uf = [s.sb([128, D], F32, "uf") for _ in range(3)]
                    ptf = [s.ps([128, 4, 128], F32, "ptf") for _ in range(4)]
                    uTf = [s.sb([128, 8, 128], F32, "uTf") for _ in range(3)]
                    rw = s.sb([128, 8, NE], F32, "rw")
                    s.dma("sp", rw.t[:], r_w[l].rearrange("(k p) e -> p k e", p=128), [], [rw])
                    rb = load_row(r_b[l:l + 1, :], NE, "rb")
                    plog = [s.ps([128, NE], F32, "plog") for _ in range(2)]
                    prk = [s.ps([128, NE], F32, "prk") for _ in range(2)]
                    lg = [s.sb([128, NE], F32, "lg") for _ in range(3)]
                    t8 = [s.sb([128, 8], F32, "t8") for _ in range(2)]
                    nv0 = [s.sb([128, 1], F32, "nv0") for _ in range(2)]
                    ex4 = [s.sb([128, 4], F32, "ex4") for _ in range(2)]
                    sm = [s.sb([128, 1], F32, "sm") for _ in range(2)]
                    msk = [s.sb([128, NE], F32, "msk") for _ in range(2)]
                    msum = s.sb([128, NE], F32, "msum")
                    oh = s.sb([128, NT, 4, NE], F32, "oh")
                    prod = s.sb([128, 4, NE], F32, "prod")
                    s.op("pool", lambda e: e.memset(msum.t[:], 0.0), [], [msum])
                else:
                    yk = [s.sb([128, D], F32, "yk") for _ in range(12)]
                    if need_u:
                        ptr = [s.ps([128, 8, 128], BF16, "ptr") for _ in range(2)]
                        uTs = [s.sb([128, 8, 128], BF16, "uTs") for _ in range(2)]
                        uf = [s.sb([128, D], F32, "uf") for _ in range(3)]

                def stageL(i):
                    x = xt[i % NB]
                    s.dma("sp", x.t[:], x_src[i * 128:(i + 1) * 128, :], [b_xres[i]] if x_src is xres else [], [x])
                    if mix:
                        y = yt[i % NB]
                        s.dma("sp", y.t[:], delta[i * 128:(i + 1) * 128, :], [b_delta[i]], [y])
                    else:
                        ys_ = [yk[(i % 3) * 4 + k] for k in range(4)]
                        for k in range(4):
                            s.idma(ys_[k].t[:], None, ys_d, bass.IndirectOffsetOnAxis(ap=posk_i.t[:, i, k:k + 1], axis=0),
                                   [b_ys_all, posk_i], [ys_[k]])

                def stageA(i):
                    x = xt[i % NB]; s12 = st12[i % NB]; m_ = mv[i % NB]; rs = rstd[i % NB]; nb_ = nbias[i % NB]
                    if mix:
                        y = yt[i % NB]
                        s.op("dve", lambda e: e.tensor_tensor(x.t[:], x.t[:], y.t[:], ALU.add), [x, y], [x])
                    else:
                        ys_ = [yk[(i % 3) * 4 + k] for k in range(4)]
                        for k in range(4):
                            s.op("act", lambda e, k=k: e.activation(out=ys_[k].t[:], in_=ys_[k].t[:], func=AF.Copy, scale=gatek.t[:, i, k:k + 1]), [ys_[k], gatek], [ys_[k]])
                        s.op("dve", lambda e: e.tensor_tensor(ys_[0].t[:], ys_[0].t[:], ys_[1].t[:], ALU.add), [ys_[0], ys_[1]], [ys_[0]])
                        s.op("dve", lambda e: e.tensor_tensor(ys_[2].t[:], ys_[2].t[:], ys_[3].t[:], ALU.add), [ys_[2], ys_[3]], [ys_[2]])
                        s.op("dve", lambda e: e.tensor_tensor(ys_[0].t[:], ys_[0].t[:], ys_[2].t[:], ALU.add), [ys_[0], ys_[2]], [ys_[0]])
                        s.op("dve", lambda e: e.tensor_tensor(x.t[:], x.t[:], ys_[0].t[:], ALU.add), [x, ys_[0]], [x])
                    s.op("dve", lambda e: e.bn_stats(s12.t[:, 0:6], x.t[:, 0:512]), [x], [s12])
                    s.op("dve", lambda e: e.bn_stats(s12.t[:, 6:12], x.t[:, 512:1024]), [x], [s12])
                    s.op("dve", lambda e: e.bn_aggr(m_.t[:], s12.t[:]), [s12], [m_])
                    s.op("act", lambda e: e.activation(out=rs.t[:], in_=m_.t[:, 1:2], func=AF.Ln, bias=eps2.t[:, 0:1]), [m_, eps2], [rs])
                    s.op("act", lambda e: e.activation(out=rs.t[:], in_=rs.t[:], func=AF.Exp, scale=-0.5), [rs], [rs])
                    s.op("dve", lambda e: e.scalar_tensor_tensor(nb_.t[:], m_.t[:, 0:1], -1.0, rs.t[:], ALU.mult, ALU.mult), [m_, rs], [nb_])

                def stageB(i):
                    x = xt[i % NB]; rs = rstd[i % NB]; nb_ = nbias[i % NB]
                    n_ = nt[i % 3]; o_ = xo[i % 3]
                    s.op("act", lambda e: e.activation(out=n_.t[:], in_=x.t[:], func=AF.Identity, scale=rs.t[:, 0:1], bias=nb_.t[:, 0:1]), [x, rs, nb_], [n_])
                    s.op("dve", lambda e: e.tensor_tensor(o_.t[:], n_.t[:], lng.t[:], ALU.mult), [n_, lng], [o_])
                    s.op("dve", lambda e: e.tensor_tensor(o_.t[:], o_.t[:], lnb.t[:], ALU.add), [o_, lnb], [o_])
                    if not need_u:
                        s.dma("sp", out[i * 128:(i + 1) * 128, :], o_.t[:], [o_], [b_xres[i]])
                        return
                    s.dma("sp", xres[i * 128:(i + 1) * 128, :], o_.t[:], [o_], [b_xres[i]])
                    u = ub[i % 3]; f = uf[i % 3]
                    s.op("dve", lambda e: e.tensor_tensor(f.t[:], n_.t[:], G2.t[:], ALU.mult), [n_, G2], [f])
                    if not mix:
                        s.op("dve", lambda e: e.tensor_tensor(u.t[:], f.t[:], B2.t[:], ALU.add), [f, B2], [u])
                        transposes_store_uT(u, i, ptr[i % 2], uTs[i % 2])
                        return
                    s.op("dve", lambda e: e.tensor_tensor(f.t[:], f.t[:], B2.t[:], ALU.add), [f, B2], [f])
                    s.op("act", lambda e: e.copy(out=u.t[:], in_=f.t[:]), [f], [u])
                    s.dma("act", u_d[i * 128:(i + 1) * 128, :], u.t[:], [u], [b_u[i]])
                    uT = uTf[i % 3]
                    for hf in range(2):
                        pt = ptf[(2 * i + hf) % 4]
                        for kk in range(4):
                            k = hf * 4 + kk
                            s.op("pe", lambda e, k=k, kk=kk: e.transpose(pt.t[:, kk, :], f.t[:, k * 128:(k + 1) * 128], ident_f.t[:]),
                                 [f, ident_f], [pt])
                        s.op("act", lambda e: e.copy(out=uT.t[:, hf * 4:hf * 4 + 4, :], in_=pt.t[:]), [pt], [uT])
                    pl = plog[i % 2]
                    for k in range(8):
                        s.mm(pl.t[:], uT.t[:, k, :], rw.t[:, k, :], k == 0, k == 7, [uT, rw], [pl])

                def stageC(i):
                    lgt = lg[i % 3]; t8_ = t8[i % 2]; mk = msk[i % 2]; pl = plog[i % 2]
                    s.op("dve", lambda e: e.tensor_tensor(lgt.t[:], pl.t[:], rb.t[:], ALU.add), [pl, rb], [lgt])
                    s.op("dve", lambda e: e.max(out=t8_.t[:], in_=lgt.t[:]), [lgt], [t8_])
                    pr = prk[i % 2]
                    s.op("dve", lambda e: e.tensor_scalar(mk.t[:], lgt.t[:], t8_.t[:, 3:4], None, ALU.is_ge), [lgt, t8_], [mk])
                    s.mm(pr.t[:], su_f.t[:], mk.t[:], True, False, [su_f, mk], [pr])
                    s.mm(pr.t[:], ones_f.t[:], msum.t[:], False, True, [ones_f, msum], [pr])
                    for k in range(4):
                        s.op("dve", lambda e, k=k: e.tensor_scalar(oh.t[:, i, k, :], lgt.t[:], t8_.t[:, k:k + 1], None, ALU.is_equal), [lgt, t8_], [oh])
                    n0 = nv0[i % 2]; e4 = ex4[i % 2]; sm_ = sm[i % 2]
                    s.op("dve", lambda e: e.tensor_scalar(n0.t[:], t8_.t[:, 0:1], -1.0, None, ALU.mult), [t8_], [n0])
                    s.op("act", lambda e: e.activation(out=e4.t[:], in_=t8_.t[:, 0:4], func=AF.Exp, bias=n0.t[:, 0:1]), [t8_, n0], [e4])
                    for k in range(4):
                        s.op("dve", lambda e, k=k: e.tensor_tensor(prod.t[:, k, :], oh.t[:, i, k, :], pr.t[:], ALU.mult), [oh, pr], [prod])
                    s.op("dve", lambda e: e.tensor_reduce(out=posk_f.t[:, i, :], in_=prod.t[:], axis=AX.X, op=ALU.add), [prod], [posk_f])
                    s.op("dve", lambda e: e.tensor_tensor(msum.t[:], msum.t[:], mk.t[:], ALU.add), [msum, mk], [msum])
                    s.op("dve", lambda e: e.tensor_reduce(out=sm_.t[:], in_=e4.t[:], axis=AX.X, op=ALU.add), [e4], [sm_])
                    s.op("dve", lambda e: e.reciprocal(sm_.t[:], sm_.t[:]), [sm_], [sm_])
                    s.op("dve", lambda e: e.tensor_scalar(gatek.t[:, i, :], e4.t[:], sm_.t[:, 0:1], None, ALU.mult), [e4, sm_], [gatek])

                yk_sets = 3
                for step in range(NT + 3):
                    if step < NT:
                        stageL(step)
                    if 2 <= step <= NT + 1:
                        stageB(step - 2)
                    if 1 <= step <= NT:
                        stageA(step - 1)
                    if mix and 3 <= step:
                        stageC(step - 3)
                if kind == "mix":
                    pc = prk[0]
                    s.mm(pc.t[:], ones_f.t[:], msum.t[:], True, True, [ones_f, msum], [pc])
                    cnt = s.sb([128, NE], F32, "cnt")
                    nch = s.sb([128, NE], F32, "nch")
                    tmpc = s.sb([128, NE], F32, "tmpc")
                    cs_a = s.sb([128, NE], F32, "csa")
                    cs_b = s.sb([128, NE], F32, "csb")
                    s.op("dve", lambda e: e.tensor_copy(out=cnt.t[:], in_=pc.t[:]), [pc], [cnt])
                    s.op("dve", lambda e: e.tensor_scalar(nch.t[:], cnt.t[:], 0.5, None, ALU.is_gt), [cnt], [nch])
                    for j in range(1, (S + CH - 1) // CH):
                        s.op("dve", lambda e, j=j: e.tensor_scalar(tmpc.t[:], cnt.t[:], CH * j + 0.5, None, ALU.is_gt), [cnt], [tmpc])
                        s.op("dve", lambda e: e.tensor_tensor(nch.t[:], nch.t[:], tmpc.t[:], ALU.add), [nch, tmpc], [nch])
                    s.op("dve", lambda e: e.tensor_copy(out=cs_a.t[:], in_=nch.t[:]), [nch], [cs_a])
                    a, b = cs_a, cs_b
                    sh_ = 1
                    while sh_ < NE:
                        s.op("dve", lambda e, a=a, b=b, sh_=sh_: e.tensor_copy(out=b.t[:, 0:sh_], in_=a.t[:, 0:sh_]), [a], [b])
                        s.op("dve", lambda e, a=a, b=b, sh_=sh_: e.tensor_tensor(b.t[:, sh_:NE], a.t[:, sh_:NE], a.t[:, 0:NE - sh_], ALU.add), [a], [b])
                        a, b = b, a
                        sh_ *= 2
                    cend = a
                    base = s.sb([128, NE], F32, "base")
                    s.op("dve", lambda e: e.tensor_tensor(base.t[:], cend.t[:], nch.t[:], ALU.subtract), [cend, nch], [base])
                    s.op("dve", lambda e: e.tensor_scalar(base.t[:], base.t[:], float(CH), None, ALU.mult), [base], [base])
                    prod2 = s.sb([128, NT, 4, NE], F32, "prod2")
                    s.op("dve", lambda e: e.tensor_tensor(prod2.t[:].rearrange("p i k e -> p (i k) e"), oh.t[:].rearrange("p i k e -> p (i k) e"),
                                                          base.t[:].unsqueeze(1).to_broadcast([128, NT * 4, NE]), ALU.mult), [oh, base], [prod2])
                    bsel = s.sb([128, NT, 4], F32, "bsel")
                    s.op("dve", lambda e: e.tensor_reduce(out=bsel.t[:], in_=prod2.t[:], axis=AX.X, op=ALU.add), [prod2], [bsel])
                    s.op("dve", lambda e: e.tensor_tensor(posk_f.t[:], posk_f.t[:], bsel.t[:], ALU.add), [posk_f, bsel], [posk_f])
                    s.op("dve", lambda e: e.tensor_copy(out=posk_i.t[:], in_=posk_f.t[:]), [posk_f], [posk_i])
                    cmp3 = s.sb([128, NCH, NE], F32, "cmp3")
                    cef = s.sb([128, NCH], F32, "cef")
                    wif = s.sb([128, NCH, 8], F32, "wif")
                    tf = s.sb([128, NCH], F32, "tf")
                    s.op("dve", lambda e: e.tensor_tensor(cmp3.t[:], cend.t[:].unsqueeze(1).to_broadcast([128, NCH, NE]),
                                                          iota_c.t[:].unsqueeze(2).to_broadcast([128, NCH, NE]), ALU.is_le), [cend, iota_c], [cmp3])
                    s.op("dve", lambda e: e.tensor_reduce(out=cef.t[:], in_=cmp3.t[:], axis=AX.X, op=ALU.add), [cmp3], [cef])
                    s.op("dve", lambda e: e.tensor_scalar(cef.t[:], cef.t[:], float(NE - 1), float(l * NE), ALU.min, ALU.add), [cef], [cef])
                    s.op("dve", lambda e: e.tensor_copy(out=bdidx.t[:], in_=cef.t[:]), [cef], [bdidx])
                    s.op("dve", lambda e: e.tensor_scalar(tf.t[:], cef.t[:], 128.0, iota_p.t[:, 0:1], ALU.mult, ALU.add), [cef, iota_p], [tf])
                    s.op("dve", lambda e: e.tensor_copy(out=bgidx.t[:], in_=tf.t[:]), [tf], [bgidx])
                    s.op("dve", lambda e: e.tensor_scalar(tf.t[:], cef.t[:], 1024.0, None, ALU.mult), [cef], [tf])
                    s.op("dve", lambda e: e.tensor_tensor(wif.t[:], tf.t[:].unsqueeze(2).to_broadcast([128, NCH, 8]),
                                                          base_pk.t[:].unsqueeze(1).to_broadcast([128, NCH, 8]), ALU.add), [tf, base_pk], [wif])
                    s.op("dve", lambda e: e.tensor_copy(out=widx.t[:], in_=wif.t[:]), [wif], [widx])
                    if "posk" in dbg_out:
                        s.dma("sp", dbg_out["posk"].rearrange("(i p) k -> p i k", p=128), posk_f.t[:], [posk_f], [])
                        s.dma("sp", dbg_out["gatek"].rearrange("(i p) k -> p i k", p=128), gatek.t[:], [gatek], [])
                        s.dma("sp", dbg_out["ctab"], cef.t[:, 0:NCH], [cef], [])

        def scatter_pass():
            with s.phase():
                ub = [s.sb([128, D], BF16, "ub") for _ in range(3)]
                for i in range(NT):
                    u = ub[i % 3]
                    s.dma("sp", u.t[:], u_d[i * 128:(i + 1) * 128, :], [b_u[i]], [u])
                    for k in range(4):
                        s.idma(xs_d, bass.IndirectOffsetOnAxis(ap=posk_i.t[:, i, k:k + 1], axis=0), u.t[:], None,
                               [u, posk_i], [b_xs_all], group="sc")

        def expert_pass(l):
            with s.phase():
                wgu = [s.sb([128, 8, 2 * D], BF16, "wgu") for _ in range(3)]
                wdn = [s.sb([128, 8, D], BF16, "wdn") for _ in range(2)]
                bgu = [s.sb([128, 16], F32, "bgu") for _ in range(3)]
                bdn = [s.sb([128, D], BF16, "bdn") for _ in range(2)]
                SBK = CH // 128
                xsb = [[s.sb([128, D], BF16, "xsb") for _ in range(SBK)] for _ in range(2)]
                xT = [s.sb([128, 8, CH], BF16, "xT") for _ in range(2)]
                ptr = [s.ps([128, 8, 128], BF16, "ptr") for _ in range(2)]
                pg = [s.ps([128, 512], F32, "pg") for _ in range(4)]
                pd = [s.ps([128, 512], F32, "pd") for _ in range(2)]
                gc = [s.sb([128, CH], F32, "gc") for _ in range(2)]
                sg = [s.sb([128, CH], F32, "sg") for _ in range(2)]
                uc = [s.sb([128, CH], F32, "uc") for _ in range(2)]
                actT = [s.sb([128, 8, CH], BF16, "actT") for _ in range(2)]
                ysb = [s.sb([128, D], F32, "ysb") for _ in range(2)]
                gpf = load_mod(l, 5, True, "gpf", 1.0 / (ALPHA * 1.702))
                wguv = w_gu.rearrange("l e r f -> (l e r) f")
                wdnv = w_dn.rearrange("l e r f -> (l e r) f")
                bguv = b_guT.rearrange("l e p c -> (l e p) c")
                bdnv = b_dn.rearrange("l e f -> (l e) f")
                IO = bass.IndirectOffsetOnAxis

                def pre_g(c):
                    wg = wgu[c % 3]; bg = bgu[c % 3]
                    for k in range(8):
                        s.idma(wg.t[:, k, :], None, wguv, IO(ap=widx.t[:, c, k:k + 1], axis=0), [widx], [wg], group=("wg", c))
                    s.idma(bg.t[:], None, bguv, IO(ap=bgidx.t[:, c:c + 1], axis=0), [bgidx], [bg])
                    s.op("dve", lambda e: e.tensor_scalar(bg.t[:, 8:16], bg.t[:, 8:16], 1.0, None, ALU.add), [bg], [bg])

                def pre_d(c):
                    wd = wdn[c % 2]; bd = bdn[c % 2]
                    for k in range(8):
                        s.idma(wd.t[:, k, :], None, wdnv, IO(ap=widx.t[:, c, k:k + 1], axis=0), [widx], [wd], group=("wd", c))
                    s.idma(bd.t[:], None, bdnv, IO(ap=bdidx.t[:, c:c + 1], axis=0), [bdidx], [bd])
                    for sb_ in range(SBK):
                        xb = xsb[c % 2][sb_]
                        r0 = c * CH + sb_ * 128
                        s.dma("sp", xb.t[:], xs_d[r0:r0 + 128, :], [b_xs_all], [xb])

                def tr(c):
                    xt_ = xT[c % 2]
                    for sb_ in range(SBK):
                        xb = xsb[c % 2][sb_]; pt = ptr[sb_ % 2]
                        for k in range(8):
                            s.op("pe", lambda e, k=k: e.transpose(pt.t[:, k, :], xb.t[:, k * 128:(k + 1) * 128], ident_b.t[:]), [xb, ident_b], [pt])
                        s.op("act", lambda e: e.copy(out=xt_.t[:, :, sb_ * 128:(sb_ + 1) * 128], in_=pt.t[:]), [pt], [xt_])

                def gu(c):
                    wg = wgu[c % 3]; bg = bgu[c % 3]
                    xt_ = xT[c % 2]; at = actT[c % 2]
                    for j in range(8):
                        p_g = pg[(2 * j) % 4]; p_u = pg[(2 * j + 1) % 4]
                        for k in range(8):
                            s.mm(p_g.t[:, 0:CH], wg.t[:, k, j * 128:(j + 1) * 128], xt_.t[:, k, :], k == 0, k == 7, [wg, xt_], [p_g])
                        for k in range(8):
                            s.mm(p_u.t[:, 0:CH], wg.t[:, k, D + j * 128:D + (j + 1) * 128], xt_.t[:, k, :], k == 0, k == 7, [wg, xt_], [p_u])
                        g_ = gc[j % 2]; s_ = sg[j % 2]; u_ = uc[j % 2]
                        s.op("dve", lambda e: e.tensor_scalar(g_.t[:], p_g.t[:, 0:CH], bg.t[:, j:j + 1], 7.0, ALU.add, ALU.min), [p_g, bg], [g_])
                        s.op("act", lambda e: e.activation(out=s_.t[:], in_=g_.t[:], func=AF.Silu, scale=1.702), [g_], [s_])
                        s.op("dve", lambda e: e.tensor_scalar(u_.t[:], p_u.t[:, 0:CH], bg.t[:, 8 + j:9 + j], 8.0, ALU.add, ALU.min), [p_u, bg], [u_])
                        s.op("dve", lambda e: e.scalar_tensor_tensor(at.t[:, j, :], u_.t[:], -6.0, s_.t[:], ALU.max, ALU.mult), [s_, u_], [at])

                def dn(c):
                    wd = wdn[c % 2]; bd = bdn[c % 2]; at = actT[c % 2]
                    for sb_ in range(SBK):
                        yb = ysb[sb_ % 2]
                        for nh in range(2):
                            p = pd[nh]
                            for j in range(8):
                                s.mm(p.t[:], at.t[:, j, sb_ * 128:(sb_ + 1) * 128], wd.t[:, j, nh * 512:(nh + 1) * 512], j == 0, False, [at, wd], [p])
                            s.mm(p.t[:], c1702.t[0:1, :], bd.t[0:1, nh * 512:(nh + 1) * 512], False, True, [c1702, bd], [p])
                            s.op("dve", lambda e: e.tensor_tensor(yb.t[:, nh * 512:(nh + 1) * 512], p.t[:], gpf.t[:, nh * 512:(nh + 1) * 512], ALU.mult), [p, gpf], [yb])
                        r0 = c * CH + sb_ * 128
                        s.dma("sp", ys_d[r0:r0 + 128, :], yb.t[:], [yb], [b_ys_all], group=("ys", l))

                pre_g(0)
                pre_d(0)
                if NCH > 1:
                    pre_g(1)
                tr(0)
                for c in range(NCH):
                    if c + 2 < NCH:
                        pre_g(c + 2)
                    if c + 1 < NCH:
                        pre_d(c + 1)
                    gu(c)
                    if c + 1 < NCH:
                        tr(c + 1)
                    dn(c)

        eps_t = s.sb([128, 1], F32, "eps")
        s.op("pool", lambda e: e.memset(eps_t.t[:], LN_EPS), [], [eps_t])

        prologue()
        premod(0)
        x_src = x_in
        stop_after = (dbg or {}).get("_stop", None)
        for l in range(layers):
            if l % 2 == 0:
                even_mixer(l)
            else:
                odd_mixer(l)
            norm_pass(l, "mix", x_src, False)
            x_src = xres
            scatter_pass()
            expert_pass(l)
            norm_pass(l, "ffn", x_src, l == layers - 1)
        s.barrier()
    return nc


def prep_inputs(inputs, S=4096, ncores=8, layers=DEPTH):
    f = lambda a: np.ascontiguousarray(np.asarray(a))
    shared = {}
    for k in ("ada_w", "ada_b", "ln_mix_g", "ln_mix_b", "ln_ffn_g", "ln_ffn_b", "ev_w_in", "ev_sg_b", "ev_vn_g", "ev_vn_b",
              "ev_w_out", "od_w_in", "od_w_uq", "od_w_ukv", "od_w_out", "moe_router_w", "moe_router_b", "moe_w_gu",
              "moe_w_dn", "moe_b_dn"):
        shared[k] = f(inputs[k][:layers]) if k in ("moe_w_gu", "moe_w_dn", "ada_w") else f(inputs[k])
    shared["ev_conv"] = f(np.asarray(inputs["ev_conv_w"]).transpose(0, 2, 1).reshape(2, 4, 128, 3).transpose(0, 2, 1, 3))
    shared["ev_sgT"] = f(np.asarray(inputs["ev_sg_w"]).transpose(0, 1, 3, 2))
    shared["od_dw"] = f(np.asarray(inputs["od_dw_w"]).transpose(0, 2, 1).reshape(2, 4, 128, 31).transpose(0, 2, 1, 3))
    col = lambda a, n: f(np.asarray(a).reshape(2, n, 128).transpose(0, 2, 1))
    shared["od_dw_b"] = col(inputs["od_dw_b"], 4)
    shared["od_cn_g"] = col(inputs["od_cn_g"], 4)
    shared["od_cn_b"] = col(inputs["od_cn_b"], 4)
    shared["od_qn_g"] = col(inputs["od_qn_g"], 2)
    shared["od_kvn_g"] = col(inputs["od_kvn_g"], 1)
    shared["moe_b_guT"] = f(np.asarray(inputs["moe_b_gu"]).reshape(DEPTH, NE, 16, 128).transpose(0, 1, 3, 2))
    invf = (10000.0 ** (-np.arange(0, 32, 2, dtype=np.float32) / 32)).astype(np.float32)
    shared["invf"] = f(np.concatenate([invf, invf]).reshape(32, 1))
    maps = []
    x = np.asarray(inputs["x"]); c = np.asarray(inputs["c"]); pos = np.asarray(inputs["positions"])
    for b in range(ncores):
        m = dict(shared)
        m["x"] = f(x[b, :S])
        m["c"] = f(c[b].reshape(8, 128).T)
        m["pos"] = f(np.broadcast_to(pos[b, :S].astype(np.int32)[None, :], (32, S)))
        maps.append(m)
    return maps


_NC_CACHE = {}


def kernel(**inputs):
    S = 4096
    if "nc" not in _NC_CACHE:
        _NC_CACHE["nc"] = build(S)
    nc = _NC_CACHE["nc"]
    maps = prep_inputs(inputs, S, 8)
    res = run_bass_kernel_spmd(nc, maps, core_ids=list(range(8)))
    return np.stack([np.asarray(r["out"]) for r in res.results], axis=0).astype(np.float32)
```

```python
import math
import numpy as np
from contextlib import ExitStack
import concourse.bass as bass
import concourse.mybir as mybir
from concourse.bass_utils import run_bass_kernel_spmd

F32 = mybir.dt.float32
BF16 = mybir.dt.bfloat16
I32 = mybir.dt.int32
AF = mybir.ActivationFunctionType
ALU = mybir.AluOpType
AX = mybir.AxisListType

D = 1024
DEPTH = 4
NE = 32
TOPK = 4
CH = 384
ALPHA = (2.0 * DEPTH) ** 0.25
LN_EPS = 1e-5
RMS_EPS = 1e-6
TWO_PI_HI = 6.28125
TWO_PI_LO = 2.0 * math.pi - 6.28125


class Buf:
    __slots__ = ("w", "r", "g")

    def __init__(self):
        self.w = []
        self.r = []
        self.g = None


class T:
    __slots__ = ("t", "b")

    def __init__(self, t):
        self.t = t
        self.b = Buf()


class Sched:
    ENG = ("pe", "act", "dve", "pool", "sp")

    def __init__(self, nc, es, ndma=12):
        self.nc = nc
        self.es = es
        self.eng = {"pe": nc.tensor, "act": nc.scalar, "dve": nc.vector, "pool": nc.gpsimd, "sp": nc.sync}
        self.sem = {}
        self.cnt = {}
        self.seen = {e: {} for e in self.ENG}
        for e in self.ENG:
            self.sem[e] = es.enter_context(nc.semaphore("s_" + e))
            self.cnt[e] = 0
        self.dq = {}
        for q in ("sp", "pool", "act"):
            lst = []
            for i in range(ndma if q != "act" else 8):
                key = "d_%s%d" % (q, i)
                self.sem[key] = es.enter_context(nc.semaphore(key))
                self.cnt[key] = 0
                lst.append(key)
            self.dq[q] = [lst, 0]
        self.uid = 0

    def sb(self, shape, dt, name=None):
        self.uid += 1
        return T(self.es_cur.enter_context(self.nc.sbuf_tensor("%s_%d" % (name or "t", self.uid), list(shape), dt)))

    def ps(self, shape, dt, name=None):
        self.uid += 1
        return T(self.es_cur.enter_context(self.nc.psum_tensor("%s_%d" % (name or "p", self.uid), list(shape), dt)))

    def _wait(self, e, evs):
        seen = self.seen[e]
        for key, val in evs:
            if key == "pe" and e == "pe":
                continue
            if seen.get(key, 0) >= val:
                continue
            self.eng[e].wait_ge(self.sem[key], val)
            seen[key] = val

    @staticmethod
    def _deps(reads, writes, group=None):
        evs = []
        for b in reads:
            b = b.b if isinstance(b, T) else b
            evs.extend(b.w)
        for b in writes:
            b = b.b if isinstance(b, T) else b
            if group is None or b.g != group:
                evs.extend(b.w)
            evs.extend(b.r)
        return evs

    @staticmethod
    def _update(ev, reads, writes, group=None):
        for b in reads:
            b = b.b if isinstance(b, T) else b
            b.r.append(ev)
            if len(b.r) > 24:
                last = {}
                for k, v in b.r:
                    if last.get(k, 0) < v:
                        last[k] = v
                b.r = list(last.items())
        for b in writes:
            b = b.b if isinstance(b, T) else b
            if group is not None and b.g == group:
                b.w.append(ev)
            else:
                b.w = [ev]
                b.g = group
            b.r = []

    def op(self, e, fn, reads=(), writes=()):
        self._wait(e, self._deps(reads, writes))
        ins = fn(self.eng[e])
        self.cnt[e] += 1
        ins.then_inc(self.sem[e], 1)
        self.seen[e][e] = max(self.seen[e].get(e, 0), 0)
        self._update((e, self.cnt[e]), reads, writes)

    def mm(self, out, lhsT, rhs, start, stop, reads, writes, **kw):
        self.op("pe", lambda pe: pe.matmul(out, lhsT=lhsT, rhs=rhs, start=start, stop=stop, **kw), reads, writes)

    def dma(self, q, out, in_, reads=(), writes=(), group=None, **kw):
        lst, idx = self.dq[q]
        key = lst[idx % len(lst)]
        self.dq[q][1] = idx + 1
        evs = self._deps(reads, writes, group)
        if self.cnt[key] > 0:
            evs.append((key, self.cnt[key]))
        self._wait(q, evs)
        ins = self.eng[q].dma_start(out=out, in_=in_, **kw)
        self.cnt[key] += 16
        ins.then_inc(self.sem[key], 16)
        self._update((key, self.cnt[key]), reads, writes, group)

    def idma(self, out, out_off, in_, in_off, reads=(), writes=(), group=None):
        q = "pool"
        lst, idx = self.dq[q]
        key = lst[idx % len(lst)]
        self.dq[q][1] = idx + 1
        evs = self._deps(reads, writes, group)
        if self.cnt[key] > 0:
            evs.append((key, self.cnt[key]))
        self._wait(q, evs)
        ins = self.eng[q].indirect_dma_start(out=out, out_offset=out_off, in_=in_, in_offset=in_off)
        self.cnt[key] += 16
        ins.then_inc(self.sem[key], 16)
        self._update((key, self.cnt[key]), reads, writes, group)

    def barrier(self):
        evs = [(k, v) for k, v in self.cnt.items() if v > 0]
        for e in self.ENG:
            self._wait(e, evs)

    def phase(self):
        return _Phase(self)


class _Phase:
    def __init__(self, s):
        self.s = s

    def __enter__(self):
        self.es = ExitStack()
        self.es.__enter__()
        self.s.es_cur = self.es
        return self.s

    def __exit__(self, *a):
        self.s.barrier()
        self.es.__exit__(*a)
        return False


def bcast(ap, n=128):
    return ap.partition_broadcast(n)


def build(S=4096, layers=DEPTH, dbg=None):
    NT = S // 128
    NG = S // 512
    NCH = (S * TOPK) // CH + NE
    NSLOT = NCH * CH
    nc = bass.Bass("TRN2", target_bir_lowering=False)

    def din(name, shape, dt=F32):
        return nc.dram_tensor(name, list(shape), dt, kind="ExternalInput").ap()

    def dscr(name, shape, dt=F32):
        return nc.dram_tensor(name, list(shape), dt, kind="Internal").ap()

    n_even = (layers + 1) // 2
    n_odd = layers // 2
    x_in = din("x", [S, D])
    c_in = din("c", [128, 8])
    pos_in = din("pos", [32, S], I32)
    invf_in = din("invf", [32, 1])
    ada_w = din("ada_w", [layers, D, 6 * D])
    ada_b = din("ada_b", [DEPTH, 6 * D])
    ln_mix_g = din("ln_mix_g", [DEPTH, D]); ln_mix_b = din("ln_mix_b", [DEPTH, D])
    ln_ffn_g = din("ln_ffn_g", [DEPTH, D]); ln_ffn_b = din("ln_ffn_b", [DEPTH, D])
    ev_w_in = din("ev_w_in", [2, D, 2560])
    ev_conv = din("ev_conv", [2, 128, 4, 3])
    ev_sgT = din("ev_sgT", [2, 8, 128, 128])
    ev_sg_b = din("ev_sg_b", [2, 8, 128])
    ev_vn_g = din("ev_vn_g", [2, 512]); ev_vn_b = din("ev_vn_b", [2, 512])
    ev_w_out = din("ev_w_out", [2, D, D])
    od_w_in = din("od_w_in", [2, D, 1440])
    od_dw = din("od_dw", [2, 128, 4, 31])
    od_dw_b = din("od_dw_b", [2, 128, 4])
    od_cn_g = din("od_cn_g", [2, 128, 4]); od_cn_b = din("od_cn_b", [2, 128, 4])
    od_qn_g = din("od_qn_g", [2, 128, 2])
    od_w_uq = din("od_w_uq", [2, 256, 768])
    od_kvn_g = din("od_kvn_g", [2, 128, 1])
    od_w_ukv = din("od_w_ukv", [2, 128, 1024])
    od_w_out = din("od_w_out", [2, D, D])
    r_w = din("moe_router_w", [DEPTH, D, NE])
    r_b = din("moe_router_b", [DEPTH, NE])
    w_gu = din("moe_w_gu", [layers, NE, D, 2 * D])
    b_guT = din("moe_b_guT", [DEPTH, NE, 128, 16])
    w_dn = din("moe_w_dn", [layers, NE, D, D])
    b_dn = din("moe_b_dn", [DEPTH, NE, D])
    out = nc.dram_tensor("out", [S, D], F32, kind="ExternalOutput").ap()

    xres = dscr("xres", [S, D])
    delta = dscr("delta", [S, D])
    uT_d = dscr("uT_d", [8, 128, S], BF16)
    u_d = dscr("u_d", [S, D], BF16)
    xs_d = dscr("xs_d", [NSLOT, D], BF16)
    ys_d = dscr("ys_d", [NSLOT, D])
    mod_d = dscr("mod_d", [DEPTH, 6 * D])
    rope_d = dscr("rope_d", [2, 32, S])
    od_scr = {}
    if layers > 1:
        od_scr = {"qT": dscr("qT_d", [8, 96, S], BF16), "kT": dscr("kT_d", [8, 96, S], BF16), "v": dscr("v_d", [S, 8, 65], BF16),
                  "yc": dscr("yc_d", [4, 128, S], BF16), "yd": dscr("yd_d", [S, 512], BF16)}
    dbg_out = {}
    if dbg:
        for name, shape in dbg.items():
            dbg_out[name] = nc.dram_tensor("dbg_" + name, list(shape), F32, kind="ExternalOutput").ap()

    tokb = lambda: [Buf() for _ in range(NT)]
    b_xres = tokb(); b_delta = tokb(); b_uT = tokb(); b_u = tokb()
    b_xs = [Buf() for _ in range(NCH)]; b_ys = [Buf() for _ in range(NCH)]
    b_xs_all = Buf(); b_ys_all = Buf()
    b_mod = Buf(); b_rope = Buf()

    with ExitStack() as es_top:
        s = Sched(nc, es_top)
        s.es_cur = es_top
        ident_b = s.sb([128, 128], BF16, "identb")
        ident_f = s.sb([128, 128], F32, "identf")
        ones_f = s.sb([128, 128], F32, "onesf")
        ones_b = s.sb([128, 128], BF16, "onesb")
        c1702 = s.sb([1, 128], BF16, "c1702")
        su_f = s.sb([128, 128], F32, "suf")
        iota_p = s.sb([128, 1], F32, "iotap")
        posk_f = s.sb([128, NT, 4], F32, "poskf")
        posk_i = s.sb([128, NT, 4], I32, "poski")
        gatek = s.sb([128, NT, 4], F32, "gatek")
        widx = s.sb([128, NCH, 8], I32, "widx")
        bgidx = s.sb([128, NCH], I32, "bgidx")
        bdidx = s.sb([128, NCH], I32, "bdidx")
        base_pk = s.sb([128, 8], F32, "basepk")
        iota_c = s.sb([128, NCH], F32, "iotac")

        def mk_consts():
            s.op("pool", lambda e: e.memset(ident_b.t[:], 0.0), [], [ident_b])
            s.op("pool", lambda e: e.affine_select(out=ident_b.t[:], in_=ident_b.t[:], pattern=[[-1, 128]],
                                                   compare_op=ALU.not_equal, fill=1.0, base=0, channel_multiplier=1),
                 [ident_b], [ident_b])
            s.op("pool", lambda e: e.memset(ident_f.t[:], 0.0), [], [ident_f])
            s.op("pool", lambda e: e.affine_select(out=ident_f.t[:], in_=ident_f.t[:], pattern=[[-1, 128]],
                                                   compare_op=ALU.not_equal, fill=1.0, base=0, channel_multiplier=1),
                 [ident_f], [ident_f])
            s.op("pool", lambda e: e.memset(ones_f.t[:], 1.0), [], [ones_f])
            s.op("pool", lambda e: e.memset(ones_b.t[:], 1.0), [], [ones_b])
            s.op("pool", lambda e: e.memset(c1702.t[:], 1.702), [], [c1702])
            s.op("pool", lambda e: e.memset(su_f.t[:], 1.0), [], [su_f])
            s.op("pool", lambda e: e.affine_select(out=su_f.t[:], in_=su_f.t[:], pattern=[[1, 128]],
                                                   compare_op=ALU.is_gt, fill=0.0, base=0, channel_multiplier=-1),
                 [su_f], [su_f])
            s.op("pool", lambda e: e.iota(iota_p.t[:], pattern=[[0, 1]], base=0, channel_multiplier=1,
                                          allow_small_or_imprecise_dtypes=True), [], [iota_p])
            s.op("pool", lambda e: e.iota(base_pk.t[:], pattern=[[128, 8]], base=0, channel_multiplier=1,
                                          allow_small_or_imprecise_dtypes=True), [], [base_pk])
            s.op("pool", lambda e: e.iota(iota_c.t[:], pattern=[[1, NCH]], base=0, channel_multiplier=0,
                                          allow_small_or_imprecise_dtypes=True), [], [iota_c])

        mk_consts()

        def prologue():
            with s.phase():
                ct = s.sb([128, 8], F32, "ct")
                cond = s.sb([128, 8], F32, "cond")
                condb = s.sb([128, 8, 128], F32, "condb")
                s.dma("sp", ct.t[:], c_in, [], [ct])
                s.op("act", lambda e: e.activation(out=cond.t[:], in_=ct.t[:], func=AF.Silu), [ct], [cond])
                for k in range(8):
                    s.op("dve", lambda e, k=k: e.tensor_scalar(condb.t[:, k, :], ones_f.t[:], cond.t[:, k:k + 1], None,
                                                               ALU.mult), [ones_f, cond], [condb])
                wst = [s.sb([128, 3072], F32, "adaw") for _ in range(3)]
                pacc = [s.ps([128, 512], F32, "pada") for _ in range(6)]
                modt = s.sb([1, 6 * D], F32, "modt")
                adab = s.sb([1, 6 * D], F32, "adab")
                it = 0
                for l in range(layers):
                    s.dma("sp", adab.t[:], ada_b[l:l + 1, :], [], [adab])
                    for half in range(2):
                        for k in range(8):
                            w = wst[it % 3]; it += 1
                            s.dma("sp", w.t[:], ada_w[l, k * 128:(k + 1) * 128, half * 3072:(half + 1) * 3072], [], [w])
                            for n in range(6):
                                s.mm(pacc[n].t[:], condb.t[:, k, :], w.t[:, n * 512:(n + 1) * 512], k == 0, k == 7,
                                     [condb, w], [pacc[n]])
                        for n in range(6):
                            c0 = half * 3072 + n * 512
                            s.op("dve", lambda e, n=n, c0=c0: e.tensor_tensor(modt.t[0:1, c0:c0 + 512], pacc[n].t[0:1, :],
                                                                              adab.t[0:1, c0:c0 + 512], ALU.add),
                                 [pacc[n], adab], [modt])
                    s.dma("sp", mod_d[l:l + 1, :], modt.t[:], [modt], [b_mod])
            if n_odd > 0:
              with s.phase():
                    pi_ = s.sb([32, S], I32, "posi")
                    ang = s.sb([32, S], F32, "ang")
                    q = s.sb([32, S], F32, "q")
                    qi = s.sb([32, S], I32, "qi")
                    r = s.sb([32, S], F32, "r")
                    m = s.sb([32, S], F32, "m")
                    invf = s.sb([32, 1], F32, "invf")
                    s.dma("sp", pi_.t[:], pos_in, [], [pi_])
                    s.dma("sp", invf.t[:], invf_in, [], [invf])
                    s.op("dve", lambda e: e.tensor_copy(out=ang.t[:], in_=pi_.t[:]), [pi_], [ang])
                    s.op("dve", lambda e: e.tensor_scalar(ang.t[:], ang.t[:], invf.t[:, 0:1], None, ALU.mult), [ang, invf], [ang])
                    s.op("dve", lambda e: e.tensor_scalar(q.t[:], ang.t[:], 1.0 / (2 * math.pi), None, ALU.mult), [ang], [q])
                    s.op("dve", lambda e: e.tensor_copy(out=qi.t[:], in_=q.t[:]), [q], [qi])
                    s.op("dve", lambda e: e.tensor_copy(out=q.t[:], in_=qi.t[:]), [qi], [q])
                    s.op("dve", lambda e: e.scalar_tensor_tensor(r.t[:], q.t[:], -TWO_PI_HI, ang.t[:], ALU.mult, ALU.add), [q, ang], [r])
                    s.op("dve", lambda e: e.scalar_tensor_tensor(r.t[:], q.t[:], -TWO_PI_LO, r.t[:], ALU.mult, ALU.add), [q, r], [r])

                    def wrap(t):
                        s.op("dve", lambda e: e.tensor_scalar(m.t[:], t.t[:], math.pi, -2 * math.pi, ALU.is_gt, ALU.mult), [t], [m])
                        s.op("dve", lambda e: e.tensor_tensor(t.t[:], t.t[:], m.t[:], ALU.add), [t, m], [t])
                        s.op("dve", lambda e: e.tensor_scalar(m.t[:], t.t[:], -math.pi, 2 * math.pi, ALU.is_lt, ALU.mult), [t], [m])
                        s.op("dve", lambda e: e.tensor_tensor(t.t[:], t.t[:], m.t[:], ALU.add), [t, m], [t])
                        s.op("dve", lambda e: e.tensor_scalar(t.t[:], t.t[:], math.pi, -math.pi, ALU.min, ALU.max), [t], [t])

                    wrap(r)
                    sn = s.sb([32, S], F32, "sn")
                    s.op("act", lambda e: e.activation(out=sn.t[:], in_=r.t[:], func=AF.Sin), [r], [sn])
                    s.dma("sp", rope_d[1], sn.t[:], [sn], [b_rope])
                    s.op("dve", lambda e: e.tensor_scalar(r.t[:], r.t[:], math.pi / 2, None, ALU.add), [r], [r])
                    wrap(r)
                    cs = s.sb([32, S], F32, "cs")
                    s.op("act", lambda e: e.activation(out=cs.t[:], in_=r.t[:], func=AF.Sin), [r], [cs])
                    s.dma("sp", rope_d[0], cs.t[:], [cs], [b_rope])

        def load_mod(l, j, plus1, name, scale=None):
            t = s.sb([128, D], F32, name)
            s.dma("sp", t.t[:], bcast(mod_d[l:l + 1, j * D:(j + 1) * D]), [b_mod], [t])
            if plus1 and scale is not None:
                s.op("pool", lambda e: e.tensor_scalar(t.t[:], t.t[:], 1.0, float(scale), ALU.add, ALU.mult), [t], [t])
            elif plus1:
                s.op("pool", lambda e: e.tensor_scalar(t.t[:], t.t[:], 1.0, None, ALU.add), [t], [t])
            return t

        def load_row(src_row, n, name):
            t = s.sb([128, n], F32, name)
            s.dma("sp", t.t[:], bcast(src_row), [], [t])
            return t

        def transposes_store_uT(ub, i, ptr, uTs):
            for k in range(8):
                s.op("pe", lambda e, k=k: e.transpose(ptr.t[:, k, :], ub.t[:, k * 128:(k + 1) * 128], ident_b.t[:]),
                     [ub, ident_b], [ptr])
            s.op("act", lambda e: e.copy(out=uTs.t[:], in_=ptr.t[:]), [ptr], [uTs])
            s.dma("act", uT_d[:, :, i * 128:(i + 1) * 128].rearrange("k p t -> p k t"), uTs.t[:], [uTs], [b_uT[i]])

        def premod(l):
            with s.phase():
                sc1 = load_mod(l, 1, True, "sc1")
                sh = load_mod(l, 0, False, "sh")
                xt = [s.sb([128, D], F32, "xt") for _ in range(2)]
                ub = [s.sb([128, D], BF16, "ub") for _ in range(2)]
                ptr = [s.ps([128, 8, 128], BF16, "ptr") for _ in range(2)]
                uTs = [s.sb([128, 8, 128], BF16, "uTs") for _ in range(2)]
                for i in range(NT):
                    x = xt[i % 2]; u = ub[i % 2]
                    s.dma("sp", x.t[:], x_in[i * 128:(i + 1) * 128, :], [], [x])
                    s.op("dve", lambda e: e.tensor_tensor(x.t[:], x.t[:], sc1.t[:], ALU.mult), [x, sc1], [x])
                    s.op("dve", lambda e: e.tensor_tensor(u.t[:], x.t[:], sh.t[:], ALU.add), [x, sh], [u])
                    transposes_store_uT(u, i, ptr[i % 2], uTs[i % 2])

        def even_mixer(l):
            li = l // 2
            with s.phase():
                win = s.sb([128, 8, 2560], BF16, "win")
                wout = s.sb([128, 8, D], BF16, "wout")
                wv = ev_w_in[li].rearrange("(k p) f -> p k f", p=128)
                gpw = load_mod(l, 2, True, "gpw", 1.0 / ALPHA)
                wstg = [s.sb([128, D], F32, "wstg") for _ in range(2)]
                for k in range(8):
                    s.dma("pool", win.t[:, k, 0:1280], wv[:, k, 0:1280], [], [win], group="w")
                    s.dma("pool", win.t[:, k, 1280:2560], wv[:, k, 1280:2560], [], [win], group="w")
                for k in range(8):
                    s.dma("sp", wstg[k % 2].t[:], ev_w_out[li, k * 128:(k + 1) * 128, :], [], [wstg[k % 2]])
                    s.op("dve", lambda e, k=k: e.tensor_tensor(wout.t[:, k, :], wstg[k % 2].t[:], gpw.t[:], ALU.mult), [wstg[k % 2], gpw], [wout])
                sgf = s.sb([128, 8, 128], F32, "sgf")
                sgm = s.sb([128, 8, 128], BF16, "sgm")
                s.dma("sp", sgf.t[:], ev_sgT[li].rearrange("h j i -> j h i"), [], [sgf])
                for h in range(8):
                    s.op("pool", lambda e, h=h: e.affine_select(out=sgf.t[:, h, :], in_=sgf.t[:, h, :], pattern=[[1, 128]],
                                                                compare_op=ALU.is_ge, fill=0.0, base=0, channel_multiplier=-1),
                         [sgf], [sgf])
                s.op("pool", lambda e: e.tensor_copy(out=sgm.t[:], in_=sgf.t[:]), [sgf], [sgm])
                sgb = s.sb([128, 4, 128], F32, "sgb")
                for h in range(8):
                    s.dma("sp", sgb.t[(h % 2) * 64:(h % 2) * 64 + 64, h // 2, :], bcast(ev_sg_b[li, h:h + 1, :], 64), [], [sgb], group="w")
                cw = s.sb([128, 4, 3], F32, "cw")
                s.dma("sp", cw.t[:], ev_conv[li], [], [cw])
                vng = load_row(ev_vn_g[li:li + 1, :], 512, "vng")
                vnb = load_row(ev_vn_b[li:li + 1, :], 512, "vnb")
                halo = s.sb([128, 4, 2], F32, "halo")
                s.op("pool", lambda e: e.memset(halo.t[:], 0.0), [], [halo])

                uTg = [s.sb([128, 8, 512], BF16, "uTg") for _ in range(2)]
                pp = [s.ps([128, 512], F32, "pp") for _ in range(6)]
                psg = [s.ps([128, 128], F32, "psg") for _ in range(2)]
                cg = [s.sb([128, 512], F32, "cg") for _ in range(2)]
                cx = [s.sb([128, 514], F32, "cx") for _ in range(2)]
                acc = [s.sb([128, 512], F32, "acc") for _ in range(2)]
                yT = [s.sb([128, 8, 512], BF16, "yT") for _ in range(2)]
                zuT = [s.sb([128, 4, 512], F32, "zuT") for _ in range(2)]
                zv = [s.sb([128, 512], F32, "zv") for _ in range(2)]
                zvn = [s.sb([128, 512], BF16, "zvn") for _ in range(2)]
                st6 = [s.sb([128, 6], F32, "st6") for _ in range(2)]
                mv = [s.sb([128, 2], F32, "mv") for _ in range(2)]
                rstd = [s.sb([128, 1], F32, "rstd") for _ in range(2)]
                tmp = [s.sb([128, 128], F32, "tmp") for _ in range(2)]
                dl = [s.sb([128, D], F32, "dl") for _ in range(2)]
                ppi = 0
                for g in range(NG):
                    u = uTg[g % 2]; y = yT[g % 2]; zu = zuT[g % 2]
                    s.dma("sp", u.t[:], uT_d[:, :, g * 512:(g + 1) * 512].rearrange("k p t -> p k t"),
                          [b_uT[4 * g + j] for j in range(4)], [u])

                    def proj(f):
                        nonlocal ppi
                        p = pp[ppi % 6]; ppi += 1
                        for k in range(8):
                            s.mm(p.t[:], win.t[:, k, f * 128:(f + 1) * 128], u.t[:, k, :], k == 0, k == 7, [win, u], [p])
                        return p

                    for cc in range(4):
                        c_ = cg[cc % 2]; x_ = cx[cc % 2]; a_ = acc[cc % 2]
                        p_c = proj(4 + cc)
                        s.op("act", lambda e: e.copy(out=c_.t[:], in_=p_c.t[:]), [p_c], [c_])
                        p_x = proj(8 + cc)
                        s.op("pool", lambda e: e.tensor_copy(out=x_.t[:, 0:2], in_=halo.t[:, cc, :]), [halo], [x_])
                        s.op("dve", lambda e: e.tensor_tensor(x_.t[:, 2:514], p_x.t[:], c_.t[:], ALU.mult), [p_x, c_], [x_])
                        s.op("pool", lambda e: e.tensor_copy(out=halo.t[:, cc, :], in_=x_.t[:, 512:514]), [x_], [halo])
                        s.op("dve", lambda e: e.tensor_scalar(a_.t[:], x_.t[:, 0:512], cw.t[:, cc, 0:1], None, ALU.mult), [x_, cw], [a_])
                        s.op("dve", lambda e: e.scalar_tensor_tensor(a_.t[:], x_.t[:, 1:513], cw.t[:, cc, 1:2], a_.t[:], ALU.mult, ALU.add),
                             [x_, cw, a_], [a_])
                        s.op("dve", lambda e: e.scalar_tensor_tensor(a_.t[:], x_.t[:, 2:514], cw.t[:, cc, 2:3], a_.t[:], ALU.mult, ALU.add),
                             [x_, cw, a_], [a_])
                        p_b = proj(cc)
                        s.op("dve", lambda e: e.tensor_tensor(y.t[:, cc, :], p_b.t[:], a_.t[:], ALU.mult), [p_b, a_], [y])
                        p_u = proj(12 + cc)
                        s.op("act", lambda e: e.activation(out=zu.t[:, cc, :], in_=p_u.t[:], func=AF.Gelu), [p_u], [zu])
                    for tt in range(4):
                        i = 4 * g + tt
                        z = zv[tt % 2]; zn = zvn[tt % 2]; s6 = st6[tt % 2]; m_ = mv[tt % 2]; rs = rstd[tt % 2]
                        p = pp[ppi % 6]; ppi += 1
                        for k in range(8):
                            s.mm(p.t[:], u.t[:, k, tt * 128:(tt + 1) * 128], win.t[:, k, 2048:2560], k == 0, k == 7, [win, u], [p])
                        s.op("act", lambda e: e.activation(out=z.t[:], in_=p.t[:], func=AF.Gelu), [p], [z])
                        s.op("dve", lambda e: e.bn_stats(s6.t[:], z.t[:]), [z], [s6])
                        s.op("dve", lambda e: e.bn_aggr(m_.t[:], s6.t[:]), [s6], [m_])
                        s.op("act", lambda e: e.activation(out=rs.t[:], in_=m_.t[:, 1:2], func=AF.Sqrt, bias=eps_t.t[:, 0:1]), [m_, eps_t], [rs])
                        s.op("dve", lambda e: e.reciprocal(rs.t[:], rs.t[:]), [rs], [rs])
                        s.op("dve", lambda e: e.tensor_scalar(z.t[:], z.t[:], m_.t[:, 0:1], rs.t[:, 0:1], ALU.subtract, ALU.mult), [z, m_, rs], [z])
                        s.op("pool", lambda e: e.tensor_tensor(z.t[:], z.t[:], vng.t[:], ALU.mult), [z, vng], [z])
                        s.op("pool", lambda e: e.tensor_tensor(zn.t[:], z.t[:], vnb.t[:], ALU.add), [z, vnb], [zn])
                        for cc in range(4):
                            for hh in range(2):
                                h = 2 * cc + hh
                                pg = psg[(cc * 2 + hh) % 2]
                                t_ = tmp[(cc * 2 + hh) % 2]
                                s.mm(pg.t[:], zn.t[:, cc * 128:(cc + 1) * 128], sgm.t[:, h, :], True, True, [zn, sgm], [pg])
                                lo, hi = hh * 64, hh * 64 + 64
                                s.op("dve", lambda e: e.tensor_tensor(t_.t[lo:hi, :], pg.t[lo:hi, :], sgb.t[lo:hi, cc, :], ALU.add), [pg, sgb], [t_])
                                s.op("dve", lambda e: e.tensor_tensor(y.t[lo:hi, 4 + cc, tt * 128:(tt + 1) * 128], t_.t[lo:hi, :],
                                                                      zu.t[lo:hi, cc, tt * 128:(tt + 1) * 128], ALU.mult), [t_, zu], [y])
                        d_ = dl[tt % 2]
                        for nh in range(2):
                            p = pp[ppi % 6]; ppi += 1
                            for k in range(8):
                                s.mm(p.t[:], y.t[:, k, tt * 128:(tt + 1) * 128], wout.t[:, k, nh * 512:(nh + 1) * 512], k == 0, k == 7, [y, wout], [p])
                            s.op("act", lambda e: e.copy(out=d_.t[:, nh * 512:(nh + 1) * 512], in_=p.t[:]), [p], [d_])
                        s.dma("act", delta[i * 128:(i + 1) * 128, :], d_.t[:], [d_], [b_delta[i]])

        def odd_mixer(l):
            li = l // 2
            SCALE = 1.0 / math.sqrt(96.0)
            qT_d = od_scr["qT"]; kT_d = od_scr["kT"]; v_d = od_scr["v"]; yc_d = od_scr["yc"]; yd_d = od_scr["yd"]
            b_q = Buf(); b_k = Buf(); b_v = Buf(); b_yc = Buf(); b_yd = Buf()
            with s.phase():
                win = s.sb([128, 8, 1440], BF16, "win")
                wv_ = od_w_in[li].rearrange("(k p) f -> p k f", p=128)
                for k in range(8):
                    s.dma("pool", win.t[:, k, :], wv_[:, k, :], [], [win], group="w")
                winr = s.sb([128, 8, 32], F32, "winr")
                s.dma("sp", winr.t[:], wv_[:, :, 1408:1440], [], [winr])
                win_sw = s.sb([128, 8, 96], BF16, "winsw")
                s.op("pool", lambda e: e.memset(win_sw.t[:], 0.0), [], [win_sw])
                s.op("dve", lambda e: e.tensor_scalar(win_sw.t[:, :, 64:80], winr.t[:, :, 16:32], -1.0, None, ALU.mult), [winr, win_sw], [win_sw])
                s.op("dve", lambda e: e.tensor_copy(out=win_sw.t[:, :, 80:96], in_=winr.t[:, :, 0:16]), [winr, win_sw], [win_sw])
                wuq = s.sb([128, 2, 768], BF16, "wuq")
                wuqf = s.sb([128, 2, 768], F32, "wuqf")
                uqv = od_w_uq[li].rearrange("(k p) f -> p k f", p=128)
                s.dma("pool", wuq.t[:], uqv, [], [wuq])
                s.dma("sp", wuqf.t[:], uqv, [], [wuqf])
                wuq_sw = s.sb([128, 2, 8, 96], BF16, "wuqsw")
                s.op("pool", lambda e: e.memset(wuq_sw.t[:], 0.0), [], [wuq_sw])
                wuqf4 = wuqf.t[:].rearrange("p k (h e) -> p k h e", e=96)
                for kc in range(2):
                    s.op("dve", lambda e, kc=kc: e.tensor_scalar(wuq_sw.t[:, kc, :, 64:80], wuqf4[:, kc, :, 80:96], -1.0, None, ALU.mult), [wuqf, wuq_sw], [wuq_sw])
                    s.op("dve", lambda e, kc=kc: e.tensor_copy(out=wuq_sw.t[:, kc, :, 80:96], in_=wuqf4[:, kc, :, 64:80]), [wuqf, wuq_sw], [wuq_sw])
                wk = s.sb([128, 8, 64], BF16, "wk")
                wvv = s.sb([128, 8, 64], BF16, "wvv")
                ukv = od_w_ukv[li].rearrange("r (h e) -> r h e", e=128)
                s.dma("pool", wk.t[:], ukv[:, :, 0:64], [], [wk])
                s.dma("pool", wvv.t[:], ukv[:, :, 64:128], [], [wvv])
                cwd = s.sb([128, 4, 31], F32, "cwd")
                s.dma("sp", cwd.t[:], od_dw[li], [], [cwd])
                dwb = s.sb([128, 4], F32, "dwb"); cng = s.sb([128, 4], F32, "cng"); cnb = s.sb([128, 4], F32, "cnb")
                qng = s.sb([128, 2], F32, "qng"); kvng = s.sb([128, 1], F32, "kvng")
                s.dma("sp", dwb.t[:], od_dw_b[li], [], [dwb]); s.dma("sp", cng.t[:], od_cn_g[li], [], [cng])
                s.dma("sp", cnb.t[:], od_cn_b[li], [], [cnb]); s.dma("sp", qng.t[:], od_qn_g[li], [], [qng])
                s.dma("sp", kvng.t[:], od_kvn_g[li], [], [kvng])
                diag = s.sb([128, 4, 31, 128], BF16, "diag")
                for cc in range(4):
                    for k in range(31):
                        eng = "dve" if (cc * 31 + k) % 2 == 0 else "pool"
                        s.op(eng, lambda e, cc=cc, k=k: e.tensor_scalar(diag.t[:, cc, k, :], ident_f.t[:], cwd.t[:, cc, k:k + 1], None, ALU.mult),
                             [ident_f, cwd], [diag])
                rms_eps = s.sb([128, 1], F32, "rmseps")
                s.op("pool", lambda e: e.memset(rms_eps.t[:], RMS_EPS), [], [rms_eps])

                uTg = [s.sb([128, 8, 512], BF16, "uTg") for _ in range(2)]
                glu = [s.sb([128, 4, 542], BF16, "glu") for _ in range(2)]
                s.op("pool", lambda e: e.memset(glu[1].t[:, :, 512:542], 0.0), [], [glu[1]])
                sgm = [s.sb([128, 512], F32, "sgm") for _ in range(2)]
                hbuf = s.sb([128, 4, 512], F32, "hbuf")
                hsq = s.sb([128, 4, 512], F32, "hsq")
                mean = s.sb([128, 512], F32, "mean"); var = s.sb([128, 512], F32, "var"); rstd = s.sb([128, 512], F32, "rstd")
                tb = [s.sb([128, 512], F32, "tb") for _ in range(2)]
                ycT = [s.sb([128, 4, 512], BF16, "ycT") for _ in range(2)]
                sq = [s.sb([128, 512], F32, "sq") for _ in range(2)]
                rq = s.sb([128, 512], F32, "rq")
                cqn = s.sb([128, 2, 512], BF16, "cqn")
                ckvn = s.sb([128, 512], BF16, "ckvn")
                cs = s.sb([128, 2, 512], F32, "cs")
                t1 = [s.sb([128, 512], F32, "t1") for _ in range(2)]
                t2 = [s.sb([128, 512], F32, "t2") for _ in range(2)]
                krT = s.sb([128, 512], BF16, "krT")
                qT = [s.sb([128, 8, 512], BF16, "qT") for _ in range(2)]
                kT = [s.sb([128, 8, 512], BF16, "kT") for _ in range(2)]
                vt = [s.sb([128, 8, 65], BF16, "vt") for _ in range(2)]
                for v_ in vt:
                    s.op("pool", lambda e, v_=v_: e.memset(v_.t[:, :, 64:65], 1.0), [], [v_])
                pp = [s.ps([128, 512], F32, "pp") for _ in range(8)]
                ppi = 0
                for g in range(NG):
                    u = uTg[g % 2]; gl = glu[g % 2]; glp = glu[(g + 1) % 2]
                    tsl = slice(g * 512, (g + 1) * 512)
                    s.dma("sp", u.t[:], uT_d[:, :, tsl].rearrange("k p t -> p k t"), [b_uT[4 * g + j] for j in range(4)], [u])
                    s.dma("sp", cs.t[64:96, 0, :], rope_d[0, :, tsl], [b_rope], [cs], group=("cs", g))
                    s.dma("sp", cs.t[64:96, 1, :], rope_d[1, :, tsl], [b_rope], [cs], group=("cs", g))

                    def proj(c0, m, wt=win):
                        nonlocal ppi
                        p = pp[ppi % 8]; ppi += 1
                        for k in range(8):
                            s.mm(p.t[0:m, :], wt.t[:, k, c0:c0 + m], u.t[:, k, :], k == 0, k == 7, [wt, u], [p])
                        return p

                    s.op("pool", lambda e: e.tensor_copy(out=gl.t[:, :, 0:30], in_=glp.t[:, :, 512:542]), [glp], [gl])
                    for cc in range(4):
                        sg_ = sgm[cc % 2]
                        p_b = proj(512 + cc * 128, 128)
                        s.op("act", lambda e: e.activation(out=sg_.t[:], in_=p_b.t[:], func=AF.Sigmoid), [p_b], [sg_])
                        p_a = proj(cc * 128, 128)
                        s.op("dve", lambda e: e.tensor_tensor(gl.t[:, cc, 30:542], p_a.t[:], sg_.t[:], ALU.mult), [p_a, sg_], [gl])
                    for cc in range(4):
                        p = pp[ppi % 8]; ppi += 1
                        for k in range(31):
                            s.mm(p.t[:], diag.t[:, cc, k, :], gl.t[:, cc, k:k + 512], k == 0, k == 30, [diag, gl], [p])
                        s.op("act", lambda e: e.activation(out=hbuf.t[:, cc, :], in_=p.t[:], func=AF.Identity, bias=dwb.t[:, cc:cc + 1]), [p, dwb], [hbuf])
                        s.op("act", lambda e: e.activation(out=hsq.t[:, cc, :], in_=p.t[:], func=AF.Square, bias=dwb.t[:, cc:cc + 1]), [p, dwb], [hsq])
                    p1 = pp[ppi % 8]; ppi += 1
                    p2 = pp[ppi % 8]; ppi += 1
                    for cc in range(4):
                        s.mm(p1.t[:], ones_f.t[:], hbuf.t[:, cc, :], cc == 0, cc == 3, [ones_f, hbuf], [p1])
                    for cc in range(4):
                        s.mm(p2.t[:], ones_f.t[:], hsq.t[:, cc, :], cc == 0, cc == 3, [ones_f, hsq], [p2])
                    s.op("dve", lambda e: e.tensor_scalar(mean.t[:], p1.t[:], 1.0 / 512, None, ALU.mult), [p1], [mean])
                    s.op("pool", lambda e: e.tensor_tensor(var.t[:], mean.t[:], mean.t[:], ALU.mult), [mean], [var])
                    s.op("dve", lambda e: e.scalar_tensor_tensor(var.t[:], p2.t[:], 1.0 / 512, var.t[:], ALU.mult, ALU.subtract), [p2, var], [var])
                    s.op("act", lambda e: e.activation(out=rstd.t[:], in_=var.t[:], func=AF.Sqrt, bias=eps_t.t[:, 0:1]), [var, eps_t], [rstd])
                    s.op("dve", lambda e: e.reciprocal(rstd.t[:], rstd.t[:]), [rstd], [rstd])
                    yc = ycT[g % 2]
                    for cc in range(4):
                        t_ = tb[cc % 2]
                        s.op("dve", lambda e: e.tensor_tensor(t_.t[:], hbuf.t[:, cc, :], mean.t[:], ALU.subtract), [hbuf, mean], [t_])
                        s.op("pool", lambda e: e.tensor_tensor(t_.t[:], t_.t[:], rstd.t[:], ALU.mult), [t_, rstd], [t_])
                        s.op("dve", lambda e: e.tensor_scalar(t_.t[:], t_.t[:], cng.t[:, cc:cc + 1], cnb.t[:, cc:cc + 1], ALU.mult, ALU.add), [t_, cng, cnb], [t_])
                        s.op("act", lambda e: e.activation(out=yc.t[:, cc, :], in_=t_.t[:], func=AF.Silu), [t_], [yc])
                    s.dma("act", yc_d[:, :, tsl].rearrange("c p t -> p c t"), yc.t[:], [yc], [b_yc], group="st")
                    pq = [proj(1024, 128), proj(1152, 128)]
                    for c2 in range(2):
                        s.op("act", lambda e, c2=c2: e.activation(out=sq[c2].t[:], in_=pq[c2].t[:], func=AF.Square), [pq[c2]], [sq[c2]])
                    ps_ = pp[ppi % 8]; ppi += 1
                    for c2 in range(2):
                        s.mm(ps_.t[:], ones_f.t[:], sq[c2].t[:], c2 == 0, c2 == 1, [ones_f, sq[c2]], [ps_])
                    s.op("act", lambda e: e.activation(out=rq.t[:], in_=ps_.t[:], func=AF.Sqrt, bias=rms_eps.t[:, 0:1], scale=1.0 / 256), [ps_, rms_eps], [rq])
                    s.op("dve", lambda e: e.reciprocal(rq.t[:], rq.t[:]), [rq], [rq])
                    for c2 in range(2):
                        s.op("dve", lambda e, c2=c2: e.scalar_tensor_tensor(cqn.t[:, c2, :], pq[c2].t[:], qng.t[:, c2:c2 + 1], rq.t[:], ALU.mult, ALU.mult),
                             [pq[c2], qng, rq], [cqn])
                    pkv = proj(1280, 128)
                    s.op("act", lambda e: e.activation(out=sq[0].t[:], in_=pkv.t[:], func=AF.Square), [pkv], [sq[0]])
                    ps_ = pp[ppi % 8]; ppi += 1
                    s.mm(ps_.t[:], ones_f.t[:], sq[0].t[:], True, True, [ones_f, sq[0]], [ps_])
                    s.op("act", lambda e: e.activation(out=rq.t[:], in_=ps_.t[:], func=AF.Sqrt, bias=rms_eps.t[:, 0:1], scale=1.0 / 128), [ps_, rms_eps], [rq])
                    s.op("dve", lambda e: e.reciprocal(rq.t[:], rq.t[:]), [rq], [rq])
                    s.op("dve", lambda e: e.scalar_tensor_tensor(ckvn.t[:], pkv.t[:], kvng.t[:, 0:1], rq.t[:], ALU.mult, ALU.mult), [pkv, kvng, rq], [ckvn])
                    pk1 = proj(1344, 96)
                    pk2 = proj(0, 96, win_sw)
                    R = slice(64, 96)
                    s.op("dve", lambda e: e.tensor_tensor(t1[0].t[R, :], pk1.t[R, :], cs.t[R, 0, :], ALU.mult), [pk1, cs], [t1[0]])
                    s.op("dve", lambda e: e.tensor_tensor(t2[0].t[R, :], pk2.t[R, :], cs.t[R, 1, :], ALU.mult), [pk2, cs], [t2[0]])
                    s.op("pool", lambda e: e.tensor_tensor(krT.t[R, :], t1[0].t[R, :], t2[0].t[R, :], ALU.add), [t1[0], t2[0]], [krT])
                    q_ = qT[g % 2]; k_ = kT[g % 2]
                    for h in range(8):
                        a1 = t1[h % 2]; a2 = t2[h % 2]
                        pq1 = pp[ppi % 8]; ppi += 1
                        pq2 = pp[ppi % 8]; ppi += 1
                        for kc in range(2):
                            s.mm(pq1.t[0:96, :], wuq.t[:, kc, h * 96:(h + 1) * 96], cqn.t[:, kc, :], kc == 0, kc == 1, [wuq, cqn], [pq1])
                        for kc in range(2):
                            s.mm(pq2.t[0:96, :], wuq_sw.t[:, kc, h, :], cqn.t[:, kc, :], kc == 0, kc == 1, [wuq_sw, cqn], [pq2])
                        s.op("act", lambda e: e.copy(out=q_.t[0:64, h, :], in_=pq1.t[0:64, :]), [pq1], [q_])
                        s.op("dve", lambda e: e.tensor_tensor(a1.t[R, :], pq1.t[R, :], cs.t[R, 0, :], ALU.mult), [pq1, cs], [a1])
                        s.op("dve", lambda e: e.tensor_tensor(a2.t[R, :], pq2.t[R, :], cs.t[R, 1, :], ALU.mult), [pq2, cs], [a2])
                        s.op("pool", lambda e: e.tensor_tensor(q_.t[R, h, :], a1.t[R, :], a2.t[R, :], ALU.add), [a1, a2], [q_])
                        pk = pp[ppi % 8]; ppi += 1
                        s.mm(pk.t[0:64, :], wk.t[:, h, :], ckvn.t[:], True, True, [wk, ckvn], [pk])
                        s.op("act", lambda e: e.copy(out=k_.t[0:64, h, :], in_=pk.t[0:64, :]), [pk], [k_])
                        s.op("pool", lambda e: e.tensor_copy(out=k_.t[R, h, :], in_=krT.t[R, :]), [krT], [k_])
                    s.dma("sp", qT_d[:, :, tsl].rearrange("h p t -> p h t"), q_.t[0:96, :, :], [q_], [b_q], group="st")
                    s.dma("sp", kT_d[:, :, tsl].rearrange("h p t -> p h t"), k_.t[0:96, :, :], [k_], [b_k], group="st")
                    for tt in range(4):
                        i = 4 * g + tt
                        v_ = vt[tt % 2]
                        pv = pp[ppi % 8]; ppi += 1
                        s.mm(pv.t[:], ckvn.t[:, tt * 128:(tt + 1) * 128], wvv.t[:].rearrange("p h e -> p (h e)"), True, True, [ckvn, wvv], [pv])
                        s.op("act", lambda e: e.copy(out=v_.t[:, :, 0:64], in_=pv.t[:].rearrange("p (h e) -> p h e", e=64)), [pv], [v_])
                        s.dma("act", v_d[i * 128:(i + 1) * 128, :, :], v_.t[:], [v_], [b_v], group="st")
            with s.phase():
                masks = s.sb([128, 4, 512], BF16, "masks")
                s.op("pool", lambda e: e.memset(masks.t[:], 1.0), [], [masks])
                for j in range(4):
                    s.op("pool", lambda e, j=j: e.affine_select(out=masks.t[:, j, :], in_=masks.t[:, j, :], pattern=[[1, 512]], compare_op=ALU.is_ge,
                                                                fill=0.0, base=-128 * j, channel_multiplier=-1), [masks], [masks])
                kTh = [s.sb([128, S], BF16, "kTh") for _ in range(2)]
                vh = [s.sb([128, NT, 65], BF16, "vh") for _ in range(2)]
                qg = [s.sb([128, 512], BF16, "qg") for _ in range(2)]
                PT = [s.sb([128, 512], BF16, "PT") for _ in range(3)]
                psT = [s.ps([128, 512], F32, "psT") for _ in range(3)]
                pacc = [s.ps([128, 65], F32, "pacc") for _ in range(4)]
                rec = [s.sb([128, 1], F32, "rec") for _ in range(2)]
                yd = [s.sb([128, 64], BF16, "yd") for _ in range(2)]
                it = 0
                qg = [s.sb([128, 512], BF16, "qg3") for _ in range(3)]

                def load_head(h):
                    s.dma("sp", kTh[h % 2].t[0:96, :], kT_d[h], [b_k], [kTh[h % 2]])
                    vsrc = v_d[:, h, :].rearrange("(t p) e -> p t e", p=128)
                    for qq in range(4):
                        t0_, t1_ = qq * NT // 4, (qq + 1) * NT // 4
                        if t1_ > t0_:
                            s.dma("sp", vh[h % 2].t[:, t0_:t1_, :], vsrc[:, t0_:t1_, :], [b_v], [vh[h % 2]], group=("vh", h))

                def load_q(n):
                    h_, G_ = divmod(n, NG)
                    s.dma("sp", qg[n % 3].t[0:96, :], qT_d[h_, :, G_ * 512:(G_ + 1) * 512], [b_q], [qg[n % 3]])

                load_head(0)
                load_q(0)
                for h in range(8):
                    kt = kTh[h % 2]; vv = vh[h % 2]
                    if h + 1 < 8:
                        load_head(h + 1)
                    for G in range(NG):
                        n = h * NG + G
                        q_ = qg[n % 3]
                        if n + 1 < 8 * NG:
                            load_q(n + 1)
                        nkb = 4 * G + 4

                        def qk(kb):
                            nonlocal it
                            ps_ = psT[it % 3]; it += 1
                            s.mm(ps_.t[:], kt.t[0:96, kb * 128:(kb + 1) * 128], q_.t[0:96, :], True, True, [kt, q_], [ps_])
                            return ps_

                        pend = [qk(0)]
                        if nkb > 1:
                            pend.append(qk(1))
                        for kb in range(nkb):
                            ps_ = pend.pop(0)
                            if kb + 2 < nkb:
                                pend.append(qk(kb + 2))
                            pt = PT[kb % 3]
                            j = kb - 4 * G
                            c0 = max(j, 0) * 128
                            s.op("act", lambda e: e.activation(out=pt.t[:, c0:512], in_=ps_.t[:, c0:512], func=AF.Exp, scale=SCALE), [ps_], [pt])
                            if j >= 0:
                                s.op("dve", lambda e: e.tensor_tensor(pt.t[:, c0:c0 + 128], pt.t[:, c0:c0 + 128], masks.t[:, 0, 0:128], ALU.mult), [pt, masks], [pt])
                            for qs in range(4):
                                last_kb = 4 * G + qs
                                if kb > last_kb:
                                    continue
                                s.mm(pacc[qs].t[:], pt.t[:, qs * 128:(qs + 1) * 128], vv.t[:, kb, :], kb == 0, kb == last_kb, [pt, vv], [pacc[qs]])
                                if kb == last_kb:
                                    r_ = rec[qs % 2]; y_ = yd[qs % 2]
                                    i = 4 * G + qs
                                    s.op("dve", lambda e: e.reciprocal(r_.t[:], pacc[qs].t[:, 64:65]), [pacc[qs]], [r_])
                                    s.op("dve", lambda e: e.tensor_scalar(y_.t[:], pacc[qs].t[:, 0:64], r_.t[:, 0:1], None, ALU.mult), [pacc[qs], r_], [y_])
                                    s.dma("sp", yd_d[i * 128:(i + 1) * 128, h * 64:(h + 1) * 64], y_.t[:], [y_], [b_yd], group="st")
            with s.phase():
                wout = s.sb([128, 8, D], BF16, "wout")
                gpw = load_mod(l, 2, True, "gpw", 1.0 / ALPHA)
                wstg = [s.sb([128, D], F32, "wstg") for _ in range(2)]
                for k in range(8):
                    s.dma("sp", wstg[k % 2].t[:], od_w_out[li, k * 128:(k + 1) * 128, :], [], [wstg[k % 2]])
                    s.op("dve", lambda e, k=k: e.tensor_tensor(wout.t[:, k, :], wstg[k % 2].t[:], gpw.t[:], ALU.mult), [wstg[k % 2], gpw], [wout])
                yT = [s.sb([128, 8, 128], BF16, "yT") for _ in range(2)]
                ydt = [s.sb([128, 512], BF16, "ydt") for _ in range(2)]
                ptr = [s.ps([128, 4, 128], BF16, "ptr") for _ in range(2)]
                pp = [s.ps([128, 512], F32, "pp") for _ in range(4)]
                dl = [s.sb([128, D], F32, "dl") for _ in range(2)]
                for i in range(NT):
                    y = yT[i % 2]; yd_ = ydt[i % 2]; pt = ptr[i % 2]; d_ = dl[i % 2]
                    s.dma("sp", y.t[:, 0:4, :], yc_d[:, :, i * 128:(i + 1) * 128].rearrange("c p t -> p c t"), [b_yc], [y])
                    s.dma("sp", yd_.t[:], yd_d[i * 128:(i + 1) * 128, :], [b_yd], [yd_])
                    for c in range(4):
                        s.op("pe", lambda e, c=c: e.transpose(pt.t[:, c, :], yd_.t[:, c * 128:(c + 1) * 128], ident_b.t[:]), [yd_, ident_b], [pt])
                    s.op("act", lambda e: e.copy(out=y.t[:, 4:8, :], in_=pt.t[:]), [pt], [y])
                    for nh in range(2):
                        p = pp[(2 * i + nh) % 4]
                        for k in range(8):
                            s.mm(p.t[:], y.t[:, k, :], wout.t[:, k, nh * 512:(nh + 1) * 512], k == 0, k == 7, [y, wout], [p])
                        s.op("act", lambda e: e.copy(out=d_.t[:, nh * 512:(nh + 1) * 512], in_=p.t[:]), [p], [d_])
                    s.dma("act", delta[i * 128:(i + 1) * 128, :], d_.t[:], [d_], [b_delta[i]])

        def norm_pass(l, kind, x_src, last):
            with s.phase():
                mix = kind == "mix"
                need_u = not (last and not mix)
                if mix:
                    lng = load_row(ln_mix_g[l:l + 1, :], D, "lng"); lnb = load_row(ln_mix_b[l:l + 1, :], D, "lnb")
                    sc1 = load_mod(l, 4, True, "sc1"); sh = load_mod(l, 3, False, "sh")
                else:
                    lng = load_row(ln_ffn_g[l:l + 1, :], D, "lng"); lnb = load_row(ln_ffn_b[l:l + 1, :], D, "lnb")
                    gpf = load_mod(l, 5, True, "gpf", 1.0 / (ALPHA * 1.702))
                    if need_u:
                        sc1 = load_mod(l + 1, 1, True, "sc1"); sh = load_mod(l + 1, 0, False, "sh")
                if need_u:
                    B2 = sh
                    tmpb = s.sb([128, D], F32, "tmpb")
                    s.op("dve", lambda e: e.tensor_tensor(tmpb.t[:], lnb.t[:], sc1.t[:], ALU.mult), [lnb, sc1], [tmpb])
                    s.op("dve", lambda e: e.tensor_tensor(B2.t[:], tmpb.t[:], sh.t[:], ALU.add), [tmpb, sh], [B2])
                    G2 = sc1
                    s.op("dve", lambda e: e.tensor_tensor(G2.t[:], sc1.t[:], lng.t[:], ALU.mult), [sc1, lng], [G2])
                eps2 = s.sb([128, 1], F32, "eps2")
                s.op("pool", lambda e: e.memset(eps2.t[:], LN_EPS / (ALPHA * ALPHA)), [], [eps2])
                NB = 4
                xt = [s.sb([128, D], F32, "xt") for _ in range(NB)]
                st12 = [s.sb([128, 12], F32, "st12") for _ in range(NB)]
                mv = [s.sb([128, 2], F32, "mv") for _ in range(NB)]
                rstd = [s.sb([128, 1], F32, "rstd") for _ in range(NB)]
                nbias = [s.sb([128, 1], F32, "nbias") for _ in range(NB)]
                nt = [s.sb([128, D], F32, "nt") for _ in range(3)]
                xo = [s.sb([128, D], F32, "xo") for _ in range(3)]
                ub = [s.sb([128, D], BF16, "ub") for _ in range(3)]
                if mix:
                    yt = [s.sb([128, D], F32, "yt") for _ in range(NB)]
                    uf = [s.sb([128, D], F32, "uf") for _ in range(3)]
                    ptf = [s.ps([128, 4, 128], F32, "ptf") for _ in range(4)]
                    uTf = [s.sb([128, 8, 128], F32, "uTf") for _ in range(3)]
                    rw = s.sb([128, 8, NE], F32, "rw")
                    s.dma("sp", rw.t[:], r_w[l].rearrange("(k p) e -> p k e", p=128), [], [rw])
                    rb = load_row(r_b[l:l + 1, :], NE, "rb")
                    plog = [s.ps([128, NE], F32, "plog") for _ in range(2)]
                    prk = [s.ps([128, NE], F32, "prk") for _ in range(2)]
                    lg = [s.sb([128, NE], F32, "lg") for _ in range(3)]
                    t8 = [s.sb([128, 8], F32, "t8") for _ in range(2)]
                    nv0 = [s.sb([128, 1], F32, "nv0") for _ in range(2)]
                    ex4 = [s.sb([128, 4], F32, "ex4") for _ in range(2)]
                    sm = [s.sb([128, 1], F32, "sm") for _ in range(2)]
                    msk = [s.sb([128, NE], F32, "msk") for _ in range(2)]
                    msum = s.sb([128, NE], F32, "msum")
                    oh = s.sb([128, NT, 4, NE], F32, "oh")
                    prod = s.sb([128, 4, NE], F32, "prod")
                    s.op("pool", lambda e: e.memset(msum.t[:], 0.0), [], [msum])
                else:
                    yk = [s.sb([128, D], F32, "yk") for _ in range(12)]
                    if need_u:
                        ptr = [s.ps([128, 8, 128], BF16, "ptr") for _ in range(2)]
                        uTs = [s.sb([128, 8, 128], BF16, "uTs") for _ in range(2)]
                        uf = [s.sb([128, D], F32, "uf") for _ in range(3)]

                def stageL(i):
                    x = xt[i % NB]
                    s.dma("sp", x.t[:], x_src[i * 128:(i + 1) * 128, :], [b_xres[i]] if x_src is xres else [], [x])
                    if mix:
                        y = yt[i % NB]
                        s.dma("sp", y.t[:], delta[i * 128:(i + 1) * 128, :], [b_delta[i]], [y])
                    else:
                        ys_ = [yk[(i % 3) * 4 + k] for k in range(4)]
                        for k in range(4):
                            s.idma(ys_[k].t[:], None, ys_d, bass.IndirectOffsetOnAxis(ap=posk_i.t[:, i, k:k + 1], axis=0),
                                   [b_ys_all, posk_i], [ys_[k]])

                def stageA(i):
                    x = xt[i % NB]; s12 = st12[i % NB]; m_ = mv[i % NB]; rs = rstd[i % NB]; nb_ = nbias[i % NB]
                    if mix:
                        y = yt[i % NB]
                        s.op("dve", lambda e: e.tensor_tensor(x.t[:], x.t[:], y.t[:], ALU.add), [x, y], [x])
                    else:
                        ys_ = [yk[(i % 3) * 4 + k] for k in range(4)]
                        for k in range(4):
                            s.op("act", lambda e, k=k: e.activation(out=ys_[k].t[:], in_=ys_[k].t[:], func=AF.Copy, scale=gatek.t[:, i, k:k + 1]), [ys_[k], gatek], [ys_[k]])
                        s.op("dve", lambda e: e.tensor_tensor(ys_[0].t[:], ys_[0].t[:], ys_[1].t[:], ALU.add), [ys_[0], ys_[1]], [ys_[0]])
                        s.op("dve", lambda e: e.tensor_tensor(ys_[2].t[:], ys_[2].t[:], ys_[3].t[:], ALU.add), [ys_[2], ys_[3]], [ys_[2]])
                        s.op("dve", lambda e: e.tensor_tensor(ys_[0].t[:], ys_[0].t[:], ys_[2].t[:], ALU.add), [ys_[0], ys_[2]], [ys_[0]])
                        s.op("dve", lambda e: e.tensor_tensor(ys_[0].t[:], ys_[0].t[:], gpf.t[:], ALU.mult), [ys_[0], gpf], [ys_[0]])
                        s.op("dve", lambda e: e.tensor_tensor(x.t[:], x.t[:], ys_[0].t[:], ALU.add), [x, ys_[0]], [x])
                    s.op("dve", lambda e: e.bn_stats(s12.t[:, 0:6], x.t[:, 0:512]), [x], [s12])
                    s.op("dve", lambda e: e.bn_stats(s12.t[:, 6:12], x.t[:, 512:1024]), [x], [s12])
                    s.op("dve", lambda e: e.bn_aggr(m_.t[:], s12.t[:]), [s12], [m_])
                    s.op("act", lambda e: e.activation(out=rs.t[:], in_=m_.t[:, 1:2], func=AF.Ln, bias=eps2.t[:, 0:1]), [m_, eps2], [rs])
                    s.op("act", lambda e: e.activation(out=rs.t[:], in_=rs.t[:], func=AF.Exp, scale=-0.5), [rs], [rs])
                    s.op("dve", lambda e: e.scalar_tensor_tensor(nb_.t[:], m_.t[:, 0:1], -1.0, rs.t[:], ALU.mult, ALU.mult), [m_, rs], [nb_])

                def stageB(i):
                    x = xt[i % NB]; rs = rstd[i % NB]; nb_ = nbias[i % NB]
                    n_ = nt[i % 3]; o_ = xo[i % 3]
                    s.op("act", lambda e: e.activation(out=n_.t[:], in_=x.t[:], func=AF.Identity, scale=rs.t[:, 0:1], bias=nb_.t[:, 0:1]), [x, rs, nb_], [n_])
                    s.op("dve", lambda e: e.tensor_tensor(o_.t[:], n_.t[:], lng.t[:], ALU.mult), [n_, lng], [o_])
                    s.op("dve", lambda e: e.tensor_tensor(o_.t[:], o_.t[:], lnb.t[:], ALU.add), [o_, lnb], [o_])
                    if not need_u:
                        s.dma("sp", out[i * 128:(i + 1) * 128, :], o_.t[:], [o_], [b_xres[i]])
                        return
                    s.dma("sp", xres[i * 128:(i + 1) * 128, :], o_.t[:], [o_], [b_xres[i]])
                    u = ub[i % 3]; f = uf[i % 3]
                    s.op("dve", lambda e: e.tensor_tensor(f.t[:], n_.t[:], G2.t[:], ALU.mult), [n_, G2], [f])
                    if not mix:
                        s.op("dve", lambda e: e.tensor_tensor(u.t[:], f.t[:], B2.t[:], ALU.add), [f, B2], [u])
                        transposes_store_uT(u, i, ptr[i % 2], uTs[i % 2])
                        return
                    s.op("dve", lambda e: e.tensor_tensor(f.t[:], f.t[:], B2.t[:], ALU.add), [f, B2], [f])
                    s.op("act", lambda e: e.copy(out=u.t[:], in_=f.t[:]), [f], [u])
                    s.dma("act", u_d[i * 128:(i + 1) * 128, :], u.t[:], [u], [b_u[i]])
                    uT = uTf[i % 3]
                    for hf in range(2):
                        pt = ptf[(2 * i + hf) % 4]
                        for kk in range(4):
                            k = hf * 4 + kk
                            s.op("pe", lambda e, k=k, kk=kk: e.transpose(pt.t[:, kk, :], f.t[:, k * 128:(k + 1) * 128], ident_f.t[:]),
                                 [f, ident_f], [pt])
                        s.op("act", lambda e: e.copy(out=uT.t[:, hf * 4:hf * 4 + 4, :], in_=pt.t[:]), [pt], [uT])
                    pl = plog[i % 2]
                    for k in range(8):
                        s.mm(pl.t[:], uT.t[:, k, :], rw.t[:, k, :], k == 0, k == 7, [uT, rw], [pl])

                def stageC(i):
                    lgt = lg[i % 3]; t8_ = t8[i % 2]; mk = msk[i % 2]; pl = plog[i % 2]
                    s.op("dve", lambda e: e.tensor_tensor(lgt.t[:], pl.t[:], rb.t[:], ALU.add), [pl, rb], [lgt])
                    s.op("dve", lambda e: e.max(out=t8_.t[:], in_=lgt.t[:]), [lgt], [t8_])
                    pr = prk[i % 2]
                    s.op("dve", lambda e: e.tensor_scalar(mk.t[:], lgt.t[:], t8_.t[:, 3:4], None, ALU.is_ge), [lgt, t8_], [mk])
                    s.mm(pr.t[:], su_f.t[:], mk.t[:], True, False, [su_f, mk], [pr])
                    s.mm(pr.t[:], ones_f.t[:], msum.t[:], False, True, [ones_f, msum], [pr])
                    for k in range(4):
                        s.op("dve", lambda e, k=k: e.tensor_scalar(oh.t[:, i, k, :], lgt.t[:], t8_.t[:, k:k + 1], None, ALU.is_equal), [lgt, t8_], [oh])
                    n0 = nv0[i % 2]; e4 = ex4[i % 2]; sm_ = sm[i % 2]
                    s.op("dve", lambda e: e.tensor_scalar(n0.t[:], t8_.t[:, 0:1], -1.0, None, ALU.mult), [t8_], [n0])
                    s.op("act", lambda e: e.activation(out=e4.t[:], in_=t8_.t[:, 0:4], func=AF.Exp, bias=n0.t[:, 0:1]), [t8_, n0], [e4])
                    for k in range(4):
                        s.op("dve", lambda e, k=k: e.tensor_tensor(prod.t[:, k, :], oh.t[:, i, k, :], pr.t[:], ALU.mult), [oh, pr], [prod])
                    s.op("dve", lambda e: e.tensor_reduce(out=posk_f.t[:, i, :], in_=prod.t[:], axis=AX.X, op=ALU.add), [prod], [posk_f])
                    s.op("dve", lambda e: e.tensor_tensor(msum.t[:], msum.t[:], mk.t[:], ALU.add), [msum, mk], [msum])
                    s.op("dve", lambda e: e.tensor_reduce(out=sm_.t[:], in_=e4.t[:], axis=AX.X, op=ALU.add), [e4], [sm_])
                    s.op("dve", lambda e: e.reciprocal(sm_.t[:], sm_.t[:]), [sm_], [sm_])
                    s.op("dve", lambda e: e.tensor_scalar(gatek.t[:, i, :], e4.t[:], sm_.t[:, 0:1], None, ALU.mult), [e4, sm_], [gatek])

                yk_sets = 3
                for step in range(NT + 3):
                    if step < NT:
                        stageL(step)
                    if 2 <= step <= NT + 1:
                        stageB(step - 2)
                    if 1 <= step <= NT:
                        stageA(step - 1)
                    if mix and 3 <= step:
                        stageC(step - 3)
                if kind == "mix":
                    pc = prk[0]
                    s.mm(pc.t[:], ones_f.t[:], msum.t[:], True, True, [ones_f, msum], [pc])
                    cnt = s.sb([128, NE], F32, "cnt")
                    nch = s.sb([128, NE], F32, "nch")
                    tmpc = s.sb([128, NE], F32, "tmpc")
                    cs_a = s.sb([128, NE], F32, "csa")
                    cs_b = s.sb([128, NE], F32, "csb")
                    s.op("dve", lambda e: e.tensor_copy(out=cnt.t[:], in_=pc.t[:]), [pc], [cnt])
                    s.op("dve", lambda e: e.tensor_scalar(nch.t[:], cnt.t[:], 0.5, None, ALU.is_gt), [cnt], [nch])
                    for j in range(1, (S + CH - 1) // CH):
                        s.op("dve", lambda e, j=j: e.tensor_scalar(tmpc.t[:], cnt.t[:], CH * j + 0.5, None, ALU.is_gt), [cnt], [tmpc])
                        s.op("dve", lambda e: e.tensor_tensor(nch.t[:], nch.t[:], tmpc.t[:], ALU.add), [nch, tmpc], [nch])
                    s.op("dve", lambda e: e.tensor_copy(out=cs_a.t[:], in_=nch.t[:]), [nch], [cs_a])
                    a, b = cs_a, cs_b
                    sh_ = 1
                    while sh_ < NE:
                        s.op("dve", lambda e, a=a, b=b, sh_=sh_: e.tensor_copy(out=b.t[:, 0:sh_], in_=a.t[:, 0:sh_]), [a], [b])
                        s.op("dve", lambda e, a=a, b=b, sh_=sh_: e.tensor_tensor(b.t[:, sh_:NE], a.t[:, sh_:NE], a.t[:, 0:NE - sh_], ALU.add), [a], [b])
                        a, b = b, a
                        sh_ *= 2
                    cend = a
                    base = s.sb([128, NE], F32, "base")
                    s.op("dve", lambda e: e.tensor_tensor(base.t[:], cend.t[:], nch.t[:], ALU.subtract), [cend, nch], [base])
                    s.op("dve", lambda e: e.tensor_scalar(base.t[:], base.t[:], float(CH), None, ALU.mult), [base], [base])
                    prod2 = s.sb([128, NT, 4, NE], F32, "prod2")
                    s.op("dve", lambda e: e.tensor_tensor(prod2.t[:].rearrange("p i k e -> p (i k) e"), oh.t[:].rearrange("p i k e -> p (i k) e"),
                                                          base.t[:].unsqueeze(1).to_broadcast([128, NT * 4, NE]), ALU.mult), [oh, base], [prod2])
                    bsel = s.sb([128, NT, 4], F32, "bsel")
                    s.op("dve", lambda e: e.tensor_reduce(out=bsel.t[:], in_=prod2.t[:], axis=AX.X, op=ALU.add), [prod2], [bsel])
                    s.op("dve", lambda e: e.tensor_tensor(posk_f.t[:], posk_f.t[:], bsel.t[:], ALU.add), [posk_f, bsel], [posk_f])
                    s.op("dve", lambda e: e.tensor_copy(out=posk_i.t[:], in_=posk_f.t[:]), [posk_f], [posk_i])
                    cmp3 = s.sb([128, NCH, NE], F32, "cmp3")
                    cef = s.sb([128, NCH], F32, "cef")
                    wif = s.sb([128, NCH, 8], F32, "wif")
                    tf = s.sb([128, NCH], F32, "tf")
                    s.op("dve", lambda e: e.tensor_tensor(cmp3.t[:], cend.t[:].unsqueeze(1).to_broadcast([128, NCH, NE]),
                                                          iota_c.t[:].unsqueeze(2).to_broadcast([128, NCH, NE]), ALU.is_le), [cend, iota_c], [cmp3])
                    s.op("dve", lambda e: e.tensor_reduce(out=cef.t[:], in_=cmp3.t[:], axis=AX.X, op=ALU.add), [cmp3], [cef])
                    s.op("dve", lambda e: e.tensor_scalar(cef.t[:], cef.t[:], float(NE - 1), float(l * NE), ALU.min, ALU.add), [cef], [cef])
                    s.op("dve", lambda e: e.tensor_copy(out=bdidx.t[:], in_=cef.t[:]), [cef], [bdidx])
                    s.op("dve", lambda e: e.tensor_scalar(tf.t[:], cef.t[:], 128.0, iota_p.t[:, 0:1], ALU.mult, ALU.add), [cef, iota_p], [tf])
                    s.op("dve", lambda e: e.tensor_copy(out=bgidx.t[:], in_=tf.t[:]), [tf], [bgidx])
                    s.op("dve", lambda e: e.tensor_scalar(tf.t[:], cef.t[:], 1024.0, None, ALU.mult), [cef], [tf])
                    s.op("dve", lambda e: e.tensor_tensor(wif.t[:], tf.t[:].unsqueeze(2).to_broadcast([128, NCH, 8]),
                                                          base_pk.t[:].unsqueeze(1).to_broadcast([128, NCH, 8]), ALU.add), [tf, base_pk], [wif])
                    s.op("dve", lambda e: e.tensor_copy(out=widx.t[:], in_=wif.t[:]), [wif], [widx])
                    if "posk" in dbg_out:
                        s.dma("sp", dbg_out["posk"].rearrange("(i p) k -> p i k", p=128), posk_f.t[:], [posk_f], [])
                        s.dma("sp", dbg_out["gatek"].rearrange("(i p) k -> p i k", p=128), gatek.t[:], [gatek], [])
                        s.dma("sp", dbg_out["ctab"], cef.t[:, 0:NCH], [cef], [])

        def scatter_pass():
            with s.phase():
                ub = [s.sb([128, D], BF16, "ub") for _ in range(3)]
                for i in range(NT):
                    u = ub[i % 3]
                    s.dma("sp", u.t[:], u_d[i * 128:(i + 1) * 128, :], [b_u[i]], [u])
                    for k in range(4):
                        s.idma(xs_d, bass.IndirectOffsetOnAxis(ap=posk_i.t[:, i, k:k + 1], axis=0), u.t[:], None,
                               [u, posk_i], [b_xs_all], group="sc")

        def expert_pass(l):
            with s.phase():
                wgu = [s.sb([128, 8, 2 * D], BF16, "wgu") for _ in range(3)]
                wdn = [s.sb([128, 8, D], BF16, "wdn") for _ in range(2)]
                bgu = [s.sb([128, 16], F32, "bgu") for _ in range(3)]
                bdn = [s.sb([128, D], BF16, "bdn") for _ in range(2)]
                SBK = CH // 128
                xsb = [[s.sb([128, D], BF16, "xsb") for _ in range(SBK)] for _ in range(2)]
                xT = [s.sb([128, 8, CH], BF16, "xT") for _ in range(2)]
                ptr = [s.ps([128, 8, 128], BF16, "ptr") for _ in range(2)]
                pg = [s.ps([128, 512], F32, "pg") for _ in range(4)]
                pd = [s.ps([128, 512], F32, "pd") for _ in range(2)]
                gc = [s.sb([128, CH], F32, "gc") for _ in range(2)]
                sg = [s.sb([128, CH], F32, "sg") for _ in range(2)]
                uc = [s.sb([128, CH], F32, "uc") for _ in range(2)]
                actT = [s.sb([128, 8, CH], BF16, "actT") for _ in range(2)]
                ysb = [s.sb([128, D], F32, "ysb") for _ in range(2)]
                wguv = w_gu.rearrange("l e r f -> (l e r) f")
                wdnv = w_dn.rearrange("l e r f -> (l e r) f")
                bguv = b_guT.rearrange("l e p c -> (l e p) c")
                bdnv = b_dn.rearrange("l e f -> (l e) f")
                IO = bass.IndirectOffsetOnAxis

                def pre_g(c):
                    wg = wgu[c % 3]; bg = bgu[c % 3]
                    for k in range(8):
                        s.idma(wg.t[:, k, :], None, wguv, IO(ap=widx.t[:, c, k:k + 1], axis=0), [widx], [wg], group=("wg", c))
                    s.idma(bg.t[:], None, bguv, IO(ap=bgidx.t[:, c:c + 1], axis=0), [bgidx], [bg])
                    s.op("dve", lambda e: e.tensor_scalar(bg.t[:, 8:16], bg.t[:, 8:16], 1.0, None, ALU.add), [bg], [bg])

                def pre_d(c):
                    wd = wdn[c % 2]; bd = bdn[c % 2]
                    for k in range(8):
                        s.idma(wd.t[:, k, :], None, wdnv, IO(ap=widx.t[:, c, k:k + 1], axis=0), [widx], [wd], group=("wd", c))
                    s.idma(bd.t[:], None, bdnv, IO(ap=bdidx.t[:, c:c + 1], axis=0), [bdidx], [bd])
                    for sb_ in range(SBK):
                        xb = xsb[c % 2][sb_]
                        r0 = c * CH + sb_ * 128
                        s.dma("sp", xb.t[:], xs_d[r0:r0 + 128, :], [b_xs_all], [xb])

                def tr(c):
                    xt_ = xT[c % 2]
                    for sb_ in range(SBK):
                        xb = xsb[c % 2][sb_]; pt = ptr[sb_ % 2]
                        for k in range(8):
                            s.op("pe", lambda e, k=k: e.transpose(pt.t[:, k, :], xb.t[:, k * 128:(k + 1) * 128], ident_b.t[:]), [xb, ident_b], [pt])
                        s.op("act", lambda e: e.copy(out=xt_.t[:, :, sb_ * 128:(sb_ + 1) * 128], in_=pt.t[:]), [pt], [xt_])

                def gu(c):
                    wg = wgu[c % 3]; bg = bgu[c % 3]
                    xt_ = xT[c % 2]; at = actT[c % 2]
                    for j in range(8):
                        p_g = pg[(2 * j) % 4]; p_u = pg[(2 * j + 1) % 4]
                        for k in range(8):
                            s.mm(p_g.t[:, 0:CH], wg.t[:, k, j * 128:(j + 1) * 128], xt_.t[:, k, :], k == 0, k == 7, [wg, xt_], [p_g])
                        for k in range(8):
                            s.mm(p_u.t[:, 0:CH], wg.t[:, k, D + j * 128:D + (j + 1) * 128], xt_.t[:, k, :], k == 0, k == 7, [wg, xt_], [p_u])
                        g_ = gc[j % 2]; s_ = sg[j % 2]; u_ = uc[j % 2]
                        s.op("dve", lambda e: e.tensor_scalar(g_.t[:], p_g.t[:, 0:CH], bg.t[:, j:j + 1], 7.0, ALU.add, ALU.min), [p_g, bg], [g_])
                        s.op("act", lambda e: e.activation(out=s_.t[:], in_=g_.t[:], func=AF.Silu, scale=1.702), [g_], [s_])
                        s.op("dve", lambda e: e.tensor_scalar(u_.t[:], p_u.t[:, 0:CH], bg.t[:, 8 + j:9 + j], 8.0, ALU.add, ALU.min), [p_u, bg], [u_])
                        s.op("dve", lambda e: e.scalar_tensor_tensor(at.t[:, j, :], u_.t[:], -6.0, s_.t[:], ALU.max, ALU.mult), [s_, u_], [at])

                def dn(c):
                    wd = wdn[c % 2]; bd = bdn[c % 2]; at = actT[c % 2]
                    for sb_ in range(SBK):
                        yb = ysb[sb_ % 2]
                        for nh in range(2):
                            p = pd[nh]
                            for j in range(8):
                                s.mm(p.t[:], at.t[:, j, sb_ * 128:(sb_ + 1) * 128], wd.t[:, j, nh * 512:(nh + 1) * 512], j == 0, False, [at, wd], [p])
                            s.mm(p.t[:], c1702.t[0:1, :], bd.t[0:1, nh * 512:(nh + 1) * 512], False, True, [c1702, bd], [p])
                            s.op("act", lambda e: e.copy(out=yb.t[:, nh * 512:(nh + 1) * 512], in_=p.t[:]), [p], [yb])
                        r0 = c * CH + sb_ * 128
                        s.dma("act", ys_d[r0:r0 + 128, :], yb.t[:], [yb], [b_ys_all], group=("ys", l))

                pre_g(0)
                pre_d(0)
                if NCH > 1:
                    pre_g(1)
                tr(0)
                for c in range(NCH):
                    if c + 2 < NCH:
                        pre_g(c + 2)
                    if c + 1 < NCH:
                        pre_d(c + 1)
                    gu(c)
                    if c + 1 < NCH:
                        tr(c + 1)
                    dn(c)

        eps_t = s.sb([128, 1], F32, "eps")
        s.op("pool", lambda e: e.memset(eps_t.t[:], LN_EPS), [], [eps_t])

        prologue()
        premod(0)
        x_src = x_in
        stop_after = (dbg or {}).get("_stop", None)
        for l in range(layers):
            if l % 2 == 0:
                even_mixer(l)
            else:
                odd_mixer(l)
            norm_pass(l, "mix", x_src, False)
            x_src = xres
            scatter_pass()
            expert_pass(l)
            norm_pass(l, "ffn", x_src, l == layers - 1)
        s.barrier()
    return nc


def prep_inputs(inputs, S=4096, ncores=8, layers=DEPTH):
    f = lambda a: np.ascontiguousarray(np.asarray(a))
    shared = {}
    for k in ("ada_w", "ada_b", "ln_mix_g", "ln_mix_b", "ln_ffn_g", "ln_ffn_b", "ev_w_in", "ev_sg_b", "ev_vn_g", "ev_vn_b",
              "ev_w_out", "od_w_in", "od_w_uq", "od_w_ukv", "od_w_out", "moe_router_w", "moe_router_b", "moe_w_gu",
              "moe_w_dn", "moe_b_dn"):
        shared[k] = f(inputs[k][:layers]) if k in ("moe_w_gu", "moe_w_dn", "ada_w") else f(inputs[k])
    shared["ev_conv"] = f(np.asarray(inputs["ev_conv_w"]).transpose(0, 2, 1).reshape(2, 4, 128, 3).transpose(0, 2, 1, 3))
    shared["ev_sgT"] = f(np.asarray(inputs["ev_sg_w"]).transpose(0, 1, 3, 2))
    shared["od_dw"] = f(np.asarray(inputs["od_dw_w"]).transpose(0, 2, 1).reshape(2, 4, 128, 31).transpose(0, 2, 1, 3))
    col = lambda a, n: f(np.asarray(a).reshape(2, n, 128).transpose(0, 2, 1))
    shared["od_dw_b"] = col(inputs["od_dw_b"], 4)
    shared["od_cn_g"] = col(inputs["od_cn_g"], 4)
    shared["od_cn_b"] = col(inputs["od_cn_b"], 4)
    shared["od_qn_g"] = col(inputs["od_qn_g"], 2)
    shared["od_kvn_g"] = col(inputs["od_kvn_g"], 1)
    shared["moe_b_guT"] = f(np.asarray(inputs["moe_b_gu"]).reshape(DEPTH, NE, 16, 128).transpose(0, 1, 3, 2))
    invf = (10000.0 ** (-np.arange(0, 32, 2, dtype=np.float32) / 32)).astype(np.float32)
    shared["invf"] = f(np.concatenate([invf, invf]).reshape(32, 1))
    maps = []
    x = np.asarray(inputs["x"]); c = np.asarray(inputs["c"]); pos = np.asarray(inputs["positions"])
    for b in range(ncores):
        m = dict(shared)
        m["x"] = f(x[b, :S])
        m["c"] = f(c[b].reshape(8, 128).T)
        m["pos"] = f(np.broadcast_to(pos[b, :S].astype(np.int32)[None, :], (32, S)))
        maps.append(m)
    return maps


_NC_CACHE = {}


def kernel(**inputs):
    S = 4096
    if "nc" not in _NC_CACHE:
        _NC_CACHE["nc"] = build(S)
    nc = _NC_CACHE["nc"]
    maps = prep_inputs(inputs, S, 8)
    res = run_bass_kernel_spmd(nc, maps, core_ids=list(range(8)))
    return np.stack([np.asarray(r["out"]) for r in res.results], axis=0).astype(np.float32)
```
